# Optimizing a Trainium2 kernel written in Bass

```python
import math
import jax, jax.numpy as jnp
from jax import lax
import numpy as np

D_MODEL = 1024
BATCH = 4
SEQ = 4096
DEPTH = 1

GRID_W = 64
CTX_LEN = 256
N_HEADS = 8
QK_NOPE_DIM = 64
QK_ROPE_DIM = 32
V_HEAD_DIM = 64
Q_LORA_RANK = 256
KV_LORA_RANK = 128
ATTN_WIDTH = N_HEADS * V_HEAD_DIM
MLA_IN_WIDTH = Q_LORA_RANK + KV_LORA_RANK + QK_ROPE_DIM
ROPE_BASE = 10000.0
SOFTMAX_SCALE = (QK_NOPE_DIM + QK_ROPE_DIM) ** -0.5
Q_BLOCK = 128
SSM_WIDTH = D_MODEL - ATTN_WIDTH
SSM_GROUP = 16
N_SSM_GROUPS = SSM_WIDTH // SSM_GROUP
SSM_STATE = 64
DT_MIN = 0.001
DT_MAX = 0.1
IN_PROJ_WIDTH = MLA_IN_WIDTH + SSM_WIDTH
MIX_WIDTH = ATTN_WIDTH + SSM_WIDTH
N_EXPERT_GROUPS = 4
EXPERTS_PER_GROUP = 8
N_EXPERTS = N_EXPERT_GROUPS * EXPERTS_PER_GROUP
EXPERT_TOP_K = 2
D_FF_EXPERT = 256
ALPHA = (2 * DEPTH) ** 0.25
BETA = (8 * DEPTH) ** -0.25
EPS = 1e-6

kernel_name = "hymba_mla_s5_hmoe_deepnorm_dit"


def layer_norm(x, g, b):
    xf = x.astype(jnp.float32)
    mu = jnp.mean(xf, -1, keepdims=True)
    var = jnp.mean(jnp.square(xf - mu), -1, keepdims=True)
    return ((xf - mu) * lax.rsqrt(var + EPS) * g + b).astype(x.dtype)


def rms_norm(x, g):
    xf = x.astype(jnp.float32)
    return (xf * lax.rsqrt(jnp.mean(jnp.square(xf), -1, keepdims=True) + EPS) * g).astype(x.dtype)


def modulate(x, shift, scale):
    return x * (1.0 + scale) + shift


def axial_rope_tables(n_tok):
    rows = n_tok // GRID_W
    row = jnp.repeat(jnp.arange(rows), GRID_W).astype(jnp.float32)
    col = jnp.tile(jnp.arange(GRID_W), rows).astype(jnp.float32)
    axis_dim = QK_ROPE_DIM // 2
    inv_freq = ROPE_BASE ** (-jnp.arange(0, axis_dim, 2, dtype=jnp.float32) / axis_dim)
    ang = jnp.concatenate([row[:, None] * inv_freq, col[:, None] * inv_freq], -1)
    ang = jnp.concatenate([ang, ang], -1)
    return jnp.cos(ang), jnp.sin(ang)


def apply_rope(t, cos, sin):
    t1, t2 = jnp.split(t, 2, axis=-1)
    rot = jnp.concatenate([-t2, t1], -1)
    return (t * cos + rot * sin).astype(t.dtype)


def mla_project(p, q_norm_g, kv_norm_g, w_uq, w_ukv, rope):
    lead = p.shape[:-1]
    cq = p[..., :Q_LORA_RANK]
    ckv = p[..., Q_LORA_RANK:Q_LORA_RANK + KV_LORA_RANK]
    k_rope = p[..., Q_LORA_RANK + KV_LORA_RANK:MLA_IN_WIDTH]
    q = (rms_norm(cq, q_norm_g) @ w_uq).reshape(*lead, N_HEADS, QK_NOPE_DIM + QK_ROPE_DIM)
    kv = (rms_norm(ckv, kv_norm_g) @ w_ukv).reshape(*lead, N_HEADS, QK_NOPE_DIM + V_HEAD_DIM)
    q_nope, q_rope = q[..., :QK_NOPE_DIM], q[..., QK_NOPE_DIM:]
    k_nope, v = kv[..., :QK_NOPE_DIM], kv[..., QK_NOPE_DIM:]
    if rope is not None:
        cos, sin = rope
        q_rope = apply_rope(q_rope, cos[:, None], sin[:, None])
        k_rope = apply_rope(k_rope, cos, sin)
    return q_nope, q_rope, k_nope, k_rope, v


def mla_attend(q_nope, q_rope, k_nope, k_rope, v):
    s = (jnp.einsum('bqhd,bkhd->bhqk', q_nope, k_nope)
         + jnp.einsum('bqhr,bkr->bhqk', q_rope, k_rope))
    p = jax.nn.softmax(s.astype(jnp.float32) * SOFTMAX_SCALE, axis=-1).astype(v.dtype)
    return jnp.einsum('bhqk,bkhd->bqhd', p, v)


def latent_attention(q_nope, q_rope, k_nope, k_rope, v):
    b, n = q_nope.shape[:2]
    nblk = n // Q_BLOCK

    def to_blocks(t):
        return jnp.moveaxis(t.reshape(b, nblk, Q_BLOCK, *t.shape[2:]), 1, 0)

    out = lax.map(lambda qs: mla_attend(qs[0], qs[1], k_nope, k_rope, v),
                  (to_blocks(q_nope), to_blocks(q_rope)))
    return jnp.moveaxis(out, 0, 1).reshape(b, n, ATTN_WIDTH)


def s5_discretise(a_re, a_im, log_dt, b_re, b_im):
    f32 = jnp.float32
    a_re, a_im = a_re.astype(f32), a_im.astype(f32)
    dt = jnp.exp(log_dt.astype(f32))[:, None]
    mag = jnp.exp(a_re * dt)
    abar_re, abar_im = mag * jnp.cos(a_im * dt), mag * jnp.sin(a_im * dt)
    den = jnp.square(a_re) + jnp.square(a_im)
    num_re = abar_re - 1.0
    coef_re = (num_re * a_re + abar_im * a_im) / den
    coef_im = (abar_im * a_re - num_re * a_im) / den
    b_re, b_im = b_re.astype(f32), b_im.astype(f32)
    bbar_re = coef_re[..., None] * b_re - coef_im[..., None] * b_im
    bbar_im = coef_re[..., None] * b_im + coef_im[..., None] * b_re
    return abar_re, abar_im, bbar_re, bbar_im


def _complex_affine_combine(e1, e2):
    a1r, a1i, b1r, b1i = e1
    a2r, a2i, b2r, b2i = e2
    return (a2r * a1r - a2i * a1i, a2r * a1i + a2i * a1r,
            a2r * b1r - a2i * b1i + b2r, a2r * b1i + a2i * b1r + b2i)


def s5_states(u, s0_re, s0_im, abar_re, abar_im, bbar_re, bbar_im, reverse):
    bsz, n = u.shape[:2]
    uf = u.astype(jnp.float32).reshape(bsz, n, N_SSM_GROUPS, SSM_GROUP)
    if reverse:
        uf = uf[:, ::-1]
    bu_re = jnp.einsum('bngh,gph->bngp', uf, bbar_re)
    bu_im = jnp.einsum('bngh,gph->bngp', uf, bbar_im)
    bu_re = bu_re.at[:, 0].add(abar_re * s0_re - abar_im * s0_im)
    bu_im = bu_im.at[:, 0].add(abar_re * s0_im + abar_im * s0_re)
    a_re_b = jnp.broadcast_to(abar_re, bu_re.shape)
    a_im_b = jnp.broadcast_to(abar_im, bu_im.shape)
    _, _, st_re, st_im = lax.associative_scan(
        _complex_affine_combine, (a_re_b, a_im_b, bu_re, bu_im), axis=1)
    return st_re, st_im


def s5_readout(st_re, st_im, c_re, c_im, reverse):
    y = (jnp.einsum('bngp,ghp->bngh', st_re, c_re.astype(jnp.float32))
         - jnp.einsum('bngp,ghp->bngh', st_im, c_im.astype(jnp.float32)))
    if reverse:
        y = y[:, ::-1]
    return y.reshape(y.shape[0], y.shape[1], SSM_WIDTH)


def bidirectional_s5(u_lat, u_ctx, a_re, a_im, log_dt, b_re, b_im, c_re, c_im, with_ctx_out):
    bsz = u_lat.shape[0]
    zeros = jnp.zeros((bsz, N_SSM_GROUPS, SSM_STATE), jnp.float32)
    y_lat, y_ctx = [], []
    for d in range(2):
        reverse = d == 1
        disc = s5_discretise(a_re[d], a_im[d], log_dt[d], b_re[d], b_im[d])
        sc_re, sc_im = s5_states(u_ctx, zeros, zeros, *disc, reverse)
        sx_re, sx_im = s5_states(u_lat, sc_re[:, -1], sc_im[:, -1], *disc, reverse)
        y_lat.append(s5_readout(sx_re, sx_im, c_re[d], c_im[d], reverse))
        if with_ctx_out:
            y_ctx.append(s5_readout(sc_re, sc_im, c_re[d], c_im[d], reverse))
    return y_lat[0] + y_lat[1], (y_ctx[0] + y_ctx[1] if with_ctx_out else None)


def s5_output(y, u, d_skip, w_glu, b_glu):
    g = jax.nn.gelu(y + d_skip.astype(jnp.float32) * u.astype(jnp.float32)).astype(u.dtype)
    return g * jax.nn.sigmoid(g @ w_glu + b_glu)


def merge_groups(attn, ssm, gn_attn_g, gn_ssm_g, w_o):
    return jnp.concatenate([rms_norm(attn, gn_attn_g), rms_norm(ssm, gn_ssm_g)], -1) @ w_o


def hier_moe(h, w_rg, b_rg, w_re, b_re, w_gate, w_up, w_down):
    g_prob = jax.nn.softmax((h @ w_rg + b_rg).astype(jnp.float32), -1)
    g_w, g_idx = lax.top_k(g_prob, 1)
    e_logits = (h @ w_re + b_re).astype(jnp.float32).reshape(
        *h.shape[:-1], N_EXPERT_GROUPS, EXPERTS_PER_GROUP)
    g_sel = jax.nn.one_hot(g_idx[..., 0], N_EXPERT_GROUPS, dtype=jnp.float32)
    e_prob = jax.nn.softmax(jnp.einsum('...g,...ge->...e', g_sel, e_logits), -1)
    e_w, e_idx = lax.top_k(e_prob, EXPERT_TOP_K)
    e_w = e_w / jnp.sum(e_w, -1, keepdims=True) * g_w
    expert_id = g_idx * EXPERTS_PER_GROUP + e_idx
    combine = jnp.sum(jax.nn.one_hot(expert_id, N_EXPERTS, dtype=jnp.float32)
                      * e_w[..., None], axis=-2).astype(h.dtype)

    def per_sample(args):
        hs, cs = args
        a = jnp.einsum('nd,edf->nef', hs, w_gate)
        u = jnp.einsum('nd,edf->nef', hs, w_up)
        return jnp.einsum('nef,efd->nd', jax.nn.silu(a) * u * cs[..., None], w_down)

    return lax.map(per_sample, (h, combine))


def setup_inputs(seed: int = 0) -> dict:
    key = jax.random.key(seed)
    ks = iter(jax.random.split(key, 40))
    f32 = jnp.float32

    def nrm(shape, scale):
        return jax.random.normal(next(ks), shape, f32) * scale

    L, G, P = DEPTH, N_SSM_GROUPS, SSM_STATE
    n_idx = jnp.arange(P, dtype=f32)
    return {
        "x": nrm((BATCH, SEQ, D_MODEL), 1.0),
        "c": nrm((BATCH, D_MODEL), 1.0),
        "ctx": nrm((BATCH, CTX_LEN, D_MODEL), 1.0),
        "c_ctx": nrm((D_MODEL,), 1.0),
        "w_ada": nrm((L, D_MODEL, 6 * D_MODEL), 0.5 * D_MODEL ** -0.5),
        "b_ada": nrm((L, 6 * D_MODEL), 0.01),
        "w_in": nrm((L, D_MODEL, IN_PROJ_WIDTH), D_MODEL ** -0.5),
        "q_norm_g": 1.0 + nrm((L, Q_LORA_RANK), 0.01),
        "kv_norm_g": 1.0 + nrm((L, KV_LORA_RANK), 0.01),
        "w_uq": nrm((L, Q_LORA_RANK, N_HEADS * (QK_NOPE_DIM + QK_ROPE_DIM)), Q_LORA_RANK ** -0.5),
        "w_ukv": nrm((L, KV_LORA_RANK, N_HEADS * (QK_NOPE_DIM + V_HEAD_DIM)), KV_LORA_RANK ** -0.5),
        "ssm_a_re": -0.5 + nrm((L, 2, G, P), 0.01),
        "ssm_a_im": math.pi * n_idx + nrm((L, 2, G, P), 0.01),
        "ssm_log_dt": jax.random.uniform(next(ks), (L, 2, G), f32, math.log(DT_MIN), math.log(DT_MAX)),
        "ssm_b_re": nrm((L, 2, G, P, SSM_GROUP), (2 * SSM_GROUP) ** -0.5),
        "ssm_b_im": nrm((L, 2, G, P, SSM_GROUP), (2 * SSM_GROUP) ** -0.5),
        "ssm_c_re": nrm((L, 2, G, SSM_GROUP, P), (2 * P) ** -0.5),
        "ssm_c_im": nrm((L, 2, G, SSM_GROUP, P), (2 * P) ** -0.5),
        "ssm_d": nrm((L, SSM_WIDTH), 1.0),
        "w_glu": nrm((L, SSM_WIDTH, SSM_WIDTH), SSM_WIDTH ** -0.5),
        "b_glu": nrm((L, SSM_WIDTH), 0.01),
        "gn_attn_g": 1.0 + nrm((L, ATTN_WIDTH), 0.01),
        "gn_ssm_g": 1.0 + nrm((L, SSM_WIDTH), 0.01),
        "w_o": nrm((L, MIX_WIDTH, D_MODEL), MIX_WIDTH ** -0.5 * BETA),
        "ln1_g": 1.0 + nrm((L, D_MODEL), 0.01),
        "ln1_b": nrm((L, D_MODEL), 0.01),
        "w_router_group": nrm((L, D_MODEL, N_EXPERT_GROUPS), D_MODEL ** -0.5),
        "b_router_group": nrm((L, N_EXPERT_GROUPS), 0.01),
        "w_router_expert": nrm((L, D_MODEL, N_EXPERTS), D_MODEL ** -0.5),
        "b_router_expert": nrm((L, N_EXPERTS), 0.01),
        "w_exp_gate": nrm((L, N_EXPERTS, D_MODEL, D_FF_EXPERT), D_MODEL ** -0.5),
        "w_exp_up": nrm((L, N_EXPERTS, D_MODEL, D_FF_EXPERT), D_MODEL ** -0.5),
        "w_exp_down": nrm((L, N_EXPERTS, D_FF_EXPERT, D_MODEL), D_FF_EXPERT ** -0.5 * BETA),
        "ln2_g": 1.0 + nrm((L, D_MODEL), 0.01),
        "ln2_b": nrm((L, D_MODEL), 0.01),
    }


def reference(x, c, ctx, c_ctx, w_ada, b_ada, w_in, q_norm_g, kv_norm_g, w_uq, w_ukv,
              ssm_a_re, ssm_a_im, ssm_log_dt, ssm_b_re, ssm_b_im, ssm_c_re, ssm_c_im, ssm_d,
              w_glu, b_glu, gn_attn_g, gn_ssm_g, w_o, ln1_g, ln1_b,
              w_router_group, b_router_group, w_router_expert, b_router_expert,
              w_exp_gate, w_exp_up, w_exp_down, ln2_g, ln2_b):
    n_lat = x.shape[1]
    rope = axial_rope_tables(n_lat)
    silu_c = jax.nn.silu(c)
    silu_cc = jax.nn.silu(c_ctx)
    for l in range(DEPTH):
        last = l == DEPTH - 1
        mod_x = (silu_c @ w_ada[l] + b_ada[l])[:, None, :]
        mod_c = silu_cc @ w_ada[l] + b_ada[l]
        sh_a_x, sc_a_x, g_a_x, sh_f_x, sc_f_x, g_f_x = jnp.split(mod_x, 6, -1)
        sh_a_c, sc_a_c, g_a_c, sh_f_c, sc_f_c, g_f_c = jnp.split(mod_c, 6, -1)

        p_x = modulate(x, sh_a_x, sc_a_x) @ w_in[l]
        p_c = modulate(ctx, sh_a_c, sc_a_c) @ w_in[l]
        qx_n, qx_r, kx_n, kx_r, vx = mla_project(p_x[..., :MLA_IN_WIDTH], q_norm_g[l], kv_norm_g[l],
                                                 w_uq[l], w_ukv[l], rope)
        qc_n, qc_r, kc_n, kc_r, vc = mla_project(p_c[..., :MLA_IN_WIDTH], q_norm_g[l], kv_norm_g[l],
                                                 w_uq[l], w_ukv[l], None)
        attn_x = latent_attention(qx_n, qx_r,
                                  jnp.concatenate([kx_n, kc_n], 1),
                                  jnp.concatenate([kx_r, kc_r], 1),
                                  jnp.concatenate([vx, vc], 1))
        u_x, u_c = p_x[..., MLA_IN_WIDTH:], p_c[..., MLA_IN_WIDTH:]
        y_x, y_c = bidirectional_s5(u_x, u_c, ssm_a_re[l], ssm_a_im[l], ssm_log_dt[l],
                                    ssm_b_re[l], ssm_b_im[l], ssm_c_re[l], ssm_c_im[l],
                                    with_ctx_out=not last)
        ssm_x = s5_output(y_x, u_x, ssm_d[l], w_glu[l], b_glu[l])
        mix_x = merge_groups(attn_x, ssm_x, gn_attn_g[l], gn_ssm_g[l], w_o[l])
        x = layer_norm(ALPHA * x + g_a_x * mix_x, ln1_g[l], ln1_b[l])
        if not last:
            attn_c = mla_attend(qc_n, qc_r, kc_n, kc_r, vc).reshape(*ctx.shape[:2], ATTN_WIDTH)
            ssm_c = s5_output(y_c, u_c, ssm_d[l], w_glu[l], b_glu[l])
            mix_c = merge_groups(attn_c, ssm_c, gn_attn_g[l], gn_ssm_g[l], w_o[l])
            ctx = layer_norm(ALPHA * ctx + g_a_c * mix_c, ln1_g[l], ln1_b[l])

        ffn_x = hier_moe(modulate(x, sh_f_x, sc_f_x), w_router_group[l], b_router_group[l],
                         w_router_expert[l], b_router_expert[l],
                         w_exp_gate[l], w_exp_up[l], w_exp_down[l])
        x = layer_norm(ALPHA * x + g_f_x * ffn_x, ln2_g[l], ln2_b[l])
        if not last:
            ffn_c = hier_moe(modulate(ctx, sh_f_c, sc_f_c), w_router_group[l], b_router_group[l],
                             w_router_expert[l], b_router_expert[l],
                             w_exp_gate[l], w_exp_up[l], w_exp_down[l])
            ctx = layer_norm(ALPHA * ctx + g_f_c * ffn_c, ln2_g[l], ln2_b[l])
    return x
```

```python
import math
import os
from contextlib import ExitStack
import numpy as np
import concourse.bass as bass
import concourse.mybir as mybir
from concourse.ap import AP
from concourse.bass_utils import run_bass_kernel_spmd

F32 = mybir.dt.float32
BF16 = mybir.dt.bfloat16
AF = mybir.ActivationFunctionType
ALU = mybir.AluOpType
AX = mybir.AxisListType

D = 1024
NTOK = 4096
NOWN = 2048
NCTX = 256
NKEY = NTOK + NCTX
NH = 8
SCALE = 96 ** -0.5
ALPHA = 2.0 ** 0.25
EPS = 1e-6
NS = 134
NE = 32


class Buf:
    __slots__ = ("name", "w", "r", "excl")

    def __init__(self, name, excl=False):
        self.name = name
        self.w = None
        self.r = []
        self.excl = excl


def bufs(name, n):
    return [Buf("%s%d" % (name, i)) for i in range(n)]


class Q:
    def __init__(self, fw, name, eng, same=False):
        self.name = name
        self.eng = eng
        self.sem = fw.es.enter_context(fw.nc.semaphore("s_" + name))
        self.cnt = 0
        self.seen = {}
        self.same = same
        self.window = False


class DmaTok:
    def __init__(self, fw, name):
        self.sem = fw.es.enter_context(fw.nc.semaphore("d_" + str(name)))
        self.cnt = 0
        self.name = name
        self.same = True


class FW:
    def __init__(self, nc, es):
        self.nc = nc
        self.es = es
        self.pe = Q(self, "pe", nc.tensor)
        self.dve = Q(self, "dve", nc.vector, same=True)
        self.act = Q(self, "act", nc.scalar, same=True)
        self.dve.window = False
        self.pool = Q(self, "pool", nc.gpsimd, same=True)
        self.sp = Q(self, "sp", nc.sync)
        self.dmasems = {}

    def _waits(self, q, reads, writes, sreads=()):
        need = {}

        def add(dep, force=False):
            if dep is None:
                return
            dq, n = dep
            if dq is q and not force:
                if not q.same:
                    return
                if q.window and n < q.cnt:
                    return
            if need.get(dq, 0) < n:
                need[dq] = n

        for b in sreads:
            add(b.w, True)
        for b in reads:
            add(b.w)
            if b.excl:
                for d in b.r:
                    add(d)
        for b in writes:
            add(b.w)
            for d in b.r:
                add(d)
        for dq, n in need.items():
            if q.seen.get(id(dq), 0) >= n:
                continue
            q.eng.wait_ge(dq.sem, n)
            q.seen[id(dq)] = n

    def op(self, q, fn, reads=(), writes=(), sreads=()):
        self._waits(q, reads, writes, sreads)
        reads = list(reads) + list(sreads)
        ins = fn(q.eng)
        ins.then_inc(q.sem, 1)
        q.cnt += 1
        tok = (q, q.cnt)
        for b in reads:
            b.r.append(tok)
        for b in writes:
            b.w = tok
            b.r = []
        return ins

    def dma(self, q, out, in_, reads=(), writes=(), sem=None, **kw):
        self._waits(q, reads, writes)
        ds = self.dmasems.get(sem)
        if ds is None:
            ds = DmaTok(self, sem)
            self.dmasems[sem] = ds
        if out.dtype != in_.dtype and in_.ap[-1][1] > 2048:
            kw.setdefault("max_dma_last_dim", 4096)
        ins = q.eng.dma_start(out=out, in_=in_, **kw)
        ds.cnt += 16
        ins.then_inc(ds.sem, 16)
        tok = (ds, ds.cnt)
        for b in reads:
            b.r.append(tok)
        for b in writes:
            b.w = tok
            b.r = []
        return tok

    def barrier(self):
        qs = [self.pe, self.dve, self.act, self.pool, self.sp]
        for q in qs:
            for dq in qs + list(self.dmasems.values()):
                if dq is q or dq.cnt == 0:
                    continue
                if q.seen.get(id(dq), 0) >= dq.cnt:
                    continue
                q.eng.wait_ge(dq.sem, dq.cnt)
                q.seen[id(dq)] = dq.cnt

    def group_done(self, sem, blist):
        ds = self.dmasems[sem]
        for b in blist:
            if b.w is not None and b.w[0] is ds:
                b.w = (ds, ds.cnt)


def sbap(t, off, dims):
    full = t[:] if not isinstance(t, AP) else t
    return AP(full.tensor, full.offset + off, [list(full.ap[0])] + [list(d) for d in dims])


def part_ap(t, p0, npart, off, dims):
    full = t[:]
    pstep = full.ap[0][0]
    sub = t[p0:p0 + npart]
    return AP(sub.tensor, sub.offset + off, [[pstep, npart]] + [list(d) for d in dims])


def build(stop_after=None, dbg_names=()):
    nc = bass.Bass("TRN2", target_bir_lowering=False)
    es = ExitStack()
    fw = FW(nc, es)
    pe, dve, act, pool, sp = fw.pe, fw.dve, fw.act, fw.pool, fw.sp
    dbg = {}

    def din(name, shape, dt=F32):
        return nc.dram_tensor(name, list(shape), dt, kind="ExternalInput").ap()

    xf = din("xf", [NTOK, D])
    ctxf = din("ctxf", [NCTX, D])
    cT_d = din("cT", [128, 8, 2])
    w_ada_d = din("w_ada", [D, 6 * D])
    b_ada_cols_d = din("b_ada_cols", [128, 96])
    b_ada_rows_d = din("b_ada_rows", [128, 4, D])
    w_in_d = din("w_in", [D, 928])
    qg_d = din("qg_cols", [128, 2])
    kvg_d = din("kvg_col", [128, 1])
    w_uq_d = din("w_uq", [256, 768])
    w_ukv_d = din("w_ukv", [128, 1024])
    rope_d = din("rope", [128, 2, NKEY])
    ssm_a_d = din("ssm_a", [128, 2, 32])
    ssm_ldt_d = din("ssm_ldt", [128, 32])
    ssm_b_d = din("ssm_b", [128, 2, 32, 16])
    ssm_c_d = din("ssm_c", [128, 2, 32, 16])
    ssm_dcols_d = din("ssm_dcols", [128, 32])
    masks_d = din("masks", [128, 2, 128])
    ident_d = din("ident", [128, 128])
    w_glu_d = din("w_glu", [512, 512])
    b_glu_d = din("b_glu_row", [128, 512])
    gn_d = din("gn_cols", [128, 8])
    w_o_d = din("w_o", [D, D])
    ln_d = din("ln_rows", [128, 4, D])
    w_r_d = din("w_r", [D, 36])
    b_r_d = din("b_r_row", [128, 36])
    wg_d = din("w_exp_gate", [NE, D, 256])
    wu_d = din("w_exp_up", [NE, D, 256])
    wd_d = din("w_exp_down", [NE, 256, D])
    sel_d = din("sel", [32, NE * 128])
    out_d = nc.dram_tensor("out", [NOWN, D], F32, kind="ExternalOutput").ap()
    ug_s = nc.dram_tensor("ug_s", [128, 32 * 2 * 272], BF16, kind="Internal").ap()
    attn_s = nc.dram_tensor("attn_s", [128, 16 * 512], F32, kind="Internal").ap()
    x1_s = nc.dram_tensor("x1_s", [128, 16 * D], F32, kind="Internal").ap()
    g_s = nc.dram_tensor("g_s", [128, 16 * 512], F32, kind="Internal").ap()
    dbg_out = {}
    for ent in dbg_names:
        nm, shp = ent[0], ent[1]
        dbg_out[nm] = nc.dram_tensor("dbg_" + nm, list(shp), BF16 if (len(ent) > 2 and ent[2] == "bf16") else F32, kind="ExternalOutput").ap()

    def sb(name, shape, dt=F32, stack=None):
        return (stack or es).enter_context(nc.sbuf_tensor("sb_" + name, list(shape), dt))

    PS = [es.enter_context(nc.psum_tensor("ps%d" % i, [128, 512], F32)) for i in range(8)]
    PSB = [Buf("ps%d" % i, excl=True) for i in range(8)]
    ps_rr = [0]

    def ps_next(k=None):
        i = ps_rr[0] % 8
        ps_rr[0] += 1
        return PS[i], PSB[i]

    ident = sb("ident", [128, 128]); B_ident = Buf("ident")
    ones_bf = sb("ones_bf", [128, 128], BF16); B_ones = Buf("ones")
    modc = sb("modc", [128, 96]); B_modc = Buf("modc")
    grow = sb("grow", [128, 4, D]); B_grow = Buf("grow")
    fw.dma(sp, ident[:], ident_d[:, :], writes=[B_ident], sem="c0")
    fw.op(dve, lambda e: e.memset(ones_bf[:], 1.0), writes=[B_ones])

    def dump(name, ap, reads):
        if name in dbg_out:
            fw.dma(sp, dbg_out[name], ap, reads=reads, writes=[], sem="dbg")

    def finish():
        for ds in fw.dmasems.values():
            sp.eng.wait_ge(ds.sem, ds.cnt)
        es.close()
        return nc

    with ExitStack() as ph:
        cT = sb("cT", [128, 8, 2], stack=ph); B_cT = Buf("cT")
        sil2 = sb("sil2", [128, 8, 2], stack=ph); B_sil = Buf("sil2")
        silrep = sb("silrep", [128, 8, 128], stack=ph); B_silrep = Buf("silrep")
        bcols = sb("bcols", [128, 96], stack=ph); B_bcols = Buf("bcols")
        brows = sb("brows", [128, 4, D], stack=ph); B_brows = Buf("brows")
        wa = [sb("wa%d" % i, [128, 8, D], stack=ph) for i in range(2)]
        B_wa = bufs("wa", 2)
        fw.dma(sp, cT[:], cT_d[:, :, :], writes=[B_cT], sem="c1")
        fw.dma(sp, bcols[:], b_ada_cols_d[:, :], writes=[B_bcols], sem="c2")
        fw.dma(sp, brows[:], b_ada_rows_d[:, :, :], writes=[B_brows], sem="c3")
        fw.op(act, lambda e: e.activation(out=sil2[:], in_=cT[:], func=AF.Silu), reads=[B_cT], writes=[B_sil])
        for k in range(8):
            fw.op(dve, lambda e: e.tensor_copy(out=silrep[:, k, :], in_=sil2[:, k, 0:1].to_broadcast([128, 128])),
                  reads=[B_sil], writes=[B_silrep])
        w_ada_v = w_ada_d.rearrange("(k p) n -> p k n", p=128)
        psc, Bpsc = PS[0], PSB[0]
        vorder = [0, 1, 2, 3, 4, 5]
        for vi, v in enumerate(vorder):
            slot = vi % 2
            fw.dma(sp if vi % 2 == 0 else act, wa[slot][:], w_ada_v[:, :, v * D:(v + 1) * D], writes=[B_wa[slot]], sem="wa%d" % slot)
            for ch in range(8):
                for k in range(8):
                    fw.op(pe, lambda e: e.matmul(psc[:, (v * 8 + ch) * 2:(v * 8 + ch) * 2 + 2], lhsT=wa[slot][:, k, ch * 128:(ch + 1) * 128],
                                                 rhs=sil2[:, k, :], start=(k == 0), stop=(k == 7)),
                          reads=[B_wa[slot], B_sil], writes=[Bpsc])
            if v in (2, 3, 4, 5):
                ri = v - 2
                for half in range(2):
                    pr, Bpr = PS[1 + half], PSB[1 + half]
                    for k in range(8):
                        fw.op(pe, lambda e: e.matmul(pr[:, :], lhsT=silrep[:, k, :], rhs=wa[slot][:, k, half * 512:(half + 1) * 512],
                                                     start=(k == 0), stop=(k == 7)),
                              reads=[B_wa[slot], B_silrep], writes=[Bpr])
                    fw.op(dve, lambda e: e.tensor_tensor(out=grow[:, ri, half * 512:(half + 1) * 512], in0=pr[:, :],
                                                         in1=brows[:, ri, half * 512:(half + 1) * 512], op=ALU.add),
                          reads=[Bpr, B_brows], writes=[B_grow])
        fw.op(dve, lambda e: e.tensor_tensor(out=modc[:], in0=psc[:, 0:96], in1=bcols[:], op=ALU.add),
              reads=[Bpsc, B_bcols], writes=[B_modc])
        for v in (1, 4):
            fw.op(dve, lambda e: e.tensor_scalar_add(out=modc[:, v * 16:(v + 1) * 16], in0=modc[:, v * 16:(v + 1) * 16], scalar1=1.0),
                  reads=[B_modc], writes=[B_modc])
        fw.op(dve, lambda e: e.tensor_scalar_add(out=grow[:, 2, :], in0=grow[:, 2, :], scalar1=1.0), reads=[B_grow], writes=[B_grow])
    fw.barrier()
    dump("modc", modc[:], [B_modc])
    dump("grow", grow[:, 0:2].rearrange("p a b -> p (a b)"), [B_grow])
    if stop_after == "adaln":
        return finish()

    def mcol(v, ch, who):
        i = (v * 8 + ch) * 2 + who
        return modc[:, i:i + 1]

    def A(q, out, in_, func, reads, writes, **kw):
        return fw.op(q, lambda e: e.activation(out=out, in_=in_, func=func, **kw), reads=reads, writes=writes)

    def TT(q, out, in0, in1, op, reads, writes):
        return fw.op(q, lambda e: e.tensor_tensor(out=out, in0=in0, in1=in1, op=op), reads=reads, writes=writes)

    def TS(q, out, in0, s1, s2, op0, op1, reads, writes, sreads=()):
        return fw.op(q, lambda e: e.tensor_scalar(out=out, in0=in0, scalar1=s1, scalar2=s2, op0=op0, op1=op1), reads=reads, writes=writes, sreads=sreads)

    def STT(out, in0, scalar, in1, op0, op1, reads, writes):
        return fw.op(dve, lambda e: e.scalar_tensor_tensor(out=out, in0=in0, scalar=scalar, in1=in1, op0=op0, op1=op1), reads=reads, writes=writes)

    def CP(q, out, in_, reads, writes):
        return fw.op(q, lambda e: e.tensor_copy(out=out, in_=in_), reads=reads, writes=writes)

    def MM(out, lhsT, rhs, start, stop, reads, writes):
        return fw.op(pe, lambda e: e.matmul(out, lhsT=lhsT, rhs=rhs, start=start, stop=stop), reads=reads, writes=writes)

    def TR(out, in_, reads, writes, n=128):
        return fw.op(pe, lambda e: e.matmul(out, lhsT=in_, rhs=ident[0:n, 0:n], start=True, stop=True, is_transpose=True),
                     reads=list(reads) + [B_ident], writes=writes)

    def rstd_from_ss(out, ss_ps, B_ss, inv_n, tmp, B_tmp, B_out):
        A(act, tmp, ss_ps, AF.Ln, [B_ss, B_eps], [B_tmp], scale=inv_n, bias=epsc[:, 0:1])
        A(act, out, tmp, AF.Exp, [B_tmp], [B_out], scale=-0.5)

    epsc = sb("epsc", [128, 1]); B_eps = Buf("eps")
    fw.op(dve, lambda e: e.memset(epsc[:], EPS), writes=[B_eps])

    att = ExitStack()
    cqT = sb("cqT", [128, 2, NOWN], BF16, stack=att); B_cqT = Buf("cqT")
    rstdq = sb("rstdq", [128, NOWN], stack=att); B_rstdq = Buf("rstdq")
    ckvnT = sb("ckvnT", [128, NKEY], BF16, stack=att); B_ckvnT = Buf("ckvnT")
    KRT = sb("KRT", [128, NKEY], BF16, stack=att); B_KRT = Buf("KRT")

    with ExitStack() as ph:
        w_in = sb("w_in", [128, 8, 928], BF16, stack=ph); B_win = Buf("w_in")
        w_krot = sb("w_krot", [128, 8, 32], BF16, stack=ph); B_wkrot = Buf("w_krot")
        fw.dma(pool, w_in[:], w_in_d.rearrange("(k p) n -> p k n", p=128), writes=[B_win], sem="w_in")
        CP(dve, w_krot[:, :, 16:32], w_in[:, :, 384:400], [B_win], [B_wkrot])
        TS(dve, w_krot[:, :, 0:16], w_in[:, :, 400:416], -1.0, None, ALU.mult, ALU.bypass, [B_win], [B_wkrot])
        xin = [sb("xin%d" % i, [128, D], stack=ph) for i in range(4)]; B_xin = bufs("xin", 4)
        xmT = [sb("xmT%d" % i, [128, 8, 512], BF16, stack=ph) for i in range(2)]; B_xmT = bufs("xmT", 2)
        ropeg = [sb("ropeg%d" % i, [128, 2, 512], stack=ph) for i in range(2)]; B_ropeg = bufs("ropeg", 2)
        u_tm = sb("u_tm", [128, 32, 128], stack=ph); B_utm = Buf("u_tm")
        Ug = sb("Ug", [128, 32, 2, 272], BF16, stack=ph); B_Ug = Buf("Ug")
        sqb = sb("sqb", [128, 512], BF16, stack=ph); B_sqb = Buf("sqb")
        sqq = sb("sqq", [128, 2, 512], BF16, stack=ph); B_sqq = Buf("sqq")
        rawkv = sb("rawkv", [128, 512], stack=ph); B_rawkv = Buf("rawkv")
        lnt = sb("lnt", [128, 512], stack=ph); B_lnt = Buf("lnt")
        rstdkv = sb("rstdkv", [128, 512], stack=ph); B_rstdkv = Buf("rstdkv")
        rt1 = sb("rt1", [128, 512], stack=ph); B_rt1 = Buf("rt1")
        rt2 = sb("rt2", [128, 512], stack=ph); B_rt2 = Buf("rt2")
        xf_v = xf.rearrange("(m j) d -> j m d", j=16)
        ctx_v = ctxf.rearrange("(m j) d -> j m d", j=16)
        gcount = [0]

        def in_group(kind, mt, jg):
            gs = gcount[0] % 2
            gcount[0] += 1
            lat = kind == "lat"
            ncols = 512 if lat else 256
            col0 = (mt * 16 + jg * 4) * 128 if lat else NTOK
            own = lat and mt == 0
            np_ = 128 if lat else 16
            nt = 4 if lat else 16
            tw = 128 if lat else 16
            if lat:
                for t in range(4):
                    j = jg * 4 + t
                    fw.dma(sp, xin[t][:], xf_v[j, mt * 128:(mt + 1) * 128, :], writes=[B_xin[t]], sem="xin%d" % t)
            fw.dma(sp, ropeg[gs][64:96, :, 0:ncols], rope_d[64:96, :, col0:col0 + ncols], writes=[B_ropeg[gs]], sem="ropeg%d" % gs)
            who = 0 if lat else 1
            if lat:
                for ch in range(8):
                    ps, Bp = ps_next()
                    for t in range(4):
                        TR(ps[:, t * 128:(t + 1) * 128], xin[t][:, ch * 128:(ch + 1) * 128], [B_xin[t]], [Bp])
                    A(act, xmT[gs][:, ch, 0:ncols], ps[:, 0:ncols], AF.Identity, [Bp, B_modc], [B_xmT[gs]],
                      scale=mcol(1, ch, who), bias=mcol(0, ch, who))
            else:
                banks = [ps_next() for _ in range(8)]
                for t in range(16):
                    fw.dma(sp, xin[t % 4][0:16, :], ctx_v[t, 0:16, :], writes=[B_xin[t % 4]], sem="xin%d" % (t % 4))
                    for ch in range(8):
                        ps, Bp = banks[ch]
                        TR(ps[:, t * 16:(t + 1) * 16], xin[t % 4][0:16, ch * 128:(ch + 1) * 128], [B_xin[t % 4]], [Bp], n=16)
                for ch in range(8):
                    ps, Bp = banks[ch]
                    A(act, xmT[gs][:, ch, 0:ncols], ps[:, 0:ncols], AF.Identity, [Bp, B_modc], [B_xmT[gs]],
                      scale=mcol(1, ch, who), bias=mcol(0, ch, who))
            X = xmT[gs]; BX = B_xmT[gs]
            kstep = int(os.environ.get("K_STEP", "9"))
            if kstep < 2:
                return
            ps, Bp = ps_next()
            for ch in range(8):
                MM(ps[:, 0:ncols], w_in[:, ch, 256:384], X[:, ch, 0:ncols], ch == 0, ch == 7, [B_win, BX], [Bp])
            A(act, sqb[:, 0:ncols], ps[:, 0:ncols], AF.Square, [Bp], [B_sqb])
            A(act, rawkv[:, 0:ncols], ps[:, 0:ncols], AF.Copy, [Bp], [B_rawkv])
            ps2, Bp2 = ps_next()
            MM(ps2[:, 0:ncols], ones_bf[:, :], sqb[:, 0:ncols], True, True, [B_ones, B_sqb], [Bp2])
            rstd_from_ss(rstdkv[:, 0:ncols], ps2[:, 0:ncols], Bp2, 1.0 / 128, lnt[:, 0:ncols], B_lnt, B_rstdkv)
            TT(dve, ckvnT[:, col0:col0 + ncols], rawkv[:, 0:ncols], rstdkv[:, 0:ncols], ALU.mult, [B_rawkv, B_rstdkv], [B_ckvnT])
            if kstep < 3:
                return
            psa, Bpa = ps_next()
            psb, Bpb = ps_next()
            for ch in range(8):
                MM(psa[64:96, 0:ncols], w_in[:, ch, 384:416], X[:, ch, 0:ncols], ch == 0, ch == 7, [B_win, BX], [Bpa])
            for ch in range(8):
                MM(psb[64:96, 0:ncols], w_krot[:, ch, :], X[:, ch, 0:ncols], ch == 0, ch == 7, [B_wkrot, BX], [Bpb])
            TT(dve, rt1[64:96, 0:ncols], psa[64:96, 0:ncols], ropeg[gs][64:96, 0, 0:ncols], ALU.mult, [Bpa, B_ropeg[gs]], [B_rt1])
            TT(dve, rt2[64:96, 0:ncols], psb[64:96, 0:ncols], ropeg[gs][64:96, 1, 0:ncols], ALU.mult, [Bpb, B_ropeg[gs]], [B_rt2])
            TT(dve, KRT[64:96, col0:col0 + ncols], rt1[64:96, 0:ncols], rt2[64:96, 0:ncols], ALU.add, [B_rt1, B_rt2], [B_KRT])
            if kstep < 4:
                return
            if own:
                pss = []
                for fc in range(2):
                    ps, Bp = ps_next()
                    for ch in range(8):
                        MM(ps[:, :], w_in[:, ch, fc * 128:(fc + 1) * 128], X[:, ch, :], ch == 0, ch == 7, [B_win, BX], [Bp])
                    A(act, cqT[:, fc, col0:col0 + 512], ps[:, :], AF.Copy, [Bp], [B_cqT])
                    pss.append((ps, Bp))
                ksub = int(os.environ.get("K_SUB", "9"))
                if ksub >= 1:
                    ps2, Bp2 = ps_next()
                    for fc in range(2):
                        ps, Bp = pss[fc]
                        A(act, sqq[:, fc, :], ps[:, :], AF.Square, [Bp], [B_sqq])
                    for fc in range(2):
                        MM(ps2[:, :], ones_bf[:, :], sqq[:, fc, :], fc == 0, fc == 1, [B_ones, B_sqq], [Bp2])
                if ksub >= 2:
                    rstd_from_ss(rstdq[:, col0:col0 + 512], ps2[:, :], Bp2, 1.0 / 256, lnt[:, :], B_lnt, B_rstdq)
            if kstep < 5:
                return
            for t in range(nt):
                ps, Bp = ps_next()
                for ch in range(8):
                    MM(ps[0:np_, :], X[:, ch, t * tw:(t + 1) * tw], w_in[:, ch, 416:928], ch == 0, ch == 7, [B_win, BX], [Bp])
                if lat:
                    jl = (jg * 4 + t) % 8
                    uo = sbap(u_tm, jl * 16, [[128, 32], [1, 16]])
                    ui = sbap(ps, 0, [[16, 32], [1, 16]])
                    if t % 2 == 0:
                        CP(dve, uo, ui, [Bp], [B_utm])
                    else:
                        A(act, uo, ui, AF.Copy, [Bp], [B_utm])
                else:
                    A(act, part_ap(u_tm, 0, 16, (t % 8) * 16, [[128, 32], [1, 16]]), part_ap(ps, 0, 16, 0, [[16, 32], [1, 16]]),
                      AF.Copy, [Bp], [B_utm])
                    if t % 8 == 7:
                        ug_transposes(u_tm, B_utm, 16, t // 8, 256, 0)

        def ug_transposes(src, Bsrc, np_, jt, ucol0, jbase):
            for g4 in range(8):
                ps, Bp = ps_next()
                for gg in range(4):
                    g = g4 * 4 + gg
                    inap = part_ap(src, 0, np_, g * 128, [[1, 128]])
                    TR(ps[:, gg * 128:gg * 128 + np_], inap, [Bsrc], [Bp], n=np_)
                outap = sbap(Ug, (g4 * 4) * 2 * 272 + jt * 272 + ucol0, [[2 * 272, 4], [1, np_]])
                inp_ = sbap(ps, 0, [[128, 4], [1, np_]])
                if g4 % 2 == 0:
                    CP(dve, outap, inp_, [Bp], [B_Ug])
                else:
                    A(act, outap, inp_, AF.Copy, [Bp], [B_Ug])

        bis = int(os.environ.get("K_BISECT", "99"))
        for mt in range(2):
            for jg in range(4):
                if mt * 4 + jg >= bis:
                    continue
                in_group("lat", mt, jg)
                if jg % 2 == 1 and bis != 2:
                    ug_transposes(u_tm, B_utm, 128, jg // 2, mt * 128, 0)
        if bis >= 99:
            in_group("ctx", 0, 0)
        dump("ckvnT", ckvnT[:], [B_ckvnT])
        dump("KRT", KRT[64:96, :], [B_KRT])
        dump("cqT", cqT[:].rearrange("p a b -> p (a b)"), [B_cqT])
        dump("rstdq", rstdq[:], [B_rstdq])
        dump("Ug", Ug[:].rearrange("p a b c -> p (a b c)"), [B_Ug])
        fw.dma(sp, ug_s[:, :], Ug[:].rearrange("p a b c -> p (a b c)"), reads=[B_Ug], writes=[], sem="ugs")
    fw.barrier()
    if stop_after == "inproj":
        return finish()

    attn_tm = sb("attn_tm", [128, 16, 512], stack=att); B_attn = Buf("attn_tm")
    B_attn_s = Buf("attn_s")
    with ExitStack() as ph:
        wq_f = sb("wq_f", [128, 2, 768], stack=ph); B_wqf = Buf("wq_f")
        wkv_f = sb("wkv_f", [128, 1024], stack=ph); B_wkvf = Buf("wkv_f")
        qg = sb("qg", [128, 2], stack=ph); kvg = sb("kvg", [128, 1], stack=ph); B_g = Buf("qkvg")
        w_uq_s = sb("w_uq_s", [128, 2, 768], BF16, stack=ph); B_wuq = Buf("w_uq_s")
        w_uq_rot = sb("w_uq_rot", [128, 2, 8, 32], BF16, stack=ph); B_wuqr = Buf("w_uq_rot")
        w_k = sb("w_k", [128, 1024], BF16, stack=ph); B_wk = Buf("w_k")
        w_v = sb("w_v", [128, 512], BF16, stack=ph); B_wv = Buf("w_v")
        V_all = sb("V_all", [128, 34, 8, 65], BF16, stack=ph); B_V = Buf("V_all")
        KT = [sb("KT%d" % i, [128, NKEY], BF16, stack=ph) for i in range(2)]; B_KT = bufs("KT", 2)
        QT = [sb("QT%d" % i, [128, NOWN], BF16, stack=ph) for i in range(2)]; B_QT = bufs("QT", 2)
        ropeq = sb("ropeq", [128, 2, NOWN], stack=ph); B_ropeq = Buf("ropeq")
        PT = [sb("PT%d" % i, [128, 512], BF16, stack=ph) for i in range(4)]; B_PT = bufs("PT", 4)
        ot = [sb("ot%d" % i, [128, 512], stack=ph) for i in range(2)]; B_ot = bufs("ot", 2)
        qt1 = sb("qt1", [128, 512], stack=ph); B_qt1 = Buf("qt1")
        qt2 = sb("qt2", [128, 512], stack=ph); B_qt2 = Buf("qt2")
        rd = sb("rd", [128, 4], stack=ph); B_rd = Buf("rd")
        fw.dma(sp, wq_f[:], w_uq_d.rearrange("(c p) n -> p c n", p=128), writes=[B_wqf], sem="a0")
        fw.dma(sp, wkv_f[:], w_ukv_d[:, :], writes=[B_wkvf], sem="a1")
        fw.dma(sp, qg[:], qg_d[:, :], writes=[B_g], sem="a2")
        fw.dma(sp, kvg[:], kvg_d[:, :], writes=[B_g], sem="a3")
        fw.dma(sp, ropeq[64:96, :, :], rope_d[64:96, :, 0:NOWN], writes=[B_ropeq], sem="a4")
        for c in range(2):
            TS(dve, w_uq_s[:, c, :], wq_f[:, c, :], qg[:, c:c + 1], None, ALU.mult, ALU.bypass, [B_wqf, B_g], [B_wuq])
            TS(dve, w_uq_rot[:, c, :, 0:16], sbap(w_uq_s, c * 768 + 80, [[96, 8], [1, 16]]), -1.0, None, ALU.mult, ALU.bypass, [B_wuq], [B_wuqr])
            CP(dve, w_uq_rot[:, c, :, 16:32], sbap(w_uq_s, c * 768 + 64, [[96, 8], [1, 16]]), [B_wuq], [B_wuqr])
        TS(dve, w_k[:, :], wkv_f[:, :], kvg[:, 0:1], None, ALU.mult, ALU.bypass, [B_wkvf, B_g], [B_wk])
        CP(dve, w_v[:].rearrange("p (h d) -> p h d", h=8), sbap(w_k, 64, [[128, 8], [1, 64]]), [B_wk], [B_wv])
        fw.op(dve, lambda e: e.memset(V_all[:, :, :, 64:65], 1.0), writes=[B_V])
        for i in range(2):
            fw.op(dve, lambda e: e.memset(KT[i][96:128, :], 0.0), writes=[B_KT[i]])
            fw.op(dve, lambda e: e.memset(QT[i][96:128, :], 0.0), writes=[B_QT[i]])
        for kt in range(34):
            ps, Bp = PS[6 + kt % 2], PSB[6 + kt % 2]
            MM(ps[:, :], ckvnT[:, kt * 128:(kt + 1) * 128], w_v[:, :], True, True, [B_ckvnT, B_wv], [Bp])
            vo = V_all[:, kt, :, 0:64]
            vi = ps[:, :].rearrange("p (h d) -> p h d", h=8)
            if kt % 2 == 0:
                CP(dve, vo, vi, [Bp], [B_V])
            else:
                A(act, vo, vi, AF.Copy, [Bp], [B_V])

        def gen(h):
            s = h % 2
            for ct in range(9):
                n = 512 if ct < 8 else 256
                c0 = ct * 512
                ps, Bp = PS[6], PSB[6]
                MM(ps[0:64, 0:n], w_k[:, h * 128:h * 128 + 64], ckvnT[:, c0:c0 + n], True, True, [B_wk, B_ckvnT], [Bp])
                CP(dve, KT[s][0:64, c0:c0 + n], ps[0:64, 0:n], [Bp], [B_KT[s]])
                yield
            CP(pool, KT[s][64:96, :], KRT[64:96, :], [B_KRT], [B_KT[s]])
            for ct in range(4):
                c0 = ct * 512
                psq, Bq = PS[6], PSB[6]
                psr, Br = PS[7], PSB[7]
                for c in range(2):
                    MM(psq[0:96, :], w_uq_s[:, c, h * 96:(h + 1) * 96], cqT[:, c, c0:c0 + 512], c == 0, c == 1, [B_wuq, B_cqT], [Bq])
                for c in range(2):
                    MM(psr[64:96, :], w_uq_rot[:, c, h, :], cqT[:, c, c0:c0 + 512], c == 0, c == 1, [B_wuqr, B_cqT], [Br])
                TT(dve, QT[s][0:64, c0:c0 + 512], psq[0:64, :], rstdq[0:64, c0:c0 + 512], ALU.mult, [Bq, B_rstdq], [B_QT[s]])
                TT(dve, qt1[64:96, :], psq[64:96, :], ropeq[64:96, 0, c0:c0 + 512], ALU.mult, [Bq, B_ropeq], [B_qt1])
                TT(dve, qt2[64:96, :], psr[64:96, :], ropeq[64:96, 1, c0:c0 + 512], ALU.mult, [Br, B_ropeq], [B_qt2])
                TT(dve, qt1[64:96, :], qt1[64:96, :], qt2[64:96, :], ALU.add, [B_qt1, B_qt2], [B_qt1])
                TT(dve, QT[s][64:96, c0:c0 + 512], qt1[64:96, :], rstdq[64:96, c0:c0 + 512], ALU.mult, [B_qt1, B_rstdq], [B_QT[s]])
                yield

        scount = [0]
        ocount = [0]

        pend = [None]

        def epilogue(h, qt, ob, osl):
            CP(dve, ot[osl][0:65, :], PS[ob][0:65, :], [PSB[ob]], [B_ot[osl]])
            pso, Bo = PS[7], PSB[7]
            for t in range(4):
                TR(pso[:, t * 128:t * 128 + 65], ot[osl][0:65, t * 128:(t + 1) * 128], [B_ot[osl]], [Bo], n=65)
            fw.op(dve, lambda e: e.reciprocal(out=rd[:, :], in_=sbap(pso, 64, [[128, 4]])), reads=[Bo], writes=[B_rd])
            for t in range(4):
                TS(dve, attn_tm[:, qt * 4 + t, h * 64:(h + 1) * 64], pso[:, t * 128:t * 128 + 64], rd[:, t:t + 1], None, ALU.mult, ALU.bypass,
                   [Bo], [B_attn], sreads=[B_rd])

        def attend(h, nxt):
            s = h % 2
            itc = [0]
            for qt in range(4):
                ob = 4 + ocount[0] % 2
                osl = ocount[0] % 2
                ocount[0] += 1
                base = scount[0]
                scount[0] += 34

                def S(kt):
                    bi = (base + kt) % 4
                    MM(PS[bi][:, :], KT[s][:, kt * 128:(kt + 1) * 128], QT[s][:, qt * 512:(qt + 1) * 512], True, True,
                       [B_KT[s], B_QT[s]], [PSB[bi]])
                S(0)
                S(1)
                if pend[0] is not None:
                    epilogue(*pend[0])
                    pend[0] = None
                for kt in range(34):
                    if kt + 2 < 34:
                        S(kt + 2)
                    bi = (base + kt) % 4
                    A(act, PT[bi][:, :], PS[bi][:, :], AF.Exp, [PSB[bi]], [B_PT[bi]], scale=SCALE)
                    MM(PS[ob][0:65, :], V_all[:, kt, h, 0:65], PT[bi][:, :], kt == 0, kt == 33, [B_V, B_PT[bi]], [PSB[ob]])
                    itc[0] += 1
                    if nxt is not None and itc[0] % 9 == 0:
                        next(nxt, None)
                pend[0] = (h, qt, ob, osl)

        nheads = int(os.environ.get("K_HEADS", "8"))
        for _ in gen(0):
            pass
        for h in range(nheads):
            nxt = gen(h + 1) if h + 1 < nheads else None
            attend(h, nxt)
            if nxt is not None:
                for _ in nxt:
                    pass
        epilogue(*pend[0])
        dump("attn", attn_tm[:].rearrange("p a b -> p (a b)"), [B_attn])
        fw.dma(sp, attn_s[:, :], attn_tm[:].rearrange("p a b -> p (a b)"), reads=[B_attn], writes=[B_attn_s], sem="attns")
        dump("KT0", KT[0][0:96, :], [B_KT[0]])
        dump("QT0", QT[0][0:96, :], [B_QT[0]])
        dump("V0", V_all[:].rearrange("p a b c -> p (a b c)"), [B_V])
    fw.barrier()
    att.close()
    if stop_after == "attn":
        return finish()

    PI = math.pi
    B_gs = Buf("g_s")
    with ExitStack() as S:
        sa = sb("ssm_a", [128, 2, 32], stack=S); sl = sb("ssm_l", [128, 32], stack=S)
        sbb = sb("ssm_b", [128, 2, 32, 16], stack=S); scc = sb("ssm_c", [128, 2, 32, 16], stack=S)
        dcols = sb("dcols", [128, 32], stack=S); masks = sb("masks4", [128, 4, 128], stack=S)
        B_par = Buf("ssm_par")
        fw.dma(sp, sa[:], ssm_a_d[:, :, :], writes=[B_par], sem="s0")
        fw.dma(sp, sl[:], ssm_ldt_d[:, :], writes=[B_par], sem="s1")
        fw.dma(sp, sbb[:], ssm_b_d[:, :, :, :], writes=[B_par], sem="s2")
        fw.dma(sp, scc[:], ssm_c_d[:, :, :, :], writes=[B_par], sem="s3")
        fw.dma(sp, dcols[:], ssm_dcols_d[:, :], writes=[B_par], sem="s4")
        fw.dma(sp, masks[:, 0:2, :], masks_d[:, :, :], writes=[B_par], sem="s5")
        fw.dma(sp, masks[:, 2:4, :], masks_d[:, :, :], writes=[B_par], sem="s6")
        w_glu = sb("w_glu", [128, 4, 512], BF16, stack=S); B_wglu = Buf("w_glu")
        fw.dma(pool, w_glu[:], w_glu_d.rearrange("(k p) n -> p k n", p=128), writes=[B_wglu], sem="s7")
        bglu = sb("bglu", [128, 512], stack=S); B_bglu = Buf("bglu")
        fw.dma(sp, bglu[:], b_glu_d[:, :], writes=[B_bglu], sem="s8")
        sm = sb("ssm_sm", [128, 32, 32], stack=S); B_sm = Buf("ssm_sm")
        _smi = [0]

        def smt():
            i = _smi[0]; _smi[0] += 1
            return sm[:, i, :]
        Bbar = sb("Bbar", [128, 2, 32, 16], stack=S)
        PWr = sb("PWr", [128, 17, 32], stack=S); PWi = sb("PWi", [128, 17, 32], stack=S)
        IPr = sb("IPr", [128, 17, 32], stack=S); IPi = sb("IPi", [128, 17, 32], stack=S)
        AA = sb("AA", [128, 2, 2, 16], stack=S); AB = sb("AB", [128, 2, 2, 16], stack=S)
        BS = [B_par, B_sm]

        def tt(out, a, b, op):
            TT(dve, out, a, b, op, BS, [B_sm])

        a_re, a_im = sa[:, 0, :], sa[:, 1, :]
        dt_ = smt(); lre = smt(); ang = smt(); mag = smt()
        A(act, dt_, sl[:, :], AF.Exp, [B_par], [B_sm])
        tt(lre, a_re, dt_, ALU.mult)
        tt(ang, a_im, dt_, ALU.mult)
        A(act, mag, lre, AF.Exp, [B_sm], [B_sm])

        def sin_of(src_ang, shift):
            a2 = smt(); k = smt(); r = smt(); o = smt()
            TS(dve, a2, src_ang, shift, None, ALU.add, ALU.bypass, BS, [B_sm])
            TS(dve, k, a2, PI, None, ALU.is_ge, ALU.bypass, BS, [B_sm])
            for i in range(2, 9):
                STT(k, a2, (2 * i - 1) * PI, k, ALU.is_ge, ALU.add, BS, [B_sm])
            STT(r, k, -2.0 * PI, a2, ALU.mult, ALU.add, BS, [B_sm])
            A(act, o, r, AF.Sin, [B_sm], [B_sm])
            return o
        sn = sin_of(ang, 0.0)
        cs = sin_of(ang, PI / 2)
        abre = smt(); abim = smt()
        tt(abre, mag, cs, ALU.mult)
        tt(abim, mag, sn, ALU.mult)
        t1 = smt(); t2 = smt(); den = smt(); rden = smt(); nre = smt(); cre = smt(); cim = smt()
        tt(t1, a_re, a_re, ALU.mult); tt(t2, a_im, a_im, ALU.mult); tt(den, t1, t2, ALU.add)
        fw.op(dve, lambda e: e.reciprocal(out=rden, in_=den), reads=BS, writes=[B_sm])
        TS(dve, nre, abre, -1.0, None, ALU.add, ALU.bypass, BS, [B_sm])
        tt(t1, nre, a_re, ALU.mult); tt(t2, abim, a_im, ALU.mult); tt(t1, t1, t2, ALU.add); tt(cre, t1, rden, ALU.mult)
        tt(t1, abim, a_re, ALU.mult); tt(t2, nre, a_im, ALU.mult); tt(t1, t1, t2, ALU.subtract); tt(cim, t1, rden, ALU.mult)
        tb = sb("tb", [128, 32, 16], stack=S)

        def bc16(v):
            return AP(v.tensor, v.offset, [list(v.ap[0]), [1, 32], [0, 16]])
        tt(Bbar[:, 0], sbb[:, 0], bc16(cre), ALU.mult); tt(tb[:], sbb[:, 1], bc16(cim), ALU.mult); tt(Bbar[:, 0], Bbar[:, 0], tb[:], ALU.subtract)
        tt(Bbar[:, 1], sbb[:, 1], bc16(cre), ALU.mult); tt(tb[:], sbb[:, 0], bc16(cim), ALU.mult); tt(Bbar[:, 1], Bbar[:, 1], tb[:], ALU.add)
        m2 = smt(); rm2 = smt(); iabre = smt(); iabim = smt()
        tt(t1, abre, abre, ALU.mult); tt(t2, abim, abim, ALU.mult); tt(m2, t1, t2, ALU.add)
        fw.op(dve, lambda e: e.reciprocal(out=rm2, in_=m2), reads=BS, writes=[B_sm])
        tt(iabre, abre, rm2, ALU.mult)
        STT(iabim, abim, -1.0, rm2, ALU.mult, ALU.mult, BS, [B_sm])
        for (Pr, Pi, br, bi) in ((PWr, PWi, abre, abim), (IPr, IPi, iabre, iabim)):
            fw.op(dve, lambda e: e.memset(Pr[:, 0, :], 1.0), writes=[B_sm])
            fw.op(dve, lambda e: e.memset(Pi[:, 0, :], 0.0), writes=[B_sm])
            CP(dve, Pr[:, 1, :], br, BS, [B_sm])
            CP(dve, Pi[:, 1, :], bi, BS, [B_sm])
            for k in range(2, 17):
                tt(t1, Pr[:, k - 1, :], br, ALU.mult); tt(t2, Pi[:, k - 1, :], bi, ALU.mult); tt(Pr[:, k, :], t1, t2, ALU.subtract)
                tt(t1, Pr[:, k - 1, :], bi, ALU.mult); tt(t2, Pi[:, k - 1, :], br, ALU.mult); tt(Pi[:, k, :], t1, t2, ALU.add)
        for d in range(2):
            for c in range(2):
                CP(dve, AA[:, d, c, :], PWr[:, 16, d * 16:(d + 1) * 16], BS, [B_sm])
            TS(dve, AB[:, d, 0, :], PWi[:, 16, d * 16:(d + 1) * 16], -1.0, None, ALU.mult, ALU.bypass, BS, [B_sm])
            CP(dve, AB[:, d, 1, :], PWi[:, 16, d * 16:(d + 1) * 16], BS, [B_sm])
        dump("sm", sm[:].rearrange("p a b -> p (a b)"), BS)
        dump("abar", sbap(PWr, 32, [[1, 32]]), BS)
        dump("abari", sbap(PWi, 32, [[1, 32]]), BS)
        dump("Bbar", Bbar[:].rearrange("p a b c -> p (a b c)"), BS)

        ctmp = [sb("ctmp%d" % i, [128, 4, 256], stack=S) for i in range(2)]; B_ct = Buf("ctmp")

        def cplx_build(q, out_t, P_r, P_i, k0, kstep, Vt, d, neg_im):
            for half in range(4):
                p0 = half * 4

                def Pv(Pt):
                    return sbap(Pt, k0 * 32 + d * 16 + p0, [[1, 4], [kstep * 32, 16], [0, 16]])

                def Vv(c):
                    return sbap(Vt, c * 512 + (d * 16 + p0) * 16, [[16, 4], [0, 16], [1, 16]])

                def Ov(c):
                    return sbap(out_t, p0 * 512 + c * 256, [[512, 4], [16, 16], [1, 16]])
                ta = ctmp[0][:].rearrange("p a (k h) -> p a k h", h=16)
                tb_ = ctmp[1][:].rearrange("p a (k h) -> p a k h", h=16)
                R = BS + [B_ct]
                TT(q, ta, Pv(P_r), Vv(0), ALU.mult, R, [B_ct])
                TT(q, tb_, Pv(P_i), Vv(1), ALU.mult, R, [B_ct])
                TT(q, Ov(0), ta, tb_, ALU.subtract, R, [B_sm])
                TT(q, ta, Pv(P_r), Vv(1), ALU.mult, R, [B_ct])
                TT(q, tb_, Pv(P_i), Vv(0), ALU.mult, R, [B_ct])
                if neg_im:
                    TT(q, ta, ta, tb_, ALU.add, R, [B_ct])
                    TS(q, Ov(1), ta, -1.0, None, ALU.mult, ALU.bypass, R, [B_sm])
                else:
                    TT(q, Ov(1), ta, tb_, ALU.add, R, [B_sm])

        Sbf = sb("Sbf", [128, 2, 2, 16, 128], BF16, stack=S); B_Sbf = Buf("Sbf")
        with ExitStack() as S2:
            Ug = sb("Ug2", [128, 32, 2, 272], BF16, stack=S2); B_Ug2 = Buf("Ug2")
            fw.dma(sp, Ug[:].rearrange("p a b c -> p (a b c)"), ug_s[:, :], writes=[B_Ug2], sem="s9")
            L = sb("L", [128, 2, 2, 16, 272], BF16, stack=S2); B_L = Buf("L")
            with ExitStack() as S2a:
                Wsrc = sb("Wsrc", [128, 16, 2, 256], stack=S2a)
                Wb = sb("Wb", [128, 16, 4, 128], BF16, stack=S2a); B_Wb = Buf("Wb")
                for d in range(2):
                    if d == 0:
                        cplx_build(dve, Wsrc, PWr, PWi, 15, -1, Bbar, 0, False)
                    else:
                        cplx_build(dve, Wsrc, PWr, PWi, 0, 1, Bbar, 1, False)
                    for pt in range(16):
                        ps, Bp = ps_next()
                        for blk in range(4):
                            TR(ps[:, blk * 128:(blk + 1) * 128], sbap(Wsrc, pt * 512 + blk * 128, [[1, 128]]), BS, [Bp])
                        if pt % 2 == 0:
                            A(act, Wb[:, pt, :, :], ps[:, :].rearrange("p (a b) -> p a b", a=4), AF.Copy, [Bp], [B_Wb])
                        else:
                            CP(dve, Wb[:, pt, :, :], ps[:, :].rearrange("p (a b) -> p a b", a=4), [Bp], [B_Wb])
                    for pt in range(16):
                        for c in range(2):
                            ps, Bp = ps_next()
                            if d == 1:
                                ranges = [(0, 272, 0)]
                                ncol = 272
                            else:
                                ranges = [(256, 16, 0), (0, 128, 16)]
                                ncol = 144
                            for (u0, n, o0) in ranges:
                                for gl in range(2):
                                    for jt in range(2):
                                        MM(ps[gl * 64:(gl + 1) * 64, o0:o0 + n], Wb[:, pt, c * 2 + jt, gl * 64:(gl + 1) * 64],
                                           Ug[:, 2 * pt + gl, jt, u0:u0 + n], jt == 0, jt == 1, [B_Wb, B_Ug2], [Bp])
                            lo = L[:, d, c, pt, 272 - ncol:272]
                            if c == 0:
                                A(act, lo, ps[:, 0:ncol], AF.Copy, [Bp], [B_L])
                            else:
                                CP(dve, lo, ps[:, 0:ncol], [Bp], [B_L])
                    if d == 0:
                        dump("Wb0", Wb[:].rearrange("p a b c -> p (a b c)"), [B_Wb])
            dump("L", L[:].rearrange("p a b c e -> p (a b c e)"), [B_L])
            fw.barrier()
            Sh = sb("Sh", [128, 2, 2, 16, NS], stack=S2); B_Sh = Buf("Sh")
            fw.op(pool, lambda e: e.memset(Sh[:], 0.0), writes=[B_Sh])
            tm1 = sb("tm1", [128, 2, 2, 16], stack=S2); tm2 = sb("tm2", [128, 2, 2, 16], stack=S2); B_tm = Buf("tm")

            def f0(st):
                return st - 140 if st >= 142 else st % 2

            def f1(st):
                return 273 - st if st >= 142 else 132 + st % 2
            B_tmh = bufs("tmh", 2); B_Shh = bufs("Shh", 2)
            for b_ in B_Shh:
                b_.w = B_Sh.w
            RSh = [[B_Shh[h_], B_L, B_sm, B_tmh[h_]] for h_ in range(2)]
            for s in range(272):
                both = s >= 128
                ops = []
                for ph_ in range(2):
                    po = ph_ * 8
                    if both:
                        def hv(t_, n_, c0, c1, rev=False):
                            dstep = 32 * n_ + (c1 - c0)
                            if rev:
                                return sbap(t_, c0 + 16 * n_ + po * n_, [[dstep, 2], [-16 * n_, 2], [n_, 8]])
                            return sbap(t_, c0 + po * n_, [[dstep, 2], [16 * n_, 2], [n_, 8]])
                        sr = hv(Sh, NS, f0(s), f1(s)); srev = hv(Sh, NS, f0(s), f1(s), True)
                        sw = hv(Sh, NS, f0(s + 1), f1(s + 1))
                        lv = hv(L, 272, s, 271 - s)
                        aa, ab = AA[:, :, :, po:po + 8], AB[:, :, :, po:po + 8]
                        x1, x2 = tm1[:, :, :, po:po + 8], tm2[:, :, :, po:po + 8]
                    else:
                        def hv(t_, n_, c1, rev=False):
                            if rev:
                                return sbap(t_, 32 * n_ + c1 + 16 * n_ + po * n_, [[-16 * n_, 2], [n_, 8]])
                            return sbap(t_, 32 * n_ + c1 + po * n_, [[16 * n_, 2], [n_, 8]])
                        sr = hv(Sh, NS, f1(s)); srev = hv(Sh, NS, f1(s), True); sw = hv(Sh, NS, f1(s + 1))
                        lv = hv(L, 272, 271 - s)
                        aa, ab = AA[:, 1, :, po:po + 8], AB[:, 1, :, po:po + 8]
                        x1, x2 = tm1[:, 1, :, po:po + 8], tm2[:, 1, :, po:po + 8]
                    ops.append((sr, srev, sw, lv, aa, ab, x1, x2))
                for h_, (sr, srev, sw, lv, aa, ab, x1, x2) in enumerate(ops):
                    TT(dve, x1, sr, aa, ALU.mult, RSh[h_], [B_tmh[h_]])
                for h_, (sr, srev, sw, lv, aa, ab, x1, x2) in enumerate(ops):
                    TT(dve, x2, srev, ab, ALU.mult, RSh[h_], [B_tmh[h_]])
                for h_, (sr, srev, sw, lv, aa, ab, x1, x2) in enumerate(ops):
                    TT(dve, x1, x1, lv, ALU.add, RSh[h_], [B_tmh[h_]])
                for h_, (sr, srev, sw, lv, aa, ab, x1, x2) in enumerate(ops):
                    TT(dve, sw, x1, x2, ALU.add, RSh[h_], [B_Shh[h_]])
            CP(dve, Sbf[:, 0].rearrange("p a b c -> p (a b) c"), sbap(Sh, 4, [[NS, 32], [1, 128]]), B_Shh, [B_Sbf])
            CP(dve, Sbf[:, 1].rearrange("p a b c -> p (a b) c"), sbap(Sh, 32 * NS + 2, [[NS, 32], [1, 128]]), B_Shh, [B_Sbf])
            dump("Sbf", Sbf[:].rearrange("p a b c e -> p (a b c e)"), [B_Sbf])
        fw.barrier()
        if stop_after == "scan":
            S.close()
            return finish()
        with ExitStack() as S3:
            Kmat = sb("Kmat", [128, 32, 4, 128], BF16, stack=S3); B_Km = Buf("Kmat")
            Ca = sb("Ca", [128, 2, 16, 2, 256], BF16, stack=S3); B_Ca = Buf("Ca")
            Ugo = sb("Ugo", [128, 64, 128], BF16, stack=S3); B_Ugo = Buf("Ugo")
            fw.dma(sp, Ugo[:], ug_s.rearrange("p (a c) -> p a c", c=272)[:, :, 0:128], writes=[B_Ugo], sem="s10")
            S3a = ExitStack()
            Xt = sb("Xt", [128, 16, 2, 256], BF16, stack=S3a); Zt = sb("Zt", [128, 16, 2, 256], BF16, stack=S3a)
            tmpK = sb("tmpK", [128, 4, 128], stack=S3a); tmpS = sb("tmpS", [128, 2, 128], stack=S3a); B_tk = Buf("tmpK")
            cplx_build(dve, Ca[:, 0], PWr, PWi, 1, 1, scc, 0, True)
            cplx_build(dve, Ca[:, 1], PWr, PWi, 16, -1, scc, 1, True)
            KS = BS + [B_tk]
            for d in range(2):
                if d == 0:
                    cplx_build(dve, Xt, IPr, IPi, 0, 1, Bbar, 0, False)
                    cplx_build(dve, Zt, PWr, PWi, 0, 1, scc, 0, True)
                else:
                    cplx_build(dve, Xt, PWr, PWi, 0, 1, Bbar, 1, False)
                    cplx_build(dve, Zt, IPr, IPi, 0, 1, scc, 1, True)
                for g in range(32):
                    pt, gl = g // 2, g % 2
                    r0, r1 = gl * 64, gl * 64 + 64
                    psA, BpA = ps_next()

                    def kblock(ps, col, jt, tt_):
                        for c in range(2):
                            MM(ps[:, col * 128:(col + 1) * 128], Xt[r0:r1, pt, c, jt * 128:(jt + 1) * 128],
                               Zt[r0:r1, pt, c, tt_ * 128:(tt_ + 1) * 128], c == 0, c == 1, BS, [BpA])
                    kblock(psA, 0, 0, 0)
                    kblock(psA, 1, 1, 1)
                    if d == 0:
                        kblock(psA, 2, 0, 1)
                    else:
                        kblock(psA, 2, 1, 0)
                    mk = sbap(masks, d * 128, [[0, 2], [1, 128]])
                    kd = sbap(Kmat, g * 512, [[384, 2], [1, 128]])
                    ko_ = Kmat[:, g, 1 if d == 0 else 2, :]
                    if d == 0:
                        TT(dve, tmpS[:], psA[:, 0:256].rearrange("p (a b) -> p a b", a=2), mk, ALU.mult, [BpA, B_par], [B_tk])
                        STT(kd, sbap(ident, 0, [[0, 2], [1, 128]]), dcols[:, g:g + 1], tmpS[:], ALU.mult, ALU.add, [B_ident, B_par, B_tk], [B_Km])
                    else:
                        TT(dve, tmpS[:], psA[:, 0:256].rearrange("p (a b) -> p a b", a=2), mk, ALU.mult, [BpA, B_par], [B_tk])
                        TT(dve, kd, kd, tmpS[:], ALU.add, [B_tk, B_Km], [B_Km])
                    A(act, ko_, psA[:, 256:384], AF.Copy, [BpA], [B_Km])
            dump("Kmat", Kmat[:].rearrange("p a b c -> p (a b c)"), [B_Km])
            dump("Ca", Ca[:].rearrange("p a b c e -> p (a b c e)"), BS)
            fw.barrier()
            S3a.close()
            g_tm = sb("g_tm", [128, 16, 512], stack=S3); B_gtm = Buf("g_tm")
            glsb = [sb("glsb%d" % i, [128, 512], stack=S3) for i in range(2)]; B_gl = bufs("glsb", 2)
            for gp in range(16):
                psY, BpY = ps_next()
                for gi in range(2):
                    g = gp * 2 + gi
                    pt, gl = g // 2, g % 2
                    r0, r1 = gl * 64, gl * 64 + 64
                    for tt_ in range(2):
                        yo = psY[:, (gi * 2 + tt_) * 128:(gi * 2 + tt_ + 1) * 128]
                        first = True
                        for jt in range(2):
                            MM(yo, Kmat[:, g, jt * 2 + tt_, :], Ugo[:, g * 2 + jt, :], first, False, [B_Km, B_Ugo], [BpY])
                            first = False
                        for d in range(2):
                            for c in range(2):
                                MM(yo, Ca[r0:r1, d, pt, c, tt_ * 128:(tt_ + 1) * 128], Sbf[r0:r1, d, c, pt, :], False, (d == 1 and c == 1),
                                   BS + [B_Sbf], [BpY])
                gs_ = gp % 2
                A(act, glsb[gs_][:, :], psY[:, :], AF.Gelu, [BpY], [B_gl[gs_]])
                psT, BpT = ps_next()
                for blk in range(4):
                    TR(psT[:, blk * 128:(blk + 1) * 128], glsb[gs_][:, blk * 128:(blk + 1) * 128], [B_gl[gs_]], [BpT])
                for gi in range(2):
                    g = gp * 2 + gi
                    oo = sbap(g_tm, 16 * g, [[512, 16], [1, 16]])
                    ii = sbap(psT, gi * 256, [[16, 16], [1, 16]])
                    CP(dve, oo, ii, [BpT], [B_gtm])
            dump("gtm", g_tm[:].rearrange("p a b -> p (a b)"), [B_gtm])
            gT = [sb("gT%d" % i, [128, 4, 128], BF16, stack=S3) for i in range(2)]; B_gT = bufs("gT", 2)
            zt = sb("zt", [128, 512], stack=S3); B_zt = Buf("zt")
            for t in range(16):
                gs_ = t % 2
                ps, Bp = ps_next()
                for fc in range(4):
                    TR(ps[:, fc * 128:(fc + 1) * 128], g_tm[:, t, fc * 128:(fc + 1) * 128], [B_gtm], [Bp])
                A(act, gT[gs_][:], ps[:, :].rearrange("p (a b) -> p a b", a=4), AF.Copy, [Bp], [B_gT[gs_]])
                ps2, Bp2 = ps_next()
                for fc in range(4):
                    MM(ps2[:, :], gT[gs_][:, fc, :], w_glu[:, fc, :], fc == 0, fc == 3, [B_gT[gs_], B_wglu], [Bp2])
                TT(dve, zt[:], ps2[:, :], bglu[:], ALU.add, [Bp2, B_bglu], [B_zt])
                A(act, zt[:], zt[:], AF.Sigmoid, [B_zt], [B_zt])
                TT(dve, g_tm[:, t, :], g_tm[:, t, :], zt[:], ALU.mult, [B_gtm, B_zt], [B_gtm])
            dump("ssm", g_tm[:].rearrange("p a b -> p (a b)"), [B_gtm])
            fw.dma(sp, g_s[:, :], g_tm[:].rearrange("p a b -> p (a b)"), reads=[B_gtm], writes=[B_gs], sem="gs")
    fw.barrier()
    if stop_after == "ssm":
        return finish()

    hT = sb("hT", [128, 8, NOWN], BF16); B_hT = Buf("hT")
    combT = sb("combT", [32, NOWN], BF16); B_combT = Buf("combT")
    lnbox = [None, None]
    B_x1s = Buf("x1_s")

    def layer_norm(pre, B_pre, gi, out, B_out, st, mv, sc_, B_st):
        for hf in range(2):
            fw.op(dve, lambda e: e.bn_stats(out=st[:, hf * 6:(hf + 1) * 6], in_=pre[:, hf * 512:(hf + 1) * 512]), reads=[B_pre], writes=[B_st])
        fw.op(dve, lambda e: e.bn_aggr(out=mv[:, 0:2], in_=st[:, 0:12]), reads=[B_st], writes=[B_st])
        A(act, sc_[:, 0:1], mv[:, 1:2], AF.Ln, [B_st, B_eps], [B_st], bias=epsc[:, 0:1])
        A(act, sc_[:, 1:2], sc_[:, 0:1], AF.Exp, [B_st], [B_st], scale=-0.5)
        TS(dve, out, pre, mv[:, 0:1], sc_[:, 1:2], ALU.subtract, ALU.mult, [B_pre], [B_out], sreads=[B_st])
        lnrows, B_ln = lnbox
        TT(dve, out, out, lnrows[:, 0, :], ALU.mult, [B_out, B_ln], [B_out])
        TT(dve, out, out, lnrows[:, 1, :], ALU.add, [B_out, B_ln], [B_out])

    with ExitStack() as M:
        ln1 = sb("ln1rows", [128, 2, D], stack=M); lnbox[0] = ln1; lnbox[1] = Buf("ln1")
        fw.dma(sp, ln1[:], ln_d[:, 0:2, :], writes=[lnbox[1]], sem="m0")
        w_o_s = sb("w_o_s", [128, 8, D], BF16, stack=M); B_wo = Buf("w_o_s")
        wof = [sb("wof%d" % i, [128, D], stack=M) for i in range(2)]; B_wof = bufs("wof", 2)
        gn = sb("gn", [128, 8], stack=M); B_gn = Buf("gn")
        w_r = sb("w_r", [128, 8, 36], stack=M); b_r = sb("b_r", [128, 36], stack=M); B_wr = Buf("w_r")
        fw.dma(sp, gn[:], gn_d[:, :], writes=[B_gn], sem="m1")
        fw.dma(sp, w_r[:], w_r_d.rearrange("(k p) n -> p k n", p=128), writes=[B_wr], sem="m2")
        fw.dma(sp, b_r[:], b_r_d[:, :], writes=[B_wr], sem="m3")
        for fc in range(8):
            fw.dma(sp, wof[fc % 2][:], w_o_d[fc * 128:(fc + 1) * 128, :], writes=[B_wof[fc % 2]], sem="wof%d" % (fc % 2))
            TS(dve, w_o_s[:, fc, :], wof[fc % 2][:], gn[:, fc:fc + 1], None, ALU.mult, ALU.bypass, [B_wof[fc % 2], B_gn], [B_wo])
        cat = [sb("cat%d" % i, [128, D], stack=M) for i in range(2)]; B_cat = bufs("cat", 2)
        xo = [sb("xo%d" % i, [128, D], stack=M) for i in range(2)]; B_xo = bufs("xo", 2)
        catT = [sb("catT%d" % i, [128, 8, 128], BF16, stack=M) for i in range(2)]; B_catT = bufs("catT", 2)
        pre = [sb("pre%d" % i, [128, D], stack=M) for i in range(2)]; B_pre = bufs("pre", 2)
        hTf = [sb("hTf%d" % i, [128, 8, 128], stack=M) for i in range(2)]; B_hTf = bufs("hTf", 2)
        htm = [sb("htm%d" % i, [128, D], stack=M) for i in range(2)]; B_htm = bufs("htm", 2)
        junk = sb("junk", [128, 512], stack=M); B_junk = Buf("junk")
        st = sb("st", [128, 12], stack=M); mv = sb("mv", [128, 2], stack=M); sc_ = sb("sc_", [128, 8], stack=M); B_st = Buf("st")
        rt = sb("rt", [128, 160], stack=M); B_rt = Buf("rt")
        scA = sb("scA", [128, 8], stack=M); B_stA = Buf("stA")
        comb = sb("comb", [128, 32], stack=M); B_comb = Buf("comb")
        def stageA(t):
            s = t % 2
            fw.dma(sp, cat[s][:, 0:512], attn_s[:, t * 512:(t + 1) * 512], reads=[B_attn_s], writes=[B_cat[s]], sem="cat%d" % s)
            fw.dma(sp, cat[s][:, 512:1024], g_s[:, t * 512:(t + 1) * 512], reads=[B_gs], writes=[B_cat[s]], sem="cat%d" % s)
            fw.group_done("cat%d" % s, [B_cat[s]])
            fw.dma(sp, xo[s][:], xf_v[t, 0:128, :], writes=[B_xo[s]], sem="xo%d" % s)
            for hf in range(2):
                A(act, junk[:], cat[s][:, hf * 512:(hf + 1) * 512], AF.Square, [B_cat[s]], [B_junk, B_stA], accum_out=scA[:, 2 + hf:3 + hf])
                A(act, scA[:, 4 + hf:5 + hf], scA[:, 2 + hf:3 + hf], AF.Ln, [B_stA, B_eps], [B_stA], scale=1.0 / 512, bias=epsc[:, 0:1])
                A(act, scA[:, 6 + hf:7 + hf], scA[:, 4 + hf:5 + hf], AF.Exp, [B_stA], [B_stA], scale=-0.5)
                A(act, cat[s][:, hf * 512:(hf + 1) * 512], cat[s][:, hf * 512:(hf + 1) * 512], AF.Copy, [B_cat[s], B_stA], [B_cat[s]],
                  scale=scA[:, 6 + hf:7 + hf])
            for hf in range(2):
                ps, Bp = ps_next()
                for q4 in range(4):
                    fc = hf * 4 + q4
                    TR(ps[:, q4 * 128:(q4 + 1) * 128], cat[s][:, fc * 128:(fc + 1) * 128], [B_cat[s]], [Bp])
                A(act, catT[s][:, hf * 4:(hf + 1) * 4, :], ps[:, :].rearrange("p (a b) -> p a b", a=4), AF.Copy, [Bp], [B_catT[s]])
            for hf in range(2):
                ps, Bp = ps_next()
                for fc in range(8):
                    MM(ps[:, :], catT[s][:, fc, :], w_o_s[:, fc, hf * 512:(hf + 1) * 512], fc == 0, fc == 7, [B_catT[s], B_wo], [Bp])
                TT(dve, pre[s][:, hf * 512:(hf + 1) * 512], ps[:, :], grow[:, 0, hf * 512:(hf + 1) * 512], ALU.mult, [Bp, B_grow], [B_pre[s]])
            STT(pre[s][:], xo[s][:], ALPHA, pre[s][:], ALU.mult, ALU.add, [B_xo[s], B_pre[s]], [B_pre[s]])

        def stageB(t):
            s = t % 2
            layer_norm(pre[s][:], B_pre[s], 0, pre[s][:], B_pre[s], st, mv, sc_, B_st)
            fw.dma(sp, x1_s[:, t * D:(t + 1) * D], pre[s][:], reads=[B_pre[s]], writes=[B_x1s], sem="x1s")
            TT(dve, htm[s][:], pre[s][:], grow[:, 2, :], ALU.mult, [B_pre[s], B_grow], [B_htm[s]])
            TT(dve, htm[s][:], htm[s][:], grow[:, 1, :], ALU.add, [B_htm[s], B_grow], [B_htm[s]])
            for hf in range(2):
                ps, Bp = ps_next()
                for q4 in range(4):
                    fc = hf * 4 + q4
                    TR(ps[:, q4 * 128:(q4 + 1) * 128], htm[s][:, fc * 128:(fc + 1) * 128], [B_htm[s]], [Bp])
                A(act, hT[:, hf * 4:(hf + 1) * 4, t * 128:(t + 1) * 128], ps[:, :].rearrange("p (a b) -> p a b", a=4), AF.Copy, [Bp], [B_hT])
                A(act, hTf[s][:, hf * 4:(hf + 1) * 4, :], ps[:, :].rearrange("p (a b) -> p a b", a=4), AF.Copy, [Bp], [B_hTf[s]])

        def stageC(t):
            s = t % 2
            ps, Bp = ps_next()
            for fc in range(8):
                MM(ps[:, 0:36], hTf[s][:, fc, :], w_r[:, fc, :], fc == 0, fc == 7, [B_hTf[s], B_wr], [Bp])
            lg = rt[:, 0:36]
            gsel = rt[:, 36:40]; gmax = rt[:, 40:41]; ngmax = rt[:, 41:42]; gsum = rt[:, 42:43]; g_w = rt[:, 43:44]
            esel = rt[:, 44:52]; m1 = rt[:, 52:53]; mask1 = rt[:, 56:64]; esel2 = rt[:, 64:72]; m2 = rt[:, 53:54]; mask2 = rt[:, 72:80]
            nm1 = rt[:, 54:55]; e2 = rt[:, 55:56]; den_ = rt[:, 80:81]; w1 = rt[:, 81:82]; w2 = rt[:, 82:83]; cw8 = rt[:, 88:96]; gex = rt[:, 96:100]
            RR = [B_rt]
            TT(dve, lg, ps[:, 0:36], b_r[:, :], ALU.add, [Bp, B_wr], RR)
            fw.op(dve, lambda e: e.reduce_max(out=gmax, in_=rt[:, 0:4], axis=AX.X), reads=RR, writes=RR)
            TS(dve, gsel, rt[:, 0:4], gmax, None, ALU.is_ge, ALU.bypass, RR, RR, sreads=RR)
            TS(dve, ngmax, gmax, -1.0, None, ALU.mult, ALU.bypass, RR, RR)
            A(act, gex, rt[:, 0:4], AF.Exp, RR, RR, bias=ngmax, accum_out=gsum)
            fw.op(dve, lambda e: e.reciprocal(out=g_w, in_=gsum), reads=RR, writes=RR)
            TS(dve, esel, rt[:, 4:12], gsel[:, 0:1], None, ALU.mult, ALU.bypass, RR, RR, sreads=RR)
            for g in range(1, 4):
                fw.op(dve, lambda e: e.scalar_tensor_tensor(out=esel, in0=rt[:, 4 + 8 * g:12 + 8 * g], scalar=gsel[:, g:g + 1], in1=esel,
                                                            op0=ALU.mult, op1=ALU.add), reads=RR, writes=RR, sreads=RR)
            fw.op(dve, lambda e: e.reduce_max(out=m1, in_=esel, axis=AX.X), reads=RR, writes=RR)
            TS(dve, mask1, esel, m1, None, ALU.is_ge, ALU.bypass, RR, RR, sreads=RR)
            STT(esel2, mask1, -1e30, esel, ALU.mult, ALU.add, RR, RR)
            fw.op(dve, lambda e: e.reduce_max(out=m2, in_=esel2, axis=AX.X), reads=RR, writes=RR)
            TS(dve, mask2, esel2, m2, None, ALU.is_ge, ALU.bypass, RR, RR, sreads=RR)
            TS(dve, nm1, m1, -1.0, None, ALU.mult, ALU.bypass, RR, RR)
            A(act, e2, m2, AF.Exp, RR, RR, bias=nm1)
            TS(dve, den_, e2, 1.0, None, ALU.add, ALU.bypass, RR, RR)
            fw.op(dve, lambda e: e.reciprocal(out=w1, in_=den_), reads=RR, writes=RR)
            TT(dve, w2, e2, w1, ALU.mult, RR, RR)
            TT(dve, w1, w1, g_w, ALU.mult, RR, RR)
            TT(dve, w2, w2, g_w, ALU.mult, RR, RR)
            TS(dve, cw8, mask1, w1, None, ALU.mult, ALU.bypass, RR, RR, sreads=RR)
            fw.op(dve, lambda e: e.scalar_tensor_tensor(out=cw8, in0=mask2, scalar=w2, in1=cw8, op0=ALU.mult, op1=ALU.add),
                  reads=RR, writes=RR, sreads=RR)
            for g in range(4):
                TS(dve, comb[:, g * 8:(g + 1) * 8], cw8, gsel[:, g:g + 1], None, ALU.mult, ALU.bypass, RR, [B_comb], sreads=RR)
            ps, Bp = ps_next()
            TR(ps[0:32, 0:128], comb[:, :], [B_comb], [Bp])
            CP(dve, combT[:, t * 128:(t + 1) * 128], ps[0:32, 0:128], [Bp], [B_combT])
            if t == 0:
                dump("x1_0", pre[s][:], [B_pre[s]])
                dump("comb0", comb[:], [B_comb])

        stageA(0)
        for t in range(17):
            if t + 1 < 16:
                stageA(t + 1)
            if t < 16:
                stageB(t)
            if t >= 1:
                stageC(t - 1)
    fw.barrier()
    if stop_after == "merge":
        return finish()

    with ExitStack() as E:
        ln2 = sb("ln2rows", [128, 2, D], stack=E); lnbox[0] = ln2; lnbox[1] = Buf("ln2")
        fw.dma(sp, ln2[:], ln_d[:, 2:4, :], writes=[lnbox[1]], sem="e9")
        acc = sb("acc", [128, 16, D], stack=E); B_acc = bufs("acc", 16)
        sel = sb("sel", [32, NE * 128], BF16, stack=E); B_sel = Buf("sel")
        fw.dma(pool, sel[:], sel_d[:, :], writes=[B_sel], sem="e0")
        NSLOT = 3
        Wg = [sb("Wg%d" % i, [128, 8, 256], BF16, stack=E) for i in range(NSLOT)]
        Wu = [sb("Wu%d" % i, [128, 8, 256], BF16, stack=E) for i in range(NSLOT)]
        Wd = [sb("Wd%d" % i, [128, 2, D], BF16, stack=E) for i in range(NSLOT)]
        B_W = bufs("Wexp", NSLOT)
        sg = [sb("sg%d" % i, [128, 512], stack=E) for i in range(2)]; B_sg = bufs("sg", 2)
        tu = [sb("tu%d" % i, [128, 512], stack=E) for i in range(2)]; B_tu = bufs("tu", 2)
        gT2 = [sb("gT2_%d" % i, [128, 2, 256], BF16, stack=E) for i in range(2)]; B_gT2 = bufs("gT2", 2)
        n_exp = int(os.environ.get("K_NEXP", str(NE)))

        stg = [sb("stg%d" % i, [128, 2048], stack=E) for i in range(2)]; B_stg = bufs("stg", 2)
        stc = [0]

        def load_expert(e):
            sl_ = e % NSLOT
            for (src_, dst, kk) in ((wg_d[e], Wg[sl_], 8), (wu_d[e], Wu[sl_], 8), (wd_d[e], Wd[sl_], 2)):
                k_ = stc[0] % 2
                stc[0] += 1
                fw.dma(sp, stg[k_][:].rearrange("p (k n) -> p k n", k=kk), src_.rearrange("(k p) n -> p k n", p=128),
                       writes=[B_stg[k_]], sem="stg%d" % k_)
                CP(pool, dst[:].rearrange("p k n -> p (k n)"), stg[k_][:], [B_stg[k_]], [B_W[sl_]])

        for e in range(min(NSLOT, n_exp)):
            load_expert(e)
        items = [(e, tg) for e in range(n_exp) for tg in range(8)]
        dcnt = [0]

        csb = [sb("csb%d" % i, [128, 256], BF16, stack=E) for i in range(2)]; B_csb = bufs("csb", 2)

        def AUC(i):
            e, tg = items[i]
            sl_ = e % NSLOT
            k2 = i % 2
            pa, Bpa = PS[k2 * 2], PSB[k2 * 2]
            pu, Bpu = PS[k2 * 2 + 1], PSB[k2 * 2 + 1]
            pc_, Bpc = PS[4], PSB[4]
            MM(pc_[:, 0:256], sel[0:32, e * 128:(e + 1) * 128], combT[0:32, tg * 256:(tg + 1) * 256], True, True, [B_sel, B_combT], [Bpc])
            for fc in range(2):
                for ch in range(8):
                    MM(pa[:, fc * 256:(fc + 1) * 256], Wg[sl_][:, ch, fc * 128:(fc + 1) * 128], hT[:, ch, tg * 256:(tg + 1) * 256],
                       ch == 0, ch == 7, [B_W[sl_], B_hT], [Bpa])
            for fc in range(2):
                for ch in range(8):
                    MM(pu[:, fc * 256:(fc + 1) * 256], Wu[sl_][:, ch, fc * 128:(fc + 1) * 128], hT[:, ch, tg * 256:(tg + 1) * 256],
                       ch == 0, ch == 7, [B_W[sl_], B_hT], [Bpu])

        def REST(i):
            e, tg = items[i]
            sl_ = e % NSLOT
            k2 = i % 2
            pa, Bpa = PS[k2 * 2], PSB[k2 * 2]
            pu, Bpu = PS[k2 * 2 + 1], PSB[k2 * 2 + 1]
            A(act, sg[k2][:, :], pa[:, :], AF.Silu, [Bpa], [B_sg[k2]])
            if i + 1 < len(items):
                A(act, csb[1 - k2][:, :], PS[4][:, 0:256], AF.Copy, [PSB[4]], [B_csb[1 - k2]])
            TT(dve, tu[k2][:, :], pu[:, :], sg[k2][:, :], ALU.mult, [Bpu, B_sg[k2]], [B_tu[k2]])
            TT(dve, gT2[k2][:], tu[k2][:, :].rearrange("p (a b) -> p a b", a=2), sbap(csb[k2], 0, [[0, 2], [1, 256]]), ALU.mult,
               [B_tu[k2], B_csb[k2]], [B_gT2[k2]])
            for tt_ in range(2):
                tile_ = tg * 2 + tt_
                for hf in range(2):
                    po, Bpo = PS[5 + dcnt[0] % 3], PSB[5 + dcnt[0] % 3]
                    dcnt[0] += 1
                    for fc in range(2):
                        MM(po[:, :], gT2[k2][:, fc, tt_ * 128:(tt_ + 1) * 128], Wd[sl_][:, fc, hf * 512:(hf + 1) * 512],
                           fc == 0, fc == 1, [B_gT2[k2], B_W[sl_]], [Bpo])
                    ao = acc[:, tile_, hf * 512:(hf + 1) * 512]
                    if e == 0:
                        CP(dve, ao, po[:, :], [Bpo], [B_acc[tile_]])
                    else:
                        TT(dve, ao, po[:, :], ao, ALU.add, [Bpo, B_acc[tile_]], [B_acc[tile_]])
            if tg == 7 and e + NSLOT < n_exp:
                load_expert(e + NSLOT)

        AUC(0)
        A(act, csb[0][:, :], PS[4][:, 0:256], AF.Copy, [PSB[4]], [B_csb[0]])
        for i in range(len(items)):
            if i + 1 < len(items):
                AUC(i + 1)
            REST(i)
        dump("acc0", acc[:, 0, :], [B_acc[0]])
        x1t = [stg[i][:, 0:D] for i in range(2)]; B_x1t = B_stg
        st2 = sb("st2", [128, 12], stack=E); mv2 = sb("mv2", [128, 2], stack=E); sc2 = sb("sc2", [128, 8], stack=E); B_st2 = Buf("st2")
        B_out = Buf("out")
        for t in range(16):
            s = t % 2
            fw.dma(sp, x1t[s], x1_s[:, t * D:(t + 1) * D], reads=[B_x1s], writes=[B_x1t[s]], sem="stg%d" % s)
            TT(dve, acc[:, t, :], acc[:, t, :], grow[:, 3, :], ALU.mult, [B_acc[t], B_grow], [B_acc[t]])
            STT(acc[:, t, :], x1t[s], ALPHA, acc[:, t, :], ALU.mult, ALU.add, [B_x1t[s], B_acc[t]], [B_acc[t]])
            layer_norm(acc[:, t, :], B_acc[t], 2, acc[:, t, :], B_acc[t], st2, mv2, sc2, B_st2)
            fw.dma(sp, out_d[t * 128:(t + 1) * 128, :], acc[:, t, :], reads=[B_acc[t]], writes=[B_out], sem="out")
    return finish()


def _rope_tables(frame_tok):
    t = np.asarray(frame_tok)
    row = (t // 64).astype(np.float32)
    col = (t % 64).astype(np.float32)
    inv = (10000.0 ** (-np.arange(0, 16, 2, dtype=np.float32) / np.float32(16))).astype(np.float32)
    ang = np.concatenate([row[:, None] * inv, col[:, None] * inv], -1)
    ang = np.concatenate([ang, ang], -1).astype(np.float32)
    return np.cos(ang).astype(np.float32), np.sin(ang).astype(np.float32)


def prep_core(inp, b, hh):
    f32 = np.float32
    m = {}
    x = inp["x"][b]
    ctx = inp["ctx"][b]
    if hh == 1:
        x = x[::-1]
        ctx = ctx[::-1]
    m["xf"] = np.ascontiguousarray(x, f32)
    m["ctxf"] = np.ascontiguousarray(ctx, f32)
    cT = np.stack([inp["c"][b].reshape(8, 128).T, inp["c_ctx"].reshape(8, 128).T], -1)
    m["cT"] = np.ascontiguousarray(cT, f32)
    m["w_ada"] = inp["w_ada"][0]
    ba = inp["b_ada"][0]
    bc = ba.reshape(6, 8, 128).transpose(2, 0, 1).reshape(128, 48)
    m["b_ada_cols"] = np.ascontiguousarray(np.repeat(bc, 2, axis=1), f32)
    br = np.stack([ba[2 * D:3 * D], ba[3 * D:4 * D], ba[4 * D:5 * D], ba[5 * D:6 * D]], 0)
    m["b_ada_rows"] = np.ascontiguousarray(np.broadcast_to(br[None], (128, 4, D)), f32)
    m["w_in"] = inp["w_in"][0]
    m["qg_cols"] = np.ascontiguousarray(inp["q_norm_g"][0].reshape(2, 128).T, f32)
    m["kvg_col"] = np.ascontiguousarray(inp["kv_norm_g"][0].reshape(1, 128).T, f32)
    m["w_uq"] = inp["w_uq"][0]
    m["w_ukv"] = inp["w_ukv"][0]
    mt, j, i = np.meshgrid(np.arange(2), np.arange(16), np.arange(128), indexing="ij")
    tau = (16 * (128 * mt + i) + j).reshape(-1)
    tok = tau if hh == 0 else (NTOK - 1 - tau)
    cos, sin = _rope_tables(tok)
    rope = np.zeros((128, 2, NKEY), f32)
    rope[64:96, 0, :NTOK] = cos.T
    rope[64:96, 1, :NTOK] = sin.T
    rope[64:96, 0, NTOK:] = 1.0
    m["rope"] = rope
    dirs = [0, 1] if hh == 0 else [1, 0]

    def lay(a):
        a = a[dirs]
        sh = a.shape[3:]
        a = a.reshape(2, 16, 2, 64, *sh)
        a = np.moveaxis(a, (2, 3), (0, 1))
        return a.reshape(128, 32, *sh)

    m["ssm_a"] = np.ascontiguousarray(np.stack([lay(inp["ssm_a_re"][0]), lay(inp["ssm_a_im"][0])], 1), f32)
    ldt = np.broadcast_to(inp["ssm_log_dt"][0][:, :, None], (2, 32, 64))
    m["ssm_ldt"] = np.ascontiguousarray(lay(ldt), f32)
    m["ssm_b"] = np.ascontiguousarray(np.stack([lay(inp["ssm_b_re"][0]), lay(inp["ssm_b_im"][0])], 1), f32)
    cre = np.swapaxes(inp["ssm_c_re"][0], 2, 3)
    cim = np.swapaxes(inp["ssm_c_im"][0], 2, 3)
    m["ssm_c"] = np.ascontiguousarray(np.stack([lay(cre), lay(cim)], 1), f32)
    dd = inp["ssm_d"][0].reshape(32, 16)
    m["ssm_dcols"] = np.ascontiguousarray(np.broadcast_to(dd.T[None], (8, 16, 32)).reshape(128, 32), f32)
    jl = np.arange(128) // 16
    m0 = (jl[None, :] >= jl[:, None]).astype(f32)
    m1 = (jl[:, None] >= jl[None, :]).astype(f32)
    m["masks"] = np.ascontiguousarray(np.stack([m0, m1], 1), f32)
    m["ident"] = np.eye(128, dtype=f32)
    m["w_glu"] = inp["w_glu"][0]
    m["b_glu_row"] = np.ascontiguousarray(np.broadcast_to(inp["b_glu"][0][None], (128, 512)), f32)
    gn = np.concatenate([inp["gn_attn_g"][0], inp["gn_ssm_g"][0]])
    m["gn_cols"] = np.ascontiguousarray(gn.reshape(8, 128).T, f32)
    m["w_o"] = inp["w_o"][0]
    ln = np.stack([inp["ln1_g"][0], inp["ln1_b"][0], inp["ln2_g"][0], inp["ln2_b"][0]], 0)
    m["ln_rows"] = np.ascontiguousarray(np.broadcast_to(ln[None], (128, 4, D)), f32)
    m["w_r"] = np.ascontiguousarray(np.concatenate([inp["w_router_group"][0], inp["w_router_expert"][0]], 1), f32)
    br_ = np.concatenate([inp["b_router_group"][0], inp["b_router_expert"][0]])
    m["b_r_row"] = np.ascontiguousarray(np.broadcast_to(br_[None], (128, 36)), f32)
    m["w_exp_gate"] = inp["w_exp_gate"][0]
    m["w_exp_up"] = inp["w_exp_up"][0]
    m["w_exp_down"] = inp["w_exp_down"][0]
    sel = np.zeros((32, NE, 128), f32)
    sel[np.arange(32), np.arange(32), :] = 1.0
    m["sel"] = sel.reshape(32, NE * 128)
    return m


def run(inputs, stop_after=None, dbg_names=(), cores=8):
    inp = {k: np.asarray(v) for k, v in inputs.items()}
    nc = build(stop_after=stop_after, dbg_names=dbg_names)
    in_maps = [prep_core(inp, c // 2, c % 2) for c in range(cores)]
    res = run_bass_kernel_spmd(nc, in_maps, core_ids=list(range(cores)))
    return res.results


def kernel(**inputs):
    res = run(inputs)
    out = np.zeros((4, NTOK, D), np.float32)
    for c in range(8):
        b, hh = c // 2, c % 2
        o = res[c]["out"]
        o = o.reshape(16, 128, D).transpose(1, 0, 2).reshape(NOWN, D)
        if hh == 0:
            out[b, :NOWN] = o
        else:
            out[b, NOWN:] = o[::-1]
    return out
```

```python
import math
import os
from contextlib import ExitStack
import numpy as np
import concourse.bass as bass
import concourse.mybir as mybir
from concourse.ap import AP
from concourse.bass_utils import run_bass_kernel_spmd

F32 = mybir.dt.float32
BF16 = mybir.dt.bfloat16
AF = mybir.ActivationFunctionType
ALU = mybir.AluOpType
AX = mybir.AxisListType

D = 1024
NTOK = 4096
NOWN = 2048
NCTX = 256
NKEY = NTOK + NCTX
NH = 8
SCALE = 96 ** -0.5
ALPHA = 2.0 ** 0.25
EPS = 1e-6
NS = 134
NE = 32


class Buf:
    __slots__ = ("name", "w", "r", "excl")

    def __init__(self, name, excl=False):
        self.name = name
        self.w = None
        self.r = []
        self.excl = excl


def bufs(name, n):
    return [Buf("%s%d" % (name, i)) for i in range(n)]


class Q:
    def __init__(self, fw, name, eng, same=False):
        self.name = name
        self.eng = eng
        self.sem = fw.es.enter_context(fw.nc.semaphore("s_" + name))
        self.cnt = 0
        self.seen = {}
        self.same = same
        self.window = False


class DmaTok:
    def __init__(self, fw, name):
        self.sem = fw.es.enter_context(fw.nc.semaphore("d_" + str(name)))
        self.cnt = 0
        self.name = name
        self.same = True


class FW:
    def __init__(self, nc, es):
        self.nc = nc
        self.es = es
        self.pe = Q(self, "pe", nc.tensor)
        self.dve = Q(self, "dve", nc.vector, same=True)
        self.act = Q(self, "act", nc.scalar, same=True)
        self.dve.window = False
        self.pool = Q(self, "pool", nc.gpsimd, same=True)
        self.sp = Q(self, "sp", nc.sync)
        self.dmasems = {}

    def _waits(self, q, reads, writes, sreads=()):
        need = {}

        def add(dep, force=False):
            if dep is None:
                return
            dq, n = dep
            if dq is q and not force:
                if not q.same:
                    return
                if q.window and n < q.cnt:
                    return
            if need.get(dq, 0) < n:
                need[dq] = n

        for b in sreads:
            add(b.w, True)
        for b in reads:
            add(b.w)
            if b.excl:
                for d in b.r:
                    add(d)
        for b in writes:
            add(b.w)
            for d in b.r:
                add(d)
        for dq, n in need.items():
            if q.seen.get(id(dq), 0) >= n:
                continue
            q.eng.wait_ge(dq.sem, n)
            q.seen[id(dq)] = n

    def op(self, q, fn, reads=(), writes=(), sreads=()):
        self._waits(q, reads, writes, sreads)
        reads = list(reads) + list(sreads)
        ins = fn(q.eng)
        ins.then_inc(q.sem, 1)
        q.cnt += 1
        tok = (q, q.cnt)
        for b in reads:
            b.r.append(tok)
        for b in writes:
            b.w = tok
            b.r = []
        return ins

    def dma(self, q, out, in_, reads=(), writes=(), sem=None, **kw):
        self._waits(q, reads, writes)
        ds = self.dmasems.get(sem)
        if ds is None:
            ds = DmaTok(self, sem)
            self.dmasems[sem] = ds
        if out.dtype != in_.dtype and in_.ap[-1][1] > 2048:
            kw.setdefault("max_dma_last_dim", 4096)
        ins = q.eng.dma_start(out=out, in_=in_, **kw)
        ds.cnt += 16
        ins.then_inc(ds.sem, 16)
        tok = (ds, ds.cnt)
        for b in reads:
            b.r.append(tok)
        for b in writes:
            b.w = tok
            b.r = []
        return tok

    def barrier(self):
        qs = [self.pe, self.dve, self.act, self.pool, self.sp]
        for q in qs:
            for dq in qs + list(self.dmasems.values()):
                if dq is q or dq.cnt == 0:
                    continue
                if q.seen.get(id(dq), 0) >= dq.cnt:
                    continue
                q.eng.wait_ge(dq.sem, dq.cnt)
                q.seen[id(dq)] = dq.cnt

    def group_done(self, sem, blist):
        ds = self.dmasems[sem]
        for b in blist:
            if b.w is not None and b.w[0] is ds:
                b.w = (ds, ds.cnt)


def sbap(t, off, dims):
    full = t[:] if not isinstance(t, AP) else t
    return AP(full.tensor, full.offset + off, [list(full.ap[0])] + [list(d) for d in dims])


def part_ap(t, p0, npart, off, dims):
    full = t[:]
    pstep = full.ap[0][0]
    sub = t[p0:p0 + npart]
    return AP(sub.tensor, sub.offset + off, [[pstep, npart]] + [list(d) for d in dims])


def build(stop_after=None, dbg_names=()):
    nc = bass.Bass("TRN2", target_bir_lowering=False)
    es = ExitStack()
    fw = FW(nc, es)
    pe, dve, act, pool, sp = fw.pe, fw.dve, fw.act, fw.pool, fw.sp
    dbg = {}

    def din(name, shape, dt=F32):
        return nc.dram_tensor(name, list(shape), dt, kind="ExternalInput").ap()

    xf = din("xf", [NTOK, D])
    ctxf = din("ctxf", [NCTX, D])
    cT_d = din("cT", [128, 8, 2])
    w_ada_d = din("w_ada", [D, 6 * D])
    b_ada_2rows_d = din("b_ada_2rows", [2, 6 * D])
    w_in_d = din("w_in", [D, 928])
    qg_d = din("qg_cols", [128, 2])
    kvg_d = din("kvg_col", [128, 1])
    w_uq_d = din("w_uq", [256, 768])
    w_ukv_d = din("w_ukv", [128, 1024])
    rope_d = din("rope", [128, 2, NKEY])
    ssm_a_d = din("ssm_a", [128, 2, 32])
    ssm_ldt_d = din("ssm_ldt", [128, 32])
    ssm_b_d = din("ssm_b", [128, 2, 32, 16])
    ssm_c_d = din("ssm_c", [128, 2, 32, 16])
    ssm_dcols_d = din("ssm_dcols", [128, 32])
    masks_d = din("masks", [128, 2, 128])
    ident_d = din("ident", [128, 128])
    w_glu_d = din("w_glu", [512, 512])
    b_glu_d = din("b_glu_row", [128, 512])
    gn_d = din("gn_cols", [128, 8])
    w_o_d = din("w_o", [D, D])
    ln_d = din("ln_rows", [128, 4, D])
    w_r_d = din("w_r", [D, 36])
    b_r_d = din("b_r_row", [128, 36])
    wg_d = din("w_exp_gate", [NE, D, 256])
    wu_d = din("w_exp_up", [NE, D, 256])
    wd_d = din("w_exp_down", [NE, 256, D])
    sel_d = din("sel", [32, NE * 128])
    out_d = nc.dram_tensor("out", [NOWN, D], F32, kind="ExternalOutput").ap()
    ug_s = nc.dram_tensor("ug_s", [128, 32 * 2 * 272], BF16, kind="Internal").ap()
    attn_s = nc.dram_tensor("attn_s", [128, 16 * 512], F32, kind="Internal").ap()
    x1_s = nc.dram_tensor("x1_s", [128, 16 * D], F32, kind="Internal").ap()
    g_s = nc.dram_tensor("g_s", [128, 16 * 512], F32, kind="Internal").ap()
    dbg_out = {}
    for ent in dbg_names:
        nm, shp = ent[0], ent[1]
        dbg_out[nm] = nc.dram_tensor("dbg_" + nm, list(shp), BF16 if (len(ent) > 2 and ent[2] == "bf16") else F32, kind="ExternalOutput").ap()

    def sb(name, shape, dt=F32, stack=None):
        return (stack or es).enter_context(nc.sbuf_tensor("sb_" + name, list(shape), dt))

    PS = [es.enter_context(nc.psum_tensor("ps%d" % i, [128, 512], F32)) for i in range(8)]
    PSB = [Buf("ps%d" % i, excl=True) for i in range(8)]
    ps_rr = [0]

    def ps_next(k=None):
        i = ps_rr[0] % 8
        ps_rr[0] += 1
        return PS[i], PSB[i]

    ident = sb("ident", [128, 128]); B_ident = Buf("ident")
    ones_bf = sb("ones_bf", [128, 128], BF16); B_ones = Buf("ones")
    modc = sb("modc", [128, 96]); B_modc = Buf("modc")
    grow = sb("grow", [128, 4, D]); B_grow = Buf("grow")
    fw.dma(sp, ident[:], ident_d[:, :], writes=[B_ident], sem="c0")
    fw.op(dve, lambda e: e.memset(ones_bf[:], 1.0), writes=[B_ones])

    def dump(name, ap, reads):
        if name in dbg_out:
            fw.dma(sp, dbg_out[name], ap, reads=reads, writes=[], sem="dbg")

    def finish():
        for ds in fw.dmasems.values():
            sp.eng.wait_ge(ds.sem, ds.cnt)
        es.close()
        return nc

    with ExitStack() as ph:
        cT = sb("cT", [128, 8, 2], stack=ph); B_cT = Buf("cT")
        sil2 = sb("sil2", [128, 8, 2], stack=ph); B_sil = Buf("sil2")
        b2 = sb("b2rows", [2, 6 * D], stack=ph); B_b2 = Buf("b2")
        modrow = sb("modrow", [2, 6 * D], stack=ph); B_mr = Buf("modrow")
        ones_f = sb("ones_f", [1, 128], stack=ph); B_onesf = Buf("ones_f")
        wa = [sb("wa%d" % i, [128, 8, D], stack=ph) for i in range(2)]
        B_wa = bufs("wa", 2)
        fw.dma(sp, cT[:], cT_d[:, :, :], writes=[B_cT], sem="c1")
        fw.dma(sp, b2[:], b_ada_2rows_d[:, :], writes=[B_b2], sem="c2")
        fw.op(dve, lambda e: e.memset(ones_f[:], 1.0), writes=[B_onesf])
        fw.op(act, lambda e: e.activation(out=sil2[:], in_=cT[:], func=AF.Silu), reads=[B_cT], writes=[B_sil])
        w_ada_v = w_ada_d.rearrange("(k p) n -> p k n", p=128)
        for v in range(6):
            slot = v % 2
            fw.dma(sp if v % 2 == 0 else act, wa[slot][:], w_ada_v[:, :, v * D:(v + 1) * D], writes=[B_wa[slot]], sem="wa%d" % slot)
            for half in range(2):
                pr, Bpr = PS[half], PSB[half]
                for k in range(8):
                    fw.op(pe, lambda e: e.matmul(pr[0:2, :], lhsT=sil2[:, k, :], rhs=wa[slot][:, k, half * 512:(half + 1) * 512],
                                                 start=(k == 0), stop=(k == 7)), reads=[B_wa[slot], B_sil], writes=[Bpr])
                c0 = v * D + half * 512
                fw.op(dve, lambda e: e.tensor_tensor(out=modrow[0:2, c0:c0 + 512], in0=pr[0:2, :], in1=b2[0:2, c0:c0 + 512], op=ALU.add),
                      reads=[Bpr, B_b2], writes=[B_mr])
        for v in (1, 4):
            fw.op(dve, lambda e: e.tensor_scalar_add(out=modrow[0:2, v * D:(v + 1) * D], in0=modrow[0:2, v * D:(v + 1) * D], scalar1=1.0),
                  reads=[B_mr], writes=[B_mr])
        psc, Bpsc = PS[2], PSB[2]
        for vc in range(48):
            fw.op(pe, lambda e: e.matmul(psc[:, vc * 2:vc * 2 + 2], lhsT=modrow[0:2, vc * 128:(vc + 1) * 128], rhs=ident[0:2, 0:2],
                                         start=True, stop=True, is_transpose=True), reads=[B_mr, B_ident], writes=[Bpsc])
        fw.op(dve, lambda e: e.tensor_copy(out=modc[:], in_=psc[:, 0:96]), reads=[Bpsc], writes=[B_modc])
        for ri, v in enumerate((2, 3, 4, 5)):
            for half in range(2):
                pr, Bpr = PS[3 + (ri * 2 + half) % 2], PSB[3 + (ri * 2 + half) % 2]
                c0 = v * D + half * 512
                fw.op(pe, lambda e: e.matmul(pr[:, :], lhsT=ones_f[0:1, :], rhs=modrow[0:1, c0:c0 + 512], start=True, stop=True),
                      reads=[B_onesf, B_mr], writes=[Bpr])
                fw.op(act, lambda e: e.activation(out=grow[:, ri, half * 512:(half + 1) * 512], in_=pr[:, :], func=AF.Copy),
                      reads=[Bpr], writes=[B_grow])
    fw.barrier()
    dump("modc", modc[:], [B_modc])
    dump("grow", grow[:, 0:2].rearrange("p a b -> p (a b)"), [B_grow])
    if stop_after == "adaln":
        return finish()

    def mcol(v, ch, who):
        i = (v * 8 + ch) * 2 + who
        return modc[:, i:i + 1]

    def A(q, out, in_, func, reads, writes, **kw):
        return fw.op(q, lambda e: e.activation(out=out, in_=in_, func=func, **kw), reads=reads, writes=writes)

    def TT(q, out, in0, in1, op, reads, writes):
        return fw.op(q, lambda e: e.tensor_tensor(out=out, in0=in0, in1=in1, op=op), reads=reads, writes=writes)

    def TS(q, out, in0, s1, s2, op0, op1, reads, writes, sreads=()):
        return fw.op(q, lambda e: e.tensor_scalar(out=out, in0=in0, scalar1=s1, scalar2=s2, op0=op0, op1=op1), reads=reads, writes=writes, sreads=sreads)

    def STT(out, in0, scalar, in1, op0, op1, reads, writes):
        return fw.op(dve, lambda e: e.scalar_tensor_tensor(out=out, in0=in0, scalar=scalar, in1=in1, op0=op0, op1=op1), reads=reads, writes=writes)

    def CP(q, out, in_, reads, writes):
        return fw.op(q, lambda e: e.tensor_copy(out=out, in_=in_), reads=reads, writes=writes)

    def MM(out, lhsT, rhs, start, stop, reads, writes):
        return fw.op(pe, lambda e: e.matmul(out, lhsT=lhsT, rhs=rhs, start=start, stop=stop), reads=reads, writes=writes)

    def TR(out, in_, reads, writes, n=128):
        return fw.op(pe, lambda e: e.matmul(out, lhsT=in_, rhs=ident[0:n, 0:n], start=True, stop=True, is_transpose=True),
                     reads=list(reads) + [B_ident], writes=writes)

    def rstd_from_ss(out, ss_ps, B_ss, inv_n, tmp, B_tmp, B_out):
        A(act, tmp, ss_ps, AF.Ln, [B_ss, B_eps], [B_tmp], scale=inv_n, bias=epsc[:, 0:1])
        A(act, out, tmp, AF.Exp, [B_tmp], [B_out], scale=-0.5)

    epsc = sb("epsc", [128, 1]); B_eps = Buf("eps")
    fw.op(dve, lambda e: e.memset(epsc[:], EPS), writes=[B_eps])

    att = ExitStack()
    cqT = sb("cqT", [128, 2, NOWN], BF16, stack=att); B_cqT = Buf("cqT")
    rstdq = sb("rstdq", [128, NOWN], stack=att); B_rstdq = Buf("rstdq")
    ckvnT = sb("ckvnT", [128, NKEY], BF16, stack=att); B_ckvnT = Buf("ckvnT")
    KRT = sb("KRT", [128, NKEY], BF16, stack=att); B_KRT = Buf("KRT")

    with ExitStack() as ph:
        w_in = sb("w_in", [128, 8, 928], BF16, stack=ph); B_win = Buf("w_in")
        w_krot = sb("w_krot", [128, 8, 32], BF16, stack=ph); B_wkrot = Buf("w_krot")
        fw.dma(pool, w_in[:], w_in_d.rearrange("(k p) n -> p k n", p=128), writes=[B_win], sem="w_in")
        CP(dve, w_krot[:, :, 16:32], w_in[:, :, 384:400], [B_win], [B_wkrot])
        TS(dve, w_krot[:, :, 0:16], w_in[:, :, 400:416], -1.0, None, ALU.mult, ALU.bypass, [B_win], [B_wkrot])
        xin = [sb("xin%d" % i, [128, D], stack=ph) for i in range(4)]; B_xin = bufs("xin", 4)
        xmT = [sb("xmT%d" % i, [128, 8, 512], BF16, stack=ph) for i in range(2)]; B_xmT = bufs("xmT", 2)
        ropeg = [sb("ropeg%d" % i, [128, 2, 512], stack=ph) for i in range(2)]; B_ropeg = bufs("ropeg", 2)
        u_tm = sb("u_tm", [128, 32, 128], stack=ph); B_utm = Buf("u_tm")
        Ug = sb("Ug", [128, 32, 2, 272], BF16, stack=ph); B_Ug = Buf("Ug")
        sqb = sb("sqb", [128, 512], BF16, stack=ph); B_sqb = Buf("sqb")
        sqq = sb("sqq", [128, 2, 512], BF16, stack=ph); B_sqq = Buf("sqq")
        rawkv = sb("rawkv", [128, 512], stack=ph); B_rawkv = Buf("rawkv")
        lnt = sb("lnt", [128, 512], stack=ph); B_lnt = Buf("lnt")
        rstdkv = sb("rstdkv", [128, 512], stack=ph); B_rstdkv = Buf("rstdkv")
        rt1 = sb("rt1", [128, 512], stack=ph); B_rt1 = Buf("rt1")
        rt2 = sb("rt2", [128, 512], stack=ph); B_rt2 = Buf("rt2")
        xf_v = xf.rearrange("(m j) d -> j m d", j=16)
        ctx_v = ctxf.rearrange("(m j) d -> j m d", j=16)
        gcount = [0]

        def in_group(kind, mt, jg):
            gs = gcount[0] % 2
            gcount[0] += 1
            lat = kind == "lat"
            ncols = 512 if lat else 256
            col0 = (mt * 16 + jg * 4) * 128 if lat else NTOK
            own = lat and mt == 0
            np_ = 128 if lat else 16
            nt = 4 if lat else 16
            tw = 128 if lat else 16
            if lat:
                for t in range(4):
                    j = jg * 4 + t
                    fw.dma(sp, xin[t][:], xf_v[j, mt * 128:(mt + 1) * 128, :], writes=[B_xin[t]], sem="xin%d" % t)
            fw.dma(sp, ropeg[gs][64:96, :, 0:ncols], rope_d[64:96, :, col0:col0 + ncols], writes=[B_ropeg[gs]], sem="ropeg%d" % gs)
            who = 0 if lat else 1
            if lat:
                for ch in range(8):
                    ps, Bp = ps_next()
                    for t in range(4):
                        TR(ps[:, t * 128:(t + 1) * 128], xin[t][:, ch * 128:(ch + 1) * 128], [B_xin[t]], [Bp])
                    A(act, xmT[gs][:, ch, 0:ncols], ps[:, 0:ncols], AF.Identity, [Bp, B_modc], [B_xmT[gs]],
                      scale=mcol(1, ch, who), bias=mcol(0, ch, who))
            else:
                banks = [ps_next() for _ in range(8)]
                for t in range(16):
                    fw.dma(sp, xin[t % 4][0:16, :], ctx_v[t, 0:16, :], writes=[B_xin[t % 4]], sem="xin%d" % (t % 4))
                    for ch in range(8):
                        ps, Bp = banks[ch]
                        TR(ps[:, t * 16:(t + 1) * 16], xin[t % 4][0:16, ch * 128:(ch + 1) * 128], [B_xin[t % 4]], [Bp], n=16)
                for ch in range(8):
                    ps, Bp = banks[ch]
                    A(act, xmT[gs][:, ch, 0:ncols], ps[:, 0:ncols], AF.Identity, [Bp, B_modc], [B_xmT[gs]],
                      scale=mcol(1, ch, who), bias=mcol(0, ch, who))
            X = xmT[gs]; BX = B_xmT[gs]
            kstep = int(os.environ.get("K_STEP", "9"))
            if kstep < 2:
                return
            ps, Bp = ps_next()
            for ch in range(8):
                MM(ps[:, 0:ncols], w_in[:, ch, 256:384], X[:, ch, 0:ncols], ch == 0, ch == 7, [B_win, BX], [Bp])
            A(act, sqb[:, 0:ncols], ps[:, 0:ncols], AF.Square, [Bp], [B_sqb])
            A(act, rawkv[:, 0:ncols], ps[:, 0:ncols], AF.Copy, [Bp], [B_rawkv])
            ps2, Bp2 = ps_next()
            MM(ps2[:, 0:ncols], ones_bf[:, :], sqb[:, 0:ncols], True, True, [B_ones, B_sqb], [Bp2])
            rstd_from_ss(rstdkv[:, 0:ncols], ps2[:, 0:ncols], Bp2, 1.0 / 128, lnt[:, 0:ncols], B_lnt, B_rstdkv)
            TT(dve, ckvnT[:, col0:col0 + ncols], rawkv[:, 0:ncols], rstdkv[:, 0:ncols], ALU.mult, [B_rawkv, B_rstdkv], [B_ckvnT])
            if kstep < 3:
                return
            psa, Bpa = ps_next()
            psb, Bpb = ps_next()
            for ch in range(8):
                MM(psa[64:96, 0:ncols], w_in[:, ch, 384:416], X[:, ch, 0:ncols], ch == 0, ch == 7, [B_win, BX], [Bpa])
            for ch in range(8):
                MM(psb[64:96, 0:ncols], w_krot[:, ch, :], X[:, ch, 0:ncols], ch == 0, ch == 7, [B_wkrot, BX], [Bpb])
            TT(dve, rt1[64:96, 0:ncols], psa[64:96, 0:ncols], ropeg[gs][64:96, 0, 0:ncols], ALU.mult, [Bpa, B_ropeg[gs]], [B_rt1])
            TT(dve, rt2[64:96, 0:ncols], psb[64:96, 0:ncols], ropeg[gs][64:96, 1, 0:ncols], ALU.mult, [Bpb, B_ropeg[gs]], [B_rt2])
            TT(dve, KRT[64:96, col0:col0 + ncols], rt1[64:96, 0:ncols], rt2[64:96, 0:ncols], ALU.add, [B_rt1, B_rt2], [B_KRT])
            if kstep < 4:
                return
            if own:
                pss = []
                for fc in range(2):
                    ps, Bp = ps_next()
                    for ch in range(8):
                        MM(ps[:, :], w_in[:, ch, fc * 128:(fc + 1) * 128], X[:, ch, :], ch == 0, ch == 7, [B_win, BX], [Bp])
                    A(act, cqT[:, fc, col0:col0 + 512], ps[:, :], AF.Copy, [Bp], [B_cqT])
                    pss.append((ps, Bp))
                ksub = int(os.environ.get("K_SUB", "9"))
                if ksub >= 1:
                    ps2, Bp2 = ps_next()
                    for fc in range(2):
                        ps, Bp = pss[fc]
                        A(act, sqq[:, fc, :], ps[:, :], AF.Square, [Bp], [B_sqq])
                    for fc in range(2):
                        MM(ps2[:, :], ones_bf[:, :], sqq[:, fc, :], fc == 0, fc == 1, [B_ones, B_sqq], [Bp2])
                if ksub >= 2:
                    rstd_from_ss(rstdq[:, col0:col0 + 512], ps2[:, :], Bp2, 1.0 / 256, lnt[:, :], B_lnt, B_rstdq)
            if kstep < 5:
                return
            for t in range(nt):
                ps, Bp = ps_next()
                for ch in range(8):
                    MM(ps[0:np_, :], X[:, ch, t * tw:(t + 1) * tw], w_in[:, ch, 416:928], ch == 0, ch == 7, [B_win, BX], [Bp])
                if lat:
                    jl = (jg * 4 + t) % 8
                    uo = sbap(u_tm, jl * 16, [[128, 32], [1, 16]])
                    ui = sbap(ps, 0, [[16, 32], [1, 16]])
                    if t % 2 == 0:
                        CP(dve, uo, ui, [Bp], [B_utm])
                    else:
                        A(act, uo, ui, AF.Copy, [Bp], [B_utm])
                else:
                    A(act, part_ap(u_tm, 0, 16, (t % 8) * 16, [[128, 32], [1, 16]]), part_ap(ps, 0, 16, 0, [[16, 32], [1, 16]]),
                      AF.Copy, [Bp], [B_utm])
                    if t % 8 == 7:
                        ug_transposes(u_tm, B_utm, 16, t // 8, 256, 0)

        def ug_transposes(src, Bsrc, np_, jt, ucol0, jbase):
            for g4 in range(8):
                ps, Bp = ps_next()
                for gg in range(4):
                    g = g4 * 4 + gg
                    inap = part_ap(src, 0, np_, g * 128, [[1, 128]])
                    TR(ps[:, gg * 128:gg * 128 + np_], inap, [Bsrc], [Bp], n=np_)
                outap = sbap(Ug, (g4 * 4) * 2 * 272 + jt * 272 + ucol0, [[2 * 272, 4], [1, np_]])
                inp_ = sbap(ps, 0, [[128, 4], [1, np_]])
                if g4 % 2 == 0:
                    CP(dve, outap, inp_, [Bp], [B_Ug])
                else:
                    A(act, outap, inp_, AF.Copy, [Bp], [B_Ug])

        bis = int(os.environ.get("K_BISECT", "99"))
        for mt in range(2):
            for jg in range(4):
                if mt * 4 + jg >= bis:
                    continue
                in_group("lat", mt, jg)
                if jg % 2 == 1 and bis != 2:
                    ug_transposes(u_tm, B_utm, 128, jg // 2, mt * 128, 0)
        if bis >= 99:
            in_group("ctx", 0, 0)
        dump("ckvnT", ckvnT[:], [B_ckvnT])
        dump("KRT", KRT[64:96, :], [B_KRT])
        dump("cqT", cqT[:].rearrange("p a b -> p (a b)"), [B_cqT])
        dump("rstdq", rstdq[:], [B_rstdq])
        dump("Ug", Ug[:].rearrange("p a b c -> p (a b c)"), [B_Ug])
        fw.dma(sp, ug_s[:, :], Ug[:].rearrange("p a b c -> p (a b c)"), reads=[B_Ug], writes=[], sem="ugs")
    fw.barrier()
    if stop_after == "inproj":
        return finish()

    attn_tm = sb("attn_tm", [128, 16, 512], stack=att); B_attn = Buf("attn_tm")
    B_attn_s = Buf("attn_s")
    with ExitStack() as ph:
        wq_f = sb("wq_f", [128, 2, 768], stack=ph); B_wqf = Buf("wq_f")
        wkv_f = sb("wkv_f", [128, 1024], stack=ph); B_wkvf = Buf("wkv_f")
        qg = sb("qg", [128, 2], stack=ph); kvg = sb("kvg", [128, 1], stack=ph); B_g = Buf("qkvg")
        w_uq_s = sb("w_uq_s", [128, 2, 768], BF16, stack=ph); B_wuq = Buf("w_uq_s")
        w_uq_rot = sb("w_uq_rot", [128, 2, 8, 32], BF16, stack=ph); B_wuqr = Buf("w_uq_rot")
        w_k = sb("w_k", [128, 1024], BF16, stack=ph); B_wk = Buf("w_k")
        w_v = sb("w_v", [128, 512], BF16, stack=ph); B_wv = Buf("w_v")
        V_all = sb("V_all", [128, 34, 8, 65], BF16, stack=ph); B_V = Buf("V_all")
        KT = [sb("KT%d" % i, [128, NKEY], BF16, stack=ph) for i in range(2)]; B_KT = bufs("KT", 2)
        QT = [sb("QT%d" % i, [128, NOWN], BF16, stack=ph) for i in range(2)]; B_QT = bufs("QT", 2)
        ropeq = sb("ropeq", [128, 2, NOWN], stack=ph); B_ropeq = Buf("ropeq")
        PT = [sb("PT%d" % i, [128, 512], BF16, stack=ph) for i in range(4)]; B_PT = bufs("PT", 4)
        ot = [sb("ot%d" % i, [128, 512], stack=ph) for i in range(2)]; B_ot = bufs("ot", 2)
        qt1 = sb("qt1", [128, 512], stack=ph); B_qt1 = Buf("qt1")
        qt2 = sb("qt2", [128, 512], stack=ph); B_qt2 = Buf("qt2")
        rd = sb("rd", [128, 4], stack=ph); B_rd = Buf("rd")
        fw.dma(sp, wq_f[:], w_uq_d.rearrange("(c p) n -> p c n", p=128), writes=[B_wqf], sem="a0")
        fw.dma(sp, wkv_f[:], w_ukv_d[:, :], writes=[B_wkvf], sem="a1")
        fw.dma(sp, qg[:], qg_d[:, :], writes=[B_g], sem="a2")
        fw.dma(sp, kvg[:], kvg_d[:, :], writes=[B_g], sem="a3")
        fw.dma(sp, ropeq[64:96, :, :], rope_d[64:96, :, 0:NOWN], writes=[B_ropeq], sem="a4")
        for c in range(2):
            TS(dve, w_uq_s[:, c, :], wq_f[:, c, :], qg[:, c:c + 1], None, ALU.mult, ALU.bypass, [B_wqf, B_g], [B_wuq])
            TS(dve, w_uq_rot[:, c, :, 0:16], sbap(w_uq_s, c * 768 + 80, [[96, 8], [1, 16]]), -1.0, None, ALU.mult, ALU.bypass, [B_wuq], [B_wuqr])
            CP(dve, w_uq_rot[:, c, :, 16:32], sbap(w_uq_s, c * 768 + 64, [[96, 8], [1, 16]]), [B_wuq], [B_wuqr])
        TS(dve, w_k[:, :], wkv_f[:, :], kvg[:, 0:1], None, ALU.mult, ALU.bypass, [B_wkvf, B_g], [B_wk])
        CP(dve, w_v[:].rearrange("p (h d) -> p h d", h=8), sbap(w_k, 64, [[128, 8], [1, 64]]), [B_wk], [B_wv])
        fw.op(dve, lambda e: e.memset(V_all[:, :, :, 64:65], 1.0), writes=[B_V])
        for i in range(2):
            fw.op(dve, lambda e: e.memset(KT[i][96:128, :], 0.0), writes=[B_KT[i]])
            fw.op(dve, lambda e: e.memset(QT[i][96:128, :], 0.0), writes=[B_QT[i]])
        for kt in range(34):
            ps, Bp = PS[6 + kt % 2], PSB[6 + kt % 2]
            MM(ps[:, :], ckvnT[:, kt * 128:(kt + 1) * 128], w_v[:, :], True, True, [B_ckvnT, B_wv], [Bp])
            vo = V_all[:, kt, :, 0:64]
            vi = ps[:, :].rearrange("p (h d) -> p h d", h=8)
            if kt % 2 == 0:
                CP(dve, vo, vi, [Bp], [B_V])
            else:
                A(act, vo, vi, AF.Copy, [Bp], [B_V])

        def gen(h):
            s = h % 2
            for ct in range(9):
                n = 512 if ct < 8 else 256
                c0 = ct * 512
                ps, Bp = PS[6], PSB[6]
                MM(ps[0:64, 0:n], w_k[:, h * 128:h * 128 + 64], ckvnT[:, c0:c0 + n], True, True, [B_wk, B_ckvnT], [Bp])
                CP(dve, KT[s][0:64, c0:c0 + n], ps[0:64, 0:n], [Bp], [B_KT[s]])
                yield
            CP(pool, KT[s][64:96, :], KRT[64:96, :], [B_KRT], [B_KT[s]])
            for ct in range(4):
                c0 = ct * 512
                psq, Bq = PS[6], PSB[6]
                psr, Br = PS[7], PSB[7]
                for c in range(2):
                    MM(psq[0:96, :], w_uq_s[:, c, h * 96:(h + 1) * 96], cqT[:, c, c0:c0 + 512], c == 0, c == 1, [B_wuq, B_cqT], [Bq])
                for c in range(2):
                    MM(psr[64:96, :], w_uq_rot[:, c, h, :], cqT[:, c, c0:c0 + 512], c == 0, c == 1, [B_wuqr, B_cqT], [Br])
                TT(dve, QT[s][0:64, c0:c0 + 512], psq[0:64, :], rstdq[0:64, c0:c0 + 512], ALU.mult, [Bq, B_rstdq], [B_QT[s]])
                TT(dve, qt1[64:96, :], psq[64:96, :], ropeq[64:96, 0, c0:c0 + 512], ALU.mult, [Bq, B_ropeq], [B_qt1])
                TT(dve, qt2[64:96, :], psr[64:96, :], ropeq[64:96, 1, c0:c0 + 512], ALU.mult, [Br, B_ropeq], [B_qt2])
                TT(dve, qt1[64:96, :], qt1[64:96, :], qt2[64:96, :], ALU.add, [B_qt1, B_qt2], [B_qt1])
                TT(dve, QT[s][64:96, c0:c0 + 512], qt1[64:96, :], rstdq[64:96, c0:c0 + 512], ALU.mult, [B_qt1, B_rstdq], [B_QT[s]])
                yield

        scount = [0]
        ocount = [0]

        pend = [None]

        def epilogue(h, qt, ob, osl):
            CP(dve, ot[osl][0:65, :], PS[ob][0:65, :], [PSB[ob]], [B_ot[osl]])
            pso, Bo = PS[7], PSB[7]
            for t in range(4):
                TR(pso[:, t * 128:t * 128 + 65], ot[osl][0:65, t * 128:(t + 1) * 128], [B_ot[osl]], [Bo], n=65)
            fw.op(dve, lambda e: e.reciprocal(out=rd[:, :], in_=sbap(pso, 64, [[128, 4]])), reads=[Bo], writes=[B_rd])
            for t in range(4):
                TS(dve, attn_tm[:, qt * 4 + t, h * 64:(h + 1) * 64], pso[:, t * 128:t * 128 + 64], rd[:, t:t + 1], None, ALU.mult, ALU.bypass,
                   [Bo], [B_attn], sreads=[B_rd])

        def attend(h, nxt):
            s = h % 2
            itc = [0]
            for qt in range(4):
                ob = 4 + ocount[0] % 2
                osl = ocount[0] % 2
                ocount[0] += 1
                base = scount[0]
                scount[0] += 34

                def S(kt):
                    bi = (base + kt) % 4
                    MM(PS[bi][:, :], KT[s][:, kt * 128:(kt + 1) * 128], QT[s][:, qt * 512:(qt + 1) * 512], True, True,
                       [B_KT[s], B_QT[s]], [PSB[bi]])
                S(0)
                S(1)
                if pend[0] is not None:
                    epilogue(*pend[0])
                    pend[0] = None
                for kt in range(34):
                    if kt + 2 < 34:
                        S(kt + 2)
                    bi = (base + kt) % 4
                    A(act, PT[bi][:, :], PS[bi][:, :], AF.Exp, [PSB[bi]], [B_PT[bi]], scale=SCALE)
                    MM(PS[ob][0:65, :], V_all[:, kt, h, 0:65], PT[bi][:, :], kt == 0, kt == 33, [B_V, B_PT[bi]], [PSB[ob]])
                    itc[0] += 1
                    if nxt is not None and itc[0] % 9 == 0:
                        next(nxt, None)
                pend[0] = (h, qt, ob, osl)

        nheads = int(os.environ.get("K_HEADS", "8"))
        for _ in gen(0):
            pass
        for h in range(nheads):
            nxt = gen(h + 1) if h + 1 < nheads else None
            attend(h, nxt)
            if nxt is not None:
                for _ in nxt:
                    pass
        epilogue(*pend[0])
        dump("attn", attn_tm[:].rearrange("p a b -> p (a b)"), [B_attn])
        fw.dma(sp, attn_s[:, :], attn_tm[:].rearrange("p a b -> p (a b)"), reads=[B_attn], writes=[B_attn_s], sem="attns")
        dump("KT0", KT[0][0:96, :], [B_KT[0]])
        dump("QT0", QT[0][0:96, :], [B_QT[0]])
        dump("V0", V_all[:].rearrange("p a b c -> p (a b c)"), [B_V])
    fw.barrier()
    att.close()
    if stop_after == "attn":
        return finish()

    PI = math.pi
    B_gs = Buf("g_s")
    with ExitStack() as S:
        sa = sb("ssm_a", [128, 2, 32], stack=S); sl = sb("ssm_l", [128, 32], stack=S)
        sbb = sb("ssm_b", [128, 2, 32, 16], stack=S); scc = sb("ssm_c", [128, 2, 32, 16], stack=S)
        dcols = sb("dcols", [128, 32], stack=S); masks = sb("masks4", [128, 4, 128], stack=S)
        B_par = Buf("ssm_par")
        fw.dma(sp, sa[:], ssm_a_d[:, :, :], writes=[B_par], sem="s0")
        fw.dma(sp, sl[:], ssm_ldt_d[:, :], writes=[B_par], sem="s1")
        fw.dma(sp, sbb[:], ssm_b_d[:, :, :, :], writes=[B_par], sem="s2")
        fw.dma(sp, scc[:], ssm_c_d[:, :, :, :], writes=[B_par], sem="s3")
        fw.dma(sp, dcols[:], ssm_dcols_d[:, :], writes=[B_par], sem="s4")
        fw.dma(sp, masks[:, 0:2, :], masks_d[:, :, :], writes=[B_par], sem="s5")
        fw.dma(sp, masks[:, 2:4, :], masks_d[:, :, :], writes=[B_par], sem="s6")
        w_glu = sb("w_glu", [128, 4, 512], BF16, stack=S); B_wglu = Buf("w_glu")
        fw.dma(pool, w_glu[:], w_glu_d.rearrange("(k p) n -> p k n", p=128), writes=[B_wglu], sem="s7")
        bglu = sb("bglu", [128, 512], stack=S); B_bglu = Buf("bglu")
        fw.dma(sp, bglu[:], b_glu_d[:, :], writes=[B_bglu], sem="s8")
        sm = sb("ssm_sm", [128, 32, 32], stack=S); B_sm = Buf("ssm_sm")
        _smi = [0]

        def smt():
            i = _smi[0]; _smi[0] += 1
            return sm[:, i, :]
        Bbar = sb("Bbar", [128, 2, 32, 16], stack=S)
        PWr = sb("PWr", [128, 17, 32], stack=S); PWi = sb("PWi", [128, 17, 32], stack=S)
        IPr = sb("IPr", [128, 17, 32], stack=S); IPi = sb("IPi", [128, 17, 32], stack=S)
        AA = sb("AA", [128, 2, 2, 16], stack=S); AB = sb("AB", [128, 2, 2, 16], stack=S)
        BS = [B_par, B_sm]

        def tt(out, a, b, op):
            TT(dve, out, a, b, op, BS, [B_sm])

        a_re, a_im = sa[:, 0, :], sa[:, 1, :]
        dt_ = smt(); lre = smt(); ang = smt(); mag = smt()
        A(act, dt_, sl[:, :], AF.Exp, [B_par], [B_sm])
        tt(lre, a_re, dt_, ALU.mult)
        tt(ang, a_im, dt_, ALU.mult)
        A(act, mag, lre, AF.Exp, [B_sm], [B_sm])

        def sin_of(src_ang, shift):
            a2 = smt(); k = smt(); r = smt(); o = smt()
            TS(dve, a2, src_ang, shift, None, ALU.add, ALU.bypass, BS, [B_sm])
            TS(dve, k, a2, PI, None, ALU.is_ge, ALU.bypass, BS, [B_sm])
            for i in range(2, 9):
                STT(k, a2, (2 * i - 1) * PI, k, ALU.is_ge, ALU.add, BS, [B_sm])
            STT(r, k, -2.0 * PI, a2, ALU.mult, ALU.add, BS, [B_sm])
            A(act, o, r, AF.Sin, [B_sm], [B_sm])
            return o
        sn = sin_of(ang, 0.0)
        cs = sin_of(ang, PI / 2)
        abre = smt(); abim = smt()
        tt(abre, mag, cs, ALU.mult)
        tt(abim, mag, sn, ALU.mult)
        t1 = smt(); t2 = smt(); den = smt(); rden = smt(); nre = smt(); cre = smt(); cim = smt()
        tt(t1, a_re, a_re, ALU.mult); tt(t2, a_im, a_im, ALU.mult); tt(den, t1, t2, ALU.add)
        fw.op(dve, lambda e: e.reciprocal(out=rden, in_=den), reads=BS, writes=[B_sm])
        TS(dve, nre, abre, -1.0, None, ALU.add, ALU.bypass, BS, [B_sm])
        tt(t1, nre, a_re, ALU.mult); tt(t2, abim, a_im, ALU.mult); tt(t1, t1, t2, ALU.add); tt(cre, t1, rden, ALU.mult)
        tt(t1, abim, a_re, ALU.mult); tt(t2, nre, a_im, ALU.mult); tt(t1, t1, t2, ALU.subtract); tt(cim, t1, rden, ALU.mult)
        tb = sb("tb", [128, 32, 16], stack=S)

        def bc16(v):
            return AP(v.tensor, v.offset, [list(v.ap[0]), [1, 32], [0, 16]])
        tt(Bbar[:, 0], sbb[:, 0], bc16(cre), ALU.mult); tt(tb[:], sbb[:, 1], bc16(cim), ALU.mult); tt(Bbar[:, 0], Bbar[:, 0], tb[:], ALU.subtract)
        tt(Bbar[:, 1], sbb[:, 1], bc16(cre), ALU.mult); tt(tb[:], sbb[:, 0], bc16(cim), ALU.mult); tt(Bbar[:, 1], Bbar[:, 1], tb[:], ALU.add)
        m2 = smt(); rm2 = smt(); iabre = smt(); iabim = smt()
        tt(t1, abre, abre, ALU.mult); tt(t2, abim, abim, ALU.mult); tt(m2, t1, t2, ALU.add)
        fw.op(dve, lambda e: e.reciprocal(out=rm2, in_=m2), reads=BS, writes=[B_sm])
        tt(iabre, abre, rm2, ALU.mult)
        STT(iabim, abim, -1.0, rm2, ALU.mult, ALU.mult, BS, [B_sm])
        for (Pr, Pi, br, bi) in ((PWr, PWi, abre, abim), (IPr, IPi, iabre, iabim)):
            fw.op(dve, lambda e: e.memset(Pr[:, 0, :], 1.0), writes=[B_sm])
            fw.op(dve, lambda e: e.memset(Pi[:, 0, :], 0.0), writes=[B_sm])
            CP(dve, Pr[:, 1, :], br, BS, [B_sm])
            CP(dve, Pi[:, 1, :], bi, BS, [B_sm])
            for k in range(2, 17):
                tt(t1, Pr[:, k - 1, :], br, ALU.mult); tt(t2, Pi[:, k - 1, :], bi, ALU.mult); tt(Pr[:, k, :], t1, t2, ALU.subtract)
                tt(t1, Pr[:, k - 1, :], bi, ALU.mult); tt(t2, Pi[:, k - 1, :], br, ALU.mult); tt(Pi[:, k, :], t1, t2, ALU.add)
        for d in range(2):
            for c in range(2):
                CP(dve, AA[:, d, c, :], PWr[:, 16, d * 16:(d + 1) * 16], BS, [B_sm])
            TS(dve, AB[:, d, 0, :], PWi[:, 16, d * 16:(d + 1) * 16], -1.0, None, ALU.mult, ALU.bypass, BS, [B_sm])
            CP(dve, AB[:, d, 1, :], PWi[:, 16, d * 16:(d + 1) * 16], BS, [B_sm])
        dump("sm", sm[:].rearrange("p a b -> p (a b)"), BS)
        dump("abar", sbap(PWr, 32, [[1, 32]]), BS)
        dump("abari", sbap(PWi, 32, [[1, 32]]), BS)
        dump("Bbar", Bbar[:].rearrange("p a b c -> p (a b c)"), BS)

        ctmp = [sb("ctmp%d" % i, [128, 4, 256], stack=S) for i in range(2)]; B_ct = Buf("ctmp")

        def cplx_build(q, out_t, P_r, P_i, k0, kstep, Vt, d, neg_im):
            for half in range(4):
                p0 = half * 4

                def Pv(Pt):
                    return sbap(Pt, k0 * 32 + d * 16 + p0, [[1, 4], [kstep * 32, 16], [0, 16]])

                def Vv(c):
                    return sbap(Vt, c * 512 + (d * 16 + p0) * 16, [[16, 4], [0, 16], [1, 16]])

                def Ov(c):
                    return sbap(out_t, p0 * 512 + c * 256, [[512, 4], [16, 16], [1, 16]])
                ta = ctmp[0][:].rearrange("p a (k h) -> p a k h", h=16)
                tb_ = ctmp[1][:].rearrange("p a (k h) -> p a k h", h=16)
                R = BS + [B_ct]
                TT(q, ta, Pv(P_r), Vv(0), ALU.mult, R, [B_ct])
                TT(q, tb_, Pv(P_i), Vv(1), ALU.mult, R, [B_ct])
                TT(q, Ov(0), ta, tb_, ALU.subtract, R, [B_sm])
                TT(q, ta, Pv(P_r), Vv(1), ALU.mult, R, [B_ct])
                TT(q, tb_, Pv(P_i), Vv(0), ALU.mult, R, [B_ct])
                if neg_im:
                    TT(q, ta, ta, tb_, ALU.add, R, [B_ct])
                    TS(q, Ov(1), ta, -1.0, None, ALU.mult, ALU.bypass, R, [B_sm])
                else:
                    TT(q, Ov(1), ta, tb_, ALU.add, R, [B_sm])

        Sbf = sb("Sbf", [128, 2, 2, 16, 128], BF16, stack=S); B_Sbf = Buf("Sbf")
        with ExitStack() as S2:
            Ug = sb("Ug2", [128, 32, 2, 272], BF16, stack=S2); B_Ug2 = Buf("Ug2")
            fw.dma(sp, Ug[:].rearrange("p a b c -> p (a b c)"), ug_s[:, :], writes=[B_Ug2], sem="s9")
            L = sb("L", [128, 2, 2, 16, 272], BF16, stack=S2); B_L = Buf("L")
            with ExitStack() as S2a:
                Wsrc = sb("Wsrc", [128, 16, 2, 256], stack=S2a)
                Wb = sb("Wb", [128, 16, 4, 128], BF16, stack=S2a); B_Wb = Buf("Wb")
                for d in range(2):
                    if d == 0:
                        cplx_build(dve, Wsrc, PWr, PWi, 15, -1, Bbar, 0, False)
                    else:
                        cplx_build(dve, Wsrc, PWr, PWi, 0, 1, Bbar, 1, False)
                    for pt in range(16):
                        ps, Bp = ps_next()
                        for blk in range(4):
                            TR(ps[:, blk * 128:(blk + 1) * 128], sbap(Wsrc, pt * 512 + blk * 128, [[1, 128]]), BS, [Bp])
                        if pt % 2 == 0:
                            A(act, Wb[:, pt, :, :], ps[:, :].rearrange("p (a b) -> p a b", a=4), AF.Copy, [Bp], [B_Wb])
                        else:
                            CP(dve, Wb[:, pt, :, :], ps[:, :].rearrange("p (a b) -> p a b", a=4), [Bp], [B_Wb])
                    for pt in range(16):
                        for c in range(2):
                            ps, Bp = ps_next()
                            if d == 1:
                                ranges = [(0, 272, 0)]
                                ncol = 272
                            else:
                                ranges = [(256, 16, 0), (0, 128, 16)]
                                ncol = 144
                            for (u0, n, o0) in ranges:
                                for gl in range(2):
                                    for jt in range(2):
                                        MM(ps[gl * 64:(gl + 1) * 64, o0:o0 + n], Wb[:, pt, c * 2 + jt, gl * 64:(gl + 1) * 64],
                                           Ug[:, 2 * pt + gl, jt, u0:u0 + n], jt == 0, jt == 1, [B_Wb, B_Ug2], [Bp])
                            lo = L[:, d, c, pt, 272 - ncol:272]
                            if c == 0:
                                A(act, lo, ps[:, 0:ncol], AF.Copy, [Bp], [B_L])
                            else:
                                CP(dve, lo, ps[:, 0:ncol], [Bp], [B_L])
                    if d == 0:
                        dump("Wb0", Wb[:].rearrange("p a b c -> p (a b c)"), [B_Wb])
            dump("L", L[:].rearrange("p a b c e -> p (a b c e)"), [B_L])
            fw.barrier()
            Sh = sb("Sh", [128, 2, 2, 16, NS], stack=S2); B_Sh = Buf("Sh")
            fw.op(pool, lambda e: e.memset(Sh[:], 0.0), writes=[B_Sh])
            tm1 = sb("tm1", [128, 2, 2, 16], stack=S2); tm2 = sb("tm2", [128, 2, 2, 16], stack=S2); B_tm = Buf("tm")

            def f0(st):
                return st - 140 if st >= 142 else st % 2

            def f1(st):
                return 273 - st if st >= 142 else 132 + st % 2
            B_tmh = bufs("tmh", 2); B_Shh = bufs("Shh", 2)
            for b_ in B_Shh:
                b_.w = B_Sh.w
            RSh = [[B_Shh[h_], B_L, B_sm, B_tmh[h_]] for h_ in range(2)]
            for s in range(272):
                both = s >= 128
                ops = []
                for ph_ in range(2):
                    po = ph_ * 8
                    if both:
                        def hv(t_, n_, c0, c1, rev=False):
                            dstep = 32 * n_ + (c1 - c0)
                            if rev:
                                return sbap(t_, c0 + 16 * n_ + po * n_, [[dstep, 2], [-16 * n_, 2], [n_, 8]])
                            return sbap(t_, c0 + po * n_, [[dstep, 2], [16 * n_, 2], [n_, 8]])
                        sr = hv(Sh, NS, f0(s), f1(s)); srev = hv(Sh, NS, f0(s), f1(s), True)
                        sw = hv(Sh, NS, f0(s + 1), f1(s + 1))
                        lv = hv(L, 272, s, 271 - s)
                        aa, ab = AA[:, :, :, po:po + 8], AB[:, :, :, po:po + 8]
                        x1, x2 = tm1[:, :, :, po:po + 8], tm2[:, :, :, po:po + 8]
                    else:
                        def hv(t_, n_, c1, rev=False):
                            if rev:
                                return sbap(t_, 32 * n_ + c1 + 16 * n_ + po * n_, [[-16 * n_, 2], [n_, 8]])
                            return sbap(t_, 32 * n_ + c1 + po * n_, [[16 * n_, 2], [n_, 8]])
                        sr = hv(Sh, NS, f1(s)); srev = hv(Sh, NS, f1(s), True); sw = hv(Sh, NS, f1(s + 1))
                        lv = hv(L, 272, 271 - s)
                        aa, ab = AA[:, 1, :, po:po + 8], AB[:, 1, :, po:po + 8]
                        x1, x2 = tm1[:, 1, :, po:po + 8], tm2[:, 1, :, po:po + 8]
                    ops.append((sr, srev, sw, lv, aa, ab, x1, x2))
                for h_, (sr, srev, sw, lv, aa, ab, x1, x2) in enumerate(ops):
                    TT(dve, x1, sr, aa, ALU.mult, RSh[h_], [B_tmh[h_]])
                for h_, (sr, srev, sw, lv, aa, ab, x1, x2) in enumerate(ops):
                    TT(dve, x2, srev, ab, ALU.mult, RSh[h_], [B_tmh[h_]])
                for h_, (sr, srev, sw, lv, aa, ab, x1, x2) in enumerate(ops):
                    TT(dve, x1, x1, lv, ALU.add, RSh[h_], [B_tmh[h_]])
                for h_, (sr, srev, sw, lv, aa, ab, x1, x2) in enumerate(ops):
                    TT(dve, sw, x1, x2, ALU.add, RSh[h_], [B_Shh[h_]])
            CP(dve, Sbf[:, 0].rearrange("p a b c -> p (a b) c"), sbap(Sh, 4, [[NS, 32], [1, 128]]), B_Shh, [B_Sbf])
            CP(dve, Sbf[:, 1].rearrange("p a b c -> p (a b) c"), sbap(Sh, 32 * NS + 2, [[NS, 32], [1, 128]]), B_Shh, [B_Sbf])
            dump("Sbf", Sbf[:].rearrange("p a b c e -> p (a b c e)"), [B_Sbf])
        fw.barrier()
        if stop_after == "scan":
            S.close()
            return finish()
        with ExitStack() as S3:
            Kmat = sb("Kmat", [128, 32, 4, 128], BF16, stack=S3); B_Km = Buf("Kmat")
            Ca = sb("Ca", [128, 2, 16, 2, 256], BF16, stack=S3); B_Ca = Buf("Ca")
            Ugo = sb("Ugo", [128, 64, 128], BF16, stack=S3); B_Ugo = Buf("Ugo")
            fw.dma(sp, Ugo[:], ug_s.rearrange("p (a c) -> p a c", c=272)[:, :, 0:128], writes=[B_Ugo], sem="s10")
            S3a = ExitStack()
            Xt = sb("Xt", [128, 16, 2, 256], BF16, stack=S3a); Zt = sb("Zt", [128, 16, 2, 256], BF16, stack=S3a)
            tmpK = sb("tmpK", [128, 4, 128], stack=S3a); tmpS = sb("tmpS", [128, 2, 128], stack=S3a); B_tk = Buf("tmpK")
            cplx_build(dve, Ca[:, 0], PWr, PWi, 1, 1, scc, 0, True)
            cplx_build(dve, Ca[:, 1], PWr, PWi, 16, -1, scc, 1, True)
            KS = BS + [B_tk]
            for d in range(2):
                if d == 0:
                    cplx_build(dve, Xt, IPr, IPi, 0, 1, Bbar, 0, False)
                    cplx_build(dve, Zt, PWr, PWi, 0, 1, scc, 0, True)
                else:
                    cplx_build(dve, Xt, PWr, PWi, 0, 1, Bbar, 1, False)
                    cplx_build(dve, Zt, IPr, IPi, 0, 1, scc, 1, True)
                for g in range(32):
                    pt, gl = g // 2, g % 2
                    r0, r1 = gl * 64, gl * 64 + 64
                    psA, BpA = ps_next()

                    def kblock(ps, col, jt, tt_):
                        for c in range(2):
                            MM(ps[:, col * 128:(col + 1) * 128], Xt[r0:r1, pt, c, jt * 128:(jt + 1) * 128],
                               Zt[r0:r1, pt, c, tt_ * 128:(tt_ + 1) * 128], c == 0, c == 1, BS, [BpA])
                    kblock(psA, 0, 0, 0)
                    kblock(psA, 1, 1, 1)
                    if d == 0:
                        kblock(psA, 2, 0, 1)
                    else:
                        kblock(psA, 2, 1, 0)
                    mk = sbap(masks, d * 128, [[0, 2], [1, 128]])
                    kd = sbap(Kmat, g * 512, [[384, 2], [1, 128]])
                    ko_ = Kmat[:, g, 1 if d == 0 else 2, :]
                    if d == 0:
                        TT(dve, tmpS[:], psA[:, 0:256].rearrange("p (a b) -> p a b", a=2), mk, ALU.mult, [BpA, B_par], [B_tk])
                        STT(kd, sbap(ident, 0, [[0, 2], [1, 128]]), dcols[:, g:g + 1], tmpS[:], ALU.mult, ALU.add, [B_ident, B_par, B_tk], [B_Km])
                    else:
                        TT(dve, tmpS[:], psA[:, 0:256].rearrange("p (a b) -> p a b", a=2), mk, ALU.mult, [BpA, B_par], [B_tk])
                        TT(dve, kd, kd, tmpS[:], ALU.add, [B_tk, B_Km], [B_Km])
                    A(act, ko_, psA[:, 256:384], AF.Copy, [BpA], [B_Km])
            dump("Kmat", Kmat[:].rearrange("p a b c -> p (a b c)"), [B_Km])
            dump("Ca", Ca[:].rearrange("p a b c e -> p (a b c e)"), BS)
            fw.barrier()
            S3a.close()
            g_tm = sb("g_tm", [128, 16, 512], stack=S3); B_gtm = Buf("g_tm")
            glsb = [sb("glsb%d" % i, [128, 512], stack=S3) for i in range(2)]; B_gl = bufs("glsb", 2)
            for gp in range(16):
                psY, BpY = ps_next()
                for gi in range(2):
                    g = gp * 2 + gi
                    pt, gl = g // 2, g % 2
                    r0, r1 = gl * 64, gl * 64 + 64
                    for tt_ in range(2):
                        yo = psY[:, (gi * 2 + tt_) * 128:(gi * 2 + tt_ + 1) * 128]
                        first = True
                        for jt in range(2):
                            MM(yo, Kmat[:, g, jt * 2 + tt_, :], Ugo[:, g * 2 + jt, :], first, False, [B_Km, B_Ugo], [BpY])
                            first = False
                        for d in range(2):
                            for c in range(2):
                                MM(yo, Ca[r0:r1, d, pt, c, tt_ * 128:(tt_ + 1) * 128], Sbf[r0:r1, d, c, pt, :], False, (d == 1 and c == 1),
                                   BS + [B_Sbf], [BpY])
                gs_ = gp % 2
                A(act, glsb[gs_][:, :], psY[:, :], AF.Gelu, [BpY], [B_gl[gs_]])
                psT, BpT = ps_next()
                for blk in range(4):
                    TR(psT[:, blk * 128:(blk + 1) * 128], glsb[gs_][:, blk * 128:(blk + 1) * 128], [B_gl[gs_]], [BpT])
                for gi in range(2):
                    g = gp * 2 + gi
                    oo = sbap(g_tm, 16 * g, [[512, 16], [1, 16]])
                    ii = sbap(psT, gi * 256, [[16, 16], [1, 16]])
                    CP(dve, oo, ii, [BpT], [B_gtm])
            dump("gtm", g_tm[:].rearrange("p a b -> p (a b)"), [B_gtm])
            gT = [sb("gT%d" % i, [128, 4, 128], BF16, stack=S3) for i in range(2)]; B_gT = bufs("gT", 2)
            zt = sb("zt", [128, 512], stack=S3); B_zt = Buf("zt")
            for t in range(16):
                gs_ = t % 2
                ps, Bp = ps_next()
                for fc in range(4):
                    TR(ps[:, fc * 128:(fc + 1) * 128], g_tm[:, t, fc * 128:(fc + 1) * 128], [B_gtm], [Bp])
                A(act, gT[gs_][:], ps[:, :].rearrange("p (a b) -> p a b", a=4), AF.Copy, [Bp], [B_gT[gs_]])
                ps2, Bp2 = ps_next()
                for fc in range(4):
                    MM(ps2[:, :], gT[gs_][:, fc, :], w_glu[:, fc, :], fc == 0, fc == 3, [B_gT[gs_], B_wglu], [Bp2])
                TT(dve, zt[:], ps2[:, :], bglu[:], ALU.add, [Bp2, B_bglu], [B_zt])
                A(act, zt[:], zt[:], AF.Sigmoid, [B_zt], [B_zt])
                TT(dve, g_tm[:, t, :], g_tm[:, t, :], zt[:], ALU.mult, [B_gtm, B_zt], [B_gtm])
            dump("ssm", g_tm[:].rearrange("p a b -> p (a b)"), [B_gtm])
            fw.dma(sp, g_s[:, :], g_tm[:].rearrange("p a b -> p (a b)"), reads=[B_gtm], writes=[B_gs], sem="gs")
    fw.barrier()
    if stop_after == "ssm":
        return finish()

    hT = sb("hT", [128, 8, NOWN], BF16); B_hT = Buf("hT")
    combT = sb("combT", [32, NOWN], BF16); B_combT = Buf("combT")
    lnbox = [None, None]
    B_x1s = Buf("x1_s")

    def layer_norm(pre, B_pre, gi, out, B_out, st, mv, sc_, B_st):
        for hf in range(2):
            fw.op(dve, lambda e: e.bn_stats(out=st[:, hf * 6:(hf + 1) * 6], in_=pre[:, hf * 512:(hf + 1) * 512]), reads=[B_pre], writes=[B_st])
        fw.op(dve, lambda e: e.bn_aggr(out=mv[:, 0:2], in_=st[:, 0:12]), reads=[B_st], writes=[B_st])
        A(act, sc_[:, 0:1], mv[:, 1:2], AF.Ln, [B_st, B_eps], [B_st], bias=epsc[:, 0:1])
        A(act, sc_[:, 1:2], sc_[:, 0:1], AF.Exp, [B_st], [B_st], scale=-0.5)
        TS(dve, out, pre, mv[:, 0:1], sc_[:, 1:2], ALU.subtract, ALU.mult, [B_pre], [B_out], sreads=[B_st])
        lnrows, B_ln = lnbox
        TT(dve, out, out, lnrows[:, 0, :], ALU.mult, [B_out, B_ln], [B_out])
        TT(dve, out, out, lnrows[:, 1, :], ALU.add, [B_out, B_ln], [B_out])

    with ExitStack() as M:
        ln1 = sb("ln1rows", [128, 2, D], stack=M); lnbox[0] = ln1; lnbox[1] = Buf("ln1")
        fw.dma(sp, ln1[:], ln_d[:, 0:2, :], writes=[lnbox[1]], sem="m0")
        w_o_s = sb("w_o_s", [128, 8, D], BF16, stack=M); B_wo = Buf("w_o_s")
        wof = [sb("wof%d" % i, [128, D], stack=M) for i in range(2)]; B_wof = bufs("wof", 2)
        gn = sb("gn", [128, 8], stack=M); B_gn = Buf("gn")
        w_r = sb("w_r", [128, 8, 36], stack=M); b_r = sb("b_r", [128, 36], stack=M); B_wr = Buf("w_r")
        fw.dma(sp, gn[:], gn_d[:, :], writes=[B_gn], sem="m1")
        fw.dma(sp, w_r[:], w_r_d.rearrange("(k p) n -> p k n", p=128), writes=[B_wr], sem="m2")
        fw.dma(sp, b_r[:], b_r_d[:, :], writes=[B_wr], sem="m3")
        for fc in range(8):
            fw.dma(sp, wof[fc % 2][:], w_o_d[fc * 128:(fc + 1) * 128, :], writes=[B_wof[fc % 2]], sem="wof%d" % (fc % 2))
            TS(dve, w_o_s[:, fc, :], wof[fc % 2][:], gn[:, fc:fc + 1], None, ALU.mult, ALU.bypass, [B_wof[fc % 2], B_gn], [B_wo])
        cat = [sb("cat%d" % i, [128, D], stack=M) for i in range(2)]; B_cat = bufs("cat", 2)
        xo = [sb("xo%d" % i, [128, D], stack=M) for i in range(2)]; B_xo = bufs("xo", 2)
        catT = [sb("catT%d" % i, [128, 8, 128], BF16, stack=M) for i in range(2)]; B_catT = bufs("catT", 2)
        pre = [sb("pre%d" % i, [128, D], stack=M) for i in range(2)]; B_pre = bufs("pre", 2)
        hTf = [sb("hTf%d" % i, [128, 8, 128], stack=M) for i in range(2)]; B_hTf = bufs("hTf", 2)
        htm = [sb("htm%d" % i, [128, D], stack=M) for i in range(2)]; B_htm = bufs("htm", 2)
        junk = sb("junk", [128, 512], stack=M); B_junk = Buf("junk")
        st = sb("st", [128, 12], stack=M); mv = sb("mv", [128, 2], stack=M); sc_ = sb("sc_", [128, 8], stack=M); B_st = Buf("st")
        rt = sb("rt", [128, 160], stack=M); B_rt = Buf("rt")
        scA = sb("scA", [128, 8], stack=M); B_stA = Buf("stA")
        comb = sb("comb", [128, 32], stack=M); B_comb = Buf("comb")
        mixps = [[], []]

        def stageA(t):
            s = t % 2
            fw.dma(sp, cat[s][:, 0:512], attn_s[:, t * 512:(t + 1) * 512], reads=[B_attn_s], writes=[B_cat[s]], sem="cat%d" % s)
            fw.dma(sp, cat[s][:, 512:1024], g_s[:, t * 512:(t + 1) * 512], reads=[B_gs], writes=[B_cat[s]], sem="cat%d" % s)
            fw.group_done("cat%d" % s, [B_cat[s]])
            fw.dma(sp, xo[s][:], xf_v[t, 0:128, :], writes=[B_xo[s]], sem="xo%d" % s)
            for hf in range(2):
                A(act, junk[:], cat[s][:, hf * 512:(hf + 1) * 512], AF.Square, [B_cat[s]], [B_junk, B_stA], accum_out=scA[:, 2 + hf:3 + hf])
                A(act, scA[:, 4 + hf:5 + hf], scA[:, 2 + hf:3 + hf], AF.Ln, [B_stA, B_eps], [B_stA], scale=1.0 / 512, bias=epsc[:, 0:1])
                A(act, scA[:, 6 + hf:7 + hf], scA[:, 4 + hf:5 + hf], AF.Exp, [B_stA], [B_stA], scale=-0.5)
                A(act, cat[s][:, hf * 512:(hf + 1) * 512], cat[s][:, hf * 512:(hf + 1) * 512], AF.Copy, [B_cat[s], B_stA], [B_cat[s]],
                  scale=scA[:, 6 + hf:7 + hf])
            for hf in range(2):
                ps, Bp = ps_next()
                for q4 in range(4):
                    fc = hf * 4 + q4
                    TR(ps[:, q4 * 128:(q4 + 1) * 128], cat[s][:, fc * 128:(fc + 1) * 128], [B_cat[s]], [Bp])
                A(act, catT[s][:, hf * 4:(hf + 1) * 4, :], ps[:, :].rearrange("p (a b) -> p a b", a=4), AF.Copy, [Bp], [B_catT[s]])
            for hf in range(2):
                ps, Bp = ps_next()
                for fc in range(8):
                    MM(ps[:, :], catT[s][:, fc, :], w_o_s[:, fc, hf * 512:(hf + 1) * 512], fc == 0, fc == 7, [B_catT[s], B_wo], [Bp])
                mixps[t % 2].append((ps, Bp))

        def stageA_pre(t):
            s = t % 2
            for hf in range(2):
                ps, Bp = mixps[t % 2][hf]
                TT(dve, pre[s][:, hf * 512:(hf + 1) * 512], ps[:, :], grow[:, 0, hf * 512:(hf + 1) * 512], ALU.mult, [Bp, B_grow], [B_pre[s]])
            mixps[t % 2].clear()
            STT(pre[s][:], xo[s][:], ALPHA, pre[s][:], ALU.mult, ALU.add, [B_xo[s], B_pre[s]], [B_pre[s]])

        def stageB(t):
            s = t % 2
            layer_norm(pre[s][:], B_pre[s], 0, pre[s][:], B_pre[s], st, mv, sc_, B_st)
            fw.dma(sp, x1_s[:, t * D:(t + 1) * D], pre[s][:], reads=[B_pre[s]], writes=[B_x1s], sem="x1s")
            TT(dve, htm[s][:], pre[s][:], grow[:, 2, :], ALU.mult, [B_pre[s], B_grow], [B_htm[s]])
            TT(dve, htm[s][:], htm[s][:], grow[:, 1, :], ALU.add, [B_htm[s], B_grow], [B_htm[s]])
            for hf in range(2):
                ps, Bp = ps_next()
                for q4 in range(4):
                    fc = hf * 4 + q4
                    TR(ps[:, q4 * 128:(q4 + 1) * 128], htm[s][:, fc * 128:(fc + 1) * 128], [B_htm[s]], [Bp])
                A(act, hT[:, hf * 4:(hf + 1) * 4, t * 128:(t + 1) * 128], ps[:, :].rearrange("p (a b) -> p a b", a=4), AF.Copy, [Bp], [B_hT])
                A(act, hTf[s][:, hf * 4:(hf + 1) * 4, :], ps[:, :].rearrange("p (a b) -> p a b", a=4), AF.Copy, [Bp], [B_hTf[s]])

        rps = [None]

        def stageC_mm(t):
            s = t % 2
            ps, Bp = ps_next()
            for fc in range(8):
                MM(ps[:, 0:36], hTf[s][:, fc, :], w_r[:, fc, :], fc == 0, fc == 7, [B_hTf[s], B_wr], [Bp])
            rps[0] = (ps, Bp)

        def stageC(t):
            s = t % 2
            ps, Bp = rps[0]
            lg = rt[:, 0:36]
            gsel = rt[:, 36:40]; gmax = rt[:, 40:41]; ngmax = rt[:, 41:42]; gsum = rt[:, 42:43]; g_w = rt[:, 43:44]
            esel = rt[:, 44:52]; m1 = rt[:, 52:53]; mask1 = rt[:, 56:64]; esel2 = rt[:, 64:72]; m2 = rt[:, 53:54]; mask2 = rt[:, 72:80]
            nm1 = rt[:, 54:55]; e2 = rt[:, 55:56]; den_ = rt[:, 80:81]; w1 = rt[:, 81:82]; w2 = rt[:, 82:83]; cw8 = rt[:, 88:96]; gex = rt[:, 96:100]
            RR = [B_rt]
            TT(dve, lg, ps[:, 0:36], b_r[:, :], ALU.add, [Bp, B_wr], RR)
            fw.op(dve, lambda e: e.reduce_max(out=gmax, in_=rt[:, 0:4], axis=AX.X), reads=RR, writes=RR)
            TS(dve, gsel, rt[:, 0:4], gmax, None, ALU.is_ge, ALU.bypass, RR, RR, sreads=RR)
            TS(dve, ngmax, gmax, -1.0, None, ALU.mult, ALU.bypass, RR, RR)
            A(act, gex, rt[:, 0:4], AF.Exp, RR, RR, bias=ngmax, accum_out=gsum)
            fw.op(dve, lambda e: e.reciprocal(out=g_w, in_=gsum), reads=RR, writes=RR)
            TS(dve, esel, rt[:, 4:12], gsel[:, 0:1], None, ALU.mult, ALU.bypass, RR, RR, sreads=RR)
            for g in range(1, 4):
                fw.op(dve, lambda e: e.scalar_tensor_tensor(out=esel, in0=rt[:, 4 + 8 * g:12 + 8 * g], scalar=gsel[:, g:g + 1], in1=esel,
                                                            op0=ALU.mult, op1=ALU.add), reads=RR, writes=RR, sreads=RR)
            fw.op(dve, lambda e: e.reduce_max(out=m1, in_=esel, axis=AX.X), reads=RR, writes=RR)
            TS(dve, mask1, esel, m1, None, ALU.is_ge, ALU.bypass, RR, RR, sreads=RR)
            STT(esel2, mask1, -1e30, esel, ALU.mult, ALU.add, RR, RR)
            fw.op(dve, lambda e: e.reduce_max(out=m2, in_=esel2, axis=AX.X), reads=RR, writes=RR)
            TS(dve, mask2, esel2, m2, None, ALU.is_ge, ALU.bypass, RR, RR, sreads=RR)
            TS(dve, nm1, m1, -1.0, None, ALU.mult, ALU.bypass, RR, RR)
            A(act, e2, m2, AF.Exp, RR, RR, bias=nm1)
            TS(dve, den_, e2, 1.0, None, ALU.add, ALU.bypass, RR, RR)
            fw.op(dve, lambda e: e.reciprocal(out=w1, in_=den_), reads=RR, writes=RR)
            TT(dve, w2, e2, w1, ALU.mult, RR, RR)
            TT(dve, w1, w1, g_w, ALU.mult, RR, RR)
            TT(dve, w2, w2, g_w, ALU.mult, RR, RR)
            TS(dve, cw8, mask1, w1, None, ALU.mult, ALU.bypass, RR, RR, sreads=RR)
            fw.op(dve, lambda e: e.scalar_tensor_tensor(out=cw8, in0=mask2, scalar=w2, in1=cw8, op0=ALU.mult, op1=ALU.add),
                  reads=RR, writes=RR, sreads=RR)
            for g in range(4):
                TS(dve, comb[:, g * 8:(g + 1) * 8], cw8, gsel[:, g:g + 1], None, ALU.mult, ALU.bypass, RR, [B_comb], sreads=RR)

        def stageC_tail(t):
            s = t % 2
            ps, Bp = ps_next()
            TR(ps[0:32, 0:128], comb[:, :], [B_comb], [Bp])
            CP(dve, combT[:, t * 128:(t + 1) * 128], ps[0:32, 0:128], [Bp], [B_combT])
            if t == 0:
                dump("x1_0", pre[s][:], [B_pre[s]])
                dump("comb0", comb[:], [B_comb])

        stageA(0)
        stageA_pre(0)
        for t in range(17):
            if t >= 1:
                stageC_mm(t - 1)
            if t + 1 < 16:
                stageA(t + 1)
            if t < 16:
                stageB(t)
            if t >= 1:
                stageC(t - 1)
            if t + 1 < 16:
                stageA_pre(t + 1)
            if t >= 1:
                stageC_tail(t - 1)
    fw.barrier()
    if stop_after == "merge":
        return finish()

    with ExitStack() as E:
        ln2 = sb("ln2rows", [128, 2, D], stack=E); lnbox[0] = ln2; lnbox[1] = Buf("ln2")
        fw.dma(sp, ln2[:], ln_d[:, 2:4, :], writes=[lnbox[1]], sem="e9")
        acc = sb("acc", [128, 16, D], stack=E); B_acc = bufs("acc", 16)
        sel = sb("sel", [32, NE * 128], BF16, stack=E); B_sel = Buf("sel")
        fw.dma(pool, sel[:], sel_d[:, :], writes=[B_sel], sem="e0")
        NSLOT = 3
        Wg = [sb("Wg%d" % i, [128, 8, 256], BF16, stack=E) for i in range(NSLOT)]
        Wu = [sb("Wu%d" % i, [128, 8, 256], BF16, stack=E) for i in range(NSLOT)]
        Wd = [sb("Wd%d" % i, [128, 2, D], BF16, stack=E) for i in range(NSLOT)]
        B_W = bufs("Wexp", NSLOT)
        sg = [sb("sg%d" % i, [128, 512], stack=E) for i in range(2)]; B_sg = bufs("sg", 2)
        tu = [sb("tu%d" % i, [128, 512], stack=E) for i in range(2)]; B_tu = bufs("tu", 2)
        gT2 = [sb("gT2_%d" % i, [128, 2, 256], BF16, stack=E) for i in range(2)]; B_gT2 = bufs("gT2", 2)
        n_exp = int(os.environ.get("K_NEXP", str(NE)))

        stg = [sb("stg%d" % i, [128, 2048], stack=E) for i in range(2)]; B_stg = bufs("stg", 2)
        stc = [0]

        def load_expert(e):
            sl_ = e % NSLOT
            for (src_, dst, kk) in ((wg_d[e], Wg[sl_], 8), (wu_d[e], Wu[sl_], 8), (wd_d[e], Wd[sl_], 2)):
                k_ = stc[0] % 2
                stc[0] += 1
                fw.dma(sp, stg[k_][:].rearrange("p (k n) -> p k n", k=kk), src_.rearrange("(k p) n -> p k n", p=128),
                       writes=[B_stg[k_]], sem="stg%d" % k_)
                CP(pool, dst[:].rearrange("p k n -> p (k n)"), stg[k_][:], [B_stg[k_]], [B_W[sl_]])

        for e in range(min(NSLOT, n_exp)):
            load_expert(e)
        items = [(e, tg) for e in range(n_exp) for tg in range(8)]
        dcnt = [0]

        csb = [sb("csb%d" % i, [128, 256], BF16, stack=E) for i in range(2)]; B_csb = bufs("csb", 2)

        def AUC(i):
            e, tg = items[i]
            sl_ = e % NSLOT
            k2 = i % 2
            pa, Bpa = PS[k2 * 2], PSB[k2 * 2]
            pu, Bpu = PS[k2 * 2 + 1], PSB[k2 * 2 + 1]
            pc_, Bpc = PS[4], PSB[4]
            MM(pc_[:, 0:256], sel[0:32, e * 128:(e + 1) * 128], combT[0:32, tg * 256:(tg + 1) * 256], True, True, [B_sel, B_combT], [Bpc])
            for fc in range(2):
                for ch in range(8):
                    MM(pa[:, fc * 256:(fc + 1) * 256], Wg[sl_][:, ch, fc * 128:(fc + 1) * 128], hT[:, ch, tg * 256:(tg + 1) * 256],
                       ch == 0, ch == 7, [B_W[sl_], B_hT], [Bpa])
            for fc in range(2):
                for ch in range(8):
                    MM(pu[:, fc * 256:(fc + 1) * 256], Wu[sl_][:, ch, fc * 128:(fc + 1) * 128], hT[:, ch, tg * 256:(tg + 1) * 256],
                       ch == 0, ch == 7, [B_W[sl_], B_hT], [Bpu])

        def REST(i):
            e, tg = items[i]
            sl_ = e % NSLOT
            k2 = i % 2
            pa, Bpa = PS[k2 * 2], PSB[k2 * 2]
            pu, Bpu = PS[k2 * 2 + 1], PSB[k2 * 2 + 1]
            A(act, sg[k2][:, :], pa[:, :], AF.Silu, [Bpa], [B_sg[k2]])
            if i + 1 < len(items):
                A(act, csb[1 - k2][:, :], PS[4][:, 0:256], AF.Copy, [PSB[4]], [B_csb[1 - k2]])
            TT(dve, tu[k2][:, :], pu[:, :], sg[k2][:, :], ALU.mult, [Bpu, B_sg[k2]], [B_tu[k2]])
            TT(dve, gT2[k2][:], tu[k2][:, :].rearrange("p (a b) -> p a b", a=2), sbap(csb[k2], 0, [[0, 2], [1, 256]]), ALU.mult,
               [B_tu[k2], B_csb[k2]], [B_gT2[k2]])
            for tt_ in range(2):
                tile_ = tg * 2 + tt_
                for hf in range(2):
                    po, Bpo = PS[5 + dcnt[0] % 3], PSB[5 + dcnt[0] % 3]
                    dcnt[0] += 1
                    for fc in range(2):
                        MM(po[:, :], gT2[k2][:, fc, tt_ * 128:(tt_ + 1) * 128], Wd[sl_][:, fc, hf * 512:(hf + 1) * 512],
                           fc == 0, fc == 1, [B_gT2[k2], B_W[sl_]], [Bpo])
                    ao = acc[:, tile_, hf * 512:(hf + 1) * 512]
                    if e == 0:
                        CP(dve, ao, po[:, :], [Bpo], [B_acc[tile_]])
                    else:
                        TT(dve, ao, po[:, :], ao, ALU.add, [Bpo, B_acc[tile_]], [B_acc[tile_]])
            if tg == 7 and e + NSLOT < n_exp:
                load_expert(e + NSLOT)

        AUC(0)
        A(act, csb[0][:, :], PS[4][:, 0:256], AF.Copy, [PSB[4]], [B_csb[0]])
        for i in range(len(items)):
            if i + 1 < len(items):
                AUC(i + 1)
            REST(i)
        dump("acc0", acc[:, 0, :], [B_acc[0]])
        x1t = [stg[i][:, 0:D] for i in range(2)]; B_x1t = B_stg
        st2 = sb("st2", [128, 12], stack=E); mv2 = sb("mv2", [128, 2], stack=E); sc2 = sb("sc2", [128, 8], stack=E); B_st2 = Buf("st2")
        B_out = Buf("out")
        for t in range(16):
            s = t % 2
            fw.dma(sp, x1t[s], x1_s[:, t * D:(t + 1) * D], reads=[B_x1s], writes=[B_x1t[s]], sem="stg%d" % s)
            TT(dve, acc[:, t, :], acc[:, t, :], grow[:, 3, :], ALU.mult, [B_acc[t], B_grow], [B_acc[t]])
            STT(acc[:, t, :], x1t[s], ALPHA, acc[:, t, :], ALU.mult, ALU.add, [B_x1t[s], B_acc[t]], [B_acc[t]])
            layer_norm(acc[:, t, :], B_acc[t], 2, acc[:, t, :], B_acc[t], st2, mv2, sc2, B_st2)
            fw.dma(sp, out_d[t * 128:(t + 1) * 128, :], acc[:, t, :], reads=[B_acc[t]], writes=[B_out], sem="out")
    return finish()


def _rope_tables(frame_tok):
    t = np.asarray(frame_tok)
    row = (t // 64).astype(np.float32)
    col = (t % 64).astype(np.float32)
    inv = (10000.0 ** (-np.arange(0, 16, 2, dtype=np.float32) / np.float32(16))).astype(np.float32)
    ang = np.concatenate([row[:, None] * inv, col[:, None] * inv], -1)
    ang = np.concatenate([ang, ang], -1).astype(np.float32)
    return np.cos(ang).astype(np.float32), np.sin(ang).astype(np.float32)


def prep_core(inp, b, hh):
    f32 = np.float32
    m = {}
    x = inp["x"][b]
    ctx = inp["ctx"][b]
    if hh == 1:
        x = x[::-1]
        ctx = ctx[::-1]
    m["xf"] = np.ascontiguousarray(x, f32)
    m["ctxf"] = np.ascontiguousarray(ctx, f32)
    cT = np.stack([inp["c"][b].reshape(8, 128).T, inp["c_ctx"].reshape(8, 128).T], -1)
    m["cT"] = np.ascontiguousarray(cT, f32)
    m["w_ada"] = inp["w_ada"][0]
    ba = inp["b_ada"][0]
    m["b_ada_2rows"] = np.ascontiguousarray(np.stack([ba, ba], 0), f32)
    m["w_in"] = inp["w_in"][0]
    m["qg_cols"] = np.ascontiguousarray(inp["q_norm_g"][0].reshape(2, 128).T, f32)
    m["kvg_col"] = np.ascontiguousarray(inp["kv_norm_g"][0].reshape(1, 128).T, f32)
    m["w_uq"] = inp["w_uq"][0]
    m["w_ukv"] = inp["w_ukv"][0]
    mt, j, i = np.meshgrid(np.arange(2), np.arange(16), np.arange(128), indexing="ij")
    tau = (16 * (128 * mt + i) + j).reshape(-1)
    tok = tau if hh == 0 else (NTOK - 1 - tau)
    cos, sin = _rope_tables(tok)
    rope = np.zeros((128, 2, NKEY), f32)
    rope[64:96, 0, :NTOK] = cos.T
    rope[64:96, 1, :NTOK] = sin.T
    rope[64:96, 0, NTOK:] = 1.0
    m["rope"] = rope
    dirs = [0, 1] if hh == 0 else [1, 0]

    def lay(a):
        a = a[dirs]
        sh = a.shape[3:]
        a = a.reshape(2, 16, 2, 64, *sh)
        a = np.moveaxis(a, (2, 3), (0, 1))
        return a.reshape(128, 32, *sh)

    m["ssm_a"] = np.ascontiguousarray(np.stack([lay(inp["ssm_a_re"][0]), lay(inp["ssm_a_im"][0])], 1), f32)
    ldt = np.broadcast_to(inp["ssm_log_dt"][0][:, :, None], (2, 32, 64))
    m["ssm_ldt"] = np.ascontiguousarray(lay(ldt), f32)
    m["ssm_b"] = np.ascontiguousarray(np.stack([lay(inp["ssm_b_re"][0]), lay(inp["ssm_b_im"][0])], 1), f32)
    cre = np.swapaxes(inp["ssm_c_re"][0], 2, 3)
    cim = np.swapaxes(inp["ssm_c_im"][0], 2, 3)
    m["ssm_c"] = np.ascontiguousarray(np.stack([lay(cre), lay(cim)], 1), f32)
    dd = inp["ssm_d"][0].reshape(32, 16)
    m["ssm_dcols"] = np.ascontiguousarray(np.broadcast_to(dd.T[None], (8, 16, 32)).reshape(128, 32), f32)
    jl = np.arange(128) // 16
    m0 = (jl[None, :] >= jl[:, None]).astype(f32)
    m1 = (jl[:, None] >= jl[None, :]).astype(f32)
    m["masks"] = np.ascontiguousarray(np.stack([m0, m1], 1), f32)
    m["ident"] = np.eye(128, dtype=f32)
    m["w_glu"] = inp["w_glu"][0]
    m["b_glu_row"] = np.ascontiguousarray(np.broadcast_to(inp["b_glu"][0][None], (128, 512)), f32)
    gn = np.concatenate([inp["gn_attn_g"][0], inp["gn_ssm_g"][0]])
    m["gn_cols"] = np.ascontiguousarray(gn.reshape(8, 128).T, f32)
    m["w_o"] = inp["w_o"][0]
    ln = np.stack([inp["ln1_g"][0], inp["ln1_b"][0], inp["ln2_g"][0], inp["ln2_b"][0]], 0)
    m["ln_rows"] = np.ascontiguousarray(np.broadcast_to(ln[None], (128, 4, D)), f32)
    m["w_r"] = np.ascontiguousarray(np.concatenate([inp["w_router_group"][0], inp["w_router_expert"][0]], 1), f32)
    br_ = np.concatenate([inp["b_router_group"][0], inp["b_router_expert"][0]])
    m["b_r_row"] = np.ascontiguousarray(np.broadcast_to(br_[None], (128, 36)), f32)
    m["w_exp_gate"] = inp["w_exp_gate"][0]
    m["w_exp_up"] = inp["w_exp_up"][0]
    m["w_exp_down"] = inp["w_exp_down"][0]
    sel = np.zeros((32, NE, 128), f32)
    sel[np.arange(32), np.arange(32), :] = 1.0
    m["sel"] = sel.reshape(32, NE * 128)
    return m


def run(inputs, stop_after=None, dbg_names=(), cores=8):
    inp = {k: np.asarray(v) for k, v in inputs.items()}
    nc = build(stop_after=stop_after, dbg_names=dbg_names)
    in_maps = [prep_core(inp, c // 2, c % 2) for c in range(cores)]
    res = run_bass_kernel_spmd(nc, in_maps, core_ids=list(range(cores)))
    return res.results


def kernel(**inputs):
    res = run(inputs)
    out = np.zeros((4, NTOK, D), np.float32)
    for c in range(8):
        b, hh = c // 2, c % 2
        o = res[c]["out"]
        o = o.reshape(16, 128, D).transpose(1, 0, 2).reshape(NOWN, D)
        if hh == 0:
            out[b, :NOWN] = o
        else:
            out[b, NOWN:] = o[::-1]
    return out
```

```python
import math
import os
from contextlib import ExitStack
import numpy as np
import concourse.bass as bass
import concourse.mybir as mybir
from concourse.ap import AP
from concourse.bass_utils import run_bass_kernel_spmd

F32 = mybir.dt.float32
BF16 = mybir.dt.bfloat16
AF = mybir.ActivationFunctionType
ALU = mybir.AluOpType
AX = mybir.AxisListType

D = 1024
NTOK = 4096
NOWN = 2048
NCTX = 256
NKEY = NTOK + NCTX
NH = 8
SCALE = 96 ** -0.5
ALPHA = 2.0 ** 0.25
EPS = 1e-6
NS = 134
WINDOW = False
NE = 32


class Buf:
    __slots__ = ("name", "w", "r", "excl")

    def __init__(self, name, excl=False):
        self.name = name
        self.w = None
        self.r = []
        self.excl = excl


def bufs(name, n):
    return [Buf("%s%d" % (name, i)) for i in range(n)]


class Q:
    def __init__(self, fw, name, eng, same=False):
        self.name = name
        self.eng = eng
        self.sem = fw.es.enter_context(fw.nc.semaphore("s_" + name))
        self.cnt = 0
        self.seen = {}
        self.same = same
        self.window = False


class DmaTok:
    def __init__(self, fw, name):
        self.sem = fw.es.enter_context(fw.nc.semaphore("d_" + str(name)))
        self.cnt = 0
        self.name = name
        self.same = True


class FW:
    def __init__(self, nc, es):
        self.nc = nc
        self.es = es
        self.pe = Q(self, "pe", nc.tensor)
        self.dve = Q(self, "dve", nc.vector, same=True)
        self.act = Q(self, "act", nc.scalar, same=True)
        self.dve.window = WINDOW
        self.pool = Q(self, "pool", nc.gpsimd, same=True)
        self.sp = Q(self, "sp", nc.sync)
        self.dmasems = {}

    def _waits(self, q, reads, writes, sreads=()):
        need = {}

        def add(dep, force=False):
            if dep is None:
                return
            dq, n = dep
            if dq is q and not force:
                if not q.same:
                    return
                if q.window and n < q.cnt:
                    return
            if need.get(dq, 0) < n:
                need[dq] = n

        for b in sreads:
            add(b.w, True)
        for b in reads:
            add(b.w)
            if b.excl:
                for d in b.r:
                    add(d)
        for b in writes:
            add(b.w)
            for d in b.r:
                add(d)
        for dq, n in need.items():
            if q.seen.get(id(dq), 0) >= n:
                continue
            q.eng.wait_ge(dq.sem, n)
            q.seen[id(dq)] = n

    def op(self, q, fn, reads=(), writes=(), sreads=()):
        self._waits(q, reads, writes, sreads)
        reads = list(reads) + list(sreads)
        ins = fn(q.eng)
        ins.then_inc(q.sem, 1)
        q.cnt += 1
        tok = (q, q.cnt)
        for b in reads:
            b.r.append(tok)
        for b in writes:
            b.w = tok
            b.r = []
        return ins

    def dma(self, q, out, in_, reads=(), writes=(), sem=None, **kw):
        self._waits(q, reads, writes)
        ds = self.dmasems.get(sem)
        if ds is None:
            ds = DmaTok(self, sem)
            self.dmasems[sem] = ds
        if out.dtype != in_.dtype and in_.ap[-1][1] > 2048:
            kw.setdefault("max_dma_last_dim", 4096)
        ins = q.eng.dma_start(out=out, in_=in_, **kw)
        ds.cnt += 16
        ins.then_inc(ds.sem, 16)
        tok = (ds, ds.cnt)
        for b in reads:
            b.r.append(tok)
        for b in writes:
            b.w = tok
            b.r = []
        return tok

    def barrier(self):
        qs = [self.pe, self.dve, self.act, self.pool, self.sp]
        for q in qs:
            for dq in qs + list(self.dmasems.values()):
                if dq is q or dq.cnt == 0:
                    continue
                if q.seen.get(id(dq), 0) >= dq.cnt:
                    continue
                q.eng.wait_ge(dq.sem, dq.cnt)
                q.seen[id(dq)] = dq.cnt

    def group_done(self, sem, blist):
        ds = self.dmasems[sem]
        for b in blist:
            if b.w is not None and b.w[0] is ds:
                b.w = (ds, ds.cnt)


def sbap(t, off, dims):
    full = t[:] if not isinstance(t, AP) else t
    return AP(full.tensor, full.offset + off, [list(full.ap[0])] + [list(d) for d in dims])


def part_ap(t, p0, npart, off, dims):
    full = t[:]
    pstep = full.ap[0][0]
    sub = t[p0:p0 + npart]
    return AP(sub.tensor, sub.offset + off, [[pstep, npart]] + [list(d) for d in dims])


def build(stop_after=None, dbg_names=()):
    nc = bass.Bass("TRN2", target_bir_lowering=False)
    es = ExitStack()
    fw = FW(nc, es)
    pe, dve, act, pool, sp = fw.pe, fw.dve, fw.act, fw.pool, fw.sp
    dbg = {}

    def din(name, shape, dt=F32):
        return nc.dram_tensor(name, list(shape), dt, kind="ExternalInput").ap()

    xf = din("xf", [NTOK, D])
    ctxf = din("ctxf", [NCTX, D])
    cT_d = din("cT", [128, 8, 2])
    w_ada_d = din("w_ada", [D, 6 * D])
    b_ada_2rows_d = din("b_ada_2rows", [2, 6 * D])
    w_in_d = din("w_in", [D, 928])
    qg_d = din("qg_cols", [128, 2])
    kvg_d = din("kvg_col", [128, 1])
    w_uq_d = din("w_uq", [256, 768])
    w_ukv_d = din("w_ukv", [128, 1024])
    rope_d = din("rope", [128, 2, NKEY])
    ssm_a_d = din("ssm_a", [128, 2, 32])
    ssm_ldt_d = din("ssm_ldt", [128, 32])
    ssm_b_d = din("ssm_b", [128, 2, 32, 16])
    ssm_c_d = din("ssm_c", [128, 2, 32, 16])
    ssm_dcols_d = din("ssm_dcols", [128, 32])
    masks_d = din("masks", [128, 2, 128])
    ident_d = din("ident", [128, 128])
    w_glu_d = din("w_glu", [512, 512])
    b_glu_d = din("b_glu_row", [128, 512])
    gn_d = din("gn_cols", [128, 8])
    w_o_d = din("w_o", [D, D])
    ln_d = din("ln_rows", [128, 4, D])
    w_r_d = din("w_r", [D, 36])
    b_r_d = din("b_r_row", [128, 36])
    wg_d = din("w_exp_gate", [NE, D, 256])
    wu_d = din("w_exp_up", [NE, D, 256])
    wd_d = din("w_exp_down", [NE, 256, D])
    sel_d = din("sel", [32, NE * 128])
    out_d = nc.dram_tensor("out", [NOWN, D], F32, kind="ExternalOutput").ap()
    ug_s = nc.dram_tensor("ug_s", [128, 32 * 2 * 272], BF16, kind="Internal").ap()
    attn_s = nc.dram_tensor("attn_s", [128, 16 * 512], F32, kind="Internal").ap()
    x1_s = nc.dram_tensor("x1_s", [128, 16 * D], F32, kind="Internal").ap()
    g_s = nc.dram_tensor("g_s", [128, 16 * 512], F32, kind="Internal").ap()
    dbg_out = {}
    for ent in dbg_names:
        nm, shp = ent[0], ent[1]
        dbg_out[nm] = nc.dram_tensor("dbg_" + nm, list(shp), BF16 if (len(ent) > 2 and ent[2] == "bf16") else F32, kind="ExternalOutput").ap()

    def sb(name, shape, dt=F32, stack=None):
        return (stack or es).enter_context(nc.sbuf_tensor("sb_" + name, list(shape), dt))

    PS = [es.enter_context(nc.psum_tensor("ps%d" % i, [128, 512], F32)) for i in range(8)]
    PSB = [Buf("ps%d" % i, excl=True) for i in range(8)]
    ps_rr = [0]

    def ps_next(k=None):
        i = ps_rr[0] % 8
        ps_rr[0] += 1
        return PS[i], PSB[i]

    ident = sb("ident", [128, 128]); B_ident = Buf("ident")
    ones_bf = sb("ones_bf", [128, 128], BF16); B_ones = Buf("ones")
    modc = sb("modc", [128, 96]); B_modc = Buf("modc")
    grow = sb("grow", [128, 4, D]); B_grow = Buf("grow")
    fw.dma(sp, ident[:], ident_d[:, :], writes=[B_ident], sem="c0")
    fw.op(dve, lambda e: e.memset(ones_bf[:], 1.0), writes=[B_ones])

    def dump(name, ap, reads):
        if name in dbg_out:
            fw.dma(sp, dbg_out[name], ap, reads=reads, writes=[], sem="dbg")

    def finish():
        for ds in fw.dmasems.values():
            sp.eng.wait_ge(ds.sem, ds.cnt)
        es.close()
        return nc

    with ExitStack() as ph:
        cT = sb("cT", [128, 8, 2], stack=ph); B_cT = Buf("cT")
        sil2 = sb("sil2", [128, 8, 2], stack=ph); B_sil = Buf("sil2")
        b2 = sb("b2rows", [2, 6 * D], stack=ph); B_b2 = Buf("b2")
        modrow = sb("modrow", [2, 6 * D], stack=ph); B_mr = Buf("modrow")
        ones_f = sb("ones_f", [1, 128], stack=ph); B_onesf = Buf("ones_f")
        wa = [sb("wa%d" % i, [128, 8, D], stack=ph) for i in range(2)]
        B_wa = bufs("wa", 2)
        fw.dma(sp, cT[:], cT_d[:, :, :], writes=[B_cT], sem="c1")
        fw.dma(sp, b2[:], b_ada_2rows_d[:, :], writes=[B_b2], sem="c2")
        fw.op(dve, lambda e: e.memset(ones_f[:], 1.0), writes=[B_onesf])
        fw.op(act, lambda e: e.activation(out=sil2[:], in_=cT[:], func=AF.Silu), reads=[B_cT], writes=[B_sil])
        w_ada_v = w_ada_d.rearrange("(k p) n -> p k n", p=128)
        for v in range(6):
            slot = v % 2
            fw.dma(sp if v % 2 == 0 else act, wa[slot][:], w_ada_v[:, :, v * D:(v + 1) * D], writes=[B_wa[slot]], sem="wa%d" % slot)
            for half in range(2):
                pr, Bpr = PS[half], PSB[half]
                for k in range(8):
                    fw.op(pe, lambda e: e.matmul(pr[0:2, :], lhsT=sil2[:, k, :], rhs=wa[slot][:, k, half * 512:(half + 1) * 512],
                                                 start=(k == 0), stop=(k == 7)), reads=[B_wa[slot], B_sil], writes=[Bpr])
                c0 = v * D + half * 512
                fw.op(dve, lambda e: e.tensor_tensor(out=modrow[0:2, c0:c0 + 512], in0=pr[0:2, :], in1=b2[0:2, c0:c0 + 512], op=ALU.add),
                      reads=[Bpr, B_b2], writes=[B_mr])
        for v in (1, 4):
            fw.op(dve, lambda e: e.tensor_scalar_add(out=modrow[0:2, v * D:(v + 1) * D], in0=modrow[0:2, v * D:(v + 1) * D], scalar1=1.0),
                  reads=[B_mr], writes=[B_mr])
        psc, Bpsc = PS[2], PSB[2]
        for vc in range(48):
            fw.op(pe, lambda e: e.matmul(psc[:, vc * 2:vc * 2 + 2], lhsT=modrow[0:2, vc * 128:(vc + 1) * 128], rhs=ident[0:2, 0:2],
                                         start=True, stop=True, is_transpose=True), reads=[B_mr, B_ident], writes=[Bpsc])
        fw.op(dve, lambda e: e.tensor_copy(out=modc[:], in_=psc[:, 0:96]), reads=[Bpsc], writes=[B_modc])
        for ri, v in enumerate((2, 3, 4, 5)):
            for half in range(2):
                pr, Bpr = PS[3 + (ri * 2 + half) % 2], PSB[3 + (ri * 2 + half) % 2]
                c0 = v * D + half * 512
                fw.op(pe, lambda e: e.matmul(pr[:, :], lhsT=ones_f[0:1, :], rhs=modrow[0:1, c0:c0 + 512], start=True, stop=True),
                      reads=[B_onesf, B_mr], writes=[Bpr])
                fw.op(act, lambda e: e.activation(out=grow[:, ri, half * 512:(half + 1) * 512], in_=pr[:, :], func=AF.Copy),
                      reads=[Bpr], writes=[B_grow])
    fw.barrier()
    dump("modc", modc[:], [B_modc])
    dump("grow", grow[:, 0:2].rearrange("p a b -> p (a b)"), [B_grow])
    if stop_after == "adaln":
        return finish()

    def mcol(v, ch, who):
        i = (v * 8 + ch) * 2 + who
        return modc[:, i:i + 1]

    def A(q, out, in_, func, reads, writes, **kw):
        return fw.op(q, lambda e: e.activation(out=out, in_=in_, func=func, **kw), reads=reads, writes=writes)

    def TT(q, out, in0, in1, op, reads, writes):
        return fw.op(q, lambda e: e.tensor_tensor(out=out, in0=in0, in1=in1, op=op), reads=reads, writes=writes)

    def TS(q, out, in0, s1, s2, op0, op1, reads, writes, sreads=()):
        return fw.op(q, lambda e: e.tensor_scalar(out=out, in0=in0, scalar1=s1, scalar2=s2, op0=op0, op1=op1), reads=reads, writes=writes, sreads=sreads)

    def STT(out, in0, scalar, in1, op0, op1, reads, writes):
        return fw.op(dve, lambda e: e.scalar_tensor_tensor(out=out, in0=in0, scalar=scalar, in1=in1, op0=op0, op1=op1), reads=reads, writes=writes)

    def CP(q, out, in_, reads, writes):
        return fw.op(q, lambda e: e.tensor_copy(out=out, in_=in_), reads=reads, writes=writes)

    def MM(out, lhsT, rhs, start, stop, reads, writes):
        return fw.op(pe, lambda e: e.matmul(out, lhsT=lhsT, rhs=rhs, start=start, stop=stop), reads=reads, writes=writes)

    def TR(out, in_, reads, writes, n=128):
        return fw.op(pe, lambda e: e.matmul(out, lhsT=in_, rhs=ident[0:n, 0:n], start=True, stop=True, is_transpose=True),
                     reads=list(reads) + [B_ident], writes=writes)

    def rstd_from_ss(out, ss_ps, B_ss, inv_n, tmp, B_tmp, B_out):
        A(act, tmp, ss_ps, AF.Ln, [B_ss, B_eps], [B_tmp], scale=inv_n, bias=epsc[:, 0:1])
        A(act, out, tmp, AF.Exp, [B_tmp], [B_out], scale=-0.5)

    epsc = sb("epsc", [128, 1]); B_eps = Buf("eps")
    fw.op(dve, lambda e: e.memset(epsc[:], EPS), writes=[B_eps])

    att = ExitStack()
    cqT = sb("cqT", [128, 2, NOWN], BF16, stack=att); B_cqT = Buf("cqT")
    rstdq = sb("rstdq", [128, NOWN], stack=att); B_rstdq = Buf("rstdq")
    ckvnT = sb("ckvnT", [128, NKEY], BF16, stack=att); B_ckvnT = Buf("ckvnT")
    KRT = sb("KRT", [128, NKEY], BF16, stack=att); B_KRT = Buf("KRT")

    with ExitStack() as ph:
        w_in = sb("w_in", [128, 8, 928], BF16, stack=ph); B_win = Buf("w_in")
        w_krot = sb("w_krot", [128, 8, 32], BF16, stack=ph); B_wkrot = Buf("w_krot")
        fw.dma(pool, w_in[:], w_in_d.rearrange("(k p) n -> p k n", p=128), writes=[B_win], sem="w_in")
        CP(dve, w_krot[:, :, 16:32], w_in[:, :, 384:400], [B_win], [B_wkrot])
        TS(dve, w_krot[:, :, 0:16], w_in[:, :, 400:416], -1.0, None, ALU.mult, ALU.bypass, [B_win], [B_wkrot])
        xin = [sb("xin%d" % i, [128, D], stack=ph) for i in range(4)]; B_xin = bufs("xin", 4)
        xmT = [sb("xmT%d" % i, [128, 8, 512], BF16, stack=ph) for i in range(2)]; B_xmT = bufs("xmT", 2)
        ropeg = [sb("ropeg%d" % i, [128, 2, 512], stack=ph) for i in range(2)]; B_ropeg = bufs("ropeg", 2)
        u_tm = sb("u_tm", [128, 32, 128], stack=ph); B_utm = Buf("u_tm")
        Ug = sb("Ug", [128, 32, 2, 272], BF16, stack=ph); B_Ug = Buf("Ug")
        sqb = sb("sqb", [128, 512], BF16, stack=ph); B_sqb = Buf("sqb")
        sqq = sb("sqq", [128, 2, 512], BF16, stack=ph); B_sqq = Buf("sqq")
        rawkv = sb("rawkv", [128, 512], stack=ph); B_rawkv = Buf("rawkv")
        lnt = sb("lnt", [128, 512], stack=ph); B_lnt = Buf("lnt")
        rstdkv = sb("rstdkv", [128, 512], stack=ph); B_rstdkv = Buf("rstdkv")
        rt1 = sb("rt1", [128, 512], stack=ph); B_rt1 = Buf("rt1")
        rt2 = sb("rt2", [128, 512], stack=ph); B_rt2 = Buf("rt2")
        xf_v = xf.rearrange("(m j) d -> j m d", j=16)
        ctx_v = ctxf.rearrange("(m j) d -> j m d", j=16)
        gcount = [0]

        def in_group(kind, mt, jg):
            gs = gcount[0] % 2
            gcount[0] += 1
            lat = kind == "lat"
            ncols = 512 if lat else 256
            col0 = (mt * 16 + jg * 4) * 128 if lat else NTOK
            own = lat and mt == 0
            np_ = 128 if lat else 16
            nt = 4 if lat else 16
            tw = 128 if lat else 16
            if lat:
                for t in range(4):
                    j = jg * 4 + t
                    fw.dma(sp, xin[t][:], xf_v[j, mt * 128:(mt + 1) * 128, :], writes=[B_xin[t]], sem="xin%d" % t)
            fw.dma(sp, ropeg[gs][64:96, :, 0:ncols], rope_d[64:96, :, col0:col0 + ncols], writes=[B_ropeg[gs]], sem="ropeg%d" % gs)
            who = 0 if lat else 1
            if lat:
                for ch in range(8):
                    ps, Bp = ps_next()
                    for t in range(4):
                        TR(ps[:, t * 128:(t + 1) * 128], xin[t][:, ch * 128:(ch + 1) * 128], [B_xin[t]], [Bp])
                    A(act, xmT[gs][:, ch, 0:ncols], ps[:, 0:ncols], AF.Identity, [Bp, B_modc], [B_xmT[gs]],
                      scale=mcol(1, ch, who), bias=mcol(0, ch, who))
            else:
                banks = [ps_next() for _ in range(8)]
                for t in range(16):
                    fw.dma(sp, xin[t % 4][0:16, :], ctx_v[t, 0:16, :], writes=[B_xin[t % 4]], sem="xin%d" % (t % 4))
                    for ch in range(8):
                        ps, Bp = banks[ch]
                        TR(ps[:, t * 16:(t + 1) * 16], xin[t % 4][0:16, ch * 128:(ch + 1) * 128], [B_xin[t % 4]], [Bp], n=16)
                for ch in range(8):
                    ps, Bp = banks[ch]
                    A(act, xmT[gs][:, ch, 0:ncols], ps[:, 0:ncols], AF.Identity, [Bp, B_modc], [B_xmT[gs]],
                      scale=mcol(1, ch, who), bias=mcol(0, ch, who))
            X = xmT[gs]; BX = B_xmT[gs]
            kstep = int(os.environ.get("K_STEP", "9"))
            if kstep < 2:
                return
            ps, Bp = ps_next()
            for ch in range(8):
                MM(ps[:, 0:ncols], w_in[:, ch, 256:384], X[:, ch, 0:ncols], ch == 0, ch == 7, [B_win, BX], [Bp])
            A(act, sqb[:, 0:ncols], ps[:, 0:ncols], AF.Square, [Bp], [B_sqb])
            A(act, rawkv[:, 0:ncols], ps[:, 0:ncols], AF.Copy, [Bp], [B_rawkv])
            ps2, Bp2 = ps_next()
            MM(ps2[:, 0:ncols], ones_bf[:, :], sqb[:, 0:ncols], True, True, [B_ones, B_sqb], [Bp2])
            rstd_from_ss(rstdkv[:, 0:ncols], ps2[:, 0:ncols], Bp2, 1.0 / 128, lnt[:, 0:ncols], B_lnt, B_rstdkv)
            TT(dve, ckvnT[:, col0:col0 + ncols], rawkv[:, 0:ncols], rstdkv[:, 0:ncols], ALU.mult, [B_rawkv, B_rstdkv], [B_ckvnT])
            if kstep < 3:
                return
            psa, Bpa = ps_next()
            psb, Bpb = ps_next()
            for ch in range(8):
                MM(psa[64:96, 0:ncols], w_in[:, ch, 384:416], X[:, ch, 0:ncols], ch == 0, ch == 7, [B_win, BX], [Bpa])
            for ch in range(8):
                MM(psb[64:96, 0:ncols], w_krot[:, ch, :], X[:, ch, 0:ncols], ch == 0, ch == 7, [B_wkrot, BX], [Bpb])
            TT(dve, rt1[64:96, 0:ncols], psa[64:96, 0:ncols], ropeg[gs][64:96, 0, 0:ncols], ALU.mult, [Bpa, B_ropeg[gs]], [B_rt1])
            TT(dve, rt2[64:96, 0:ncols], psb[64:96, 0:ncols], ropeg[gs][64:96, 1, 0:ncols], ALU.mult, [Bpb, B_ropeg[gs]], [B_rt2])
            TT(dve, KRT[64:96, col0:col0 + ncols], rt1[64:96, 0:ncols], rt2[64:96, 0:ncols], ALU.add, [B_rt1, B_rt2], [B_KRT])
            if kstep < 4:
                return
            if own:
                pss = []
                for fc in range(2):
                    ps, Bp = ps_next()
                    for ch in range(8):
                        MM(ps[:, :], w_in[:, ch, fc * 128:(fc + 1) * 128], X[:, ch, :], ch == 0, ch == 7, [B_win, BX], [Bp])
                    A(act, cqT[:, fc, col0:col0 + 512], ps[:, :], AF.Copy, [Bp], [B_cqT])
                    pss.append((ps, Bp))
                ksub = int(os.environ.get("K_SUB", "9"))
                if ksub >= 1:
                    ps2, Bp2 = ps_next()
                    for fc in range(2):
                        ps, Bp = pss[fc]
                        A(act, sqq[:, fc, :], ps[:, :], AF.Square, [Bp], [B_sqq])
                    for fc in range(2):
                        MM(ps2[:, :], ones_bf[:, :], sqq[:, fc, :], fc == 0, fc == 1, [B_ones, B_sqq], [Bp2])
                if ksub >= 2:
                    rstd_from_ss(rstdq[:, col0:col0 + 512], ps2[:, :], Bp2, 1.0 / 256, lnt[:, :], B_lnt, B_rstdq)
            if kstep < 5:
                return
            for t in range(nt):
                ps, Bp = ps_next()
                for ch in range(8):
                    MM(ps[0:np_, :], X[:, ch, t * tw:(t + 1) * tw], w_in[:, ch, 416:928], ch == 0, ch == 7, [B_win, BX], [Bp])
                if lat:
                    jl = (jg * 4 + t) % 8
                    uo = sbap(u_tm, jl * 16, [[128, 32], [1, 16]])
                    ui = sbap(ps, 0, [[16, 32], [1, 16]])
                    if t % 2 == 0:
                        CP(dve, uo, ui, [Bp], [B_utm])
                    else:
                        A(act, uo, ui, AF.Copy, [Bp], [B_utm])
                else:
                    A(act, part_ap(u_tm, 0, 16, (t % 8) * 16, [[128, 32], [1, 16]]), part_ap(ps, 0, 16, 0, [[16, 32], [1, 16]]),
                      AF.Copy, [Bp], [B_utm])
                    if t % 8 == 7:
                        ug_transposes(u_tm, B_utm, 16, t // 8, 256, 0)

        def ug_transposes(src, Bsrc, np_, jt, ucol0, jbase):
            for g4 in range(8):
                ps, Bp = ps_next()
                for gg in range(4):
                    g = g4 * 4 + gg
                    inap = part_ap(src, 0, np_, g * 128, [[1, 128]])
                    TR(ps[:, gg * 128:gg * 128 + np_], inap, [Bsrc], [Bp], n=np_)
                outap = sbap(Ug, (g4 * 4) * 2 * 272 + jt * 272 + ucol0, [[2 * 272, 4], [1, np_]])
                inp_ = sbap(ps, 0, [[128, 4], [1, np_]])
                if g4 % 2 == 0:
                    CP(dve, outap, inp_, [Bp], [B_Ug])
                else:
                    A(act, outap, inp_, AF.Copy, [Bp], [B_Ug])

        bis = int(os.environ.get("K_BISECT", "99"))
        for mt in range(2):
            for jg in range(4):
                if mt * 4 + jg >= bis:
                    continue
                in_group("lat", mt, jg)
                if jg % 2 == 1 and bis != 2:
                    ug_transposes(u_tm, B_utm, 128, jg // 2, mt * 128, 0)
        if bis >= 99:
            in_group("ctx", 0, 0)
        dump("ckvnT", ckvnT[:], [B_ckvnT])
        dump("KRT", KRT[64:96, :], [B_KRT])
        dump("cqT", cqT[:].rearrange("p a b -> p (a b)"), [B_cqT])
        dump("rstdq", rstdq[:], [B_rstdq])
        dump("Ug", Ug[:].rearrange("p a b c -> p (a b c)"), [B_Ug])
        fw.dma(sp, ug_s[:, :], Ug[:].rearrange("p a b c -> p (a b c)"), reads=[B_Ug], writes=[], sem="ugs")
    fw.barrier()
    if stop_after == "inproj":
        return finish()

    attn_tm = sb("attn_tm", [128, 16, 512], stack=att); B_attn = Buf("attn_tm")
    B_attn_s = Buf("attn_s")
    with ExitStack() as ph:
        wq_f = sb("wq_f", [128, 2, 768], stack=ph); B_wqf = Buf("wq_f")
        wkv_f = sb("wkv_f", [128, 1024], stack=ph); B_wkvf = Buf("wkv_f")
        qg = sb("qg", [128, 2], stack=ph); kvg = sb("kvg", [128, 1], stack=ph); B_g = Buf("qkvg")
        w_uq_s = sb("w_uq_s", [128, 2, 768], BF16, stack=ph); B_wuq = Buf("w_uq_s")
        w_uq_rot = sb("w_uq_rot", [128, 2, 8, 32], BF16, stack=ph); B_wuqr = Buf("w_uq_rot")
        w_k = sb("w_k", [128, 1024], BF16, stack=ph); B_wk = Buf("w_k")
        w_v = sb("w_v", [128, 512], BF16, stack=ph); B_wv = Buf("w_v")
        V_all = sb("V_all", [128, 34, 8, 65], BF16, stack=ph); B_V = Buf("V_all")
        KT = [sb("KT%d" % i, [128, NKEY], BF16, stack=ph) for i in range(2)]; B_KT = bufs("KT", 2)
        QT = [sb("QT%d" % i, [128, NOWN], BF16, stack=ph) for i in range(2)]; B_QT = bufs("QT", 2)
        ropeq = sb("ropeq", [128, 2, NOWN], stack=ph); B_ropeq = Buf("ropeq")
        PT = [sb("PT%d" % i, [128, 512], BF16, stack=ph) for i in range(4)]; B_PT = bufs("PT", 4)
        ot = [sb("ot%d" % i, [128, 512], stack=ph) for i in range(2)]; B_ot = bufs("ot", 2)
        qt1 = sb("qt1", [128, 512], stack=ph); B_qt1 = Buf("qt1")
        qt2 = sb("qt2", [128, 512], stack=ph); B_qt2 = Buf("qt2")
        rd = sb("rd", [128, 4], stack=ph); B_rd = Buf("rd")
        fw.dma(sp, wq_f[:], w_uq_d.rearrange("(c p) n -> p c n", p=128), writes=[B_wqf], sem="a0")
        fw.dma(sp, wkv_f[:], w_ukv_d[:, :], writes=[B_wkvf], sem="a1")
        fw.dma(sp, qg[:], qg_d[:, :], writes=[B_g], sem="a2")
        fw.dma(sp, kvg[:], kvg_d[:, :], writes=[B_g], sem="a3")
        fw.dma(sp, ropeq[64:96, :, :], rope_d[64:96, :, 0:NOWN], writes=[B_ropeq], sem="a4")
        for c in range(2):
            TS(dve, w_uq_s[:, c, :], wq_f[:, c, :], qg[:, c:c + 1], None, ALU.mult, ALU.bypass, [B_wqf, B_g], [B_wuq])
            TS(dve, w_uq_rot[:, c, :, 0:16], sbap(w_uq_s, c * 768 + 80, [[96, 8], [1, 16]]), -1.0, None, ALU.mult, ALU.bypass, [B_wuq], [B_wuqr])
            CP(dve, w_uq_rot[:, c, :, 16:32], sbap(w_uq_s, c * 768 + 64, [[96, 8], [1, 16]]), [B_wuq], [B_wuqr])
        TS(dve, w_k[:, :], wkv_f[:, :], kvg[:, 0:1], None, ALU.mult, ALU.bypass, [B_wkvf, B_g], [B_wk])
        CP(dve, w_v[:].rearrange("p (h d) -> p h d", h=8), sbap(w_k, 64, [[128, 8], [1, 64]]), [B_wk], [B_wv])
        fw.op(dve, lambda e: e.memset(V_all[:, :, :, 64:65], 1.0), writes=[B_V])
        for i in range(2):
            fw.op(dve, lambda e: e.memset(KT[i][96:128, :], 0.0), writes=[B_KT[i]])
            fw.op(dve, lambda e: e.memset(QT[i][96:128, :], 0.0), writes=[B_QT[i]])
        for kt in range(34):
            ps, Bp = PS[6 + kt % 2], PSB[6 + kt % 2]
            MM(ps[:, :], ckvnT[:, kt * 128:(kt + 1) * 128], w_v[:, :], True, True, [B_ckvnT, B_wv], [Bp])
            vo = V_all[:, kt, :, 0:64]
            vi = ps[:, :].rearrange("p (h d) -> p h d", h=8)
            if kt % 2 == 0:
                CP(dve, vo, vi, [Bp], [B_V])
            else:
                A(act, vo, vi, AF.Copy, [Bp], [B_V])

        def gen(h):
            s = h % 2
            for ct in range(9):
                n = 512 if ct < 8 else 256
                c0 = ct * 512
                ps, Bp = PS[6], PSB[6]
                MM(ps[0:64, 0:n], w_k[:, h * 128:h * 128 + 64], ckvnT[:, c0:c0 + n], True, True, [B_wk, B_ckvnT], [Bp])
                CP(dve, KT[s][0:64, c0:c0 + n], ps[0:64, 0:n], [Bp], [B_KT[s]])
                yield
            CP(pool, KT[s][64:96, :], KRT[64:96, :], [B_KRT], [B_KT[s]])
            for ct in range(4):
                c0 = ct * 512
                psq, Bq = PS[6], PSB[6]
                psr, Br = PS[7], PSB[7]
                for c in range(2):
                    MM(psq[0:96, :], w_uq_s[:, c, h * 96:(h + 1) * 96], cqT[:, c, c0:c0 + 512], c == 0, c == 1, [B_wuq, B_cqT], [Bq])
                for c in range(2):
                    MM(psr[64:96, :], w_uq_rot[:, c, h, :], cqT[:, c, c0:c0 + 512], c == 0, c == 1, [B_wuqr, B_cqT], [Br])
                TT(dve, QT[s][0:64, c0:c0 + 512], psq[0:64, :], rstdq[0:64, c0:c0 + 512], ALU.mult, [Bq, B_rstdq], [B_QT[s]])
                TT(dve, qt1[64:96, :], psq[64:96, :], ropeq[64:96, 0, c0:c0 + 512], ALU.mult, [Bq, B_ropeq], [B_qt1])
                TT(dve, qt2[64:96, :], psr[64:96, :], ropeq[64:96, 1, c0:c0 + 512], ALU.mult, [Br, B_ropeq], [B_qt2])
                TT(dve, qt1[64:96, :], qt1[64:96, :], qt2[64:96, :], ALU.add, [B_qt1, B_qt2], [B_qt1])
                TT(dve, QT[s][64:96, c0:c0 + 512], qt1[64:96, :], rstdq[64:96, c0:c0 + 512], ALU.mult, [B_qt1, B_rstdq], [B_QT[s]])
                yield

        scount = [0]
        ocount = [0]

        pend = [None]

        def epilogue(h, qt, ob, osl):
            CP(dve, ot[osl][0:65, :], PS[ob][0:65, :], [PSB[ob]], [B_ot[osl]])
            pso, Bo = PS[7], PSB[7]
            for t in range(4):
                TR(pso[:, t * 128:t * 128 + 65], ot[osl][0:65, t * 128:(t + 1) * 128], [B_ot[osl]], [Bo], n=65)
            fw.op(dve, lambda e: e.reciprocal(out=rd[:, :], in_=sbap(pso, 64, [[128, 4]])), reads=[Bo], writes=[B_rd])
            for t in range(4):
                TS(dve, attn_tm[:, qt * 4 + t, h * 64:(h + 1) * 64], pso[:, t * 128:t * 128 + 64], rd[:, t:t + 1], None, ALU.mult, ALU.bypass,
                   [Bo], [B_attn], sreads=[B_rd])

        def attend(h, nxt):
            s = h % 2
            itc = [0]
            for qt in range(4):
                ob = 4 + ocount[0] % 2
                osl = ocount[0] % 2
                ocount[0] += 1
                base = scount[0]
                scount[0] += 34

                def S(kt):
                    bi = (base + kt) % 4
                    MM(PS[bi][:, :], KT[s][:, kt * 128:(kt + 1) * 128], QT[s][:, qt * 512:(qt + 1) * 512], True, True,
                       [B_KT[s], B_QT[s]], [PSB[bi]])
                S(0)
                S(1)
                if pend[0] is not None:
                    epilogue(*pend[0])
                    pend[0] = None
                for kt in range(34):
                    if kt + 2 < 34:
                        S(kt + 2)
                    bi = (base + kt) % 4
                    A(act, PT[bi][:, :], PS[bi][:, :], AF.Exp, [PSB[bi]], [B_PT[bi]], scale=SCALE)
                    MM(PS[ob][0:65, :], V_all[:, kt, h, 0:65], PT[bi][:, :], kt == 0, kt == 33, [B_V, B_PT[bi]], [PSB[ob]])
                    itc[0] += 1
                    if nxt is not None and itc[0] % 9 == 0:
                        next(nxt, None)
                pend[0] = (h, qt, ob, osl)

        nheads = int(os.environ.get("K_HEADS", "8"))
        for _ in gen(0):
            pass
        for h in range(nheads):
            nxt = gen(h + 1) if h + 1 < nheads else None
            attend(h, nxt)
            if nxt is not None:
                for _ in nxt:
                    pass
        epilogue(*pend[0])
        dump("attn", attn_tm[:].rearrange("p a b -> p (a b)"), [B_attn])
        fw.dma(sp, attn_s[:, :], attn_tm[:].rearrange("p a b -> p (a b)"), reads=[B_attn], writes=[B_attn_s], sem="attns")
        dump("KT0", KT[0][0:96, :], [B_KT[0]])
        dump("QT0", QT[0][0:96, :], [B_QT[0]])
        dump("V0", V_all[:].rearrange("p a b c -> p (a b c)"), [B_V])
    fw.barrier()
    att.close()
    if stop_after == "attn":
        return finish()

    PI = math.pi
    B_gs = Buf("g_s")
    with ExitStack() as S:
        sa = sb("ssm_a", [128, 2, 32], stack=S); sl = sb("ssm_l", [128, 32], stack=S)
        sbb = sb("ssm_b", [128, 2, 32, 16], stack=S); scc = sb("ssm_c", [128, 2, 32, 16], stack=S)
        dcols = sb("dcols", [128, 32], stack=S); masks = sb("masks4", [128, 4, 128], stack=S)
        B_par = Buf("ssm_par")
        fw.dma(sp, sa[:], ssm_a_d[:, :, :], writes=[B_par], sem="s0")
        fw.dma(sp, sl[:], ssm_ldt_d[:, :], writes=[B_par], sem="s1")
        fw.dma(sp, sbb[:], ssm_b_d[:, :, :, :], writes=[B_par], sem="s2")
        fw.dma(sp, scc[:], ssm_c_d[:, :, :, :], writes=[B_par], sem="s3")
        fw.dma(sp, dcols[:], ssm_dcols_d[:, :], writes=[B_par], sem="s4")
        fw.dma(sp, masks[:, 0:2, :], masks_d[:, :, :], writes=[B_par], sem="s5")
        fw.dma(sp, masks[:, 2:4, :], masks_d[:, :, :], writes=[B_par], sem="s6")
        w_glu = sb("w_glu", [128, 4, 512], BF16, stack=S); B_wglu = Buf("w_glu")
        fw.dma(pool, w_glu[:], w_glu_d.rearrange("(k p) n -> p k n", p=128), writes=[B_wglu], sem="s7")
        bglu = sb("bglu", [128, 512], stack=S); B_bglu = Buf("bglu")
        fw.dma(sp, bglu[:], b_glu_d[:, :], writes=[B_bglu], sem="s8")
        sm = sb("ssm_sm", [128, 32, 32], stack=S); B_sm = Buf("ssm_sm")
        _smi = [0]

        def smt():
            i = _smi[0]; _smi[0] += 1
            return sm[:, i, :]
        Bbar = sb("Bbar", [128, 2, 32, 16], stack=S)
        PWr = sb("PWr", [128, 17, 32], stack=S); PWi = sb("PWi", [128, 17, 32], stack=S)
        IPr = sb("IPr", [128, 17, 32], stack=S); IPi = sb("IPi", [128, 17, 32], stack=S)
        AA = sb("AA", [128, 2, 2, 16], stack=S); AB = sb("AB", [128, 2, 2, 16], stack=S)
        BS = [B_par, B_sm]

        def tt(out, a, b, op):
            TT(dve, out, a, b, op, BS, [B_sm])

        a_re, a_im = sa[:, 0, :], sa[:, 1, :]
        dt_ = smt(); lre = smt(); ang = smt(); mag = smt()
        A(act, dt_, sl[:, :], AF.Exp, [B_par], [B_sm])
        tt(lre, a_re, dt_, ALU.mult)
        tt(ang, a_im, dt_, ALU.mult)
        A(act, mag, lre, AF.Exp, [B_sm], [B_sm])

        def sin_of(src_ang, shift):
            a2 = smt(); k = smt(); r = smt(); o = smt()
            TS(dve, a2, src_ang, shift, None, ALU.add, ALU.bypass, BS, [B_sm])
            TS(dve, k, a2, PI, None, ALU.is_ge, ALU.bypass, BS, [B_sm])
            for i in range(2, 9):
                STT(k, a2, (2 * i - 1) * PI, k, ALU.is_ge, ALU.add, BS, [B_sm])
            STT(r, k, -2.0 * PI, a2, ALU.mult, ALU.add, BS, [B_sm])
            A(act, o, r, AF.Sin, [B_sm], [B_sm])
            return o
        sn = sin_of(ang, 0.0)
        cs = sin_of(ang, PI / 2)
        abre = smt(); abim = smt()
        tt(abre, mag, cs, ALU.mult)
        tt(abim, mag, sn, ALU.mult)
        t1 = smt(); t2 = smt(); den = smt(); rden = smt(); nre = smt(); cre = smt(); cim = smt()
        tt(t1, a_re, a_re, ALU.mult); tt(t2, a_im, a_im, ALU.mult); tt(den, t1, t2, ALU.add)
        fw.op(dve, lambda e: e.reciprocal(out=rden, in_=den), reads=BS, writes=[B_sm])
        TS(dve, nre, abre, -1.0, None, ALU.add, ALU.bypass, BS, [B_sm])
        tt(t1, nre, a_re, ALU.mult); tt(t2, abim, a_im, ALU.mult); tt(t1, t1, t2, ALU.add); tt(cre, t1, rden, ALU.mult)
        tt(t1, abim, a_re, ALU.mult); tt(t2, nre, a_im, ALU.mult); tt(t1, t1, t2, ALU.subtract); tt(cim, t1, rden, ALU.mult)
        tb = sb("tb", [128, 32, 16], stack=S)

        def bc16(v):
            return AP(v.tensor, v.offset, [list(v.ap[0]), [1, 32], [0, 16]])
        tt(Bbar[:, 0], sbb[:, 0], bc16(cre), ALU.mult); tt(tb[:], sbb[:, 1], bc16(cim), ALU.mult); tt(Bbar[:, 0], Bbar[:, 0], tb[:], ALU.subtract)
        tt(Bbar[:, 1], sbb[:, 1], bc16(cre), ALU.mult); tt(tb[:], sbb[:, 0], bc16(cim), ALU.mult); tt(Bbar[:, 1], Bbar[:, 1], tb[:], ALU.add)
        m2 = smt(); rm2 = smt(); iabre = smt(); iabim = smt()
        tt(t1, abre, abre, ALU.mult); tt(t2, abim, abim, ALU.mult); tt(m2, t1, t2, ALU.add)
        fw.op(dve, lambda e: e.reciprocal(out=rm2, in_=m2), reads=BS, writes=[B_sm])
        tt(iabre, abre, rm2, ALU.mult)
        STT(iabim, abim, -1.0, rm2, ALU.mult, ALU.mult, BS, [B_sm])
        for (Pr, Pi, br, bi) in ((PWr, PWi, abre, abim), (IPr, IPi, iabre, iabim)):
            fw.op(dve, lambda e: e.memset(Pr[:, 0, :], 1.0), writes=[B_sm])
            fw.op(dve, lambda e: e.memset(Pi[:, 0, :], 0.0), writes=[B_sm])
            CP(dve, Pr[:, 1, :], br, BS, [B_sm])
            CP(dve, Pi[:, 1, :], bi, BS, [B_sm])
            for k in range(2, 17):
                tt(t1, Pr[:, k - 1, :], br, ALU.mult); tt(t2, Pi[:, k - 1, :], bi, ALU.mult); tt(Pr[:, k, :], t1, t2, ALU.subtract)
                tt(t1, Pr[:, k - 1, :], bi, ALU.mult); tt(t2, Pi[:, k - 1, :], br, ALU.mult); tt(Pi[:, k, :], t1, t2, ALU.add)
        for d in range(2):
            for c in range(2):
                CP(dve, AA[:, d, c, :], PWr[:, 16, d * 16:(d + 1) * 16], BS, [B_sm])
            TS(dve, AB[:, d, 0, :], PWi[:, 16, d * 16:(d + 1) * 16], -1.0, None, ALU.mult, ALU.bypass, BS, [B_sm])
            CP(dve, AB[:, d, 1, :], PWi[:, 16, d * 16:(d + 1) * 16], BS, [B_sm])
        dump("sm", sm[:].rearrange("p a b -> p (a b)"), BS)
        dump("abar", sbap(PWr, 32, [[1, 32]]), BS)
        dump("abari", sbap(PWi, 32, [[1, 32]]), BS)
        dump("Bbar", Bbar[:].rearrange("p a b c -> p (a b c)"), BS)

        ctmp = [sb("ctmp%d" % i, [128, 4, 256], stack=S) for i in range(2)]; B_ct = Buf("ctmp")

        def cplx_build(q, out_t, P_r, P_i, k0, kstep, Vt, d, neg_im):
            for half in range(4):
                p0 = half * 4

                def Pv(Pt):
                    return sbap(Pt, k0 * 32 + d * 16 + p0, [[1, 4], [kstep * 32, 16], [0, 16]])

                def Vv(c):
                    return sbap(Vt, c * 512 + (d * 16 + p0) * 16, [[16, 4], [0, 16], [1, 16]])

                def Ov(c):
                    return sbap(out_t, p0 * 512 + c * 256, [[512, 4], [16, 16], [1, 16]])
                ta = ctmp[0][:].rearrange("p a (k h) -> p a k h", h=16)
                tb_ = ctmp[1][:].rearrange("p a (k h) -> p a k h", h=16)
                R = BS + [B_ct]
                TT(q, ta, Pv(P_r), Vv(0), ALU.mult, R, [B_ct])
                TT(q, tb_, Pv(P_i), Vv(1), ALU.mult, R, [B_ct])
                TT(q, Ov(0), ta, tb_, ALU.subtract, R, [B_sm])
                TT(q, ta, Pv(P_r), Vv(1), ALU.mult, R, [B_ct])
                TT(q, tb_, Pv(P_i), Vv(0), ALU.mult, R, [B_ct])
                if neg_im:
                    TT(q, ta, ta, tb_, ALU.add, R, [B_ct])
                    TS(q, Ov(1), ta, -1.0, None, ALU.mult, ALU.bypass, R, [B_sm])
                else:
                    TT(q, Ov(1), ta, tb_, ALU.add, R, [B_sm])

        Sbf = sb("Sbf", [128, 2, 2, 16, 128], BF16, stack=S); B_Sbf = Buf("Sbf")
        with ExitStack() as S2:
            Ug = sb("Ug2", [128, 32, 2, 272], BF16, stack=S2); B_Ug2 = Buf("Ug2")
            fw.dma(sp, Ug[:].rearrange("p a b c -> p (a b c)"), ug_s[:, :], writes=[B_Ug2], sem="s9")
            L = sb("L", [128, 2, 2, 16, 272], BF16, stack=S2); B_L = Buf("L")
            with ExitStack() as S2a:
                Wsrc = sb("Wsrc", [128, 16, 2, 256], stack=S2a)
                Wb = sb("Wb", [128, 16, 4, 128], BF16, stack=S2a); B_Wb = Buf("Wb")
                for d in range(2):
                    if d == 0:
                        cplx_build(dve, Wsrc, PWr, PWi, 15, -1, Bbar, 0, False)
                    else:
                        cplx_build(dve, Wsrc, PWr, PWi, 0, 1, Bbar, 1, False)
                    for pt in range(16):
                        ps, Bp = ps_next()
                        for blk in range(4):
                            TR(ps[:, blk * 128:(blk + 1) * 128], sbap(Wsrc, pt * 512 + blk * 128, [[1, 128]]), BS, [Bp])
                        if pt % 2 == 0:
                            A(act, Wb[:, pt, :, :], ps[:, :].rearrange("p (a b) -> p a b", a=4), AF.Copy, [Bp], [B_Wb])
                        else:
                            CP(dve, Wb[:, pt, :, :], ps[:, :].rearrange("p (a b) -> p a b", a=4), [Bp], [B_Wb])
                    for pt in range(16):
                        for c in range(2):
                            ps, Bp = ps_next()
                            if d == 1:
                                ranges = [(0, 272, 0)]
                                ncol = 272
                            else:
                                ranges = [(256, 16, 0), (0, 128, 16)]
                                ncol = 144
                            for (u0, n, o0) in ranges:
                                for gl in range(2):
                                    for jt in range(2):
                                        MM(ps[gl * 64:(gl + 1) * 64, o0:o0 + n], Wb[:, pt, c * 2 + jt, gl * 64:(gl + 1) * 64],
                                           Ug[:, 2 * pt + gl, jt, u0:u0 + n], jt == 0, jt == 1, [B_Wb, B_Ug2], [Bp])
                            lo = L[:, d, c, pt, 272 - ncol:272]
                            if c == 0:
                                A(act, lo, ps[:, 0:ncol], AF.Copy, [Bp], [B_L])
                            else:
                                CP(dve, lo, ps[:, 0:ncol], [Bp], [B_L])
                    if d == 0:
                        dump("Wb0", Wb[:].rearrange("p a b c -> p (a b c)"), [B_Wb])
            dump("L", L[:].rearrange("p a b c e -> p (a b c e)"), [B_L])
            fw.barrier()
            Sh = sb("Sh", [128, 2, 2, 16, NS], stack=S2); B_Sh = Buf("Sh")
            fw.op(pool, lambda e: e.memset(Sh[:], 0.0), writes=[B_Sh])
            tm1 = sb("tm1", [128, 2, 2, 16], stack=S2); tm2 = sb("tm2", [128, 2, 2, 16], stack=S2); B_tm = Buf("tm")

            def f0(st):
                return st - 140 if st >= 142 else st % 2

            def f1(st):
                return 273 - st if st >= 142 else 132 + st % 2
            B_tmh = bufs("tmh", 2); B_Shh = bufs("Shh", 2)
            for b_ in B_Shh:
                b_.w = B_Sh.w
            RSh = [[B_Shh[h_], B_L, B_sm, B_tmh[h_]] for h_ in range(2)]
            for s in range(272):
                both = s >= 128
                ops = []
                NH_ = 2 if WINDOW else 1
                PW_ = 16 // NH_
                for ph_ in range(NH_):
                    po = ph_ * PW_
                    if both:
                        def hv(t_, n_, c0, c1, rev=False):
                            dstep = 32 * n_ + (c1 - c0)
                            if rev:
                                return sbap(t_, c0 + 16 * n_ + po * n_, [[dstep, 2], [-16 * n_, 2], [n_, PW_]])
                            return sbap(t_, c0 + po * n_, [[dstep, 2], [16 * n_, 2], [n_, PW_]])
                        sr = hv(Sh, NS, f0(s), f1(s)); srev = hv(Sh, NS, f0(s), f1(s), True)
                        sw = hv(Sh, NS, f0(s + 1), f1(s + 1))
                        lv = hv(L, 272, s, 271 - s)
                        aa, ab = AA[:, :, :, po:po + PW_], AB[:, :, :, po:po + PW_]
                        x1, x2 = tm1[:, :, :, po:po + PW_], tm2[:, :, :, po:po + PW_]
                    else:
                        def hv(t_, n_, c1, rev=False):
                            if rev:
                                return sbap(t_, 32 * n_ + c1 + 16 * n_ + po * n_, [[-16 * n_, 2], [n_, PW_]])
                            return sbap(t_, 32 * n_ + c1 + po * n_, [[16 * n_, 2], [n_, PW_]])
                        sr = hv(Sh, NS, f1(s)); srev = hv(Sh, NS, f1(s), True); sw = hv(Sh, NS, f1(s + 1))
                        lv = hv(L, 272, 271 - s)
                        aa, ab = AA[:, 1, :, po:po + PW_], AB[:, 1, :, po:po + PW_]
                        x1, x2 = tm1[:, 1, :, po:po + PW_], tm2[:, 1, :, po:po + PW_]
                    ops.append((sr, srev, sw, lv, aa, ab, x1, x2))
                for h_, (sr, srev, sw, lv, aa, ab, x1, x2) in enumerate(ops):
                    TT(dve, x1, sr, aa, ALU.mult, RSh[h_], [B_tmh[h_]])
                for h_, (sr, srev, sw, lv, aa, ab, x1, x2) in enumerate(ops):
                    TT(dve, x2, srev, ab, ALU.mult, RSh[h_], [B_tmh[h_]])
                for h_, (sr, srev, sw, lv, aa, ab, x1, x2) in enumerate(ops):
                    TT(dve, x1, x1, lv, ALU.add, RSh[h_], [B_tmh[h_]])
                for h_, (sr, srev, sw, lv, aa, ab, x1, x2) in enumerate(ops):
                    TT(dve, sw, x1, x2, ALU.add, RSh[h_], [B_Shh[h_]])
            CP(dve, Sbf[:, 0].rearrange("p a b c -> p (a b) c"), sbap(Sh, 4, [[NS, 32], [1, 128]]), B_Shh, [B_Sbf])
            CP(dve, Sbf[:, 1].rearrange("p a b c -> p (a b) c"), sbap(Sh, 32 * NS + 2, [[NS, 32], [1, 128]]), B_Shh, [B_Sbf])
            dump("Sbf", Sbf[:].rearrange("p a b c e -> p (a b c e)"), [B_Sbf])
        fw.barrier()
        if stop_after == "scan":
            S.close()
            return finish()
        with ExitStack() as S3:
            Kmat = sb("Kmat", [128, 32, 4, 128], BF16, stack=S3); B_Km = Buf("Kmat")
            Ca = sb("Ca", [128, 2, 16, 2, 256], BF16, stack=S3); B_Ca = Buf("Ca")
            Ugo = sb("Ugo", [128, 64, 128], BF16, stack=S3); B_Ugo = Buf("Ugo")
            fw.dma(sp, Ugo[:], ug_s.rearrange("p (a c) -> p a c", c=272)[:, :, 0:128], writes=[B_Ugo], sem="s10")
            S3a = ExitStack()
            Xt = sb("Xt", [128, 16, 2, 256], BF16, stack=S3a); Zt = sb("Zt", [128, 16, 2, 256], BF16, stack=S3a)
            tmpK = sb("tmpK", [128, 4, 128], stack=S3a); tmpS = sb("tmpS", [128, 2, 128], stack=S3a); B_tk = Buf("tmpK")
            cplx_build(dve, Ca[:, 0], PWr, PWi, 1, 1, scc, 0, True)
            cplx_build(dve, Ca[:, 1], PWr, PWi, 16, -1, scc, 1, True)
            KS = BS + [B_tk]
            for d in range(2):
                if d == 0:
                    cplx_build(dve, Xt, IPr, IPi, 0, 1, Bbar, 0, False)
                    cplx_build(dve, Zt, PWr, PWi, 0, 1, scc, 0, True)
                else:
                    cplx_build(dve, Xt, PWr, PWi, 0, 1, Bbar, 1, False)
                    cplx_build(dve, Zt, IPr, IPi, 0, 1, scc, 1, True)
                for g in range(32):
                    pt, gl = g // 2, g % 2
                    r0, r1 = gl * 64, gl * 64 + 64
                    psA, BpA = ps_next()

                    def kblock(ps, col, jt, tt_):
                        for c in range(2):
                            MM(ps[:, col * 128:(col + 1) * 128], Xt[r0:r1, pt, c, jt * 128:(jt + 1) * 128],
                               Zt[r0:r1, pt, c, tt_ * 128:(tt_ + 1) * 128], c == 0, c == 1, BS, [BpA])
                    kblock(psA, 0, 0, 0)
                    kblock(psA, 1, 1, 1)
                    if d == 0:
                        kblock(psA, 2, 0, 1)
                    else:
                        kblock(psA, 2, 1, 0)
                    mk = sbap(masks, d * 128, [[0, 2], [1, 128]])
                    kd = sbap(Kmat, g * 512, [[384, 2], [1, 128]])
                    ko_ = Kmat[:, g, 1 if d == 0 else 2, :]
                    if d == 0:
                        TT(dve, tmpS[:], psA[:, 0:256].rearrange("p (a b) -> p a b", a=2), mk, ALU.mult, [BpA, B_par], [B_tk])
                        STT(kd, sbap(ident, 0, [[0, 2], [1, 128]]), dcols[:, g:g + 1], tmpS[:], ALU.mult, ALU.add, [B_ident, B_par, B_tk], [B_Km])
                    else:
                        TT(dve, tmpS[:], psA[:, 0:256].rearrange("p (a b) -> p a b", a=2), mk, ALU.mult, [BpA, B_par], [B_tk])
                        TT(dve, kd, kd, tmpS[:], ALU.add, [B_tk, B_Km], [B_Km])
                    A(act, ko_, psA[:, 256:384], AF.Copy, [BpA], [B_Km])
            dump("Kmat", Kmat[:].rearrange("p a b c -> p (a b c)"), [B_Km])
            dump("Ca", Ca[:].rearrange("p a b c e -> p (a b c e)"), BS)
            fw.barrier()
            S3a.close()
            g_tm = sb("g_tm", [128, 16, 512], stack=S3); B_gtm = Buf("g_tm")
            glsb = [sb("glsb%d" % i, [128, 512], stack=S3) for i in range(2)]; B_gl = bufs("glsb", 2)
            for gp in range(16):
                psY, BpY = ps_next()
                for gi in range(2):
                    g = gp * 2 + gi
                    pt, gl = g // 2, g % 2
                    r0, r1 = gl * 64, gl * 64 + 64
                    for tt_ in range(2):
                        yo = psY[:, (gi * 2 + tt_) * 128:(gi * 2 + tt_ + 1) * 128]
                        first = True
                        for jt in range(2):
                            MM(yo, Kmat[:, g, jt * 2 + tt_, :], Ugo[:, g * 2 + jt, :], first, False, [B_Km, B_Ugo], [BpY])
                            first = False
                        for d in range(2):
                            for c in range(2):
                                MM(yo, Ca[r0:r1, d, pt, c, tt_ * 128:(tt_ + 1) * 128], Sbf[r0:r1, d, c, pt, :], False, (d == 1 and c == 1),
                                   BS + [B_Sbf], [BpY])
                gs_ = gp % 2
                A(act, glsb[gs_][:, :], psY[:, :], AF.Gelu, [BpY], [B_gl[gs_]])
                psT, BpT = ps_next()
                for blk in range(4):
                    TR(psT[:, blk * 128:(blk + 1) * 128], glsb[gs_][:, blk * 128:(blk + 1) * 128], [B_gl[gs_]], [BpT])
                for gi in range(2):
                    g = gp * 2 + gi
                    oo = sbap(g_tm, 16 * g, [[512, 16], [1, 16]])
                    ii = sbap(psT, gi * 256, [[16, 16], [1, 16]])
                    CP(dve, oo, ii, [BpT], [B_gtm])
            dump("gtm", g_tm[:].rearrange("p a b -> p (a b)"), [B_gtm])
            gT = [sb("gT%d" % i, [128, 4, 128], BF16, stack=S3) for i in range(2)]; B_gT = bufs("gT", 2)
            zt = sb("zt", [128, 512], stack=S3); B_zt = Buf("zt")
            for t in range(16):
                gs_ = t % 2
                ps, Bp = ps_next()
                for fc in range(4):
                    TR(ps[:, fc * 128:(fc + 1) * 128], g_tm[:, t, fc * 128:(fc + 1) * 128], [B_gtm], [Bp])
                A(act, gT[gs_][:], ps[:, :].rearrange("p (a b) -> p a b", a=4), AF.Copy, [Bp], [B_gT[gs_]])
                ps2, Bp2 = ps_next()
                for fc in range(4):
                    MM(ps2[:, :], gT[gs_][:, fc, :], w_glu[:, fc, :], fc == 0, fc == 3, [B_gT[gs_], B_wglu], [Bp2])
                TT(dve, zt[:], ps2[:, :], bglu[:], ALU.add, [Bp2, B_bglu], [B_zt])
                A(act, zt[:], zt[:], AF.Sigmoid, [B_zt], [B_zt])
                TT(dve, g_tm[:, t, :], g_tm[:, t, :], zt[:], ALU.mult, [B_gtm, B_zt], [B_gtm])
            dump("ssm", g_tm[:].rearrange("p a b -> p (a b)"), [B_gtm])
            fw.dma(sp, g_s[:, :], g_tm[:].rearrange("p a b -> p (a b)"), reads=[B_gtm], writes=[B_gs], sem="gs")
    fw.barrier()
    if stop_after == "ssm":
        return finish()

    hT = sb("hT", [128, 8, NOWN], BF16); B_hT = Buf("hT")
    combT = sb("combT", [32, NOWN], BF16); B_combT = Buf("combT")
    lnbox = [None, None]
    B_x1s = Buf("x1_s")

    def layer_norm(pre, B_pre, gi, out, B_out, st, mv, sc_, B_st):
        for hf in range(2):
            fw.op(dve, lambda e: e.bn_stats(out=st[:, hf * 6:(hf + 1) * 6], in_=pre[:, hf * 512:(hf + 1) * 512]), reads=[B_pre], writes=[B_st])
        fw.op(dve, lambda e: e.bn_aggr(out=mv[:, 0:2], in_=st[:, 0:12]), reads=[B_st], writes=[B_st])
        A(act, sc_[:, 0:1], mv[:, 1:2], AF.Ln, [B_st, B_eps], [B_st], bias=epsc[:, 0:1])
        A(act, sc_[:, 1:2], sc_[:, 0:1], AF.Exp, [B_st], [B_st], scale=-0.5)
        TS(dve, out, pre, mv[:, 0:1], sc_[:, 1:2], ALU.subtract, ALU.mult, [B_pre], [B_out], sreads=[B_st])
        lnrows, B_ln = lnbox
        TT(dve, out, out, lnrows[:, 0, :], ALU.mult, [B_out, B_ln], [B_out])
        TT(dve, out, out, lnrows[:, 1, :], ALU.add, [B_out, B_ln], [B_out])

    with ExitStack() as M:
        ln1 = sb("ln1rows", [128, 2, D], stack=M); lnbox[0] = ln1; lnbox[1] = Buf("ln1")
        fw.dma(sp, ln1[:], ln_d[:, 0:2, :], writes=[lnbox[1]], sem="m0")
        w_o_s = sb("w_o_s", [128, 8, D], BF16, stack=M); B_wo = Buf("w_o_s")
        wof = [sb("wof%d" % i, [128, D], stack=M) for i in range(2)]; B_wof = bufs("wof", 2)
        gn = sb("gn", [128, 8], stack=M); B_gn = Buf("gn")
        w_r = sb("w_r", [128, 8, 36], stack=M); b_r = sb("b_r", [128, 36], stack=M); B_wr = Buf("w_r")
        fw.dma(sp, gn[:], gn_d[:, :], writes=[B_gn], sem="m1")
        fw.dma(sp, w_r[:], w_r_d.rearrange("(k p) n -> p k n", p=128), writes=[B_wr], sem="m2")
        fw.dma(sp, b_r[:], b_r_d[:, :], writes=[B_wr], sem="m3")
        for fc in range(8):
            fw.dma(sp, wof[fc % 2][:], w_o_d[fc * 128:(fc + 1) * 128, :], writes=[B_wof[fc % 2]], sem="wof%d" % (fc % 2))
            TS(dve, w_o_s[:, fc, :], wof[fc % 2][:], gn[:, fc:fc + 1], None, ALU.mult, ALU.bypass, [B_wof[fc % 2], B_gn], [B_wo])
        cat = [sb("cat%d" % i, [128, D], stack=M) for i in range(2)]; B_cat = bufs("cat", 2)
        xo = [sb("xo%d" % i, [128, D], stack=M) for i in range(2)]; B_xo = bufs("xo", 2)
        catT = [sb("catT%d" % i, [128, 8, 128], BF16, stack=M) for i in range(2)]; B_catT = bufs("catT", 2)
        pre = [sb("pre%d" % i, [128, D], stack=M) for i in range(2)]; B_pre = bufs("pre", 2)
        hTf = [sb("hTf%d" % i, [128, 8, 128], stack=M) for i in range(2)]; B_hTf = bufs("hTf", 2)
        htm = [sb("htm%d" % i, [128, D], stack=M) for i in range(2)]; B_htm = bufs("htm", 2)
        junk = sb("junk", [128, 512], stack=M); B_junk = Buf("junk")
        st = sb("st", [128, 12], stack=M); mv = sb("mv", [128, 2], stack=M); sc_ = sb("sc_", [128, 8], stack=M); B_st = Buf("st")
        rt = sb("rt", [128, 160], stack=M); B_rt = Buf("rt")
        scA = sb("scA", [128, 8], stack=M); B_stA = Buf("stA")
        comb = sb("comb", [128, 32], stack=M); B_comb = Buf("comb")
        mixps = [[], []]

        def stageA(t):
            s = t % 2
            fw.dma(sp, cat[s][:, 0:512], attn_s[:, t * 512:(t + 1) * 512], reads=[B_attn_s], writes=[B_cat[s]], sem="cat%d" % s)
            fw.dma(sp, cat[s][:, 512:1024], g_s[:, t * 512:(t + 1) * 512], reads=[B_gs], writes=[B_cat[s]], sem="cat%d" % s)
            fw.group_done("cat%d" % s, [B_cat[s]])
            fw.dma(sp, xo[s][:], xf_v[t, 0:128, :], writes=[B_xo[s]], sem="xo%d" % s)
            for hf in range(2):
                A(act, junk[:], cat[s][:, hf * 512:(hf + 1) * 512], AF.Square, [B_cat[s]], [B_junk, B_stA], accum_out=scA[:, 2 + hf:3 + hf])
                A(act, scA[:, 4 + hf:5 + hf], scA[:, 2 + hf:3 + hf], AF.Ln, [B_stA, B_eps], [B_stA], scale=1.0 / 512, bias=epsc[:, 0:1])
                A(act, scA[:, 6 + hf:7 + hf], scA[:, 4 + hf:5 + hf], AF.Exp, [B_stA], [B_stA], scale=-0.5)
                A(act, cat[s][:, hf * 512:(hf + 1) * 512], cat[s][:, hf * 512:(hf + 1) * 512], AF.Copy, [B_cat[s], B_stA], [B_cat[s]],
                  scale=scA[:, 6 + hf:7 + hf])
            for hf in range(2):
                ps, Bp = ps_next()
                for q4 in range(4):
                    fc = hf * 4 + q4
                    TR(ps[:, q4 * 128:(q4 + 1) * 128], cat[s][:, fc * 128:(fc + 1) * 128], [B_cat[s]], [Bp])
                A(act, catT[s][:, hf * 4:(hf + 1) * 4, :], ps[:, :].rearrange("p (a b) -> p a b", a=4), AF.Copy, [Bp], [B_catT[s]])
            for hf in range(2):
                ps, Bp = ps_next()
                for fc in range(8):
                    MM(ps[:, :], catT[s][:, fc, :], w_o_s[:, fc, hf * 512:(hf + 1) * 512], fc == 0, fc == 7, [B_catT[s], B_wo], [Bp])
                mixps[t % 2].append((ps, Bp))

        def stageA_pre(t):
            s = t % 2
            for hf in range(2):
                ps, Bp = mixps[t % 2][hf]
                TT(dve, pre[s][:, hf * 512:(hf + 1) * 512], ps[:, :], grow[:, 0, hf * 512:(hf + 1) * 512], ALU.mult, [Bp, B_grow], [B_pre[s]])
            mixps[t % 2].clear()
            STT(pre[s][:], xo[s][:], ALPHA, pre[s][:], ALU.mult, ALU.add, [B_xo[s], B_pre[s]], [B_pre[s]])

        def stageB(t):
            s = t % 2
            layer_norm(pre[s][:], B_pre[s], 0, pre[s][:], B_pre[s], st, mv, sc_, B_st)
            fw.dma(sp, x1_s[:, t * D:(t + 1) * D], pre[s][:], reads=[B_pre[s]], writes=[B_x1s], sem="x1s")
            TT(dve, htm[s][:], pre[s][:], grow[:, 2, :], ALU.mult, [B_pre[s], B_grow], [B_htm[s]])
            TT(dve, htm[s][:], htm[s][:], grow[:, 1, :], ALU.add, [B_htm[s], B_grow], [B_htm[s]])
            for hf in range(2):
                ps, Bp = ps_next()
                for q4 in range(4):
                    fc = hf * 4 + q4
                    TR(ps[:, q4 * 128:(q4 + 1) * 128], htm[s][:, fc * 128:(fc + 1) * 128], [B_htm[s]], [Bp])
                A(act, hT[:, hf * 4:(hf + 1) * 4, t * 128:(t + 1) * 128], ps[:, :].rearrange("p (a b) -> p a b", a=4), AF.Copy, [Bp], [B_hT])
                A(act, hTf[s][:, hf * 4:(hf + 1) * 4, :], ps[:, :].rearrange("p (a b) -> p a b", a=4), AF.Copy, [Bp], [B_hTf[s]])

        rps = [None]

        def stageC_mm(t):
            s = t % 2
            ps, Bp = ps_next()
            for fc in range(8):
                MM(ps[:, 0:36], hTf[s][:, fc, :], w_r[:, fc, :], fc == 0, fc == 7, [B_hTf[s], B_wr], [Bp])
            rps[0] = (ps, Bp)

        def stageC(t):
            s = t % 2
            ps, Bp = rps[0]
            lg = rt[:, 0:36]
            gsel = rt[:, 36:40]; gmax = rt[:, 40:41]; ngmax = rt[:, 41:42]; gsum = rt[:, 42:43]; g_w = rt[:, 43:44]
            esel = rt[:, 44:52]; m1 = rt[:, 52:53]; mask1 = rt[:, 56:64]; esel2 = rt[:, 64:72]; m2 = rt[:, 53:54]; mask2 = rt[:, 72:80]
            nm1 = rt[:, 54:55]; e2 = rt[:, 55:56]; den_ = rt[:, 80:81]; w1 = rt[:, 81:82]; w2 = rt[:, 82:83]; cw8 = rt[:, 88:96]; gex = rt[:, 96:100]
            RR = [B_rt]
            TT(dve, lg, ps[:, 0:36], b_r[:, :], ALU.add, [Bp, B_wr], RR)
            fw.op(dve, lambda e: e.reduce_max(out=gmax, in_=rt[:, 0:4], axis=AX.X), reads=RR, writes=RR)
            TS(dve, gsel, rt[:, 0:4], gmax, None, ALU.is_ge, ALU.bypass, RR, RR, sreads=RR)
            TS(dve, ngmax, gmax, -1.0, None, ALU.mult, ALU.bypass, RR, RR)
            A(act, gex, rt[:, 0:4], AF.Exp, RR, RR, bias=ngmax, accum_out=gsum)
            fw.op(dve, lambda e: e.reciprocal(out=g_w, in_=gsum), reads=RR, writes=RR)
            TS(dve, esel, rt[:, 4:12], gsel[:, 0:1], None, ALU.mult, ALU.bypass, RR, RR, sreads=RR)
            for g in range(1, 4):
                fw.op(dve, lambda e: e.scalar_tensor_tensor(out=esel, in0=rt[:, 4 + 8 * g:12 + 8 * g], scalar=gsel[:, g:g + 1], in1=esel,
                                                            op0=ALU.mult, op1=ALU.add), reads=RR, writes=RR, sreads=RR)
            fw.op(dve, lambda e: e.reduce_max(out=m1, in_=esel, axis=AX.X), reads=RR, writes=RR)
            TS(dve, mask1, esel, m1, None, ALU.is_ge, ALU.bypass, RR, RR, sreads=RR)
            STT(esel2, mask1, -1e30, esel, ALU.mult, ALU.add, RR, RR)
            fw.op(dve, lambda e: e.reduce_max(out=m2, in_=esel2, axis=AX.X), reads=RR, writes=RR)
            TS(dve, mask2, esel2, m2, None, ALU.is_ge, ALU.bypass, RR, RR, sreads=RR)
            TS(dve, nm1, m1, -1.0, None, ALU.mult, ALU.bypass, RR, RR)
            A(act, e2, m2, AF.Exp, RR, RR, bias=nm1)
            TS(dve, den_, e2, 1.0, None, ALU.add, ALU.bypass, RR, RR)
            fw.op(dve, lambda e: e.reciprocal(out=w1, in_=den_), reads=RR, writes=RR)
            TT(dve, w2, e2, w1, ALU.mult, RR, RR)
            TT(dve, w1, w1, g_w, ALU.mult, RR, RR)
            TT(dve, w2, w2, g_w, ALU.mult, RR, RR)
            TS(dve, cw8, mask1, w1, None, ALU.mult, ALU.bypass, RR, RR, sreads=RR)
            fw.op(dve, lambda e: e.scalar_tensor_tensor(out=cw8, in0=mask2, scalar=w2, in1=cw8, op0=ALU.mult, op1=ALU.add),
                  reads=RR, writes=RR, sreads=RR)
            for g in range(4):
                TS(dve, comb[:, g * 8:(g + 1) * 8], cw8, gsel[:, g:g + 1], None, ALU.mult, ALU.bypass, RR, [B_comb], sreads=RR)

        def stageC_tail(t):
            s = t % 2
            ps, Bp = ps_next()
            TR(ps[0:32, 0:128], comb[:, :], [B_comb], [Bp])
            CP(dve, combT[:, t * 128:(t + 1) * 128], ps[0:32, 0:128], [Bp], [B_combT])
            if t == 0:
                dump("x1_0", pre[s][:], [B_pre[s]])
                dump("comb0", comb[:], [B_comb])

        stageA(0)
        stageA_pre(0)
        for t in range(17):
            if t >= 1:
                stageC_mm(t - 1)
            if t + 1 < 16:
                stageA(t + 1)
            if t < 16:
                stageB(t)
            if t >= 1:
                stageC(t - 1)
            if t + 1 < 16:
                stageA_pre(t + 1)
            if t >= 1:
                stageC_tail(t - 1)
    fw.barrier()
    if stop_after == "merge":
        return finish()

    with ExitStack() as E:
        ln2 = sb("ln2rows", [128, 2, D], stack=E); lnbox[0] = ln2; lnbox[1] = Buf("ln2")
        fw.dma(sp, ln2[:], ln_d[:, 2:4, :], writes=[lnbox[1]], sem="e9")
        acc = sb("acc", [128, 16, D], stack=E); B_acc = bufs("acc", 16)
        sel = sb("sel", [32, NE * 128], BF16, stack=E); B_sel = Buf("sel")
        fw.dma(pool, sel[:], sel_d[:, :], writes=[B_sel], sem="e0")
        NSLOT = 3
        Wg = [sb("Wg%d" % i, [128, 8, 256], BF16, stack=E) for i in range(NSLOT)]
        Wu = [sb("Wu%d" % i, [128, 8, 256], BF16, stack=E) for i in range(NSLOT)]
        Wd = [sb("Wd%d" % i, [128, 2, D], BF16, stack=E) for i in range(NSLOT)]
        B_W = bufs("Wexp", NSLOT)
        sg = [sb("sg%d" % i, [128, 512], stack=E) for i in range(2)]; B_sg = bufs("sg", 2)
        tu = [sb("tu%d" % i, [128, 512], stack=E) for i in range(2)]; B_tu = bufs("tu", 2)
        gT2 = [sb("gT2_%d" % i, [128, 2, 256], BF16, stack=E) for i in range(2)]; B_gT2 = bufs("gT2", 2)
        n_exp = int(os.environ.get("K_NEXP", str(NE)))

        stg = [sb("stg%d" % i, [128, 2048], stack=E) for i in range(2)]; B_stg = bufs("stg", 2)
        stc = [0]

        def load_expert(e):
            sl_ = e % NSLOT
            for (src_, dst, kk) in ((wg_d[e], Wg[sl_], 8), (wu_d[e], Wu[sl_], 8), (wd_d[e], Wd[sl_], 2)):
                k_ = stc[0] % 2
                stc[0] += 1
                fw.dma(sp, stg[k_][:].rearrange("p (k n) -> p k n", k=kk), src_.rearrange("(k p) n -> p k n", p=128),
                       writes=[B_stg[k_]], sem="stg%d" % k_)
                CP(pool, dst[:].rearrange("p k n -> p (k n)"), stg[k_][:], [B_stg[k_]], [B_W[sl_]])

        for e in range(min(NSLOT, n_exp)):
            load_expert(e)
        items = [(e, tg) for e in range(n_exp) for tg in range(8)]
        dcnt = [0]

        csb = [sb("csb%d" % i, [128, 256], BF16, stack=E) for i in range(2)]; B_csb = bufs("csb", 2)

        def AUC(i):
            e, tg = items[i]
            sl_ = e % NSLOT
            k2 = i % 2
            pa, Bpa = PS[k2 * 2], PSB[k2 * 2]
            pu, Bpu = PS[k2 * 2 + 1], PSB[k2 * 2 + 1]
            pc_, Bpc = PS[4], PSB[4]
            MM(pc_[:, 0:256], sel[0:32, e * 128:(e + 1) * 128], combT[0:32, tg * 256:(tg + 1) * 256], True, True, [B_sel, B_combT], [Bpc])
            for fc in range(2):
                for ch in range(8):
                    MM(pa[:, fc * 256:(fc + 1) * 256], Wg[sl_][:, ch, fc * 128:(fc + 1) * 128], hT[:, ch, tg * 256:(tg + 1) * 256],
                       ch == 0, ch == 7, [B_W[sl_], B_hT], [Bpa])
            for fc in range(2):
                for ch in range(8):
                    MM(pu[:, fc * 256:(fc + 1) * 256], Wu[sl_][:, ch, fc * 128:(fc + 1) * 128], hT[:, ch, tg * 256:(tg + 1) * 256],
                       ch == 0, ch == 7, [B_W[sl_], B_hT], [Bpu])

        def REST(i):
            e, tg = items[i]
            sl_ = e % NSLOT
            k2 = i % 2
            pa, Bpa = PS[k2 * 2], PSB[k2 * 2]
            pu, Bpu = PS[k2 * 2 + 1], PSB[k2 * 2 + 1]
            A(act, sg[k2][:, :], pa[:, :], AF.Silu, [Bpa], [B_sg[k2]])
            if i + 1 < len(items):
                A(act, csb[1 - k2][:, :], PS[4][:, 0:256], AF.Copy, [PSB[4]], [B_csb[1 - k2]])
            TT(dve, tu[k2][:, :], pu[:, :], sg[k2][:, :], ALU.mult, [Bpu, B_sg[k2]], [B_tu[k2]])
            TT(dve, gT2[k2][:], tu[k2][:, :].rearrange("p (a b) -> p a b", a=2), sbap(csb[k2], 0, [[0, 2], [1, 256]]), ALU.mult,
               [B_tu[k2], B_csb[k2]], [B_gT2[k2]])
            for tt_ in range(2):
                tile_ = tg * 2 + tt_
                for hf in range(2):
                    po, Bpo = PS[5 + dcnt[0] % 3], PSB[5 + dcnt[0] % 3]
                    dcnt[0] += 1
                    for fc in range(2):
                        MM(po[:, :], gT2[k2][:, fc, tt_ * 128:(tt_ + 1) * 128], Wd[sl_][:, fc, hf * 512:(hf + 1) * 512],
                           fc == 0, fc == 1, [B_gT2[k2], B_W[sl_]], [Bpo])
                    ao = acc[:, tile_, hf * 512:(hf + 1) * 512]
                    if e == 0:
                        CP(dve, ao, po[:, :], [Bpo], [B_acc[tile_]])
                    else:
                        TT(dve, ao, po[:, :], ao, ALU.add, [Bpo, B_acc[tile_]], [B_acc[tile_]])
            if tg == 7 and e + NSLOT < n_exp:
                load_expert(e + NSLOT)

        AUC(0)
        A(act, csb[0][:, :], PS[4][:, 0:256], AF.Copy, [PSB[4]], [B_csb[0]])
        for i in range(len(items)):
            if i + 1 < len(items):
                AUC(i + 1)
            REST(i)
        dump("acc0", acc[:, 0, :], [B_acc[0]])
        x1t = [stg[i][:, 0:D] for i in range(2)]; B_x1t = B_stg
        st2 = sb("st2", [128, 12], stack=E); mv2 = sb("mv2", [128, 2], stack=E); sc2 = sb("sc2", [128, 8], stack=E); B_st2 = Buf("st2")
        B_out = Buf("out")
        st2s = [sb("st2_%d" % i, [128, 12], stack=E) for i in range(2)]; mv2s = [sb("mv2_%d" % i, [128, 2], stack=E) for i in range(2)]
        sc2s = [sb("sc2_%d" % i, [128, 8], stack=E) for i in range(2)]; B_st2s = bufs("st2s", 2)

        def F1(t):
            s = t % 2
            fw.dma(sp, x1t[s], x1_s[:, t * D:(t + 1) * D], reads=[B_x1s], writes=[B_x1t[s]], sem="stg%d" % s)
            TT(dve, acc[:, t, :], acc[:, t, :], grow[:, 3, :], ALU.mult, [B_acc[t], B_grow], [B_acc[t]])
            STT(acc[:, t, :], x1t[s], ALPHA, acc[:, t, :], ALU.mult, ALU.add, [B_x1t[s], B_acc[t]], [B_acc[t]])
            pre_ = acc[:, t, :]
            for hf in range(2):
                fw.op(dve, lambda e: e.bn_stats(out=st2s[s][:, hf * 6:(hf + 1) * 6], in_=pre_[:, hf * 512:(hf + 1) * 512]), reads=[B_acc[t]], writes=[B_st2s[s]])
            fw.op(dve, lambda e: e.bn_aggr(out=mv2s[s][:, 0:2], in_=st2s[s][:, 0:12]), reads=[B_st2s[s]], writes=[B_st2s[s]])
            A(act, sc2s[s][:, 0:1], mv2s[s][:, 1:2], AF.Ln, [B_st2s[s], B_eps], [B_st2s[s]], bias=epsc[:, 0:1])
            A(act, sc2s[s][:, 1:2], sc2s[s][:, 0:1], AF.Exp, [B_st2s[s]], [B_st2s[s]], scale=-0.5)

        def F2(t):
            s = t % 2
            o_ = acc[:, t, :]
            lnrows, B_ln = lnbox
            TS(dve, o_, o_, mv2s[s][:, 0:1], sc2s[s][:, 1:2], ALU.subtract, ALU.mult, [B_acc[t]], [B_acc[t]], sreads=[B_st2s[s]])
            TT(dve, o_, o_, lnrows[:, 0, :], ALU.mult, [B_acc[t], B_ln], [B_acc[t]])
            TT(dve, o_, o_, lnrows[:, 1, :], ALU.add, [B_acc[t], B_ln], [B_acc[t]])
            fw.dma(sp, out_d[t * 128:(t + 1) * 128, :], o_, reads=[B_acc[t]], writes=[B_out], sem="out")
        F1(0)
        for t in range(16):
            if t + 1 < 16:
                F1(t + 1)
            F2(t)
    return finish()


def _rope_tables(frame_tok):
    t = np.asarray(frame_tok)
    row = (t // 64).astype(np.float32)
    col = (t % 64).astype(np.float32)
    inv = (10000.0 ** (-np.arange(0, 16, 2, dtype=np.float32) / np.float32(16))).astype(np.float32)
    ang = np.concatenate([row[:, None] * inv, col[:, None] * inv], -1)
    ang = np.concatenate([ang, ang], -1).astype(np.float32)
    return np.cos(ang).astype(np.float32), np.sin(ang).astype(np.float32)


def prep_core(inp, b, hh):
    f32 = np.float32
    m = {}
    x = inp["x"][b]
    ctx = inp["ctx"][b]
    if hh == 1:
        x = x[::-1]
        ctx = ctx[::-1]
    m["xf"] = np.ascontiguousarray(x, f32)
    m["ctxf"] = np.ascontiguousarray(ctx, f32)
    cT = np.stack([inp["c"][b].reshape(8, 128).T, inp["c_ctx"].reshape(8, 128).T], -1)
    m["cT"] = np.ascontiguousarray(cT, f32)
    m["w_ada"] = inp["w_ada"][0]
    ba = inp["b_ada"][0]
    m["b_ada_2rows"] = np.ascontiguousarray(np.stack([ba, ba], 0), f32)
    m["w_in"] = inp["w_in"][0]
    m["qg_cols"] = np.ascontiguousarray(inp["q_norm_g"][0].reshape(2, 128).T, f32)
    m["kvg_col"] = np.ascontiguousarray(inp["kv_norm_g"][0].reshape(1, 128).T, f32)
    m["w_uq"] = inp["w_uq"][0]
    m["w_ukv"] = inp["w_ukv"][0]
    mt, j, i = np.meshgrid(np.arange(2), np.arange(16), np.arange(128), indexing="ij")
    tau = (16 * (128 * mt + i) + j).reshape(-1)
    tok = tau if hh == 0 else (NTOK - 1 - tau)
    cos, sin = _rope_tables(tok)
    rope = np.zeros((128, 2, NKEY), f32)
    rope[64:96, 0, :NTOK] = cos.T
    rope[64:96, 1, :NTOK] = sin.T
    rope[64:96, 0, NTOK:] = 1.0
    m["rope"] = rope
    dirs = [0, 1] if hh == 0 else [1, 0]

    def lay(a):
        a = a[dirs]
        sh = a.shape[3:]
        a = a.reshape(2, 16, 2, 64, *sh)
        a = np.moveaxis(a, (2, 3), (0, 1))
        return a.reshape(128, 32, *sh)

    m["ssm_a"] = np.ascontiguousarray(np.stack([lay(inp["ssm_a_re"][0]), lay(inp["ssm_a_im"][0])], 1), f32)
    ldt = np.broadcast_to(inp["ssm_log_dt"][0][:, :, None], (2, 32, 64))
    m["ssm_ldt"] = np.ascontiguousarray(lay(ldt), f32)
    m["ssm_b"] = np.ascontiguousarray(np.stack([lay(inp["ssm_b_re"][0]), lay(inp["ssm_b_im"][0])], 1), f32)
    cre = np.swapaxes(inp["ssm_c_re"][0], 2, 3)
    cim = np.swapaxes(inp["ssm_c_im"][0], 2, 3)
    m["ssm_c"] = np.ascontiguousarray(np.stack([lay(cre), lay(cim)], 1), f32)
    dd = inp["ssm_d"][0].reshape(32, 16)
    m["ssm_dcols"] = np.ascontiguousarray(np.broadcast_to(dd.T[None], (8, 16, 32)).reshape(128, 32), f32)
    jl = np.arange(128) // 16
    m0 = (jl[None, :] >= jl[:, None]).astype(f32)
    m1 = (jl[:, None] >= jl[None, :]).astype(f32)
    m["masks"] = np.ascontiguousarray(np.stack([m0, m1], 1), f32)
    m["ident"] = np.eye(128, dtype=f32)
    m["w_glu"] = inp["w_glu"][0]
    m["b_glu_row"] = np.ascontiguousarray(np.broadcast_to(inp["b_glu"][0][None], (128, 512)), f32)
    gn = np.concatenate([inp["gn_attn_g"][0], inp["gn_ssm_g"][0]])
    m["gn_cols"] = np.ascontiguousarray(gn.reshape(8, 128).T, f32)
    m["w_o"] = inp["w_o"][0]
    ln = np.stack([inp["ln1_g"][0], inp["ln1_b"][0], inp["ln2_g"][0], inp["ln2_b"][0]], 0)
    m["ln_rows"] = np.ascontiguousarray(np.broadcast_to(ln[None], (128, 4, D)), f32)
    m["w_r"] = np.ascontiguousarray(np.concatenate([inp["w_router_group"][0], inp["w_router_expert"][0]], 1), f32)
    br_ = np.concatenate([inp["b_router_group"][0], inp["b_router_expert"][0]])
    m["b_r_row"] = np.ascontiguousarray(np.broadcast_to(br_[None], (128, 36)), f32)
    m["w_exp_gate"] = inp["w_exp_gate"][0]
    m["w_exp_up"] = inp["w_exp_up"][0]
    m["w_exp_down"] = inp["w_exp_down"][0]
    sel = np.zeros((32, NE, 128), f32)
    sel[np.arange(32), np.arange(32), :] = 1.0
    m["sel"] = sel.reshape(32, NE * 128)
    return m


def run(inputs, stop_after=None, dbg_names=(), cores=8):
    inp = {k: np.asarray(v) for k, v in inputs.items()}
    nc = build(stop_after=stop_after, dbg_names=dbg_names)
    in_maps = [prep_core(inp, c // 2, c % 2) for c in range(cores)]
    res = run_bass_kernel_spmd(nc, in_maps, core_ids=list(range(cores)))
    return res.results


def kernel(**inputs):
    res = run(inputs)
    out = np.zeros((4, NTOK, D), np.float32)
    for c in range(8):
        b, hh = c // 2, c % 2
        o = res[c]["out"]
        o = o.reshape(16, 128, D).transpose(1, 0, 2).reshape(NOWN, D)
        if hh == 0:
            out[b, :NOWN] = o
        else:
            out[b, NOWN:] = o[::-1]
    return out
```

```python
import math
import os
from contextlib import ExitStack
import numpy as np
import concourse.bass as bass
import concourse.mybir as mybir
from concourse.ap import AP
from concourse.bass_utils import run_bass_kernel_spmd

F32 = mybir.dt.float32
BF16 = mybir.dt.bfloat16
AF = mybir.ActivationFunctionType
ALU = mybir.AluOpType
AX = mybir.AxisListType

D = 1024
NTOK = 4096
NOWN = 2048
NCTX = 256
NKEY = NTOK + NCTX
NH = 8
SCALE = 96 ** -0.5
ALPHA = 2.0 ** 0.25
EPS = 1e-6
NS = 134
WINDOW = False
NE = 32


class Buf:
    __slots__ = ("name", "w", "r", "excl")

    def __init__(self, name, excl=False):
        self.name = name
        self.w = None
        self.r = []
        self.excl = excl


def bufs(name, n):
    return [Buf("%s%d" % (name, i)) for i in range(n)]


class Q:
    def __init__(self, fw, name, eng, same=False):
        self.name = name
        self.eng = eng
        self.sem = fw.es.enter_context(fw.nc.semaphore("s_" + name))
        self.cnt = 0
        self.seen = {}
        self.same = same
        self.window = False


class DmaTok:
    def __init__(self, fw, name):
        self.sem = fw.es.enter_context(fw.nc.semaphore("d_" + str(name)))
        self.cnt = 0
        self.name = name
        self.same = True


class FW:
    def __init__(self, nc, es):
        self.nc = nc
        self.es = es
        self.pe = Q(self, "pe", nc.tensor)
        self.dve = Q(self, "dve", nc.vector, same=True)
        self.act = Q(self, "act", nc.scalar, same=True)
        self.dve.window = WINDOW
        self.pool = Q(self, "pool", nc.gpsimd, same=True)
        self.sp = Q(self, "sp", nc.sync)
        self.dmasems = {}

    def _waits(self, q, reads, writes, sreads=()):
        need = {}

        def add(dep, force=False):
            if dep is None:
                return
            dq, n = dep
            if dq is q and not force:
                if not q.same:
                    return
                if q.window and n < q.cnt:
                    return
            if need.get(dq, 0) < n:
                need[dq] = n

        for b in sreads:
            add(b.w, True)
        for b in reads:
            add(b.w)
            if b.excl:
                for d in b.r:
                    add(d)
        for b in writes:
            add(b.w)
            for d in b.r:
                add(d)
        for dq, n in need.items():
            if q.seen.get(id(dq), 0) >= n:
                continue
            q.eng.wait_ge(dq.sem, n)
            q.seen[id(dq)] = n

    def op(self, q, fn, reads=(), writes=(), sreads=()):
        self._waits(q, reads, writes, sreads)
        reads = list(reads) + list(sreads)
        ins = fn(q.eng)
        ins.then_inc(q.sem, 1)
        q.cnt += 1
        tok = (q, q.cnt)
        for b in reads:
            b.r.append(tok)
        for b in writes:
            b.w = tok
            b.r = []
        return ins

    def dma(self, q, out, in_, reads=(), writes=(), sem=None, **kw):
        self._waits(q, reads, writes)
        ds = self.dmasems.get(sem)
        if ds is None:
            ds = DmaTok(self, sem)
            self.dmasems[sem] = ds
        if out.dtype != in_.dtype and in_.ap[-1][1] > 2048:
            kw.setdefault("max_dma_last_dim", 4096)
        ins = q.eng.dma_start(out=out, in_=in_, **kw)
        ds.cnt += 16
        ins.then_inc(ds.sem, 16)
        tok = (ds, ds.cnt)
        for b in reads:
            b.r.append(tok)
        for b in writes:
            b.w = tok
            b.r = []
        return tok

    def barrier(self):
        qs = [self.pe, self.dve, self.act, self.pool, self.sp]
        for q in qs:
            for dq in qs + list(self.dmasems.values()):
                if dq is q or dq.cnt == 0:
                    continue
                if q.seen.get(id(dq), 0) >= dq.cnt:
                    continue
                q.eng.wait_ge(dq.sem, dq.cnt)
                q.seen[id(dq)] = dq.cnt

    def group_done(self, sem, blist):
        ds = self.dmasems[sem]
        for b in blist:
            if b.w is not None and b.w[0] is ds:
                b.w = (ds, ds.cnt)


def sbap(t, off, dims):
    full = t[:] if not isinstance(t, AP) else t
    return AP(full.tensor, full.offset + off, [list(full.ap[0])] + [list(d) for d in dims])


def part_ap(t, p0, npart, off, dims):
    full = t[:]
    pstep = full.ap[0][0]
    sub = t[p0:p0 + npart]
    return AP(sub.tensor, sub.offset + off, [[pstep, npart]] + [list(d) for d in dims])


def build(stop_after=None, dbg_names=()):
    nc = bass.Bass("TRN2", target_bir_lowering=False)
    es = ExitStack()
    fw = FW(nc, es)
    pe, dve, act, pool, sp = fw.pe, fw.dve, fw.act, fw.pool, fw.sp
    dbg = {}

    def din(name, shape, dt=F32):
        return nc.dram_tensor(name, list(shape), dt, kind="ExternalInput").ap()

    xf = din("xf", [NTOK, D])
    ctxf = din("ctxf", [NCTX, D])
    cT_d = din("cT", [128, 8, 2])
    w_ada_d = din("w_ada", [D, 6 * D])
    b_ada_2rows_d = din("b_ada_2rows", [2, 6 * D])
    w_in_d = din("w_in", [D, 928])
    qg_d = din("qg_cols", [128, 2])
    kvg_d = din("kvg_col", [128, 1])
    w_uq_d = din("w_uq", [256, 768])
    w_ukv_d = din("w_ukv", [128, 1024])
    rope_d = din("rope", [128, 2, NKEY])
    ssm_a_d = din("ssm_a", [128, 2, 32])
    ssm_ldt_d = din("ssm_ldt", [128, 32])
    ssm_b_d = din("ssm_b", [128, 2, 32, 16])
    ssm_c_d = din("ssm_c", [128, 2, 32, 16])
    ssm_dcols_d = din("ssm_dcols", [128, 32])
    masks_d = din("masks", [128, 2, 128])
    ident_d = din("ident", [128, 128])
    w_glu_d = din("w_glu", [512, 512])
    b_glu_d = din("b_glu_row", [128, 512])
    gn_d = din("gn_cols", [128, 8])
    w_o_d = din("w_o", [D, D])
    ln_d = din("ln_rows", [128, 4, D])
    w_r_d = din("w_r", [D, 36])
    b_r_d = din("b_r_row", [128, 36])
    wg_d = din("w_exp_gate", [NE, D, 256])
    wu_d = din("w_exp_up", [NE, D, 256])
    wd_d = din("w_exp_down", [NE, 256, D])
    sel_d = din("sel", [32, NE * 128])
    out_d = nc.dram_tensor("out", [NOWN, D], F32, kind="ExternalOutput").ap()
    ug_s = nc.dram_tensor("ug_s", [128, 32 * 2 * 272], BF16, kind="Internal").ap()
    attn_s = nc.dram_tensor("attn_s", [128, 16 * 512], F32, kind="Internal").ap()
    x1_s = nc.dram_tensor("x1_s", [128, 16 * D], F32, kind="Internal").ap()
    g_s = nc.dram_tensor("g_s", [128, 16 * 512], F32, kind="Internal").ap()
    dbg_out = {}
    for ent in dbg_names:
        nm, shp = ent[0], ent[1]
        dbg_out[nm] = nc.dram_tensor("dbg_" + nm, list(shp), BF16 if (len(ent) > 2 and ent[2] == "bf16") else F32, kind="ExternalOutput").ap()

    def sb(name, shape, dt=F32, stack=None):
        return (stack or es).enter_context(nc.sbuf_tensor("sb_" + name, list(shape), dt))

    PS = [es.enter_context(nc.psum_tensor("ps%d" % i, [128, 512], F32)) for i in range(8)]
    PSB = [Buf("ps%d" % i, excl=True) for i in range(8)]
    ps_rr = [0]

    def ps_next(k=None):
        i = ps_rr[0] % 8
        ps_rr[0] += 1
        return PS[i], PSB[i]

    ident = sb("ident", [128, 128]); B_ident = Buf("ident")
    ones_bf = sb("ones_bf", [128, 128], BF16); B_ones = Buf("ones")
    modc = sb("modc", [128, 96]); B_modc = Buf("modc")
    grow = sb("grow", [128, 4, D]); B_grow = Buf("grow")
    fw.dma(sp, ident[:], ident_d[:, :], writes=[B_ident], sem="c0")
    fw.op(dve, lambda e: e.memset(ones_bf[:], 1.0), writes=[B_ones])

    def dump(name, ap, reads):
        if name in dbg_out:
            fw.dma(sp, dbg_out[name], ap, reads=reads, writes=[], sem="dbg")

    def finish():
        for ds in fw.dmasems.values():
            sp.eng.wait_ge(ds.sem, ds.cnt)
        es.close()
        return nc

    with ExitStack() as ph:
        cT = sb("cT", [128, 8, 2], stack=ph); B_cT = Buf("cT")
        sil2 = sb("sil2", [128, 8, 2], stack=ph); B_sil = Buf("sil2")
        b2 = sb("b2rows", [2, 6 * D], stack=ph); B_b2 = Buf("b2")
        modrow = sb("modrow", [2, 6 * D], stack=ph); B_mr = Buf("modrow")
        ones_f = sb("ones_f", [1, 128], stack=ph); B_onesf = Buf("ones_f")
        wa = [sb("wa%d" % i, [128, 8, D], stack=ph) for i in range(2)]
        B_wa = bufs("wa", 2)
        fw.dma(sp, cT[:], cT_d[:, :, :], writes=[B_cT], sem="c1")
        fw.dma(sp, b2[:], b_ada_2rows_d[:, :], writes=[B_b2], sem="c2")
        fw.op(dve, lambda e: e.memset(ones_f[:], 1.0), writes=[B_onesf])
        fw.op(act, lambda e: e.activation(out=sil2[:], in_=cT[:], func=AF.Silu), reads=[B_cT], writes=[B_sil])
        w_ada_v = w_ada_d.rearrange("(k p) n -> p k n", p=128)
        for v in range(6):
            slot = v % 2
            fw.dma(sp if v % 2 == 0 else act, wa[slot][:], w_ada_v[:, :, v * D:(v + 1) * D], writes=[B_wa[slot]], sem="wa%d" % slot)
            for half in range(2):
                pr, Bpr = PS[half], PSB[half]
                for k in range(8):
                    fw.op(pe, lambda e: e.matmul(pr[0:2, :], lhsT=sil2[:, k, :], rhs=wa[slot][:, k, half * 512:(half + 1) * 512],
                                                 start=(k == 0), stop=(k == 7)), reads=[B_wa[slot], B_sil], writes=[Bpr])
                c0 = v * D + half * 512
                fw.op(dve, lambda e: e.tensor_tensor(out=modrow[0:2, c0:c0 + 512], in0=pr[0:2, :], in1=b2[0:2, c0:c0 + 512], op=ALU.add),
                      reads=[Bpr, B_b2], writes=[B_mr])
        for v in (1, 4):
            fw.op(dve, lambda e: e.tensor_scalar_add(out=modrow[0:2, v * D:(v + 1) * D], in0=modrow[0:2, v * D:(v + 1) * D], scalar1=1.0),
                  reads=[B_mr], writes=[B_mr])
        psc, Bpsc = PS[2], PSB[2]
        for vc in range(48):
            fw.op(pe, lambda e: e.matmul(psc[:, vc * 2:vc * 2 + 2], lhsT=modrow[0:2, vc * 128:(vc + 1) * 128], rhs=ident[0:2, 0:2],
                                         start=True, stop=True, is_transpose=True), reads=[B_mr, B_ident], writes=[Bpsc])
        fw.op(dve, lambda e: e.tensor_copy(out=modc[:], in_=psc[:, 0:96]), reads=[Bpsc], writes=[B_modc])
        for ri, v in enumerate((2, 3, 4, 5)):
            for half in range(2):
                pr, Bpr = PS[3 + (ri * 2 + half) % 2], PSB[3 + (ri * 2 + half) % 2]
                c0 = v * D + half * 512
                fw.op(pe, lambda e: e.matmul(pr[:, :], lhsT=ones_f[0:1, :], rhs=modrow[0:1, c0:c0 + 512], start=True, stop=True),
                      reads=[B_onesf, B_mr], writes=[Bpr])
                fw.op(act, lambda e: e.activation(out=grow[:, ri, half * 512:(half + 1) * 512], in_=pr[:, :], func=AF.Copy),
                      reads=[Bpr], writes=[B_grow])
    fw.barrier()
    dump("modc", modc[:], [B_modc])
    dump("grow", grow[:, 0:2].rearrange("p a b -> p (a b)"), [B_grow])
    if stop_after == "adaln":
        return finish()

    def mcol(v, ch, who):
        i = (v * 8 + ch) * 2 + who
        return modc[:, i:i + 1]

    def A(q, out, in_, func, reads, writes, **kw):
        return fw.op(q, lambda e: e.activation(out=out, in_=in_, func=func, **kw), reads=reads, writes=writes)

    def TT(q, out, in0, in1, op, reads, writes):
        return fw.op(q, lambda e: e.tensor_tensor(out=out, in0=in0, in1=in1, op=op), reads=reads, writes=writes)

    def TS(q, out, in0, s1, s2, op0, op1, reads, writes, sreads=()):
        return fw.op(q, lambda e: e.tensor_scalar(out=out, in0=in0, scalar1=s1, scalar2=s2, op0=op0, op1=op1), reads=reads, writes=writes, sreads=sreads)

    def STT(out, in0, scalar, in1, op0, op1, reads, writes):
        return fw.op(dve, lambda e: e.scalar_tensor_tensor(out=out, in0=in0, scalar=scalar, in1=in1, op0=op0, op1=op1), reads=reads, writes=writes)

    def CP(q, out, in_, reads, writes):
        return fw.op(q, lambda e: e.tensor_copy(out=out, in_=in_), reads=reads, writes=writes)

    def MM(out, lhsT, rhs, start, stop, reads, writes):
        return fw.op(pe, lambda e: e.matmul(out, lhsT=lhsT, rhs=rhs, start=start, stop=stop), reads=reads, writes=writes)

    def TR(out, in_, reads, writes, n=128):
        return fw.op(pe, lambda e: e.matmul(out, lhsT=in_, rhs=ident[0:n, 0:n], start=True, stop=True, is_transpose=True),
                     reads=list(reads) + [B_ident], writes=writes)

    def rstd_from_ss(out, ss_ps, B_ss, inv_n, tmp, B_tmp, B_out):
        A(act, tmp, ss_ps, AF.Ln, [B_ss, B_eps], [B_tmp], scale=inv_n, bias=epsc[:, 0:1])
        A(act, out, tmp, AF.Exp, [B_tmp], [B_out], scale=-0.5)

    epsc = sb("epsc", [128, 1]); B_eps = Buf("eps")
    fw.op(dve, lambda e: e.memset(epsc[:], EPS), writes=[B_eps])

    att = ExitStack()
    cqT = sb("cqT", [128, 2, NOWN], BF16, stack=att); B_cqT = Buf("cqT")
    rstdq = sb("rstdq", [128, NOWN], stack=att); B_rstdq = Buf("rstdq")
    ckvnT = sb("ckvnT", [128, NKEY], BF16, stack=att); B_ckvnT = Buf("ckvnT")
    KRT = sb("KRT", [128, NKEY], BF16, stack=att); B_KRT = Buf("KRT")

    with ExitStack() as ph:
        w_in = sb("w_in", [128, 8, 928], BF16, stack=ph); B_win = Buf("w_in")
        w_krot = sb("w_krot", [128, 8, 32], BF16, stack=ph); B_wkrot = Buf("w_krot")
        fw.dma(pool, w_in[:], w_in_d.rearrange("(k p) n -> p k n", p=128), writes=[B_win], sem="w_in")
        CP(dve, w_krot[:, :, 16:32], w_in[:, :, 384:400], [B_win], [B_wkrot])
        TS(dve, w_krot[:, :, 0:16], w_in[:, :, 400:416], -1.0, None, ALU.mult, ALU.bypass, [B_win], [B_wkrot])
        xin = [sb("xin%d" % i, [128, D], stack=ph) for i in range(4)]; B_xin = bufs("xin", 4)
        xmT = [sb("xmT%d" % i, [128, 8, 512], BF16, stack=ph) for i in range(2)]; B_xmT = bufs("xmT", 2)
        ropeg = [sb("ropeg%d" % i, [128, 2, 512], stack=ph) for i in range(2)]; B_ropeg = bufs("ropeg", 2)
        u_tm = sb("u_tm", [128, 32, 128], stack=ph); B_utm = Buf("u_tm")
        Ug = sb("Ug", [128, 32, 2, 272], BF16, stack=ph); B_Ug = Buf("Ug")
        sqb = sb("sqb", [128, 512], BF16, stack=ph); B_sqb = Buf("sqb")
        sqq = sb("sqq", [128, 2, 512], BF16, stack=ph); B_sqq = Buf("sqq")
        rawkv = sb("rawkv", [128, 512], stack=ph); B_rawkv = Buf("rawkv")
        lnt = sb("lnt", [128, 512], stack=ph); B_lnt = Buf("lnt")
        rstdkv = sb("rstdkv", [128, 512], stack=ph); B_rstdkv = Buf("rstdkv")
        rt1 = sb("rt1", [128, 512], stack=ph); B_rt1 = Buf("rt1")
        rt2 = sb("rt2", [128, 512], stack=ph); B_rt2 = Buf("rt2")
        xf_v = xf.rearrange("(m j) d -> j m d", j=16)
        ctx_v = ctxf.rearrange("(m j) d -> j m d", j=16)
        gcount = [0]

        def in_group(kind, mt, jg):
            gs = gcount[0] % 2
            gcount[0] += 1
            lat = kind == "lat"
            ncols = 512 if lat else 256
            col0 = (mt * 16 + jg * 4) * 128 if lat else NTOK
            own = lat and mt == 0
            np_ = 128 if lat else 16
            nt = 4 if lat else 16
            tw = 128 if lat else 16
            if lat:
                for t in range(4):
                    j = jg * 4 + t
                    fw.dma(sp, xin[t][:], xf_v[j, mt * 128:(mt + 1) * 128, :], writes=[B_xin[t]], sem="xin%d" % t)
            fw.dma(sp, ropeg[gs][64:96, :, 0:ncols], rope_d[64:96, :, col0:col0 + ncols], writes=[B_ropeg[gs]], sem="ropeg%d" % gs)
            who = 0 if lat else 1
            if lat:
                for ch in range(8):
                    ps, Bp = ps_next()
                    for t in range(4):
                        TR(ps[:, t * 128:(t + 1) * 128], xin[t][:, ch * 128:(ch + 1) * 128], [B_xin[t]], [Bp])
                    A(act, xmT[gs][:, ch, 0:ncols], ps[:, 0:ncols], AF.Identity, [Bp, B_modc], [B_xmT[gs]],
                      scale=mcol(1, ch, who), bias=mcol(0, ch, who))
            else:
                banks = [ps_next() for _ in range(8)]
                for t in range(16):
                    fw.dma(sp, xin[t % 4][0:16, :], ctx_v[t, 0:16, :], writes=[B_xin[t % 4]], sem="xin%d" % (t % 4))
                    for ch in range(8):
                        ps, Bp = banks[ch]
                        TR(ps[:, t * 16:(t + 1) * 16], xin[t % 4][0:16, ch * 128:(ch + 1) * 128], [B_xin[t % 4]], [Bp], n=16)
                for ch in range(8):
                    ps, Bp = banks[ch]
                    A(act, xmT[gs][:, ch, 0:ncols], ps[:, 0:ncols], AF.Identity, [Bp, B_modc], [B_xmT[gs]],
                      scale=mcol(1, ch, who), bias=mcol(0, ch, who))
            X = xmT[gs]; BX = B_xmT[gs]
            kstep = 9
            yield
            ps, Bp = ps_next()
            for ch in range(8):
                MM(ps[:, 0:ncols], w_in[:, ch, 256:384], X[:, ch, 0:ncols], ch == 0, ch == 7, [B_win, BX], [Bp])
            A(act, sqb[:, 0:ncols], ps[:, 0:ncols], AF.Square, [Bp], [B_sqb])
            A(act, rawkv[:, 0:ncols], ps[:, 0:ncols], AF.Copy, [Bp], [B_rawkv])
            ps2, Bp2 = ps_next()
            MM(ps2[:, 0:ncols], ones_bf[:, :], sqb[:, 0:ncols], True, True, [B_ones, B_sqb], [Bp2])
            rstd_from_ss(rstdkv[:, 0:ncols], ps2[:, 0:ncols], Bp2, 1.0 / 128, lnt[:, 0:ncols], B_lnt, B_rstdkv)
            TT(dve, ckvnT[:, col0:col0 + ncols], rawkv[:, 0:ncols], rstdkv[:, 0:ncols], ALU.mult, [B_rawkv, B_rstdkv], [B_ckvnT])
            psa, Bpa = ps_next()
            psb, Bpb = ps_next()
            for ch in range(8):
                MM(psa[64:96, 0:ncols], w_in[:, ch, 384:416], X[:, ch, 0:ncols], ch == 0, ch == 7, [B_win, BX], [Bpa])
            for ch in range(8):
                MM(psb[64:96, 0:ncols], w_krot[:, ch, :], X[:, ch, 0:ncols], ch == 0, ch == 7, [B_wkrot, BX], [Bpb])
            TT(dve, rt1[64:96, 0:ncols], psa[64:96, 0:ncols], ropeg[gs][64:96, 0, 0:ncols], ALU.mult, [Bpa, B_ropeg[gs]], [B_rt1])
            TT(dve, rt2[64:96, 0:ncols], psb[64:96, 0:ncols], ropeg[gs][64:96, 1, 0:ncols], ALU.mult, [Bpb, B_ropeg[gs]], [B_rt2])
            TT(dve, KRT[64:96, col0:col0 + ncols], rt1[64:96, 0:ncols], rt2[64:96, 0:ncols], ALU.add, [B_rt1, B_rt2], [B_KRT])
            if own:
                pss = []
                for fc in range(2):
                    ps, Bp = ps_next()
                    for ch in range(8):
                        MM(ps[:, :], w_in[:, ch, fc * 128:(fc + 1) * 128], X[:, ch, :], ch == 0, ch == 7, [B_win, BX], [Bp])
                    A(act, cqT[:, fc, col0:col0 + 512], ps[:, :], AF.Copy, [Bp], [B_cqT])
                    pss.append((ps, Bp))
                ksub = int(os.environ.get("K_SUB", "9"))
                if ksub >= 1:
                    ps2, Bp2 = ps_next()
                    for fc in range(2):
                        ps, Bp = pss[fc]
                        A(act, sqq[:, fc, :], ps[:, :], AF.Square, [Bp], [B_sqq])
                    for fc in range(2):
                        MM(ps2[:, :], ones_bf[:, :], sqq[:, fc, :], fc == 0, fc == 1, [B_ones, B_sqq], [Bp2])
                if ksub >= 2:
                    rstd_from_ss(rstdq[:, col0:col0 + 512], ps2[:, :], Bp2, 1.0 / 256, lnt[:, :], B_lnt, B_rstdq)
            for t in range(nt):
                ps, Bp = ps_next()
                for ch in range(8):
                    MM(ps[0:np_, :], X[:, ch, t * tw:(t + 1) * tw], w_in[:, ch, 416:928], ch == 0, ch == 7, [B_win, BX], [Bp])
                if lat:
                    jl = (jg * 4 + t) % 8
                    uo = sbap(u_tm, jl * 16, [[128, 32], [1, 16]])
                    ui = sbap(ps, 0, [[16, 32], [1, 16]])
                    if t % 2 == 0:
                        CP(dve, uo, ui, [Bp], [B_utm])
                    else:
                        A(act, uo, ui, AF.Copy, [Bp], [B_utm])
                else:
                    A(act, part_ap(u_tm, 0, 16, (t % 8) * 16, [[128, 32], [1, 16]]), part_ap(ps, 0, 16, 0, [[16, 32], [1, 16]]),
                      AF.Copy, [Bp], [B_utm])
                    if t % 8 == 7:
                        ug_transposes(u_tm, B_utm, 16, t // 8, 256, 0)

        def ug_transposes(src, Bsrc, np_, jt, ucol0, jbase):
            for g4 in range(8):
                ps, Bp = ps_next()
                for gg in range(4):
                    g = g4 * 4 + gg
                    inap = part_ap(src, 0, np_, g * 128, [[1, 128]])
                    TR(ps[:, gg * 128:gg * 128 + np_], inap, [Bsrc], [Bp], n=np_)
                outap = sbap(Ug, (g4 * 4) * 2 * 272 + jt * 272 + ucol0, [[2 * 272, 4], [1, np_]])
                inp_ = sbap(ps, 0, [[128, 4], [1, np_]])
                if g4 % 2 == 0:
                    CP(dve, outap, inp_, [Bp], [B_Ug])
                else:
                    A(act, outap, inp_, AF.Copy, [Bp], [B_Ug])

        glist = [("lat", mt, jg) for mt in range(2) for jg in range(4)] + [("ctx", 0, 0)]
        gens = [in_group(*g_) for g_ in glist]
        next(gens[0])
        for gi_, g_ in enumerate(glist):
            if gi_ + 1 < len(glist):
                next(gens[gi_ + 1])
            for _ in gens[gi_]:
                pass
            if g_[0] == "lat" and g_[2] % 2 == 1:
                ug_transposes(u_tm, B_utm, 128, g_[2] // 2, g_[1] * 128, 0)
        dump("ckvnT", ckvnT[:], [B_ckvnT])
        dump("KRT", KRT[64:96, :], [B_KRT])
        dump("cqT", cqT[:].rearrange("p a b -> p (a b)"), [B_cqT])
        dump("rstdq", rstdq[:], [B_rstdq])
        dump("Ug", Ug[:].rearrange("p a b c -> p (a b c)"), [B_Ug])
        fw.dma(sp, ug_s[:, :], Ug[:].rearrange("p a b c -> p (a b c)"), reads=[B_Ug], writes=[], sem="ugs")
    fw.barrier()
    if stop_after == "inproj":
        return finish()

    attn_tm = sb("attn_tm", [128, 16, 512], stack=att); B_attn = Buf("attn_tm")
    B_attn_s = Buf("attn_s")
    with ExitStack() as ph:
        wq_f = sb("wq_f", [128, 2, 768], stack=ph); B_wqf = Buf("wq_f")
        wkv_f = sb("wkv_f", [128, 1024], stack=ph); B_wkvf = Buf("wkv_f")
        qg = sb("qg", [128, 2], stack=ph); kvg = sb("kvg", [128, 1], stack=ph); B_g = Buf("qkvg")
        w_uq_s = sb("w_uq_s", [128, 2, 768], BF16, stack=ph); B_wuq = Buf("w_uq_s")
        w_uq_rot = sb("w_uq_rot", [128, 2, 8, 32], BF16, stack=ph); B_wuqr = Buf("w_uq_rot")
        w_k = sb("w_k", [128, 1024], BF16, stack=ph); B_wk = Buf("w_k")
        w_v = sb("w_v", [128, 512], BF16, stack=ph); B_wv = Buf("w_v")
        V_all = sb("V_all", [128, 34, 8, 65], BF16, stack=ph); B_V = Buf("V_all")
        KT = [sb("KT%d" % i, [128, NKEY], BF16, stack=ph) for i in range(2)]; B_KT = bufs("KT", 2)
        QT = [sb("QT%d" % i, [128, NOWN], BF16, stack=ph) for i in range(2)]; B_QT = bufs("QT", 2)
        ropeq = sb("ropeq", [128, 2, NOWN], stack=ph); B_ropeq = Buf("ropeq")
        PT = [sb("PT%d" % i, [128, 512], BF16, stack=ph) for i in range(4)]; B_PT = bufs("PT", 4)
        ot = [sb("ot%d" % i, [128, 512], stack=ph) for i in range(2)]; B_ot = bufs("ot", 2)
        qt1 = sb("qt1", [128, 512], stack=ph); B_qt1 = Buf("qt1")
        qt2 = sb("qt2", [128, 512], stack=ph); B_qt2 = Buf("qt2")
        rd = sb("rd", [128, 4], stack=ph); B_rd = Buf("rd")
        fw.dma(sp, wq_f[:], w_uq_d.rearrange("(c p) n -> p c n", p=128), writes=[B_wqf], sem="a0")
        fw.dma(sp, wkv_f[:], w_ukv_d[:, :], writes=[B_wkvf], sem="a1")
        fw.dma(sp, qg[:], qg_d[:, :], writes=[B_g], sem="a2")
        fw.dma(sp, kvg[:], kvg_d[:, :], writes=[B_g], sem="a3")
        fw.dma(sp, ropeq[64:96, :, :], rope_d[64:96, :, 0:NOWN], writes=[B_ropeq], sem="a4")
        for c in range(2):
            TS(dve, w_uq_s[:, c, :], wq_f[:, c, :], qg[:, c:c + 1], None, ALU.mult, ALU.bypass, [B_wqf, B_g], [B_wuq])
            TS(dve, w_uq_rot[:, c, :, 0:16], sbap(w_uq_s, c * 768 + 80, [[96, 8], [1, 16]]), -1.0, None, ALU.mult, ALU.bypass, [B_wuq], [B_wuqr])
            CP(dve, w_uq_rot[:, c, :, 16:32], sbap(w_uq_s, c * 768 + 64, [[96, 8], [1, 16]]), [B_wuq], [B_wuqr])
        TS(dve, w_k[:, :], wkv_f[:, :], kvg[:, 0:1], None, ALU.mult, ALU.bypass, [B_wkvf, B_g], [B_wk])
        CP(dve, w_v[:].rearrange("p (h d) -> p h d", h=8), sbap(w_k, 64, [[128, 8], [1, 64]]), [B_wk], [B_wv])
        fw.op(dve, lambda e: e.memset(V_all[:, :, :, 64:65], 1.0), writes=[B_V])
        for i in range(2):
            fw.op(dve, lambda e: e.memset(KT[i][96:128, :], 0.0), writes=[B_KT[i]])
            fw.op(dve, lambda e: e.memset(QT[i][96:128, :], 0.0), writes=[B_QT[i]])
        for kt in range(34):
            ps, Bp = PS[6 + kt % 2], PSB[6 + kt % 2]
            MM(ps[:, :], ckvnT[:, kt * 128:(kt + 1) * 128], w_v[:, :], True, True, [B_ckvnT, B_wv], [Bp])
            vo = V_all[:, kt, :, 0:64]
            vi = ps[:, :].rearrange("p (h d) -> p h d", h=8)
            if kt % 2 == 0:
                CP(dve, vo, vi, [Bp], [B_V])
            else:
                A(act, vo, vi, AF.Copy, [Bp], [B_V])

        def gen(h):
            s = h % 2
            for ct in range(9):
                n = 512 if ct < 8 else 256
                c0 = ct * 512
                ps, Bp = PS[6], PSB[6]
                MM(ps[0:64, 0:n], w_k[:, h * 128:h * 128 + 64], ckvnT[:, c0:c0 + n], True, True, [B_wk, B_ckvnT], [Bp])
                CP(dve, KT[s][0:64, c0:c0 + n], ps[0:64, 0:n], [Bp], [B_KT[s]])
                yield
            CP(pool, KT[s][64:96, :], KRT[64:96, :], [B_KRT], [B_KT[s]])
            for ct in range(4):
                c0 = ct * 512
                psq, Bq = PS[6], PSB[6]
                psr, Br = PS[7], PSB[7]
                for c in range(2):
                    MM(psq[0:96, :], w_uq_s[:, c, h * 96:(h + 1) * 96], cqT[:, c, c0:c0 + 512], c == 0, c == 1, [B_wuq, B_cqT], [Bq])
                for c in range(2):
                    MM(psr[64:96, :], w_uq_rot[:, c, h, :], cqT[:, c, c0:c0 + 512], c == 0, c == 1, [B_wuqr, B_cqT], [Br])
                TT(dve, QT[s][0:64, c0:c0 + 512], psq[0:64, :], rstdq[0:64, c0:c0 + 512], ALU.mult, [Bq, B_rstdq], [B_QT[s]])
                TT(dve, qt1[64:96, :], psq[64:96, :], ropeq[64:96, 0, c0:c0 + 512], ALU.mult, [Bq, B_ropeq], [B_qt1])
                TT(dve, qt2[64:96, :], psr[64:96, :], ropeq[64:96, 1, c0:c0 + 512], ALU.mult, [Br, B_ropeq], [B_qt2])
                TT(dve, qt1[64:96, :], qt1[64:96, :], qt2[64:96, :], ALU.add, [B_qt1, B_qt2], [B_qt1])
                TT(dve, QT[s][64:96, c0:c0 + 512], qt1[64:96, :], rstdq[64:96, c0:c0 + 512], ALU.mult, [B_qt1, B_rstdq], [B_QT[s]])
                yield

        scount = [0]
        ocount = [0]

        pend = [None]

        def epilogue(h, qt, ob, osl):
            CP(dve, ot[osl][0:65, :], PS[ob][0:65, :], [PSB[ob]], [B_ot[osl]])
            pso, Bo = PS[7], PSB[7]
            for t in range(4):
                TR(pso[:, t * 128:t * 128 + 65], ot[osl][0:65, t * 128:(t + 1) * 128], [B_ot[osl]], [Bo], n=65)
            fw.op(dve, lambda e: e.reciprocal(out=rd[:, :], in_=sbap(pso, 64, [[128, 4]])), reads=[Bo], writes=[B_rd])
            for t in range(4):
                TS(dve, attn_tm[:, qt * 4 + t, h * 64:(h + 1) * 64], pso[:, t * 128:t * 128 + 64], rd[:, t:t + 1], None, ALU.mult, ALU.bypass,
                   [Bo], [B_attn], sreads=[B_rd])

        def attend(h, nxt):
            s = h % 2
            itc = [0]
            for qt in range(4):
                ob = 4 + ocount[0] % 2
                osl = ocount[0] % 2
                ocount[0] += 1
                base = scount[0]
                scount[0] += 34

                def S(kt):
                    bi = (base + kt) % 4
                    MM(PS[bi][:, :], KT[s][:, kt * 128:(kt + 1) * 128], QT[s][:, qt * 512:(qt + 1) * 512], True, True,
                       [B_KT[s], B_QT[s]], [PSB[bi]])
                S(0)
                S(1)
                if pend[0] is not None:
                    epilogue(*pend[0])
                    pend[0] = None
                for kt in range(34):
                    if kt + 2 < 34:
                        S(kt + 2)
                    bi = (base + kt) % 4
                    A(act, PT[bi][:, :], PS[bi][:, :], AF.Exp, [PSB[bi]], [B_PT[bi]], scale=SCALE)
                    MM(PS[ob][0:65, :], V_all[:, kt, h, 0:65], PT[bi][:, :], kt == 0, kt == 33, [B_V, B_PT[bi]], [PSB[ob]])
                    itc[0] += 1
                    if nxt is not None and itc[0] % 9 == 0:
                        next(nxt, None)
                pend[0] = (h, qt, ob, osl)

        nheads = int(os.environ.get("K_HEADS", "8"))
        for _ in gen(0):
            pass
        for h in range(nheads):
            nxt = gen(h + 1) if h + 1 < nheads else None
            attend(h, nxt)
            if nxt is not None:
                for _ in nxt:
                    pass
        epilogue(*pend[0])
        dump("attn", attn_tm[:].rearrange("p a b -> p (a b)"), [B_attn])
        fw.dma(sp, attn_s[:, :], attn_tm[:].rearrange("p a b -> p (a b)"), reads=[B_attn], writes=[B_attn_s], sem="attns")
        dump("KT0", KT[0][0:96, :], [B_KT[0]])
        dump("QT0", QT[0][0:96, :], [B_QT[0]])
        dump("V0", V_all[:].rearrange("p a b c -> p (a b c)"), [B_V])
    fw.barrier()
    att.close()
    if stop_after == "attn":
        return finish()

    PI = math.pi
    B_gs = Buf("g_s")
    with ExitStack() as S:
        sa = sb("ssm_a", [128, 2, 32], stack=S); sl = sb("ssm_l", [128, 32], stack=S)
        sbb = sb("ssm_b", [128, 2, 32, 16], stack=S); scc = sb("ssm_c", [128, 2, 32, 16], stack=S)
        dcols = sb("dcols", [128, 32], stack=S); masks = sb("masks4", [128, 4, 128], stack=S)
        B_par = Buf("ssm_par")
        fw.dma(sp, sa[:], ssm_a_d[:, :, :], writes=[B_par], sem="s0")
        fw.dma(sp, sl[:], ssm_ldt_d[:, :], writes=[B_par], sem="s1")
        fw.dma(sp, sbb[:], ssm_b_d[:, :, :, :], writes=[B_par], sem="s2")
        fw.dma(sp, scc[:], ssm_c_d[:, :, :, :], writes=[B_par], sem="s3")
        fw.dma(sp, dcols[:], ssm_dcols_d[:, :], writes=[B_par], sem="s4")
        fw.dma(sp, masks[:, 0:2, :], masks_d[:, :, :], writes=[B_par], sem="s5")
        fw.dma(sp, masks[:, 2:4, :], masks_d[:, :, :], writes=[B_par], sem="s6")
        w_glu = sb("w_glu", [128, 4, 512], BF16, stack=S); B_wglu = Buf("w_glu")
        fw.dma(pool, w_glu[:], w_glu_d.rearrange("(k p) n -> p k n", p=128), writes=[B_wglu], sem="s7")
        bglu = sb("bglu", [128, 512], stack=S); B_bglu = Buf("bglu")
        fw.dma(sp, bglu[:], b_glu_d[:, :], writes=[B_bglu], sem="s8")
        sm = sb("ssm_sm", [128, 32, 32], stack=S); B_sm = Buf("ssm_sm")
        _smi = [0]

        def smt():
            i = _smi[0]; _smi[0] += 1
            return sm[:, i, :]
        Bbar = sb("Bbar", [128, 2, 32, 16], stack=S)
        PWr = sb("PWr", [128, 17, 32], stack=S); PWi = sb("PWi", [128, 17, 32], stack=S)
        IPr = sb("IPr", [128, 17, 32], stack=S); IPi = sb("IPi", [128, 17, 32], stack=S)
        AA = sb("AA", [128, 2, 2, 16], stack=S); AB = sb("AB", [128, 2, 2, 16], stack=S)
        BS = [B_par, B_sm]

        def tt(out, a, b, op):
            TT(dve, out, a, b, op, BS, [B_sm])

        a_re, a_im = sa[:, 0, :], sa[:, 1, :]
        dt_ = smt(); lre = smt(); ang = smt(); mag = smt()
        A(act, dt_, sl[:, :], AF.Exp, [B_par], [B_sm])
        tt(lre, a_re, dt_, ALU.mult)
        tt(ang, a_im, dt_, ALU.mult)
        A(act, mag, lre, AF.Exp, [B_sm], [B_sm])

        def sin_of(src_ang, shift):
            a2 = smt(); k = smt(); r = smt(); o = smt()
            TS(dve, a2, src_ang, shift, None, ALU.add, ALU.bypass, BS, [B_sm])
            TS(dve, k, a2, PI, None, ALU.is_ge, ALU.bypass, BS, [B_sm])
            for i in range(2, 9):
                STT(k, a2, (2 * i - 1) * PI, k, ALU.is_ge, ALU.add, BS, [B_sm])
            STT(r, k, -2.0 * PI, a2, ALU.mult, ALU.add, BS, [B_sm])
            A(act, o, r, AF.Sin, [B_sm], [B_sm])
            return o
        sn = sin_of(ang, 0.0)
        cs = sin_of(ang, PI / 2)
        abre = smt(); abim = smt()
        tt(abre, mag, cs, ALU.mult)
        tt(abim, mag, sn, ALU.mult)
        t1 = smt(); t2 = smt(); den = smt(); rden = smt(); nre = smt(); cre = smt(); cim = smt()
        tt(t1, a_re, a_re, ALU.mult); tt(t2, a_im, a_im, ALU.mult); tt(den, t1, t2, ALU.add)
        fw.op(dve, lambda e: e.reciprocal(out=rden, in_=den), reads=BS, writes=[B_sm])
        TS(dve, nre, abre, -1.0, None, ALU.add, ALU.bypass, BS, [B_sm])
        tt(t1, nre, a_re, ALU.mult); tt(t2, abim, a_im, ALU.mult); tt(t1, t1, t2, ALU.add); tt(cre, t1, rden, ALU.mult)
        tt(t1, abim, a_re, ALU.mult); tt(t2, nre, a_im, ALU.mult); tt(t1, t1, t2, ALU.subtract); tt(cim, t1, rden, ALU.mult)
        tb = sb("tb", [128, 32, 16], stack=S)

        def bc16(v):
            return AP(v.tensor, v.offset, [list(v.ap[0]), [1, 32], [0, 16]])
        tt(Bbar[:, 0], sbb[:, 0], bc16(cre), ALU.mult); tt(tb[:], sbb[:, 1], bc16(cim), ALU.mult); tt(Bbar[:, 0], Bbar[:, 0], tb[:], ALU.subtract)
        tt(Bbar[:, 1], sbb[:, 1], bc16(cre), ALU.mult); tt(tb[:], sbb[:, 0], bc16(cim), ALU.mult); tt(Bbar[:, 1], Bbar[:, 1], tb[:], ALU.add)
        m2 = smt(); rm2 = smt(); iabre = smt(); iabim = smt()
        tt(t1, abre, abre, ALU.mult); tt(t2, abim, abim, ALU.mult); tt(m2, t1, t2, ALU.add)
        fw.op(dve, lambda e: e.reciprocal(out=rm2, in_=m2), reads=BS, writes=[B_sm])
        tt(iabre, abre, rm2, ALU.mult)
        STT(iabim, abim, -1.0, rm2, ALU.mult, ALU.mult, BS, [B_sm])
        for (Pr, Pi, br, bi) in ((PWr, PWi, abre, abim), (IPr, IPi, iabre, iabim)):
            fw.op(dve, lambda e: e.memset(Pr[:, 0, :], 1.0), writes=[B_sm])
            fw.op(dve, lambda e: e.memset(Pi[:, 0, :], 0.0), writes=[B_sm])
            CP(dve, Pr[:, 1, :], br, BS, [B_sm])
            CP(dve, Pi[:, 1, :], bi, BS, [B_sm])
            for k in range(2, 17):
                tt(t1, Pr[:, k - 1, :], br, ALU.mult); tt(t2, Pi[:, k - 1, :], bi, ALU.mult); tt(Pr[:, k, :], t1, t2, ALU.subtract)
                tt(t1, Pr[:, k - 1, :], bi, ALU.mult); tt(t2, Pi[:, k - 1, :], br, ALU.mult); tt(Pi[:, k, :], t1, t2, ALU.add)
        for d in range(2):
            for c in range(2):
                CP(dve, AA[:, d, c, :], PWr[:, 16, d * 16:(d + 1) * 16], BS, [B_sm])
            TS(dve, AB[:, d, 0, :], PWi[:, 16, d * 16:(d + 1) * 16], -1.0, None, ALU.mult, ALU.bypass, BS, [B_sm])
            CP(dve, AB[:, d, 1, :], PWi[:, 16, d * 16:(d + 1) * 16], BS, [B_sm])
        dump("sm", sm[:].rearrange("p a b -> p (a b)"), BS)
        dump("abar", sbap(PWr, 32, [[1, 32]]), BS)
        dump("abari", sbap(PWi, 32, [[1, 32]]), BS)
        dump("Bbar", Bbar[:].rearrange("p a b c -> p (a b c)"), BS)

        ctmp = [sb("ctmp%d" % i, [128, 4, 256], stack=S) for i in range(2)]; B_ct = Buf("ctmp")

        def cplx_build(q, out_t, P_r, P_i, k0, kstep, Vt, d, neg_im):
            for half in range(4):
                p0 = half * 4

                def Pv(Pt):
                    return sbap(Pt, k0 * 32 + d * 16 + p0, [[1, 4], [kstep * 32, 16], [0, 16]])

                def Vv(c):
                    return sbap(Vt, c * 512 + (d * 16 + p0) * 16, [[16, 4], [0, 16], [1, 16]])

                def Ov(c):
                    return sbap(out_t, p0 * 512 + c * 256, [[512, 4], [16, 16], [1, 16]])
                ta = ctmp[0][:].rearrange("p a (k h) -> p a k h", h=16)
                tb_ = ctmp[1][:].rearrange("p a (k h) -> p a k h", h=16)
                R = BS + [B_ct]
                TT(q, ta, Pv(P_r), Vv(0), ALU.mult, R, [B_ct])
                TT(q, tb_, Pv(P_i), Vv(1), ALU.mult, R, [B_ct])
                TT(q, Ov(0), ta, tb_, ALU.subtract, R, [B_sm])
                TT(q, ta, Pv(P_r), Vv(1), ALU.mult, R, [B_ct])
                TT(q, tb_, Pv(P_i), Vv(0), ALU.mult, R, [B_ct])
                if neg_im:
                    TT(q, ta, ta, tb_, ALU.add, R, [B_ct])
                    TS(q, Ov(1), ta, -1.0, None, ALU.mult, ALU.bypass, R, [B_sm])
                else:
                    TT(q, Ov(1), ta, tb_, ALU.add, R, [B_sm])

        Sbf = sb("Sbf", [128, 2, 2, 16, 128], BF16, stack=S); B_Sbf = Buf("Sbf")
        with ExitStack() as S2:
            Ug = sb("Ug2", [128, 32, 2, 272], BF16, stack=S2); B_Ug2 = Buf("Ug2")
            fw.dma(sp, Ug[:].rearrange("p a b c -> p (a b c)"), ug_s[:, :], writes=[B_Ug2], sem="s9")
            L = sb("L", [128, 2, 2, 16, 272], BF16, stack=S2); B_L = Buf("L")
            with ExitStack() as S2a:
                Wsrc = sb("Wsrc", [128, 16, 2, 256], stack=S2a)
                Wb = sb("Wb", [128, 16, 4, 128], BF16, stack=S2a); B_Wb = Buf("Wb")
                for d in range(2):
                    if d == 0:
                        cplx_build(dve, Wsrc, PWr, PWi, 15, -1, Bbar, 0, False)
                    else:
                        cplx_build(dve, Wsrc, PWr, PWi, 0, 1, Bbar, 1, False)
                    for pt in range(16):
                        ps, Bp = ps_next()
                        for blk in range(4):
                            TR(ps[:, blk * 128:(blk + 1) * 128], sbap(Wsrc, pt * 512 + blk * 128, [[1, 128]]), BS, [Bp])
                        if pt % 2 == 0:
                            A(act, Wb[:, pt, :, :], ps[:, :].rearrange("p (a b) -> p a b", a=4), AF.Copy, [Bp], [B_Wb])
                        else:
                            CP(dve, Wb[:, pt, :, :], ps[:, :].rearrange("p (a b) -> p a b", a=4), [Bp], [B_Wb])
                    for pt in range(16):
                        for c in range(2):
                            ps, Bp = ps_next()
                            if d == 1:
                                ranges = [(0, 272, 0)]
                                ncol = 272
                            else:
                                ranges = [(256, 16, 0), (0, 128, 16)]
                                ncol = 144
                            for (u0, n, o0) in ranges:
                                for gl in range(2):
                                    for jt in range(2):
                                        MM(ps[gl * 64:(gl + 1) * 64, o0:o0 + n], Wb[:, pt, c * 2 + jt, gl * 64:(gl + 1) * 64],
                                           Ug[:, 2 * pt + gl, jt, u0:u0 + n], jt == 0, jt == 1, [B_Wb, B_Ug2], [Bp])
                            lo = L[:, d, c, pt, 272 - ncol:272]
                            if c == 0:
                                A(act, lo, ps[:, 0:ncol], AF.Copy, [Bp], [B_L])
                            else:
                                CP(dve, lo, ps[:, 0:ncol], [Bp], [B_L])
                    if d == 0:
                        dump("Wb0", Wb[:].rearrange("p a b c -> p (a b c)"), [B_Wb])
            dump("L", L[:].rearrange("p a b c e -> p (a b c e)"), [B_L])
            fw.barrier()
            Sh = sb("Sh", [128, 2, 2, 16, NS], stack=S2); B_Sh = Buf("Sh")
            fw.op(pool, lambda e: e.memset(Sh[:], 0.0), writes=[B_Sh])
            tm1 = sb("tm1", [128, 2, 2, 16], stack=S2); tm2 = sb("tm2", [128, 2, 2, 16], stack=S2); B_tm = Buf("tm")

            def f0(st):
                return st - 140 if st >= 142 else st % 2

            def f1(st):
                return 273 - st if st >= 142 else 132 + st % 2
            B_tmh = bufs("tmh", 2); B_Shh = bufs("Shh", 2)
            for b_ in B_Shh:
                b_.w = B_Sh.w
            RSh = [[B_Shh[h_], B_L, B_sm, B_tmh[h_]] for h_ in range(2)]
            for s in range(272):
                both = s >= 128
                ops = []
                NH_ = 2 if WINDOW else 1
                PW_ = 16 // NH_
                for ph_ in range(NH_):
                    po = ph_ * PW_
                    if both:
                        def hv(t_, n_, c0, c1, rev=False):
                            dstep = 32 * n_ + (c1 - c0)
                            if rev:
                                return sbap(t_, c0 + 16 * n_ + po * n_, [[dstep, 2], [-16 * n_, 2], [n_, PW_]])
                            return sbap(t_, c0 + po * n_, [[dstep, 2], [16 * n_, 2], [n_, PW_]])
                        sr = hv(Sh, NS, f0(s), f1(s)); srev = hv(Sh, NS, f0(s), f1(s), True)
                        sw = hv(Sh, NS, f0(s + 1), f1(s + 1))
                        lv = hv(L, 272, s, 271 - s)
                        aa, ab = AA[:, :, :, po:po + PW_], AB[:, :, :, po:po + PW_]
                        x1, x2 = tm1[:, :, :, po:po + PW_], tm2[:, :, :, po:po + PW_]
                    else:
                        def hv(t_, n_, c1, rev=False):
                            if rev:
                                return sbap(t_, 32 * n_ + c1 + 16 * n_ + po * n_, [[-16 * n_, 2], [n_, PW_]])
                            return sbap(t_, 32 * n_ + c1 + po * n_, [[16 * n_, 2], [n_, PW_]])
                        sr = hv(Sh, NS, f1(s)); srev = hv(Sh, NS, f1(s), True); sw = hv(Sh, NS, f1(s + 1))
                        lv = hv(L, 272, 271 - s)
                        aa, ab = AA[:, 1, :, po:po + PW_], AB[:, 1, :, po:po + PW_]
                        x1, x2 = tm1[:, 1, :, po:po + PW_], tm2[:, 1, :, po:po + PW_]
                    ops.append((sr, srev, sw, lv, aa, ab, x1, x2))
                for h_, (sr, srev, sw, lv, aa, ab, x1, x2) in enumerate(ops):
                    TT(dve, x1, sr, aa, ALU.mult, RSh[h_], [B_tmh[h_]])
                for h_, (sr, srev, sw, lv, aa, ab, x1, x2) in enumerate(ops):
                    TT(dve, x2, srev, ab, ALU.mult, RSh[h_], [B_tmh[h_]])
                for h_, (sr, srev, sw, lv, aa, ab, x1, x2) in enumerate(ops):
                    TT(dve, x1, x1, lv, ALU.add, RSh[h_], [B_tmh[h_]])
                for h_, (sr, srev, sw, lv, aa, ab, x1, x2) in enumerate(ops):
                    TT(dve, sw, x1, x2, ALU.add, RSh[h_], [B_Shh[h_]])
            CP(dve, Sbf[:, 0].rearrange("p a b c -> p (a b) c"), sbap(Sh, 4, [[NS, 32], [1, 128]]), B_Shh, [B_Sbf])
            CP(dve, Sbf[:, 1].rearrange("p a b c -> p (a b) c"), sbap(Sh, 32 * NS + 2, [[NS, 32], [1, 128]]), B_Shh, [B_Sbf])
            dump("Sbf", Sbf[:].rearrange("p a b c e -> p (a b c e)"), [B_Sbf])
        fw.barrier()
        if stop_after == "scan":
            S.close()
            return finish()
        with ExitStack() as S3:
            Kmat = sb("Kmat", [128, 32, 4, 128], BF16, stack=S3); B_Km = Buf("Kmat")
            Ca = sb("Ca", [128, 2, 16, 2, 256], BF16, stack=S3); B_Ca = Buf("Ca")
            Ugo = sb("Ugo", [128, 64, 128], BF16, stack=S3); B_Ugo = Buf("Ugo")
            fw.dma(sp, Ugo[:], ug_s.rearrange("p (a c) -> p a c", c=272)[:, :, 0:128], writes=[B_Ugo], sem="s10")
            S3a = ExitStack()
            Xt = sb("Xt", [128, 16, 2, 256], BF16, stack=S3a); Zt = sb("Zt", [128, 16, 2, 256], BF16, stack=S3a)
            tmpK = sb("tmpK", [128, 4, 128], stack=S3a); tmpS = sb("tmpS", [128, 2, 128], stack=S3a); B_tk = Buf("tmpK")
            cplx_build(dve, Ca[:, 0], PWr, PWi, 1, 1, scc, 0, True)
            cplx_build(dve, Ca[:, 1], PWr, PWi, 16, -1, scc, 1, True)
            KS = BS + [B_tk]
            for d in range(2):
                if d == 0:
                    cplx_build(dve, Xt, IPr, IPi, 0, 1, Bbar, 0, False)
                    cplx_build(dve, Zt, PWr, PWi, 0, 1, scc, 0, True)
                else:
                    cplx_build(dve, Xt, PWr, PWi, 0, 1, Bbar, 1, False)
                    cplx_build(dve, Zt, IPr, IPi, 0, 1, scc, 1, True)
                for g in range(32):
                    pt, gl = g // 2, g % 2
                    r0, r1 = gl * 64, gl * 64 + 64
                    psA, BpA = ps_next()

                    def kblock(ps, col, jt, tt_):
                        for c in range(2):
                            MM(ps[:, col * 128:(col + 1) * 128], Xt[r0:r1, pt, c, jt * 128:(jt + 1) * 128],
                               Zt[r0:r1, pt, c, tt_ * 128:(tt_ + 1) * 128], c == 0, c == 1, BS, [BpA])
                    kblock(psA, 0, 0, 0)
                    kblock(psA, 1, 1, 1)
                    if d == 0:
                        kblock(psA, 2, 0, 1)
                    else:
                        kblock(psA, 2, 1, 0)
                    mk = sbap(masks, d * 128, [[0, 2], [1, 128]])
                    kd = sbap(Kmat, g * 512, [[384, 2], [1, 128]])
                    ko_ = Kmat[:, g, 1 if d == 0 else 2, :]
                    if d == 0:
                        TT(dve, tmpS[:], psA[:, 0:256].rearrange("p (a b) -> p a b", a=2), mk, ALU.mult, [BpA, B_par], [B_tk])
                        STT(kd, sbap(ident, 0, [[0, 2], [1, 128]]), dcols[:, g:g + 1], tmpS[:], ALU.mult, ALU.add, [B_ident, B_par, B_tk], [B_Km])
                    else:
                        TT(dve, tmpS[:], psA[:, 0:256].rearrange("p (a b) -> p a b", a=2), mk, ALU.mult, [BpA, B_par], [B_tk])
                        TT(dve, kd, kd, tmpS[:], ALU.add, [B_tk, B_Km], [B_Km])
                    A(act, ko_, psA[:, 256:384], AF.Copy, [BpA], [B_Km])
            dump("Kmat", Kmat[:].rearrange("p a b c -> p (a b c)"), [B_Km])
            dump("Ca", Ca[:].rearrange("p a b c e -> p (a b c e)"), BS)
            fw.barrier()
            S3a.close()
            g_tm = sb("g_tm", [128, 16, 512], stack=S3); B_gtm = Buf("g_tm")
            glsb = [sb("glsb%d" % i, [128, 512], stack=S3) for i in range(2)]; B_gl = bufs("glsb", 2)
            for gp in range(16):
                psY, BpY = ps_next()
                for gi in range(2):
                    g = gp * 2 + gi
                    pt, gl = g // 2, g % 2
                    r0, r1 = gl * 64, gl * 64 + 64
                    for tt_ in range(2):
                        yo = psY[:, (gi * 2 + tt_) * 128:(gi * 2 + tt_ + 1) * 128]
                        first = True
                        for jt in range(2):
                            MM(yo, Kmat[:, g, jt * 2 + tt_, :], Ugo[:, g * 2 + jt, :], first, False, [B_Km, B_Ugo], [BpY])
                            first = False
                        for d in range(2):
                            for c in range(2):
                                MM(yo, Ca[r0:r1, d, pt, c, tt_ * 128:(tt_ + 1) * 128], Sbf[r0:r1, d, c, pt, :], False, (d == 1 and c == 1),
                                   BS + [B_Sbf], [BpY])
                gs_ = gp % 2
                A(act, glsb[gs_][:, :], psY[:, :], AF.Gelu, [BpY], [B_gl[gs_]])
                psT, BpT = ps_next()
                for blk in range(4):
                    TR(psT[:, blk * 128:(blk + 1) * 128], glsb[gs_][:, blk * 128:(blk + 1) * 128], [B_gl[gs_]], [BpT])
                for gi in range(2):
                    g = gp * 2 + gi
                    oo = sbap(g_tm, 16 * g, [[512, 16], [1, 16]])
                    ii = sbap(psT, gi * 256, [[16, 16], [1, 16]])
                    CP(dve, oo, ii, [BpT], [B_gtm])
            dump("gtm", g_tm[:].rearrange("p a b -> p (a b)"), [B_gtm])
            gT = [sb("gT%d" % i, [128, 4, 128], BF16, stack=S3) for i in range(2)]; B_gT = bufs("gT", 2)
            zt = sb("zt", [128, 512], stack=S3); B_zt = Buf("zt")
            for t in range(16):
                gs_ = t % 2
                ps, Bp = ps_next()
                for fc in range(4):
                    TR(ps[:, fc * 128:(fc + 1) * 128], g_tm[:, t, fc * 128:(fc + 1) * 128], [B_gtm], [Bp])
                A(act, gT[gs_][:], ps[:, :].rearrange("p (a b) -> p a b", a=4), AF.Copy, [Bp], [B_gT[gs_]])
                ps2, Bp2 = ps_next()
                for fc in range(4):
                    MM(ps2[:, :], gT[gs_][:, fc, :], w_glu[:, fc, :], fc == 0, fc == 3, [B_gT[gs_], B_wglu], [Bp2])
                TT(dve, zt[:], ps2[:, :], bglu[:], ALU.add, [Bp2, B_bglu], [B_zt])
                A(act, zt[:], zt[:], AF.Sigmoid, [B_zt], [B_zt])
                TT(dve, g_tm[:, t, :], g_tm[:, t, :], zt[:], ALU.mult, [B_gtm, B_zt], [B_gtm])
            dump("ssm", g_tm[:].rearrange("p a b -> p (a b)"), [B_gtm])
            fw.dma(sp, g_s[:, :], g_tm[:].rearrange("p a b -> p (a b)"), reads=[B_gtm], writes=[B_gs], sem="gs")
    fw.barrier()
    if stop_after == "ssm":
        return finish()

    hT = sb("hT", [128, 8, NOWN], BF16); B_hT = Buf("hT")
    combT = sb("combT", [32, NOWN], BF16); B_combT = Buf("combT")
    lnbox = [None, None]
    B_x1s = Buf("x1_s")

    def layer_norm(pre, B_pre, gi, out, B_out, st, mv, sc_, B_st):
        for hf in range(2):
            fw.op(dve, lambda e: e.bn_stats(out=st[:, hf * 6:(hf + 1) * 6], in_=pre[:, hf * 512:(hf + 1) * 512]), reads=[B_pre], writes=[B_st])
        fw.op(dve, lambda e: e.bn_aggr(out=mv[:, 0:2], in_=st[:, 0:12]), reads=[B_st], writes=[B_st])
        A(act, sc_[:, 0:1], mv[:, 1:2], AF.Ln, [B_st, B_eps], [B_st], bias=epsc[:, 0:1])
        A(act, sc_[:, 1:2], sc_[:, 0:1], AF.Exp, [B_st], [B_st], scale=-0.5)
        TS(dve, out, pre, mv[:, 0:1], sc_[:, 1:2], ALU.subtract, ALU.mult, [B_pre], [B_out], sreads=[B_st])
        lnrows, B_ln = lnbox
        TT(dve, out, out, lnrows[:, 0, :], ALU.mult, [B_out, B_ln], [B_out])
        TT(dve, out, out, lnrows[:, 1, :], ALU.add, [B_out, B_ln], [B_out])

    with ExitStack() as M:
        ln1 = sb("ln1rows", [128, 2, D], stack=M); lnbox[0] = ln1; lnbox[1] = Buf("ln1")
        fw.dma(sp, ln1[:], ln_d[:, 0:2, :], writes=[lnbox[1]], sem="m0")
        w_o_s = sb("w_o_s", [128, 8, D], BF16, stack=M); B_wo = Buf("w_o_s")
        wof = [sb("wof%d" % i, [128, D], stack=M) for i in range(2)]; B_wof = bufs("wof", 2)
        gn = sb("gn", [128, 8], stack=M); B_gn = Buf("gn")
        w_r = sb("w_r", [128, 8, 36], stack=M); b_r = sb("b_r", [128, 36], stack=M); B_wr = Buf("w_r")
        fw.dma(sp, gn[:], gn_d[:, :], writes=[B_gn], sem="m1")
        fw.dma(sp, w_r[:], w_r_d.rearrange("(k p) n -> p k n", p=128), writes=[B_wr], sem="m2")
        fw.dma(sp, b_r[:], b_r_d[:, :], writes=[B_wr], sem="m3")
        for fc in range(8):
            fw.dma(sp, wof[fc % 2][:], w_o_d[fc * 128:(fc + 1) * 128, :], writes=[B_wof[fc % 2]], sem="wof%d" % (fc % 2))
            TS(dve, w_o_s[:, fc, :], wof[fc % 2][:], gn[:, fc:fc + 1], None, ALU.mult, ALU.bypass, [B_wof[fc % 2], B_gn], [B_wo])
        cat = [sb("cat%d" % i, [128, D], stack=M) for i in range(2)]; B_cat = bufs("cat", 2)
        xo = [sb("xo%d" % i, [128, D], stack=M) for i in range(2)]; B_xo = bufs("xo", 2)
        catT = [sb("catT%d" % i, [128, 8, 128], BF16, stack=M) for i in range(2)]; B_catT = bufs("catT", 2)
        pre = [sb("pre%d" % i, [128, D], stack=M) for i in range(2)]; B_pre = bufs("pre", 2)
        hTf = [sb("hTf%d" % i, [128, 8, 128], stack=M) for i in range(2)]; B_hTf = bufs("hTf", 2)
        htm = [sb("htm%d" % i, [128, D], stack=M) for i in range(2)]; B_htm = bufs("htm", 2)
        junk = sb("junk", [128, 512], stack=M); B_junk = Buf("junk")
        st = sb("st", [128, 12], stack=M); mv = sb("mv", [128, 2], stack=M); sc_ = sb("sc_", [128, 8], stack=M); B_st = Buf("st")
        rt = sb("rt", [128, 160], stack=M); B_rt = Buf("rt")
        scA = sb("scA", [128, 8], stack=M); B_stA = Buf("stA")
        comb = sb("comb", [128, 32], stack=M); B_comb = Buf("comb")
        mixps = [[], []]

        def stageA(t):
            s = t % 2
            fw.dma(sp, cat[s][:, 0:512], attn_s[:, t * 512:(t + 1) * 512], reads=[B_attn_s], writes=[B_cat[s]], sem="cat%d" % s)
            fw.dma(sp, cat[s][:, 512:1024], g_s[:, t * 512:(t + 1) * 512], reads=[B_gs], writes=[B_cat[s]], sem="cat%d" % s)
            fw.group_done("cat%d" % s, [B_cat[s]])
            fw.dma(sp, xo[s][:], xf_v[t, 0:128, :], writes=[B_xo[s]], sem="xo%d" % s)
            for hf in range(2):
                A(act, junk[:], cat[s][:, hf * 512:(hf + 1) * 512], AF.Square, [B_cat[s]], [B_junk, B_stA], accum_out=scA[:, 2 + hf:3 + hf])
                A(act, scA[:, 4 + hf:5 + hf], scA[:, 2 + hf:3 + hf], AF.Ln, [B_stA, B_eps], [B_stA], scale=1.0 / 512, bias=epsc[:, 0:1])
                A(act, scA[:, 6 + hf:7 + hf], scA[:, 4 + hf:5 + hf], AF.Exp, [B_stA], [B_stA], scale=-0.5)
                A(act, cat[s][:, hf * 512:(hf + 1) * 512], cat[s][:, hf * 512:(hf + 1) * 512], AF.Copy, [B_cat[s], B_stA], [B_cat[s]],
                  scale=scA[:, 6 + hf:7 + hf])
            for hf in range(2):
                ps, Bp = ps_next()
                for q4 in range(4):
                    fc = hf * 4 + q4
                    TR(ps[:, q4 * 128:(q4 + 1) * 128], cat[s][:, fc * 128:(fc + 1) * 128], [B_cat[s]], [Bp])
                A(act, catT[s][:, hf * 4:(hf + 1) * 4, :], ps[:, :].rearrange("p (a b) -> p a b", a=4), AF.Copy, [Bp], [B_catT[s]])
            for hf in range(2):
                ps, Bp = ps_next()
                for fc in range(8):
                    MM(ps[:, :], catT[s][:, fc, :], w_o_s[:, fc, hf * 512:(hf + 1) * 512], fc == 0, fc == 7, [B_catT[s], B_wo], [Bp])
                mixps[t % 2].append((ps, Bp))

        def stageA_pre(t):
            s = t % 2
            for hf in range(2):
                ps, Bp = mixps[t % 2][hf]
                TT(dve, pre[s][:, hf * 512:(hf + 1) * 512], ps[:, :], grow[:, 0, hf * 512:(hf + 1) * 512], ALU.mult, [Bp, B_grow], [B_pre[s]])
            mixps[t % 2].clear()
            STT(pre[s][:], xo[s][:], ALPHA, pre[s][:], ALU.mult, ALU.add, [B_xo[s], B_pre[s]], [B_pre[s]])

        def stageB(t):
            s = t % 2
            layer_norm(pre[s][:], B_pre[s], 0, pre[s][:], B_pre[s], st, mv, sc_, B_st)
            fw.dma(sp, x1_s[:, t * D:(t + 1) * D], pre[s][:], reads=[B_pre[s]], writes=[B_x1s], sem="x1s")
            TT(dve, htm[s][:], pre[s][:], grow[:, 2, :], ALU.mult, [B_pre[s], B_grow], [B_htm[s]])
            TT(dve, htm[s][:], htm[s][:], grow[:, 1, :], ALU.add, [B_htm[s], B_grow], [B_htm[s]])
            for hf in range(2):
                ps, Bp = ps_next()
                for q4 in range(4):
                    fc = hf * 4 + q4
                    TR(ps[:, q4 * 128:(q4 + 1) * 128], htm[s][:, fc * 128:(fc + 1) * 128], [B_htm[s]], [Bp])
                A(act, hT[:, hf * 4:(hf + 1) * 4, t * 128:(t + 1) * 128], ps[:, :].rearrange("p (a b) -> p a b", a=4), AF.Copy, [Bp], [B_hT])
                A(act, hTf[s][:, hf * 4:(hf + 1) * 4, :], ps[:, :].rearrange("p (a b) -> p a b", a=4), AF.Copy, [Bp], [B_hTf[s]])

        rps = [None]

        def stageC_mm(t):
            s = t % 2
            ps, Bp = ps_next()
            for fc in range(8):
                MM(ps[:, 0:36], hTf[s][:, fc, :], w_r[:, fc, :], fc == 0, fc == 7, [B_hTf[s], B_wr], [Bp])
            rps[0] = (ps, Bp)

        def stageC(t):
            s = t % 2
            ps, Bp = rps[0]
            lg = rt[:, 0:36]
            gsel = rt[:, 36:40]; gmax = rt[:, 40:41]; ngmax = rt[:, 41:42]; gsum = rt[:, 42:43]; g_w = rt[:, 43:44]
            esel = rt[:, 44:52]; m1 = rt[:, 52:53]; mask1 = rt[:, 56:64]; esel2 = rt[:, 64:72]; m2 = rt[:, 53:54]; mask2 = rt[:, 72:80]
            nm1 = rt[:, 54:55]; e2 = rt[:, 55:56]; den_ = rt[:, 80:81]; w1 = rt[:, 81:82]; w2 = rt[:, 82:83]; cw8 = rt[:, 88:96]; gex = rt[:, 96:100]
            RR = [B_rt]
            TT(dve, lg, ps[:, 0:36], b_r[:, :], ALU.add, [Bp, B_wr], RR)
            fw.op(dve, lambda e: e.reduce_max(out=gmax, in_=rt[:, 0:4], axis=AX.X), reads=RR, writes=RR)
            TS(dve, gsel, rt[:, 0:4], gmax, None, ALU.is_ge, ALU.bypass, RR, RR, sreads=RR)
            TS(dve, ngmax, gmax, -1.0, None, ALU.mult, ALU.bypass, RR, RR)
            A(act, gex, rt[:, 0:4], AF.Exp, RR, RR, bias=ngmax, accum_out=gsum)
            fw.op(dve, lambda e: e.reciprocal(out=g_w, in_=gsum), reads=RR, writes=RR)
            TS(dve, esel, rt[:, 4:12], gsel[:, 0:1], None, ALU.mult, ALU.bypass, RR, RR, sreads=RR)
            for g in range(1, 4):
                fw.op(dve, lambda e: e.scalar_tensor_tensor(out=esel, in0=rt[:, 4 + 8 * g:12 + 8 * g], scalar=gsel[:, g:g + 1], in1=esel,
                                                            op0=ALU.mult, op1=ALU.add), reads=RR, writes=RR, sreads=RR)
            fw.op(dve, lambda e: e.reduce_max(out=m1, in_=esel, axis=AX.X), reads=RR, writes=RR)
            TS(dve, mask1, esel, m1, None, ALU.is_ge, ALU.bypass, RR, RR, sreads=RR)
            STT(esel2, mask1, -1e30, esel, ALU.mult, ALU.add, RR, RR)
            fw.op(dve, lambda e: e.reduce_max(out=m2, in_=esel2, axis=AX.X), reads=RR, writes=RR)
            TS(dve, mask2, esel2, m2, None, ALU.is_ge, ALU.bypass, RR, RR, sreads=RR)
            TS(dve, nm1, m1, -1.0, None, ALU.mult, ALU.bypass, RR, RR)
            A(act, e2, m2, AF.Exp, RR, RR, bias=nm1)
            TS(dve, den_, e2, 1.0, None, ALU.add, ALU.bypass, RR, RR)
            fw.op(dve, lambda e: e.reciprocal(out=w1, in_=den_), reads=RR, writes=RR)
            TT(dve, w2, e2, w1, ALU.mult, RR, RR)
            TT(dve, w1, w1, g_w, ALU.mult, RR, RR)
            TT(dve, w2, w2, g_w, ALU.mult, RR, RR)
            TS(dve, cw8, mask1, w1, None, ALU.mult, ALU.bypass, RR, RR, sreads=RR)
            fw.op(dve, lambda e: e.scalar_tensor_tensor(out=cw8, in0=mask2, scalar=w2, in1=cw8, op0=ALU.mult, op1=ALU.add),
                  reads=RR, writes=RR, sreads=RR)
            for g in range(4):
                TS(dve, comb[:, g * 8:(g + 1) * 8], cw8, gsel[:, g:g + 1], None, ALU.mult, ALU.bypass, RR, [B_comb], sreads=RR)

        def stageC_tail(t):
            s = t % 2
            ps, Bp = ps_next()
            TR(ps[0:32, 0:128], comb[:, :], [B_comb], [Bp])
            CP(dve, combT[:, t * 128:(t + 1) * 128], ps[0:32, 0:128], [Bp], [B_combT])
            if t == 0:
                dump("x1_0", pre[s][:], [B_pre[s]])
                dump("comb0", comb[:], [B_comb])

        stageA(0)
        stageA_pre(0)
        for t in range(17):
            if t >= 1:
                stageC_mm(t - 1)
            if t + 1 < 16:
                stageA(t + 1)
            if t < 16:
                stageB(t)
            if t >= 1:
                stageC(t - 1)
            if t + 1 < 16:
                stageA_pre(t + 1)
            if t >= 1:
                stageC_tail(t - 1)
    fw.barrier()
    if stop_after == "merge":
        return finish()

    with ExitStack() as E:
        ln2 = sb("ln2rows", [128, 2, D], stack=E); lnbox[0] = ln2; lnbox[1] = Buf("ln2")
        fw.dma(sp, ln2[:], ln_d[:, 2:4, :], writes=[lnbox[1]], sem="e9")
        acc = sb("acc", [128, 16, D], stack=E); B_acc = bufs("acc", 16)
        sel = sb("sel", [32, NE * 128], BF16, stack=E); B_sel = Buf("sel")
        fw.dma(pool, sel[:], sel_d[:, :], writes=[B_sel], sem="e0")
        NSLOT = 3
        Wg = [sb("Wg%d" % i, [128, 8, 256], BF16, stack=E) for i in range(NSLOT)]
        Wu = [sb("Wu%d" % i, [128, 8, 256], BF16, stack=E) for i in range(NSLOT)]
        Wd = [sb("Wd%d" % i, [128, 2, D], BF16, stack=E) for i in range(NSLOT)]
        B_W = bufs("Wexp", NSLOT)
        sg = [sb("sg%d" % i, [128, 512], stack=E) for i in range(2)]; B_sg = bufs("sg", 2)
        tu = [sb("tu%d" % i, [128, 512], stack=E) for i in range(2)]; B_tu = bufs("tu", 2)
        gT2 = [sb("gT2_%d" % i, [128, 2, 256], BF16, stack=E) for i in range(2)]; B_gT2 = bufs("gT2", 2)
        n_exp = int(os.environ.get("K_NEXP", str(NE)))

        stg = [sb("stg%d" % i, [128, 2048], stack=E) for i in range(2)]; B_stg = bufs("stg", 2)
        stc = [0]

        def load_expert(e):
            sl_ = e % NSLOT
            for (src_, dst, kk) in ((wg_d[e], Wg[sl_], 8), (wu_d[e], Wu[sl_], 8), (wd_d[e], Wd[sl_], 2)):
                k_ = stc[0] % 2
                stc[0] += 1
                fw.dma(sp, stg[k_][:].rearrange("p (k n) -> p k n", k=kk), src_.rearrange("(k p) n -> p k n", p=128),
                       writes=[B_stg[k_]], sem="stg%d" % k_)
                CP(pool, dst[:].rearrange("p k n -> p (k n)"), stg[k_][:], [B_stg[k_]], [B_W[sl_]])

        for e in range(min(NSLOT, n_exp)):
            load_expert(e)
        items = [(e, tg) for e in range(n_exp) for tg in range(8)]
        dcnt = [0]

        csb = [sb("csb%d" % i, [128, 256], BF16, stack=E) for i in range(2)]; B_csb = bufs("csb", 2)

        def AUC(i):
            e, tg = items[i]
            sl_ = e % NSLOT
            k2 = i % 2
            pa, Bpa = PS[k2 * 2], PSB[k2 * 2]
            pu, Bpu = PS[k2 * 2 + 1], PSB[k2 * 2 + 1]
            pc_, Bpc = PS[4], PSB[4]
            MM(pc_[:, 0:256], sel[0:32, e * 128:(e + 1) * 128], combT[0:32, tg * 256:(tg + 1) * 256], True, True, [B_sel, B_combT], [Bpc])
            for fc in range(2):
                for ch in range(8):
                    MM(pa[:, fc * 256:(fc + 1) * 256], Wg[sl_][:, ch, fc * 128:(fc + 1) * 128], hT[:, ch, tg * 256:(tg + 1) * 256],
                       ch == 0, ch == 7, [B_W[sl_], B_hT], [Bpa])
            for fc in range(2):
                for ch in range(8):
                    MM(pu[:, fc * 256:(fc + 1) * 256], Wu[sl_][:, ch, fc * 128:(fc + 1) * 128], hT[:, ch, tg * 256:(tg + 1) * 256],
                       ch == 0, ch == 7, [B_W[sl_], B_hT], [Bpu])

        def REST(i):
            e, tg = items[i]
            sl_ = e % NSLOT
            k2 = i % 2
            pa, Bpa = PS[k2 * 2], PSB[k2 * 2]
            pu, Bpu = PS[k2 * 2 + 1], PSB[k2 * 2 + 1]
            A(act, sg[k2][:, :], pa[:, :], AF.Silu, [Bpa], [B_sg[k2]])
            if i + 1 < len(items):
                A(act, csb[1 - k2][:, :], PS[4][:, 0:256], AF.Copy, [PSB[4]], [B_csb[1 - k2]])
            TT(dve, tu[k2][:, :], pu[:, :], sg[k2][:, :], ALU.mult, [Bpu, B_sg[k2]], [B_tu[k2]])
            TT(dve, gT2[k2][:], tu[k2][:, :].rearrange("p (a b) -> p a b", a=2), sbap(csb[k2], 0, [[0, 2], [1, 256]]), ALU.mult,
               [B_tu[k2], B_csb[k2]], [B_gT2[k2]])
            for tt_ in range(2):
                tile_ = tg * 2 + tt_
                for hf in range(2):
                    po, Bpo = PS[5 + dcnt[0] % 3], PSB[5 + dcnt[0] % 3]
                    dcnt[0] += 1
                    for fc in range(2):
                        MM(po[:, :], gT2[k2][:, fc, tt_ * 128:(tt_ + 1) * 128], Wd[sl_][:, fc, hf * 512:(hf + 1) * 512],
                           fc == 0, fc == 1, [B_gT2[k2], B_W[sl_]], [Bpo])
                    ao = acc[:, tile_, hf * 512:(hf + 1) * 512]
                    if e == 0:
                        CP(dve, ao, po[:, :], [Bpo], [B_acc[tile_]])
                    else:
                        TT(dve, ao, po[:, :], ao, ALU.add, [Bpo, B_acc[tile_]], [B_acc[tile_]])
            if tg == 7 and e + NSLOT < n_exp:
                load_expert(e + NSLOT)

        AUC(0)
        A(act, csb[0][:, :], PS[4][:, 0:256], AF.Copy, [PSB[4]], [B_csb[0]])
        for i in range(len(items)):
            if i + 1 < len(items):
                AUC(i + 1)
            REST(i)
        dump("acc0", acc[:, 0, :], [B_acc[0]])
        x1t = [stg[i][:, 0:D] for i in range(2)]; B_x1t = B_stg
        st2 = sb("st2", [128, 12], stack=E); mv2 = sb("mv2", [128, 2], stack=E); sc2 = sb("sc2", [128, 8], stack=E); B_st2 = Buf("st2")
        B_out = Buf("out")
        st2s = [sb("st2_%d" % i, [128, 12], stack=E) for i in range(2)]; mv2s = [sb("mv2_%d" % i, [128, 2], stack=E) for i in range(2)]
        sc2s = [sb("sc2_%d" % i, [128, 8], stack=E) for i in range(2)]; B_st2s = bufs("st2s", 2)

        def F1(t):
            s = t % 2
            fw.dma(sp, x1t[s], x1_s[:, t * D:(t + 1) * D], reads=[B_x1s], writes=[B_x1t[s]], sem="stg%d" % s)
            TT(dve, acc[:, t, :], acc[:, t, :], grow[:, 3, :], ALU.mult, [B_acc[t], B_grow], [B_acc[t]])
            STT(acc[:, t, :], x1t[s], ALPHA, acc[:, t, :], ALU.mult, ALU.add, [B_x1t[s], B_acc[t]], [B_acc[t]])
            pre_ = acc[:, t, :]
            for hf in range(2):
                fw.op(dve, lambda e: e.bn_stats(out=st2s[s][:, hf * 6:(hf + 1) * 6], in_=pre_[:, hf * 512:(hf + 1) * 512]), reads=[B_acc[t]], writes=[B_st2s[s]])
            fw.op(dve, lambda e: e.bn_aggr(out=mv2s[s][:, 0:2], in_=st2s[s][:, 0:12]), reads=[B_st2s[s]], writes=[B_st2s[s]])
            A(act, sc2s[s][:, 0:1], mv2s[s][:, 1:2], AF.Ln, [B_st2s[s], B_eps], [B_st2s[s]], bias=epsc[:, 0:1])
            A(act, sc2s[s][:, 1:2], sc2s[s][:, 0:1], AF.Exp, [B_st2s[s]], [B_st2s[s]], scale=-0.5)

        def F2(t):
            s = t % 2
            o_ = acc[:, t, :]
            lnrows, B_ln = lnbox
            TS(dve, o_, o_, mv2s[s][:, 0:1], sc2s[s][:, 1:2], ALU.subtract, ALU.mult, [B_acc[t]], [B_acc[t]], sreads=[B_st2s[s]])
            TT(dve, o_, o_, lnrows[:, 0, :], ALU.mult, [B_acc[t], B_ln], [B_acc[t]])
            TT(dve, o_, o_, lnrows[:, 1, :], ALU.add, [B_acc[t], B_ln], [B_acc[t]])
            fw.dma(sp, out_d[t * 128:(t + 1) * 128, :], o_, reads=[B_acc[t]], writes=[B_out], sem="out")
        F1(0)
        for t in range(16):
            if t + 1 < 16:
                F1(t + 1)
            F2(t)
    return finish()


def _rope_tables(frame_tok):
    t = np.asarray(frame_tok)
    row = (t // 64).astype(np.float32)
    col = (t % 64).astype(np.float32)
    inv = (10000.0 ** (-np.arange(0, 16, 2, dtype=np.float32) / np.float32(16))).astype(np.float32)
    ang = np.concatenate([row[:, None] * inv, col[:, None] * inv], -1)
    ang = np.concatenate([ang, ang], -1).astype(np.float32)
    return np.cos(ang).astype(np.float32), np.sin(ang).astype(np.float32)


def prep_core(inp, b, hh):
    f32 = np.float32
    m = {}
    x = inp["x"][b]
    ctx = inp["ctx"][b]
    if hh == 1:
        x = x[::-1]
        ctx = ctx[::-1]
    m["xf"] = np.ascontiguousarray(x, f32)
    m["ctxf"] = np.ascontiguousarray(ctx, f32)
    cT = np.stack([inp["c"][b].reshape(8, 128).T, inp["c_ctx"].reshape(8, 128).T], -1)
    m["cT"] = np.ascontiguousarray(cT, f32)
    m["w_ada"] = inp["w_ada"][0]
    ba = inp["b_ada"][0]
    m["b_ada_2rows"] = np.ascontiguousarray(np.stack([ba, ba], 0), f32)
    m["w_in"] = inp["w_in"][0]
    m["qg_cols"] = np.ascontiguousarray(inp["q_norm_g"][0].reshape(2, 128).T, f32)
    m["kvg_col"] = np.ascontiguousarray(inp["kv_norm_g"][0].reshape(1, 128).T, f32)
    m["w_uq"] = inp["w_uq"][0]
    m["w_ukv"] = inp["w_ukv"][0]
    mt, j, i = np.meshgrid(np.arange(2), np.arange(16), np.arange(128), indexing="ij")
    tau = (16 * (128 * mt + i) + j).reshape(-1)
    tok = tau if hh == 0 else (NTOK - 1 - tau)
    cos, sin = _rope_tables(tok)
    rope = np.zeros((128, 2, NKEY), f32)
    rope[64:96, 0, :NTOK] = cos.T
    rope[64:96, 1, :NTOK] = sin.T
    rope[64:96, 0, NTOK:] = 1.0
    m["rope"] = rope
    dirs = [0, 1] if hh == 0 else [1, 0]

    def lay(a):
        a = a[dirs]
        sh = a.shape[3:]
        a = a.reshape(2, 16, 2, 64, *sh)
        a = np.moveaxis(a, (2, 3), (0, 1))
        return a.reshape(128, 32, *sh)

    m["ssm_a"] = np.ascontiguousarray(np.stack([lay(inp["ssm_a_re"][0]), lay(inp["ssm_a_im"][0])], 1), f32)
    ldt = np.broadcast_to(inp["ssm_log_dt"][0][:, :, None], (2, 32, 64))
    m["ssm_ldt"] = np.ascontiguousarray(lay(ldt), f32)
    m["ssm_b"] = np.ascontiguousarray(np.stack([lay(inp["ssm_b_re"][0]), lay(inp["ssm_b_im"][0])], 1), f32)
    cre = np.swapaxes(inp["ssm_c_re"][0], 2, 3)
    cim = np.swapaxes(inp["ssm_c_im"][0], 2, 3)
    m["ssm_c"] = np.ascontiguousarray(np.stack([lay(cre), lay(cim)], 1), f32)
    dd = inp["ssm_d"][0].reshape(32, 16)
    m["ssm_dcols"] = np.ascontiguousarray(np.broadcast_to(dd.T[None], (8, 16, 32)).reshape(128, 32), f32)
    jl = np.arange(128) // 16
    m0 = (jl[None, :] >= jl[:, None]).astype(f32)
    m1 = (jl[:, None] >= jl[None, :]).astype(f32)
    m["masks"] = np.ascontiguousarray(np.stack([m0, m1], 1), f32)
    m["ident"] = np.eye(128, dtype=f32)
    m["w_glu"] = inp["w_glu"][0]
    m["b_glu_row"] = np.ascontiguousarray(np.broadcast_to(inp["b_glu"][0][None], (128, 512)), f32)
    gn = np.concatenate([inp["gn_attn_g"][0], inp["gn_ssm_g"][0]])
    m["gn_cols"] = np.ascontiguousarray(gn.reshape(8, 128).T, f32)
    m["w_o"] = inp["w_o"][0]
    ln = np.stack([inp["ln1_g"][0], inp["ln1_b"][0], inp["ln2_g"][0], inp["ln2_b"][0]], 0)
    m["ln_rows"] = np.ascontiguousarray(np.broadcast_to(ln[None], (128, 4, D)), f32)
    m["w_r"] = np.ascontiguousarray(np.concatenate([inp["w_router_group"][0], inp["w_router_expert"][0]], 1), f32)
    br_ = np.concatenate([inp["b_router_group"][0], inp["b_router_expert"][0]])
    m["b_r_row"] = np.ascontiguousarray(np.broadcast_to(br_[None], (128, 36)), f32)
    m["w_exp_gate"] = inp["w_exp_gate"][0]
    m["w_exp_up"] = inp["w_exp_up"][0]
    m["w_exp_down"] = inp["w_exp_down"][0]
    sel = np.zeros((32, NE, 128), f32)
    sel[np.arange(32), np.arange(32), :] = 1.0
    m["sel"] = sel.reshape(32, NE * 128)
    return m


def run(inputs, stop_after=None, dbg_names=(), cores=8):
    inp = {k: np.asarray(v) for k, v in inputs.items()}
    nc = build(stop_after=stop_after, dbg_names=dbg_names)
    in_maps = [prep_core(inp, c // 2, c % 2) for c in range(cores)]
    res = run_bass_kernel_spmd(nc, in_maps, core_ids=list(range(cores)))
    return res.results


def kernel(**inputs):
    res = run(inputs)
    out = np.zeros((4, NTOK, D), np.float32)
    for c in range(8):
        b, hh = c // 2, c % 2
        o = res[c]["out"]
        o = o.reshape(16, 128, D).transpose(1, 0, 2).reshape(NOWN, D)
        if hh == 0:
            out[b, :NOWN] = o
        else:
            out[b, NOWN:] = o[::-1]
    return out
```

```python
import math
import os
from contextlib import ExitStack
import numpy as np
import concourse.bass as bass
import concourse.mybir as mybir
from concourse.ap import AP
from concourse.bass_utils import run_bass_kernel_spmd

F32 = mybir.dt.float32
BF16 = mybir.dt.bfloat16
AF = mybir.ActivationFunctionType
ALU = mybir.AluOpType
AX = mybir.AxisListType

D = 1024
NTOK = 4096
NOWN = 2048
NCTX = 256
NKEY = NTOK + NCTX
NH = 8
SCALE = 96 ** -0.5
ALPHA = 2.0 ** 0.25
EPS = 1e-6
NS = 134
WINDOW = False
NE = 32


class Buf:
    __slots__ = ("name", "w", "r", "excl")

    def __init__(self, name, excl=False):
        self.name = name
        self.w = None
        self.r = []
        self.excl = excl


def bufs(name, n):
    return [Buf("%s%d" % (name, i)) for i in range(n)]


class Q:
    def __init__(self, fw, name, eng, same=False):
        self.name = name
        self.eng = eng
        self.sem = fw.es.enter_context(fw.nc.semaphore("s_" + name))
        self.cnt = 0
        self.seen = {}
        self.same = same
        self.window = False


class DmaTok:
    def __init__(self, fw, name):
        self.sem = fw.es.enter_context(fw.nc.semaphore("d_" + str(name)))
        self.cnt = 0
        self.name = name
        self.same = True


class FW:
    def __init__(self, nc, es):
        self.nc = nc
        self.es = es
        self.pe = Q(self, "pe", nc.tensor)
        self.dve = Q(self, "dve", nc.vector, same=True)
        self.act = Q(self, "act", nc.scalar, same=True)
        self.dve.window = WINDOW
        self.pool = Q(self, "pool", nc.gpsimd, same=True)
        self.sp = Q(self, "sp", nc.sync)
        self.dmasems = {}

    def _waits(self, q, reads, writes, sreads=()):
        need = {}

        def add(dep, force=False):
            if dep is None:
                return
            dq, n = dep
            if dq is q and not force:
                if not q.same:
                    return
                if q.window and n < q.cnt:
                    return
            if need.get(dq, 0) < n:
                need[dq] = n

        for b in sreads:
            add(b.w, True)
        for b in reads:
            add(b.w)
            if b.excl:
                for d in b.r:
                    add(d)
        for b in writes:
            add(b.w)
            for d in b.r:
                add(d)
        for dq, n in need.items():
            if q.seen.get(id(dq), 0) >= n:
                continue
            q.eng.wait_ge(dq.sem, n)
            q.seen[id(dq)] = n

    def op(self, q, fn, reads=(), writes=(), sreads=()):
        self._waits(q, reads, writes, sreads)
        reads = list(reads) + list(sreads)
        ins = fn(q.eng)
        ins.then_inc(q.sem, 1)
        q.cnt += 1
        tok = (q, q.cnt)
        for b in reads:
            b.r.append(tok)
        for b in writes:
            b.w = tok
            b.r = []
        return ins

    def dma(self, q, out, in_, reads=(), writes=(), sem=None, **kw):
        self._waits(q, reads, writes)
        ds = self.dmasems.get(sem)
        if ds is None:
            ds = DmaTok(self, sem)
            self.dmasems[sem] = ds
        if out.dtype != in_.dtype and in_.ap[-1][1] > 2048:
            kw.setdefault("max_dma_last_dim", 4096)
        ins = q.eng.dma_start(out=out, in_=in_, **kw)
        ds.cnt += 16
        ins.then_inc(ds.sem, 16)
        tok = (ds, ds.cnt)
        for b in reads:
            b.r.append(tok)
        for b in writes:
            b.w = tok
            b.r = []
        return tok

    def barrier(self):
        qs = [self.pe, self.dve, self.act, self.pool, self.sp]
        for q in qs:
            for dq in qs + list(self.dmasems.values()):
                if dq is q or dq.cnt == 0:
                    continue
                if q.seen.get(id(dq), 0) >= dq.cnt:
                    continue
                q.eng.wait_ge(dq.sem, dq.cnt)
                q.seen[id(dq)] = dq.cnt

    def group_done(self, sem, blist):
        ds = self.dmasems[sem]
        for b in blist:
            if b.w is not None and b.w[0] is ds:
                b.w = (ds, ds.cnt)


def sbap(t, off, dims):
    full = t[:] if not isinstance(t, AP) else t
    return AP(full.tensor, full.offset + off, [list(full.ap[0])] + [list(d) for d in dims])


def part_ap(t, p0, npart, off, dims):
    full = t[:]
    pstep = full.ap[0][0]
    sub = t[p0:p0 + npart]
    return AP(sub.tensor, sub.offset + off, [[pstep, npart]] + [list(d) for d in dims])


def build(stop_after=None, dbg_names=()):
    nc = bass.Bass("TRN2", target_bir_lowering=False)
    es = ExitStack()
    fw = FW(nc, es)
    pe, dve, act, pool, sp = fw.pe, fw.dve, fw.act, fw.pool, fw.sp
    dbg = {}

    def din(name, shape, dt=F32):
        return nc.dram_tensor(name, list(shape), dt, kind="ExternalInput").ap()

    xf = din("xf", [NTOK, D])
    ctxf = din("ctxf", [NCTX, D])
    cT_d = din("cT", [128, 8, 2])
    w_ada_d = din("w_ada", [D, 6 * D])
    b_ada_2rows_d = din("b_ada_2rows", [2, 6 * D])
    w_in_d = din("w_in", [D, 928])
    qg_d = din("qg_cols", [128, 2])
    kvg_d = din("kvg_col", [128, 1])
    w_uq_d = din("w_uq", [256, 768])
    w_ukv_d = din("w_ukv", [128, 1024])
    rope_d = din("rope", [128, 2, NKEY])
    ssm_a_d = din("ssm_a", [128, 2, 32])
    ssm_ldt_d = din("ssm_ldt", [128, 32])
    ssm_b_d = din("ssm_b", [128, 2, 32, 16])
    ssm_c_d = din("ssm_c", [128, 2, 32, 16])
    ssm_dcols_d = din("ssm_dcols", [128, 32])
    masks_d = din("masks", [128, 2, 128])
    ident_d = din("ident", [128, 128])
    w_glu_d = din("w_glu", [512, 512])
    b_glu_d = din("b_glu_row", [128, 512])
    gn_d = din("gn_cols", [128, 8])
    w_o_d = din("w_o", [D, D])
    ln_d = din("ln_rows", [128, 4, D])
    w_r_d = din("w_r", [D, 36])
    b_r_d = din("b_r_row", [128, 36])
    wg_d = din("w_exp_gate", [NE, D, 256])
    wu_d = din("w_exp_up", [NE, D, 256])
    wd_d = din("w_exp_down", [NE, 256, D])
    sel_d = din("sel", [32, NE * 128])
    out_d = nc.dram_tensor("out", [NOWN, D], F32, kind="ExternalOutput").ap()
    ug_s = nc.dram_tensor("ug_s", [128, 32 * 2 * 272], BF16, kind="Internal").ap()
    attn_s = nc.dram_tensor("attn_s", [128, 16 * 512], F32, kind="Internal").ap()
    x1_s = nc.dram_tensor("x1_s", [128, 16 * D], F32, kind="Internal").ap()
    g_s = nc.dram_tensor("g_s", [128, 16 * 512], F32, kind="Internal").ap()
    dbg_out = {}
    for ent in dbg_names:
        nm, shp = ent[0], ent[1]
        dbg_out[nm] = nc.dram_tensor("dbg_" + nm, list(shp), BF16 if (len(ent) > 2 and ent[2] == "bf16") else F32, kind="ExternalOutput").ap()

    def sb(name, shape, dt=F32, stack=None):
        return (stack or es).enter_context(nc.sbuf_tensor("sb_" + name, list(shape), dt))

    PS = [es.enter_context(nc.psum_tensor("ps%d" % i, [128, 512], F32)) for i in range(8)]
    PSB = [Buf("ps%d" % i, excl=True) for i in range(8)]
    ps_rr = [0]

    def ps_next(k=None):
        i = ps_rr[0] % 8
        ps_rr[0] += 1
        return PS[i], PSB[i]

    ident = sb("ident", [128, 128]); B_ident = Buf("ident")
    ones_bf = sb("ones_bf", [128, 128], BF16); B_ones = Buf("ones")
    modc = sb("modc", [128, 96]); B_modc = Buf("modc")
    grow = sb("grow", [128, 4, D]); B_grow = Buf("grow")
    fw.dma(sp, ident[:], ident_d[:, :], writes=[B_ident], sem="c0")
    fw.op(dve, lambda e: e.memset(ones_bf[:], 1.0), writes=[B_ones])

    def dump(name, ap, reads):
        if name in dbg_out:
            fw.dma(sp, dbg_out[name], ap, reads=reads, writes=[], sem="dbg")

    def finish():
        for ds in fw.dmasems.values():
            sp.eng.wait_ge(ds.sem, ds.cnt)
        es.close()
        return nc

    with ExitStack() as ph:
        cT = sb("cT", [128, 8, 2], stack=ph); B_cT = Buf("cT")
        sil2 = sb("sil2", [128, 8, 2], stack=ph); B_sil = Buf("sil2")
        b2 = sb("b2rows", [2, 6 * D], stack=ph); B_b2 = Buf("b2")
        modrow = sb("modrow", [2, 6 * D], stack=ph); B_mr = Buf("modrow")
        ones_f = sb("ones_f", [1, 128], stack=ph); B_onesf = Buf("ones_f")
        wa = [sb("wa%d" % i, [128, 8, D], stack=ph) for i in range(2)]
        B_wa = bufs("wa", 2)
        fw.dma(sp, cT[:], cT_d[:, :, :], writes=[B_cT], sem="c1")
        fw.dma(sp, b2[:], b_ada_2rows_d[:, :], writes=[B_b2], sem="c2")
        fw.op(dve, lambda e: e.memset(ones_f[:], 1.0), writes=[B_onesf])
        fw.op(act, lambda e: e.activation(out=sil2[:], in_=cT[:], func=AF.Silu), reads=[B_cT], writes=[B_sil])
        w_ada_v = w_ada_d.rearrange("(k p) n -> p k n", p=128)
        for v in range(6):
            slot = v % 2
            fw.dma(sp if v % 2 == 0 else act, wa[slot][:], w_ada_v[:, :, v * D:(v + 1) * D], writes=[B_wa[slot]], sem="wa%d" % slot)
            for half in range(2):
                pr, Bpr = PS[half], PSB[half]
                for k in range(8):
                    fw.op(pe, lambda e: e.matmul(pr[0:2, :], lhsT=sil2[:, k, :], rhs=wa[slot][:, k, half * 512:(half + 1) * 512],
                                                 start=(k == 0), stop=(k == 7)), reads=[B_wa[slot], B_sil], writes=[Bpr])
                c0 = v * D + half * 512
                fw.op(dve, lambda e: e.tensor_tensor(out=modrow[0:2, c0:c0 + 512], in0=pr[0:2, :], in1=b2[0:2, c0:c0 + 512], op=ALU.add),
                      reads=[Bpr, B_b2], writes=[B_mr])
        for v in (1, 4):
            fw.op(dve, lambda e: e.tensor_scalar_add(out=modrow[0:2, v * D:(v + 1) * D], in0=modrow[0:2, v * D:(v + 1) * D], scalar1=1.0),
                  reads=[B_mr], writes=[B_mr])
        psc, Bpsc = PS[2], PSB[2]
        for vc in range(48):
            fw.op(pe, lambda e: e.matmul(psc[:, vc * 2:vc * 2 + 2], lhsT=modrow[0:2, vc * 128:(vc + 1) * 128], rhs=ident[0:2, 0:2],
                                         start=True, stop=True, is_transpose=True), reads=[B_mr, B_ident], writes=[Bpsc])
        fw.op(dve, lambda e: e.tensor_copy(out=modc[:], in_=psc[:, 0:96]), reads=[Bpsc], writes=[B_modc])
        for ri, v in enumerate((2, 3, 4, 5)):
            for half in range(2):
                pr, Bpr = PS[3 + (ri * 2 + half) % 2], PSB[3 + (ri * 2 + half) % 2]
                c0 = v * D + half * 512
                fw.op(pe, lambda e: e.matmul(pr[:, :], lhsT=ones_f[0:1, :], rhs=modrow[0:1, c0:c0 + 512], start=True, stop=True),
                      reads=[B_onesf, B_mr], writes=[Bpr])
                fw.op(act, lambda e: e.activation(out=grow[:, ri, half * 512:(half + 1) * 512], in_=pr[:, :], func=AF.Copy),
                      reads=[Bpr], writes=[B_grow])
    fw.barrier()
    dump("modc", modc[:], [B_modc])
    dump("grow", grow[:, 0:2].rearrange("p a b -> p (a b)"), [B_grow])
    if stop_after == "adaln":
        return finish()

    def mcol(v, ch, who):
        i = (v * 8 + ch) * 2 + who
        return modc[:, i:i + 1]

    def A(q, out, in_, func, reads, writes, **kw):
        return fw.op(q, lambda e: e.activation(out=out, in_=in_, func=func, **kw), reads=reads, writes=writes)

    def TT(q, out, in0, in1, op, reads, writes):
        return fw.op(q, lambda e: e.tensor_tensor(out=out, in0=in0, in1=in1, op=op), reads=reads, writes=writes)

    def TS(q, out, in0, s1, s2, op0, op1, reads, writes, sreads=()):
        return fw.op(q, lambda e: e.tensor_scalar(out=out, in0=in0, scalar1=s1, scalar2=s2, op0=op0, op1=op1), reads=reads, writes=writes, sreads=sreads)

    def STT(out, in0, scalar, in1, op0, op1, reads, writes):
        return fw.op(dve, lambda e: e.scalar_tensor_tensor(out=out, in0=in0, scalar=scalar, in1=in1, op0=op0, op1=op1), reads=reads, writes=writes)

    def CP(q, out, in_, reads, writes):
        return fw.op(q, lambda e: e.tensor_copy(out=out, in_=in_), reads=reads, writes=writes)

    def MM(out, lhsT, rhs, start, stop, reads, writes):
        return fw.op(pe, lambda e: e.matmul(out, lhsT=lhsT, rhs=rhs, start=start, stop=stop), reads=reads, writes=writes)

    def TR(out, in_, reads, writes, n=128):
        return fw.op(pe, lambda e: e.matmul(out, lhsT=in_, rhs=ident[0:n, 0:n], start=True, stop=True, is_transpose=True),
                     reads=list(reads) + [B_ident], writes=writes)

    def rstd_from_ss(out, ss_ps, B_ss, inv_n, tmp, B_tmp, B_out):
        A(act, tmp, ss_ps, AF.Ln, [B_ss, B_eps], [B_tmp], scale=inv_n, bias=epsc[:, 0:1])
        A(act, out, tmp, AF.Exp, [B_tmp], [B_out], scale=-0.5)

    epsc = sb("epsc", [128, 1]); B_eps = Buf("eps")
    fw.op(dve, lambda e: e.memset(epsc[:], EPS), writes=[B_eps])

    att = ExitStack()
    cqT = sb("cqT", [128, 2, NOWN], BF16, stack=att); B_cqT = Buf("cqT")
    rstdq = sb("rstdq", [128, NOWN], stack=att); B_rstdq = Buf("rstdq")
    ckvnT = sb("ckvnT", [128, NKEY], BF16, stack=att); B_ckvnT = Buf("ckvnT")
    KRT = sb("KRT", [128, NKEY], BF16, stack=att); B_KRT = Buf("KRT")

    with ExitStack() as ph:
        w_in = sb("w_in", [128, 8, 928], BF16, stack=ph); B_win = Buf("w_in")
        w_krot = sb("w_krot", [128, 8, 32], BF16, stack=ph); B_wkrot = Buf("w_krot")
        fw.dma(pool, w_in[:], w_in_d.rearrange("(k p) n -> p k n", p=128), writes=[B_win], sem="w_in")
        CP(dve, w_krot[:, :, 16:32], w_in[:, :, 384:400], [B_win], [B_wkrot])
        TS(dve, w_krot[:, :, 0:16], w_in[:, :, 400:416], -1.0, None, ALU.mult, ALU.bypass, [B_win], [B_wkrot])
        xin = [sb("xin%d" % i, [128, D], stack=ph) for i in range(4)]; B_xin = bufs("xin", 4)
        xmT = [sb("xmT%d" % i, [128, 8, 512], BF16, stack=ph) for i in range(2)]; B_xmT = bufs("xmT", 2)
        ropeg = [sb("ropeg%d" % i, [128, 2, 512], stack=ph) for i in range(2)]; B_ropeg = bufs("ropeg", 2)
        u_tm = sb("u_tm", [128, 32, 128], stack=ph); B_utm = Buf("u_tm")
        Ug = sb("Ug", [128, 32, 2, 272], BF16, stack=ph); B_Ug = Buf("Ug")
        sqb = sb("sqb", [128, 512], BF16, stack=ph); B_sqb = Buf("sqb")
        sqq = sb("sqq", [128, 2, 512], BF16, stack=ph); B_sqq = Buf("sqq")
        rawkv = sb("rawkv", [128, 512], stack=ph); B_rawkv = Buf("rawkv")
        lnt = sb("lnt", [128, 512], stack=ph); B_lnt = Buf("lnt")
        rstdkv = sb("rstdkv", [128, 512], stack=ph); B_rstdkv = Buf("rstdkv")
        rt1 = sb("rt1", [128, 512], stack=ph); B_rt1 = Buf("rt1")
        rt2 = sb("rt2", [128, 512], stack=ph); B_rt2 = Buf("rt2")
        xf_v = xf.rearrange("(m j) d -> j m d", j=16)
        ctx_v = ctxf.rearrange("(m j) d -> j m d", j=16)
        gcount = [0]

        def in_group(kind, mt, jg):
            gs = gcount[0] % 2
            gcount[0] += 1
            lat = kind == "lat"
            ncols = 512 if lat else 256
            col0 = (mt * 16 + jg * 4) * 128 if lat else NTOK
            own = lat and mt == 0
            np_ = 128 if lat else 16
            nt = 4 if lat else 16
            tw = 128 if lat else 16
            if lat:
                for t in range(4):
                    j = jg * 4 + t
                    fw.dma(sp, xin[t][:], xf_v[j, mt * 128:(mt + 1) * 128, :], writes=[B_xin[t]], sem="xin%d" % t)
            fw.dma(sp, ropeg[gs][64:96, :, 0:ncols], rope_d[64:96, :, col0:col0 + ncols], writes=[B_ropeg[gs]], sem="ropeg%d" % gs)
            who = 0 if lat else 1
            if lat:
                for ch in range(8):
                    ps, Bp = ps_next()
                    for t in range(4):
                        TR(ps[:, t * 128:(t + 1) * 128], xin[t][:, ch * 128:(ch + 1) * 128], [B_xin[t]], [Bp])
                    A(act, xmT[gs][:, ch, 0:ncols], ps[:, 0:ncols], AF.Identity, [Bp, B_modc], [B_xmT[gs]],
                      scale=mcol(1, ch, who), bias=mcol(0, ch, who))
            else:
                banks = [ps_next() for _ in range(8)]
                for t in range(16):
                    fw.dma(sp, xin[t % 4][0:16, :], ctx_v[t, 0:16, :], writes=[B_xin[t % 4]], sem="xin%d" % (t % 4))
                    for ch in range(8):
                        ps, Bp = banks[ch]
                        TR(ps[:, t * 16:(t + 1) * 16], xin[t % 4][0:16, ch * 128:(ch + 1) * 128], [B_xin[t % 4]], [Bp], n=16)
                for ch in range(8):
                    ps, Bp = banks[ch]
                    A(act, xmT[gs][:, ch, 0:ncols], ps[:, 0:ncols], AF.Identity, [Bp, B_modc], [B_xmT[gs]],
                      scale=mcol(1, ch, who), bias=mcol(0, ch, who))
            X = xmT[gs]; BX = B_xmT[gs]
            kstep = 9
            yield
            ps, Bp = ps_next()
            for ch in range(8):
                MM(ps[:, 0:ncols], w_in[:, ch, 256:384], X[:, ch, 0:ncols], ch == 0, ch == 7, [B_win, BX], [Bp])
            A(act, sqb[:, 0:ncols], ps[:, 0:ncols], AF.Square, [Bp], [B_sqb])
            A(act, rawkv[:, 0:ncols], ps[:, 0:ncols], AF.Copy, [Bp], [B_rawkv])
            ps2, Bp2 = ps_next()
            MM(ps2[:, 0:ncols], ones_bf[:, :], sqb[:, 0:ncols], True, True, [B_ones, B_sqb], [Bp2])
            rstd_from_ss(rstdkv[:, 0:ncols], ps2[:, 0:ncols], Bp2, 1.0 / 128, lnt[:, 0:ncols], B_lnt, B_rstdkv)
            TT(dve, ckvnT[:, col0:col0 + ncols], rawkv[:, 0:ncols], rstdkv[:, 0:ncols], ALU.mult, [B_rawkv, B_rstdkv], [B_ckvnT])
            psa, Bpa = ps_next()
            psb, Bpb = ps_next()
            for ch in range(8):
                MM(psa[64:96, 0:ncols], w_in[:, ch, 384:416], X[:, ch, 0:ncols], ch == 0, ch == 7, [B_win, BX], [Bpa])
            for ch in range(8):
                MM(psb[64:96, 0:ncols], w_krot[:, ch, :], X[:, ch, 0:ncols], ch == 0, ch == 7, [B_wkrot, BX], [Bpb])
            TT(dve, rt1[64:96, 0:ncols], psa[64:96, 0:ncols], ropeg[gs][64:96, 0, 0:ncols], ALU.mult, [Bpa, B_ropeg[gs]], [B_rt1])
            TT(dve, rt2[64:96, 0:ncols], psb[64:96, 0:ncols], ropeg[gs][64:96, 1, 0:ncols], ALU.mult, [Bpb, B_ropeg[gs]], [B_rt2])
            TT(dve, KRT[64:96, col0:col0 + ncols], rt1[64:96, 0:ncols], rt2[64:96, 0:ncols], ALU.add, [B_rt1, B_rt2], [B_KRT])
            if own:
                pss = []
                for fc in range(2):
                    ps, Bp = ps_next()
                    for ch in range(8):
                        MM(ps[:, :], w_in[:, ch, fc * 128:(fc + 1) * 128], X[:, ch, :], ch == 0, ch == 7, [B_win, BX], [Bp])
                    A(act, cqT[:, fc, col0:col0 + 512], ps[:, :], AF.Copy, [Bp], [B_cqT])
                    pss.append((ps, Bp))
                ksub = int(os.environ.get("K_SUB", "9"))
                if ksub >= 1:
                    ps2, Bp2 = ps_next()
                    for fc in range(2):
                        ps, Bp = pss[fc]
                        A(act, sqq[:, fc, :], ps[:, :], AF.Square, [Bp], [B_sqq])
                    for fc in range(2):
                        MM(ps2[:, :], ones_bf[:, :], sqq[:, fc, :], fc == 0, fc == 1, [B_ones, B_sqq], [Bp2])
                if ksub >= 2:
                    rstd_from_ss(rstdq[:, col0:col0 + 512], ps2[:, :], Bp2, 1.0 / 256, lnt[:, :], B_lnt, B_rstdq)
            for t in range(nt):
                ps, Bp = ps_next()
                for ch in range(8):
                    MM(ps[0:np_, :], X[:, ch, t * tw:(t + 1) * tw], w_in[:, ch, 416:928], ch == 0, ch == 7, [B_win, BX], [Bp])
                if lat:
                    jl = (jg * 4 + t) % 8
                    uo = sbap(u_tm, jl * 16, [[128, 32], [1, 16]])
                    ui = sbap(ps, 0, [[16, 32], [1, 16]])
                    if t % 2 == 0:
                        CP(dve, uo, ui, [Bp], [B_utm])
                    else:
                        A(act, uo, ui, AF.Copy, [Bp], [B_utm])
                else:
                    A(act, part_ap(u_tm, 0, 16, (t % 8) * 16, [[128, 32], [1, 16]]), part_ap(ps, 0, 16, 0, [[16, 32], [1, 16]]),
                      AF.Copy, [Bp], [B_utm])
                    if t % 8 == 7:
                        ug_transposes(u_tm, B_utm, 16, t // 8, 256, 0)

        def ug_transposes(src, Bsrc, np_, jt, ucol0, jbase):
            for g4 in range(8):
                ps, Bp = ps_next()
                for gg in range(4):
                    g = g4 * 4 + gg
                    inap = part_ap(src, 0, np_, g * 128, [[1, 128]])
                    TR(ps[:, gg * 128:gg * 128 + np_], inap, [Bsrc], [Bp], n=np_)
                outap = sbap(Ug, (g4 * 4) * 2 * 272 + jt * 272 + ucol0, [[2 * 272, 4], [1, np_]])
                inp_ = sbap(ps, 0, [[128, 4], [1, np_]])
                if g4 % 2 == 0:
                    CP(dve, outap, inp_, [Bp], [B_Ug])
                else:
                    A(act, outap, inp_, AF.Copy, [Bp], [B_Ug])

        glist = [("lat", mt, jg) for mt in range(2) for jg in range(4)] + [("ctx", 0, 0)]
        gens = [in_group(*g_) for g_ in glist]
        next(gens[0])
        for gi_, g_ in enumerate(glist):
            if gi_ + 1 < len(glist):
                next(gens[gi_ + 1])
            for _ in gens[gi_]:
                pass
            if g_[0] == "lat" and g_[2] % 2 == 1:
                ug_transposes(u_tm, B_utm, 128, g_[2] // 2, g_[1] * 128, 0)
        dump("ckvnT", ckvnT[:], [B_ckvnT])
        dump("KRT", KRT[64:96, :], [B_KRT])
        dump("cqT", cqT[:].rearrange("p a b -> p (a b)"), [B_cqT])
        dump("rstdq", rstdq[:], [B_rstdq])
        dump("Ug", Ug[:].rearrange("p a b c -> p (a b c)"), [B_Ug])
        fw.dma(sp, ug_s[:, :], Ug[:].rearrange("p a b c -> p (a b c)"), reads=[B_Ug], writes=[], sem="ugs")
    fw.barrier()
    if stop_after == "inproj":
        return finish()

    attn_tm = sb("attn_tm", [128, 16, 512], stack=att); B_attn = Buf("attn_tm")
    B_attn_s = Buf("attn_s")
    with ExitStack() as ph:
        wq_f = sb("wq_f", [128, 2, 768], stack=ph); B_wqf = Buf("wq_f")
        wkv_f = sb("wkv_f", [128, 1024], stack=ph); B_wkvf = Buf("wkv_f")
        qg = sb("qg", [128, 2], stack=ph); kvg = sb("kvg", [128, 1], stack=ph); B_g = Buf("qkvg")
        w_uq_s = sb("w_uq_s", [128, 2, 768], BF16, stack=ph); B_wuq = Buf("w_uq_s")
        w_uq_rot = sb("w_uq_rot", [128, 2, 8, 32], BF16, stack=ph); B_wuqr = Buf("w_uq_rot")
        w_k = sb("w_k", [128, 1024], BF16, stack=ph); B_wk = Buf("w_k")
        w_v = sb("w_v", [128, 512], BF16, stack=ph); B_wv = Buf("w_v")
        V_all = sb("V_all", [128, 34, 8, 65], BF16, stack=ph); B_V = Buf("V_all")
        KT = [sb("KT%d" % i, [128, NKEY], BF16, stack=ph) for i in range(2)]; B_KT = bufs("KT", 2)
        QT = [sb("QT%d" % i, [128, NOWN], BF16, stack=ph) for i in range(2)]; B_QT = bufs("QT", 2)
        ropeq = sb("ropeq", [128, 2, NOWN], stack=ph); B_ropeq = Buf("ropeq")
        PT = [sb("PT%d" % i, [128, 512], BF16, stack=ph) for i in range(4)]; B_PT = bufs("PT", 4)
        ot = [sb("ot%d" % i, [128, 512], stack=ph) for i in range(2)]; B_ot = bufs("ot", 2)
        qt1 = sb("qt1", [128, 512], stack=ph); B_qt1 = Buf("qt1")
        qt2 = sb("qt2", [128, 512], stack=ph); B_qt2 = Buf("qt2")
        rd = sb("rd", [128, 4], stack=ph); B_rd = Buf("rd")
        fw.dma(sp, wq_f[:], w_uq_d.rearrange("(c p) n -> p c n", p=128), writes=[B_wqf], sem="a0")
        fw.dma(sp, wkv_f[:], w_ukv_d[:, :], writes=[B_wkvf], sem="a1")
        fw.dma(sp, qg[:], qg_d[:, :], writes=[B_g], sem="a2")
        fw.dma(sp, kvg[:], kvg_d[:, :], writes=[B_g], sem="a3")
        fw.dma(sp, ropeq[64:96, :, :], rope_d[64:96, :, 0:NOWN], writes=[B_ropeq], sem="a4")
        for c in range(2):
            TS(dve, w_uq_s[:, c, :], wq_f[:, c, :], qg[:, c:c + 1], None, ALU.mult, ALU.bypass, [B_wqf, B_g], [B_wuq])
            TS(dve, w_uq_rot[:, c, :, 0:16], sbap(w_uq_s, c * 768 + 80, [[96, 8], [1, 16]]), -1.0, None, ALU.mult, ALU.bypass, [B_wuq], [B_wuqr])
            CP(dve, w_uq_rot[:, c, :, 16:32], sbap(w_uq_s, c * 768 + 64, [[96, 8], [1, 16]]), [B_wuq], [B_wuqr])
        TS(dve, w_k[:, :], wkv_f[:, :], kvg[:, 0:1], None, ALU.mult, ALU.bypass, [B_wkvf, B_g], [B_wk])
        CP(dve, w_v[:].rearrange("p (h d) -> p h d", h=8), sbap(w_k, 64, [[128, 8], [1, 64]]), [B_wk], [B_wv])
        fw.op(dve, lambda e: e.memset(V_all[:, :, :, 64:65], 1.0), writes=[B_V])
        for i in range(2):
            fw.op(dve, lambda e: e.memset(KT[i][96:128, :], 0.0), writes=[B_KT[i]])
            fw.op(dve, lambda e: e.memset(QT[i][96:128, :], 0.0), writes=[B_QT[i]])
        for kt in range(34):
            ps, Bp = PS[6 + kt % 2], PSB[6 + kt % 2]
            MM(ps[:, :], ckvnT[:, kt * 128:(kt + 1) * 128], w_v[:, :], True, True, [B_ckvnT, B_wv], [Bp])
            vo = V_all[:, kt, :, 0:64]
            vi = ps[:, :].rearrange("p (h d) -> p h d", h=8)
            if kt % 2 == 0:
                CP(dve, vo, vi, [Bp], [B_V])
            else:
                A(act, vo, vi, AF.Copy, [Bp], [B_V])

        def gen(h):
            s = h % 2
            for ct in range(9):
                n = 512 if ct < 8 else 256
                c0 = ct * 512
                ps, Bp = PS[6], PSB[6]
                MM(ps[0:64, 0:n], w_k[:, h * 128:h * 128 + 64], ckvnT[:, c0:c0 + n], True, True, [B_wk, B_ckvnT], [Bp])
                CP(dve, KT[s][0:64, c0:c0 + n], ps[0:64, 0:n], [Bp], [B_KT[s]])
                yield
            CP(pool, KT[s][64:96, :], KRT[64:96, :], [B_KRT], [B_KT[s]])
            for ct in range(4):
                c0 = ct * 512
                psq, Bq = PS[6], PSB[6]
                psr, Br = PS[7], PSB[7]
                for c in range(2):
                    MM(psq[0:96, :], w_uq_s[:, c, h * 96:(h + 1) * 96], cqT[:, c, c0:c0 + 512], c == 0, c == 1, [B_wuq, B_cqT], [Bq])
                for c in range(2):
                    MM(psr[64:96, :], w_uq_rot[:, c, h, :], cqT[:, c, c0:c0 + 512], c == 0, c == 1, [B_wuqr, B_cqT], [Br])
                TT(dve, QT[s][0:64, c0:c0 + 512], psq[0:64, :], rstdq[0:64, c0:c0 + 512], ALU.mult, [Bq, B_rstdq], [B_QT[s]])
                TT(dve, qt1[64:96, :], psq[64:96, :], ropeq[64:96, 0, c0:c0 + 512], ALU.mult, [Bq, B_ropeq], [B_qt1])
                TT(dve, qt2[64:96, :], psr[64:96, :], ropeq[64:96, 1, c0:c0 + 512], ALU.mult, [Br, B_ropeq], [B_qt2])
                TT(dve, qt1[64:96, :], qt1[64:96, :], qt2[64:96, :], ALU.add, [B_qt1, B_qt2], [B_qt1])
                TT(dve, QT[s][64:96, c0:c0 + 512], qt1[64:96, :], rstdq[64:96, c0:c0 + 512], ALU.mult, [B_qt1, B_rstdq], [B_QT[s]])
                yield

        scount = [0]
        ocount = [0]

        pend = [None]

        def epilogue(h, qt, ob, osl):
            CP(dve, ot[osl][0:65, :], PS[ob][0:65, :], [PSB[ob]], [B_ot[osl]])
            pso, Bo = PS[7], PSB[7]
            for t in range(4):
                TR(pso[:, t * 128:t * 128 + 65], ot[osl][0:65, t * 128:(t + 1) * 128], [B_ot[osl]], [Bo], n=65)
            fw.op(dve, lambda e: e.reciprocal(out=rd[:, :], in_=sbap(pso, 64, [[128, 4]])), reads=[Bo], writes=[B_rd])
            for t in range(4):
                TS(dve, attn_tm[:, qt * 4 + t, h * 64:(h + 1) * 64], pso[:, t * 128:t * 128 + 64], rd[:, t:t + 1], None, ALU.mult, ALU.bypass,
                   [Bo], [B_attn], sreads=[B_rd])

        def attend(h, nxt):
            s = h % 2
            itc = [0]
            for qt in range(4):
                ob = 4 + ocount[0] % 2
                osl = ocount[0] % 2
                ocount[0] += 1
                base = scount[0]
                scount[0] += 34

                def S(kt):
                    bi = (base + kt) % 4
                    MM(PS[bi][:, :], KT[s][:, kt * 128:(kt + 1) * 128], QT[s][:, qt * 512:(qt + 1) * 512], True, True,
                       [B_KT[s], B_QT[s]], [PSB[bi]])
                S(0)
                S(1)
                if pend[0] is not None:
                    epilogue(*pend[0])
                    pend[0] = None
                for kt in range(34):
                    if kt + 2 < 34:
                        S(kt + 2)
                    bi = (base + kt) % 4
                    A(act, PT[bi][:, :], PS[bi][:, :], AF.Exp, [PSB[bi]], [B_PT[bi]], scale=SCALE)
                    MM(PS[ob][0:65, :], V_all[:, kt, h, 0:65], PT[bi][:, :], kt == 0, kt == 33, [B_V, B_PT[bi]], [PSB[ob]])
                    itc[0] += 1
                    if nxt is not None and itc[0] % 9 == 0:
                        next(nxt, None)
                pend[0] = (h, qt, ob, osl)

        nheads = int(os.environ.get("K_HEADS", "8"))
        for _ in gen(0):
            pass
        for h in range(nheads):
            nxt = gen(h + 1) if h + 1 < nheads else None
            attend(h, nxt)
            if nxt is not None:
                for _ in nxt:
                    pass
        epilogue(*pend[0])
        dump("attn", attn_tm[:].rearrange("p a b -> p (a b)"), [B_attn])
        fw.dma(sp, attn_s[:, :], attn_tm[:].rearrange("p a b -> p (a b)"), reads=[B_attn], writes=[B_attn_s], sem="attns")
        dump("KT0", KT[0][0:96, :], [B_KT[0]])
        dump("QT0", QT[0][0:96, :], [B_QT[0]])
        dump("V0", V_all[:].rearrange("p a b c -> p (a b c)"), [B_V])
    fw.barrier()
    att.close()
    if stop_after == "attn":
        return finish()

    PI = math.pi
    B_gs = Buf("g_s")
    with ExitStack() as S:
        sa = sb("ssm_a", [128, 2, 32], stack=S); sl = sb("ssm_l", [128, 32], stack=S)
        sbb = sb("ssm_b", [128, 2, 32, 16], stack=S); scc = sb("ssm_c", [128, 2, 32, 16], stack=S)
        dcols = sb("dcols", [128, 32], stack=S); masks = sb("masks4", [128, 4, 128], stack=S)
        B_par = Buf("ssm_par")
        fw.dma(sp, sa[:], ssm_a_d[:, :, :], writes=[B_par], sem="s0")
        fw.dma(sp, sl[:], ssm_ldt_d[:, :], writes=[B_par], sem="s1")
        fw.dma(sp, sbb[:], ssm_b_d[:, :, :, :], writes=[B_par], sem="s2")
        fw.dma(sp, scc[:], ssm_c_d[:, :, :, :], writes=[B_par], sem="s3")
        fw.dma(sp, dcols[:], ssm_dcols_d[:, :], writes=[B_par], sem="s4")
        fw.dma(sp, masks[:, 0:2, :], masks_d[:, :, :], writes=[B_par], sem="s5")
        fw.dma(sp, masks[:, 2:4, :], masks_d[:, :, :], writes=[B_par], sem="s6")
        w_glu = sb("w_glu", [128, 4, 512], BF16, stack=S); B_wglu = Buf("w_glu")
        fw.dma(pool, w_glu[:], w_glu_d.rearrange("(k p) n -> p k n", p=128), writes=[B_wglu], sem="s7")
        bglu = sb("bglu", [128, 512], stack=S); B_bglu = Buf("bglu")
        fw.dma(sp, bglu[:], b_glu_d[:, :], writes=[B_bglu], sem="s8")
        sm = sb("ssm_sm", [128, 32, 32], stack=S); B_sm = Buf("ssm_sm")
        _smi = [0]

        def smt():
            i = _smi[0]; _smi[0] += 1
            return sm[:, i, :]
        Bbar = sb("Bbar", [128, 2, 32, 16], stack=S)
        PWr = sb("PWr", [128, 17, 32], stack=S); PWi = sb("PWi", [128, 17, 32], stack=S)
        IPr = sb("IPr", [128, 17, 32], stack=S); IPi = sb("IPi", [128, 17, 32], stack=S)
        AA = sb("AA", [128, 2, 2, 16], stack=S); AB = sb("AB", [128, 2, 2, 16], stack=S)
        BS = [B_par, B_sm]

        def tt(out, a, b, op):
            TT(dve, out, a, b, op, BS, [B_sm])

        a_re, a_im = sa[:, 0, :], sa[:, 1, :]
        dt_ = smt(); lre = smt(); ang = smt(); mag = smt()
        A(act, dt_, sl[:, :], AF.Exp, [B_par], [B_sm])
        tt(lre, a_re, dt_, ALU.mult)
        tt(ang, a_im, dt_, ALU.mult)
        A(act, mag, lre, AF.Exp, [B_sm], [B_sm])

        def sin_of(src_ang, shift):
            a2 = smt(); k = smt(); r = smt(); o = smt()
            TS(dve, a2, src_ang, shift, None, ALU.add, ALU.bypass, BS, [B_sm])
            TS(dve, k, a2, PI, None, ALU.is_ge, ALU.bypass, BS, [B_sm])
            for i in range(2, 9):
                STT(k, a2, (2 * i - 1) * PI, k, ALU.is_ge, ALU.add, BS, [B_sm])
            STT(r, k, -2.0 * PI, a2, ALU.mult, ALU.add, BS, [B_sm])
            A(act, o, r, AF.Sin, [B_sm], [B_sm])
            return o
        sn = sin_of(ang, 0.0)
        cs = sin_of(ang, PI / 2)
        abre = smt(); abim = smt()
        tt(abre, mag, cs, ALU.mult)
        tt(abim, mag, sn, ALU.mult)
        t1 = smt(); t2 = smt(); den = smt(); rden = smt(); nre = smt(); cre = smt(); cim = smt()
        tt(t1, a_re, a_re, ALU.mult); tt(t2, a_im, a_im, ALU.mult); tt(den, t1, t2, ALU.add)
        fw.op(dve, lambda e: e.reciprocal(out=rden, in_=den), reads=BS, writes=[B_sm])
        TS(dve, nre, abre, -1.0, None, ALU.add, ALU.bypass, BS, [B_sm])
        tt(t1, nre, a_re, ALU.mult); tt(t2, abim, a_im, ALU.mult); tt(t1, t1, t2, ALU.add); tt(cre, t1, rden, ALU.mult)
        tt(t1, abim, a_re, ALU.mult); tt(t2, nre, a_im, ALU.mult); tt(t1, t1, t2, ALU.subtract); tt(cim, t1, rden, ALU.mult)
        tb = sb("tb", [128, 32, 16], stack=S)

        def bc16(v):
            return AP(v.tensor, v.offset, [list(v.ap[0]), [1, 32], [0, 16]])
        tt(Bbar[:, 0], sbb[:, 0], bc16(cre), ALU.mult); tt(tb[:], sbb[:, 1], bc16(cim), ALU.mult); tt(Bbar[:, 0], Bbar[:, 0], tb[:], ALU.subtract)
        tt(Bbar[:, 1], sbb[:, 1], bc16(cre), ALU.mult); tt(tb[:], sbb[:, 0], bc16(cim), ALU.mult); tt(Bbar[:, 1], Bbar[:, 1], tb[:], ALU.add)
        m2 = smt(); rm2 = smt(); iabre = smt(); iabim = smt()
        tt(t1, abre, abre, ALU.mult); tt(t2, abim, abim, ALU.mult); tt(m2, t1, t2, ALU.add)
        fw.op(dve, lambda e: e.reciprocal(out=rm2, in_=m2), reads=BS, writes=[B_sm])
        tt(iabre, abre, rm2, ALU.mult)
        STT(iabim, abim, -1.0, rm2, ALU.mult, ALU.mult, BS, [B_sm])
        for (Pr, Pi, br, bi) in ((PWr, PWi, abre, abim), (IPr, IPi, iabre, iabim)):
            fw.op(dve, lambda e: e.memset(Pr[:, 0, :], 1.0), writes=[B_sm])
            fw.op(dve, lambda e: e.memset(Pi[:, 0, :], 0.0), writes=[B_sm])
            CP(dve, Pr[:, 1, :], br, BS, [B_sm])
            CP(dve, Pi[:, 1, :], bi, BS, [B_sm])
            for k in range(2, 17):
                tt(t1, Pr[:, k - 1, :], br, ALU.mult); tt(t2, Pi[:, k - 1, :], bi, ALU.mult); tt(Pr[:, k, :], t1, t2, ALU.subtract)
                tt(t1, Pr[:, k - 1, :], bi, ALU.mult); tt(t2, Pi[:, k - 1, :], br, ALU.mult); tt(Pi[:, k, :], t1, t2, ALU.add)
        for d in range(2):
            for c in range(2):
                CP(dve, AA[:, d, c, :], PWr[:, 16, d * 16:(d + 1) * 16], BS, [B_sm])
            TS(dve, AB[:, d, 0, :], PWi[:, 16, d * 16:(d + 1) * 16], -1.0, None, ALU.mult, ALU.bypass, BS, [B_sm])
            CP(dve, AB[:, d, 1, :], PWi[:, 16, d * 16:(d + 1) * 16], BS, [B_sm])
        dump("sm", sm[:].rearrange("p a b -> p (a b)"), BS)
        dump("abar", sbap(PWr, 32, [[1, 32]]), BS)
        dump("abari", sbap(PWi, 32, [[1, 32]]), BS)
        dump("Bbar", Bbar[:].rearrange("p a b c -> p (a b c)"), BS)

        ctmp = [sb("ctmp%d" % i, [128, 4, 256], stack=S) for i in range(2)]; B_ct = Buf("ctmp")

        def cplx_build(q, out_t, P_r, P_i, k0, kstep, Vt, d, neg_im):
            for half in range(4):
                p0 = half * 4

                def Pv(Pt):
                    return sbap(Pt, k0 * 32 + d * 16 + p0, [[1, 4], [kstep * 32, 16], [0, 16]])

                def Vv(c):
                    return sbap(Vt, c * 512 + (d * 16 + p0) * 16, [[16, 4], [0, 16], [1, 16]])

                def Ov(c):
                    return sbap(out_t, p0 * 512 + c * 256, [[512, 4], [16, 16], [1, 16]])
                ta = ctmp[0][:].rearrange("p a (k h) -> p a k h", h=16)
                tb_ = ctmp[1][:].rearrange("p a (k h) -> p a k h", h=16)
                R = BS + [B_ct]
                TT(q, ta, Pv(P_r), Vv(0), ALU.mult, R, [B_ct])
                TT(q, tb_, Pv(P_i), Vv(1), ALU.mult, R, [B_ct])
                TT(q, Ov(0), ta, tb_, ALU.subtract, R, [B_sm])
                TT(q, ta, Pv(P_r), Vv(1), ALU.mult, R, [B_ct])
                TT(q, tb_, Pv(P_i), Vv(0), ALU.mult, R, [B_ct])
                if neg_im:
                    TT(q, ta, ta, tb_, ALU.add, R, [B_ct])
                    TS(q, Ov(1), ta, -1.0, None, ALU.mult, ALU.bypass, R, [B_sm])
                else:
                    TT(q, Ov(1), ta, tb_, ALU.add, R, [B_sm])

        Sbf = sb("Sbf", [128, 2, 2, 16, 128], BF16, stack=S); B_Sbf = Buf("Sbf")
        with ExitStack() as S2:
            Ug = sb("Ug2", [128, 32, 2, 272], BF16, stack=S2); B_Ug2 = Buf("Ug2")
            fw.dma(sp, Ug[:].rearrange("p a b c -> p (a b c)"), ug_s[:, :], writes=[B_Ug2], sem="s9")
            L = sb("L", [128, 2, 2, 16, 272], BF16, stack=S2); B_L = Buf("L")
            with ExitStack() as S2a:
                Wsrc = sb("Wsrc", [128, 16, 2, 256], stack=S2a)
                Wb = sb("Wb", [128, 16, 4, 128], BF16, stack=S2a); B_Wb = Buf("Wb")
                for d in range(2):
                    if d == 0:
                        cplx_build(dve, Wsrc, PWr, PWi, 15, -1, Bbar, 0, False)
                    else:
                        cplx_build(dve, Wsrc, PWr, PWi, 0, 1, Bbar, 1, False)
                    for pt in range(16):
                        ps, Bp = ps_next()
                        for blk in range(4):
                            TR(ps[:, blk * 128:(blk + 1) * 128], sbap(Wsrc, pt * 512 + blk * 128, [[1, 128]]), BS, [Bp])
                        if pt % 2 == 0:
                            A(act, Wb[:, pt, :, :], ps[:, :].rearrange("p (a b) -> p a b", a=4), AF.Copy, [Bp], [B_Wb])
                        else:
                            CP(dve, Wb[:, pt, :, :], ps[:, :].rearrange("p (a b) -> p a b", a=4), [Bp], [B_Wb])
                    for pt in range(16):
                        for c in range(2):
                            ps, Bp = ps_next()
                            if d == 1:
                                ranges = [(0, 272, 0)]
                                ncol = 272
                            else:
                                ranges = [(256, 16, 0), (0, 128, 16)]
                                ncol = 144
                            for (u0, n, o0) in ranges:
                                for gl in range(2):
                                    for jt in range(2):
                                        MM(ps[gl * 64:(gl + 1) * 64, o0:o0 + n], Wb[:, pt, c * 2 + jt, gl * 64:(gl + 1) * 64],
                                           Ug[:, 2 * pt + gl, jt, u0:u0 + n], jt == 0, jt == 1, [B_Wb, B_Ug2], [Bp])
                            lo = L[:, d, c, pt, 272 - ncol:272]
                            if c == 0:
                                A(act, lo, ps[:, 0:ncol], AF.Copy, [Bp], [B_L])
                            else:
                                CP(dve, lo, ps[:, 0:ncol], [Bp], [B_L])
                    if d == 0:
                        dump("Wb0", Wb[:].rearrange("p a b c -> p (a b c)"), [B_Wb])
            dump("L", L[:].rearrange("p a b c e -> p (a b c e)"), [B_L])
            fw.barrier()
            Sh = sb("Sh", [128, 2, 2, 16, NS], stack=S2); B_Sh = Buf("Sh")
            fw.op(pool, lambda e: e.memset(Sh[:], 0.0), writes=[B_Sh])
            tm1 = sb("tm1", [128, 2, 2, 16], stack=S2); tm2 = sb("tm2", [128, 2, 2, 16], stack=S2); B_tm = Buf("tm")

            def f0(st):
                return st - 140 if st >= 142 else st % 2

            def f1(st):
                return 273 - st if st >= 142 else 132 + st % 2
            B_tmh = bufs("tmh", 2); B_Shh = bufs("Shh", 2)
            for b_ in B_Shh:
                b_.w = B_Sh.w
            RSh = [[B_Shh[h_], B_L, B_sm, B_tmh[h_]] for h_ in range(2)]
            for s in range(272):
                both = s >= 128
                ops = []
                NH_ = 2 if WINDOW else 1
                PW_ = 16 // NH_
                for ph_ in range(NH_):
                    po = ph_ * PW_
                    if both:
                        def hv(t_, n_, c0, c1, rev=False):
                            dstep = 32 * n_ + (c1 - c0)
                            if rev:
                                return sbap(t_, c0 + 16 * n_ + po * n_, [[dstep, 2], [-16 * n_, 2], [n_, PW_]])
                            return sbap(t_, c0 + po * n_, [[dstep, 2], [16 * n_, 2], [n_, PW_]])
                        sr = hv(Sh, NS, f0(s), f1(s)); srev = hv(Sh, NS, f0(s), f1(s), True)
                        sw = hv(Sh, NS, f0(s + 1), f1(s + 1))
                        lv = hv(L, 272, s, 271 - s)
                        aa, ab = AA[:, :, :, po:po + PW_], AB[:, :, :, po:po + PW_]
                        x1, x2 = tm1[:, :, :, po:po + PW_], tm2[:, :, :, po:po + PW_]
                    else:
                        def hv(t_, n_, c1, rev=False):
                            if rev:
                                return sbap(t_, 32 * n_ + c1 + 16 * n_ + po * n_, [[-16 * n_, 2], [n_, PW_]])
                            return sbap(t_, 32 * n_ + c1 + po * n_, [[16 * n_, 2], [n_, PW_]])
                        sr = hv(Sh, NS, f1(s)); srev = hv(Sh, NS, f1(s), True); sw = hv(Sh, NS, f1(s + 1))
                        lv = hv(L, 272, 271 - s)
                        aa, ab = AA[:, 1, :, po:po + PW_], AB[:, 1, :, po:po + PW_]
                        x1, x2 = tm1[:, 1, :, po:po + PW_], tm2[:, 1, :, po:po + PW_]
                    ops.append((sr, srev, sw, lv, aa, ab, x1, x2))
                for h_, (sr, srev, sw, lv, aa, ab, x1, x2) in enumerate(ops):
                    TT(dve, x1, sr, aa, ALU.mult, RSh[h_], [B_tmh[h_]])
                for h_, (sr, srev, sw, lv, aa, ab, x1, x2) in enumerate(ops):
                    TT(dve, x2, srev, ab, ALU.mult, RSh[h_], [B_tmh[h_]])
                for h_, (sr, srev, sw, lv, aa, ab, x1, x2) in enumerate(ops):
                    TT(dve, x1, x1, lv, ALU.add, RSh[h_], [B_tmh[h_]])
                for h_, (sr, srev, sw, lv, aa, ab, x1, x2) in enumerate(ops):
                    TT(dve, sw, x1, x2, ALU.add, RSh[h_], [B_Shh[h_]])
            CP(dve, Sbf[:, 0].rearrange("p a b c -> p (a b) c"), sbap(Sh, 4, [[NS, 32], [1, 128]]), B_Shh, [B_Sbf])
            CP(dve, Sbf[:, 1].rearrange("p a b c -> p (a b) c"), sbap(Sh, 32 * NS + 2, [[NS, 32], [1, 128]]), B_Shh, [B_Sbf])
            dump("Sbf", Sbf[:].rearrange("p a b c e -> p (a b c e)"), [B_Sbf])
        fw.barrier()
        if stop_after == "scan":
            S.close()
            return finish()
        with ExitStack() as S3:
            Kmat = sb("Kmat", [128, 32, 4, 128], BF16, stack=S3); B_Km = Buf("Kmat")
            Ca = sb("Ca", [128, 2, 16, 2, 256], BF16, stack=S3); B_Ca = Buf("Ca")
            Ugo = sb("Ugo", [128, 64, 128], BF16, stack=S3); B_Ugo = Buf("Ugo")
            fw.dma(sp, Ugo[:], ug_s.rearrange("p (a c) -> p a c", c=272)[:, :, 0:128], writes=[B_Ugo], sem="s10")
            S3a = ExitStack()
            Xt = sb("Xt", [128, 16, 2, 256], BF16, stack=S3a); Zt = sb("Zt", [128, 16, 2, 256], BF16, stack=S3a)
            tmpK = sb("tmpK", [128, 4, 128], stack=S3a); tmpS = sb("tmpS", [128, 2, 128], stack=S3a); B_tk = Buf("tmpK")
            cplx_build(dve, Ca[:, 0], PWr, PWi, 1, 1, scc, 0, True)
            cplx_build(dve, Ca[:, 1], PWr, PWi, 16, -1, scc, 1, True)
            KS = BS + [B_tk]
            for d in range(2):
                if d == 0:
                    cplx_build(dve, Xt, IPr, IPi, 0, 1, Bbar, 0, False)
                    cplx_build(dve, Zt, PWr, PWi, 0, 1, scc, 0, True)
                else:
                    cplx_build(dve, Xt, PWr, PWi, 0, 1, Bbar, 1, False)
                    cplx_build(dve, Zt, IPr, IPi, 0, 1, scc, 1, True)
                for g in range(32):
                    pt, gl = g // 2, g % 2
                    r0, r1 = gl * 64, gl * 64 + 64
                    psA, BpA = ps_next()

                    def kblock(ps, col, jt, tt_):
                        for c in range(2):
                            MM(ps[:, col * 128:(col + 1) * 128], Xt[r0:r1, pt, c, jt * 128:(jt + 1) * 128],
                               Zt[r0:r1, pt, c, tt_ * 128:(tt_ + 1) * 128], c == 0, c == 1, BS, [BpA])
                    kblock(psA, 0, 0, 0)
                    kblock(psA, 1, 1, 1)
                    if d == 0:
                        kblock(psA, 2, 0, 1)
                    else:
                        kblock(psA, 2, 1, 0)
                    mk = sbap(masks, d * 128, [[0, 2], [1, 128]])
                    kd = sbap(Kmat, g * 512, [[384, 2], [1, 128]])
                    ko_ = Kmat[:, g, 1 if d == 0 else 2, :]
                    if d == 0:
                        TT(dve, tmpS[:], psA[:, 0:256].rearrange("p (a b) -> p a b", a=2), mk, ALU.mult, [BpA, B_par], [B_tk])
                        STT(kd, sbap(ident, 0, [[0, 2], [1, 128]]), dcols[:, g:g + 1], tmpS[:], ALU.mult, ALU.add, [B_ident, B_par, B_tk], [B_Km])
                    else:
                        TT(dve, tmpS[:], psA[:, 0:256].rearrange("p (a b) -> p a b", a=2), mk, ALU.mult, [BpA, B_par], [B_tk])
                        TT(dve, kd, kd, tmpS[:], ALU.add, [B_tk, B_Km], [B_Km])
                    A(act, ko_, psA[:, 256:384], AF.Copy, [BpA], [B_Km])
            dump("Kmat", Kmat[:].rearrange("p a b c -> p (a b c)"), [B_Km])
            dump("Ca", Ca[:].rearrange("p a b c e -> p (a b c e)"), BS)
            fw.barrier()
            S3a.close()
            g_tm = sb("g_tm", [128, 16, 512], stack=S3); B_gtm = Buf("g_tm")
            glsb = [sb("glsb%d" % i, [128, 512], stack=S3) for i in range(2)]; B_gl = bufs("glsb", 2)
            for gp in range(16):
                psY, BpY = ps_next()
                for gi in range(2):
                    g = gp * 2 + gi
                    pt, gl = g // 2, g % 2
                    r0, r1 = gl * 64, gl * 64 + 64
                    for tt_ in range(2):
                        yo = psY[:, (gi * 2 + tt_) * 128:(gi * 2 + tt_ + 1) * 128]
                        first = True
                        for jt in range(2):
                            MM(yo, Kmat[:, g, jt * 2 + tt_, :], Ugo[:, g * 2 + jt, :], first, False, [B_Km, B_Ugo], [BpY])
                            first = False
                        for d in range(2):
                            for c in range(2):
                                MM(yo, Ca[r0:r1, d, pt, c, tt_ * 128:(tt_ + 1) * 128], Sbf[r0:r1, d, c, pt, :], False, (d == 1 and c == 1),
                                   BS + [B_Sbf], [BpY])
                gs_ = gp % 2
                A(act, glsb[gs_][:, :], psY[:, :], AF.Gelu, [BpY], [B_gl[gs_]])
                psT, BpT = ps_next()
                for blk in range(4):
                    TR(psT[:, blk * 128:(blk + 1) * 128], glsb[gs_][:, blk * 128:(blk + 1) * 128], [B_gl[gs_]], [BpT])
                for gi in range(2):
                    g = gp * 2 + gi
                    oo = sbap(g_tm, 16 * g, [[512, 16], [1, 16]])
                    ii = sbap(psT, gi * 256, [[16, 16], [1, 16]])
                    CP(dve, oo, ii, [BpT], [B_gtm])
            dump("gtm", g_tm[:].rearrange("p a b -> p (a b)"), [B_gtm])
            gT = [sb("gT%d" % i, [128, 4, 128], BF16, stack=S3) for i in range(2)]; B_gT = bufs("gT", 2)
            zt = sb("zt", [128, 512], stack=S3); B_zt = Buf("zt")
            for t in range(16):
                gs_ = t % 2
                ps, Bp = ps_next()
                for fc in range(4):
                    TR(ps[:, fc * 128:(fc + 1) * 128], g_tm[:, t, fc * 128:(fc + 1) * 128], [B_gtm], [Bp])
                A(act, gT[gs_][:], ps[:, :].rearrange("p (a b) -> p a b", a=4), AF.Copy, [Bp], [B_gT[gs_]])
                ps2, Bp2 = ps_next()
                for fc in range(4):
                    MM(ps2[:, :], gT[gs_][:, fc, :], w_glu[:, fc, :], fc == 0, fc == 3, [B_gT[gs_], B_wglu], [Bp2])
                TT(dve, zt[:], ps2[:, :], bglu[:], ALU.add, [Bp2, B_bglu], [B_zt])
                A(act, zt[:], zt[:], AF.Sigmoid, [B_zt], [B_zt])
                TT(dve, g_tm[:, t, :], g_tm[:, t, :], zt[:], ALU.mult, [B_gtm, B_zt], [B_gtm])
            dump("ssm", g_tm[:].rearrange("p a b -> p (a b)"), [B_gtm])
            fw.dma(sp, g_s[:, :], g_tm[:].rearrange("p a b -> p (a b)"), reads=[B_gtm], writes=[B_gs], sem="gs")
    fw.barrier()
    if stop_after == "ssm":
        return finish()

    hT = sb("hT", [128, 8, NOWN], BF16); B_hT = Buf("hT")
    combT = sb("combT", [32, NOWN], BF16); B_combT = Buf("combT")
    lnbox = [None, None]
    B_x1s = Buf("x1_s")

    def layer_norm(pre, B_pre, gi, out, B_out, st, mv, sc_, B_st):
        for hf in range(2):
            fw.op(dve, lambda e: e.bn_stats(out=st[:, hf * 6:(hf + 1) * 6], in_=pre[:, hf * 512:(hf + 1) * 512]), reads=[B_pre], writes=[B_st])
        fw.op(dve, lambda e: e.bn_aggr(out=mv[:, 0:2], in_=st[:, 0:12]), reads=[B_st], writes=[B_st])
        A(act, sc_[:, 0:1], mv[:, 1:2], AF.Ln, [B_st, B_eps], [B_st], bias=epsc[:, 0:1])
        A(act, sc_[:, 1:2], sc_[:, 0:1], AF.Exp, [B_st], [B_st], scale=-0.5)
        TS(dve, out, pre, mv[:, 0:1], sc_[:, 1:2], ALU.subtract, ALU.mult, [B_pre], [B_out], sreads=[B_st])
        lnrows, B_ln = lnbox
        TT(dve, out, out, lnrows[:, 0, :], ALU.mult, [B_out, B_ln], [B_out])
        TT(dve, out, out, lnrows[:, 1, :], ALU.add, [B_out, B_ln], [B_out])

    n_exp = int(os.environ.get("K_NEXP", str(NE)))
    NSLOT = 3
    Wg = [sb("Wg%d" % i, [128, 8, 256], BF16, stack=es) for i in range(NSLOT)]
    Wu = [sb("Wu%d" % i, [128, 8, 256], BF16, stack=es) for i in range(NSLOT)]
    Wd = [sb("Wd%d" % i, [128, 2, D], BF16, stack=es) for i in range(NSLOT)]
    B_W = bufs("Wexp", NSLOT)
    stg = [sb("stg%d" % i, [128, 2048], stack=es) for i in range(2)]; B_stg = bufs("stg", 2)
    stc = [0]

    def load_expert(e):
        sl_ = e % NSLOT
        for (src_, dst, kk) in ((wg_d[e], Wg[sl_], 8), (wu_d[e], Wu[sl_], 8), (wd_d[e], Wd[sl_], 2)):
            k_ = stc[0] % 2
            stc[0] += 1
            fw.dma(sp, stg[k_][:].rearrange("p (k n) -> p k n", k=kk), src_.rearrange("(k p) n -> p k n", p=128),
                   writes=[B_stg[k_]], sem="stg%d" % k_)
            CP(pool, dst[:].rearrange("p k n -> p (k n)"), stg[k_][:], [B_stg[k_]], [B_W[sl_]])


    with ExitStack() as M:
        ln1 = sb("ln1rows", [128, 2, D], stack=M); lnbox[0] = ln1; lnbox[1] = Buf("ln1")
        fw.dma(sp, ln1[:], ln_d[:, 0:2, :], writes=[lnbox[1]], sem="m0")
        w_o_s = sb("w_o_s", [128, 8, D], BF16, stack=M); B_wo = Buf("w_o_s")
        wof = [sb("wof%d" % i, [128, D], stack=M) for i in range(2)]; B_wof = bufs("wof", 2)
        gn = sb("gn", [128, 8], stack=M); B_gn = Buf("gn")
        w_r = sb("w_r", [128, 8, 36], stack=M); b_r = sb("b_r", [128, 36], stack=M); B_wr = Buf("w_r")
        fw.dma(sp, gn[:], gn_d[:, :], writes=[B_gn], sem="m1")
        fw.dma(sp, w_r[:], w_r_d.rearrange("(k p) n -> p k n", p=128), writes=[B_wr], sem="m2")
        fw.dma(sp, b_r[:], b_r_d[:, :], writes=[B_wr], sem="m3")
        for fc in range(8):
            fw.dma(sp, wof[fc % 2][:], w_o_d[fc * 128:(fc + 1) * 128, :], writes=[B_wof[fc % 2]], sem="wof%d" % (fc % 2))
            TS(dve, w_o_s[:, fc, :], wof[fc % 2][:], gn[:, fc:fc + 1], None, ALU.mult, ALU.bypass, [B_wof[fc % 2], B_gn], [B_wo])
        for e in range(min(NSLOT, n_exp)):
            load_expert(e)
        cat = [sb("cat%d" % i, [128, D], stack=M) for i in range(2)]; B_cat = bufs("cat", 2)
        xo = [sb("xo%d" % i, [128, D], stack=M) for i in range(2)]; B_xo = bufs("xo", 2)
        catT = [sb("catT%d" % i, [128, 8, 128], BF16, stack=M) for i in range(2)]; B_catT = bufs("catT", 2)
        pre = [sb("pre%d" % i, [128, D], stack=M) for i in range(2)]; B_pre = bufs("pre", 2)
        hTf = [sb("hTf%d" % i, [128, 8, 128], stack=M) for i in range(2)]; B_hTf = bufs("hTf", 2)
        htm = [sb("htm%d" % i, [128, D], stack=M) for i in range(2)]; B_htm = bufs("htm", 2)
        junk = sb("junk", [128, 512], stack=M); B_junk = Buf("junk")
        st = sb("st", [128, 12], stack=M); mv = sb("mv", [128, 2], stack=M); sc_ = sb("sc_", [128, 8], stack=M); B_st = Buf("st")
        rt = sb("rt", [128, 160], stack=M); B_rt = Buf("rt")
        scA = sb("scA", [128, 8], stack=M); B_stA = Buf("stA")
        comb = sb("comb", [128, 32], stack=M); B_comb = Buf("comb")
        mixps = [[], []]

        def stageA(t):
            s = t % 2
            fw.dma(sp, cat[s][:, 0:512], attn_s[:, t * 512:(t + 1) * 512], reads=[B_attn_s], writes=[B_cat[s]], sem="cat%d" % s)
            fw.dma(sp, cat[s][:, 512:1024], g_s[:, t * 512:(t + 1) * 512], reads=[B_gs], writes=[B_cat[s]], sem="cat%d" % s)
            fw.group_done("cat%d" % s, [B_cat[s]])
            fw.dma(sp, xo[s][:], xf_v[t, 0:128, :], writes=[B_xo[s]], sem="xo%d" % s)
            for hf in range(2):
                A(act, junk[:], cat[s][:, hf * 512:(hf + 1) * 512], AF.Square, [B_cat[s]], [B_junk, B_stA], accum_out=scA[:, 2 + hf:3 + hf])
                A(act, scA[:, 4 + hf:5 + hf], scA[:, 2 + hf:3 + hf], AF.Ln, [B_stA, B_eps], [B_stA], scale=1.0 / 512, bias=epsc[:, 0:1])
                A(act, scA[:, 6 + hf:7 + hf], scA[:, 4 + hf:5 + hf], AF.Exp, [B_stA], [B_stA], scale=-0.5)
                A(act, cat[s][:, hf * 512:(hf + 1) * 512], cat[s][:, hf * 512:(hf + 1) * 512], AF.Copy, [B_cat[s], B_stA], [B_cat[s]],
                  scale=scA[:, 6 + hf:7 + hf])
            for hf in range(2):
                ps, Bp = ps_next()
                for q4 in range(4):
                    fc = hf * 4 + q4
                    TR(ps[:, q4 * 128:(q4 + 1) * 128], cat[s][:, fc * 128:(fc + 1) * 128], [B_cat[s]], [Bp])
                A(act, catT[s][:, hf * 4:(hf + 1) * 4, :], ps[:, :].rearrange("p (a b) -> p a b", a=4), AF.Copy, [Bp], [B_catT[s]])
            for hf in range(2):
                ps, Bp = ps_next()
                for fc in range(8):
                    MM(ps[:, :], catT[s][:, fc, :], w_o_s[:, fc, hf * 512:(hf + 1) * 512], fc == 0, fc == 7, [B_catT[s], B_wo], [Bp])
                mixps[t % 2].append((ps, Bp))

        def stageA_pre(t):
            s = t % 2
            for hf in range(2):
                ps, Bp = mixps[t % 2][hf]
                TT(dve, pre[s][:, hf * 512:(hf + 1) * 512], ps[:, :], grow[:, 0, hf * 512:(hf + 1) * 512], ALU.mult, [Bp, B_grow], [B_pre[s]])
            mixps[t % 2].clear()
            STT(pre[s][:], xo[s][:], ALPHA, pre[s][:], ALU.mult, ALU.add, [B_xo[s], B_pre[s]], [B_pre[s]])

        def stageB(t):
            s = t % 2
            layer_norm(pre[s][:], B_pre[s], 0, pre[s][:], B_pre[s], st, mv, sc_, B_st)
            fw.dma(sp, x1_s[:, t * D:(t + 1) * D], pre[s][:], reads=[B_pre[s]], writes=[B_x1s], sem="x1s")
            TT(dve, htm[s][:], pre[s][:], grow[:, 2, :], ALU.mult, [B_pre[s], B_grow], [B_htm[s]])
            TT(dve, htm[s][:], htm[s][:], grow[:, 1, :], ALU.add, [B_htm[s], B_grow], [B_htm[s]])
            for hf in range(2):
                ps, Bp = ps_next()
                for q4 in range(4):
                    fc = hf * 4 + q4
                    TR(ps[:, q4 * 128:(q4 + 1) * 128], htm[s][:, fc * 128:(fc + 1) * 128], [B_htm[s]], [Bp])
                A(act, hT[:, hf * 4:(hf + 1) * 4, t * 128:(t + 1) * 128], ps[:, :].rearrange("p (a b) -> p a b", a=4), AF.Copy, [Bp], [B_hT])
                A(act, hTf[s][:, hf * 4:(hf + 1) * 4, :], ps[:, :].rearrange("p (a b) -> p a b", a=4), AF.Copy, [Bp], [B_hTf[s]])

        rps = [None]

        def stageC_mm(t):
            s = t % 2
            ps, Bp = ps_next()
            for fc in range(8):
                MM(ps[:, 0:36], hTf[s][:, fc, :], w_r[:, fc, :], fc == 0, fc == 7, [B_hTf[s], B_wr], [Bp])
            rps[0] = (ps, Bp)

        def stageC(t):
            s = t % 2
            ps, Bp = rps[0]
            lg = rt[:, 0:36]
            gsel = rt[:, 36:40]; gmax = rt[:, 40:41]; ngmax = rt[:, 41:42]; gsum = rt[:, 42:43]; g_w = rt[:, 43:44]
            esel = rt[:, 44:52]; m1 = rt[:, 52:53]; mask1 = rt[:, 56:64]; esel2 = rt[:, 64:72]; m2 = rt[:, 53:54]; mask2 = rt[:, 72:80]
            nm1 = rt[:, 54:55]; e2 = rt[:, 55:56]; den_ = rt[:, 80:81]; w1 = rt[:, 81:82]; w2 = rt[:, 82:83]; cw8 = rt[:, 88:96]; gex = rt[:, 96:100]
            RR = [B_rt]
            TT(dve, lg, ps[:, 0:36], b_r[:, :], ALU.add, [Bp, B_wr], RR)
            fw.op(dve, lambda e: e.reduce_max(out=gmax, in_=rt[:, 0:4], axis=AX.X), reads=RR, writes=RR)
            TS(dve, gsel, rt[:, 0:4], gmax, None, ALU.is_ge, ALU.bypass, RR, RR, sreads=RR)
            TS(dve, ngmax, gmax, -1.0, None, ALU.mult, ALU.bypass, RR, RR)
            A(act, gex, rt[:, 0:4], AF.Exp, RR, RR, bias=ngmax, accum_out=gsum)
            fw.op(dve, lambda e: e.reciprocal(out=g_w, in_=gsum), reads=RR, writes=RR)
            TS(dve, esel, rt[:, 4:12], gsel[:, 0:1], None, ALU.mult, ALU.bypass, RR, RR, sreads=RR)
            for g in range(1, 4):
                fw.op(dve, lambda e: e.scalar_tensor_tensor(out=esel, in0=rt[:, 4 + 8 * g:12 + 8 * g], scalar=gsel[:, g:g + 1], in1=esel,
                                                            op0=ALU.mult, op1=ALU.add), reads=RR, writes=RR, sreads=RR)
            fw.op(dve, lambda e: e.reduce_max(out=m1, in_=esel, axis=AX.X), reads=RR, writes=RR)
            TS(dve, mask1, esel, m1, None, ALU.is_ge, ALU.bypass, RR, RR, sreads=RR)
            STT(esel2, mask1, -1e30, esel, ALU.mult, ALU.add, RR, RR)
            fw.op(dve, lambda e: e.reduce_max(out=m2, in_=esel2, axis=AX.X), reads=RR, writes=RR)
            TS(dve, mask2, esel2, m2, None, ALU.is_ge, ALU.bypass, RR, RR, sreads=RR)
            TS(dve, nm1, m1, -1.0, None, ALU.mult, ALU.bypass, RR, RR)
            A(act, e2, m2, AF.Exp, RR, RR, bias=nm1)
            TS(dve, den_, e2, 1.0, None, ALU.add, ALU.bypass, RR, RR)
            fw.op(dve, lambda e: e.reciprocal(out=w1, in_=den_), reads=RR, writes=RR)
            TT(dve, w2, e2, w1, ALU.mult, RR, RR)
            TT(dve, w1, w1, g_w, ALU.mult, RR, RR)
            TT(dve, w2, w2, g_w, ALU.mult, RR, RR)
            TS(dve, cw8, mask1, w1, None, ALU.mult, ALU.bypass, RR, RR, sreads=RR)
            fw.op(dve, lambda e: e.scalar_tensor_tensor(out=cw8, in0=mask2, scalar=w2, in1=cw8, op0=ALU.mult, op1=ALU.add),
                  reads=RR, writes=RR, sreads=RR)
            for g in range(4):
                TS(dve, comb[:, g * 8:(g + 1) * 8], cw8, gsel[:, g:g + 1], None, ALU.mult, ALU.bypass, RR, [B_comb], sreads=RR)

        def stageC_tail(t):
            s = t % 2
            ps, Bp = ps_next()
            TR(ps[0:32, 0:128], comb[:, :], [B_comb], [Bp])
            CP(dve, combT[:, t * 128:(t + 1) * 128], ps[0:32, 0:128], [Bp], [B_combT])
            if t == 0:
                dump("x1_0", pre[s][:], [B_pre[s]])
                dump("comb0", comb[:], [B_comb])

        stageA(0)
        stageA_pre(0)
        for t in range(17):
            if t >= 1:
                stageC_mm(t - 1)
            if t + 1 < 16:
                stageA(t + 1)
            if t < 16:
                stageB(t)
            if t >= 1:
                stageC(t - 1)
            if t + 1 < 16:
                stageA_pre(t + 1)
            if t >= 1:
                stageC_tail(t - 1)
    fw.barrier()
    if stop_after == "merge":
        return finish()

    with ExitStack() as E:
        ln2 = sb("ln2rows", [128, 2, D], stack=E); lnbox[0] = ln2; lnbox[1] = Buf("ln2")
        fw.dma(sp, ln2[:], ln_d[:, 2:4, :], writes=[lnbox[1]], sem="e9")
        acc = sb("acc", [128, 16, D], stack=E); B_acc = bufs("acc", 16)
        sel = sb("sel", [32, NE * 128], BF16, stack=E); B_sel = Buf("sel")
        fw.dma(pool, sel[:], sel_d[:, :], writes=[B_sel], sem="e0")
        sg = [sb("sg%d" % i, [128, 512], stack=E) for i in range(2)]; B_sg = bufs("sg", 2)
        tu = [sb("tu%d" % i, [128, 512], stack=E) for i in range(2)]; B_tu = bufs("tu", 2)
        gT2 = [sb("gT2_%d" % i, [128, 2, 256], BF16, stack=E) for i in range(2)]; B_gT2 = bufs("gT2", 2)
        n_exp = int(os.environ.get("K_NEXP", str(NE)))

        items = [(e, tg) for e in range(n_exp) for tg in range(8)]
        dcnt = [0]

        csb = [sb("csb%d" % i, [128, 256], BF16, stack=E) for i in range(2)]; B_csb = bufs("csb", 2)

        def AUC(i):
            e, tg = items[i]
            sl_ = e % NSLOT
            k2 = i % 2
            pa, Bpa = PS[k2 * 2], PSB[k2 * 2]
            pu, Bpu = PS[k2 * 2 + 1], PSB[k2 * 2 + 1]
            pc_, Bpc = PS[4], PSB[4]
            MM(pc_[:, 0:256], sel[0:32, e * 128:(e + 1) * 128], combT[0:32, tg * 256:(tg + 1) * 256], True, True, [B_sel, B_combT], [Bpc])
            for fc in range(2):
                for ch in range(8):
                    MM(pa[:, fc * 256:(fc + 1) * 256], Wg[sl_][:, ch, fc * 128:(fc + 1) * 128], hT[:, ch, tg * 256:(tg + 1) * 256],
                       ch == 0, ch == 7, [B_W[sl_], B_hT], [Bpa])
            for fc in range(2):
                for ch in range(8):
                    MM(pu[:, fc * 256:(fc + 1) * 256], Wu[sl_][:, ch, fc * 128:(fc + 1) * 128], hT[:, ch, tg * 256:(tg + 1) * 256],
                       ch == 0, ch == 7, [B_W[sl_], B_hT], [Bpu])

        def REST(i):
            e, tg = items[i]
            sl_ = e % NSLOT
            k2 = i % 2
            pa, Bpa = PS[k2 * 2], PSB[k2 * 2]
            pu, Bpu = PS[k2 * 2 + 1], PSB[k2 * 2 + 1]
            A(act, sg[k2][:, :], pa[:, :], AF.Silu, [Bpa], [B_sg[k2]])
            if i + 1 < len(items):
                A(act, csb[1 - k2][:, :], PS[4][:, 0:256], AF.Copy, [PSB[4]], [B_csb[1 - k2]])
            TT(dve, tu[k2][:, :], pu[:, :], sg[k2][:, :], ALU.mult, [Bpu, B_sg[k2]], [B_tu[k2]])
            TT(dve, gT2[k2][:], tu[k2][:, :].rearrange("p (a b) -> p a b", a=2), sbap(csb[k2], 0, [[0, 2], [1, 256]]), ALU.mult,
               [B_tu[k2], B_csb[k2]], [B_gT2[k2]])
            for tt_ in range(2):
                tile_ = tg * 2 + tt_
                for hf in range(2):
                    po, Bpo = PS[5 + dcnt[0] % 3], PSB[5 + dcnt[0] % 3]
                    dcnt[0] += 1
                    for fc in range(2):
                        MM(po[:, :], gT2[k2][:, fc, tt_ * 128:(tt_ + 1) * 128], Wd[sl_][:, fc, hf * 512:(hf + 1) * 512],
                           fc == 0, fc == 1, [B_gT2[k2], B_W[sl_]], [Bpo])
                    ao = acc[:, tile_, hf * 512:(hf + 1) * 512]
                    if e == 0:
                        CP(dve, ao, po[:, :], [Bpo], [B_acc[tile_]])
                    else:
                        TT(dve, ao, po[:, :], ao, ALU.add, [Bpo, B_acc[tile_]], [B_acc[tile_]])
            if tg == 7 and e + NSLOT < n_exp:
                load_expert(e + NSLOT)

        x1t = [stg[i][:, 0:D] for i in range(2)]; B_x1t = B_stg
        st2 = sb("st2", [128, 12], stack=E); mv2 = sb("mv2", [128, 2], stack=E); sc2 = sb("sc2", [128, 8], stack=E); B_st2 = Buf("st2")
        B_out = Buf("out")
        st2s = [sb("st2_%d" % i, [128, 12], stack=E) for i in range(2)]; mv2s = [sb("mv2_%d" % i, [128, 2], stack=E) for i in range(2)]
        sc2s = [sb("sc2_%d" % i, [128, 8], stack=E) for i in range(2)]; B_st2s = bufs("st2s", 2)

        def F1(t):
            s = t % 2
            fw.dma(sp, x1t[s], x1_s[:, t * D:(t + 1) * D], reads=[B_x1s], writes=[B_x1t[s]], sem="stg%d" % s)
            TT(dve, acc[:, t, :], acc[:, t, :], grow[:, 3, :], ALU.mult, [B_acc[t], B_grow], [B_acc[t]])
            STT(acc[:, t, :], x1t[s], ALPHA, acc[:, t, :], ALU.mult, ALU.add, [B_x1t[s], B_acc[t]], [B_acc[t]])
            pre_ = acc[:, t, :]
            for hf in range(2):
                fw.op(dve, lambda e: e.bn_stats(out=st2s[s][:, hf * 6:(hf + 1) * 6], in_=pre_[:, hf * 512:(hf + 1) * 512]), reads=[B_acc[t]], writes=[B_st2s[s]])
            fw.op(dve, lambda e: e.bn_aggr(out=mv2s[s][:, 0:2], in_=st2s[s][:, 0:12]), reads=[B_st2s[s]], writes=[B_st2s[s]])
            A(act, sc2s[s][:, 0:1], mv2s[s][:, 1:2], AF.Ln, [B_st2s[s], B_eps], [B_st2s[s]], bias=epsc[:, 0:1])
            A(act, sc2s[s][:, 1:2], sc2s[s][:, 0:1], AF.Exp, [B_st2s[s]], [B_st2s[s]], scale=-0.5)

        def F2(t):
            s = t % 2
            o_ = acc[:, t, :]
            lnrows, B_ln = lnbox
            TS(dve, o_, o_, mv2s[s][:, 0:1], sc2s[s][:, 1:2], ALU.subtract, ALU.mult, [B_acc[t]], [B_acc[t]], sreads=[B_st2s[s]])
            TT(dve, o_, o_, lnrows[:, 0, :], ALU.mult, [B_acc[t], B_ln], [B_acc[t]])
            TT(dve, o_, o_, lnrows[:, 1, :], ALU.add, [B_acc[t], B_ln], [B_acc[t]])
            fw.dma(sp, out_d[t * 128:(t + 1) * 128, :], o_, reads=[B_acc[t]], writes=[B_out], sem="out")
        AUC(0)
        A(act, csb[0][:, :], PS[4][:, 0:256], AF.Copy, [PSB[4]], [B_csb[0]])
        for i in range(len(items)):
            if i + 1 < len(items):
                AUC(i + 1)
            REST(i)
            if items[i][0] == n_exp - 1:
                tg_ = items[i][1]
                F1(2 * tg_); F1(2 * tg_ + 1); F2(2 * tg_); F2(2 * tg_ + 1)

    return finish()


def _rope_tables(frame_tok):
    t = np.asarray(frame_tok)
    row = (t // 64).astype(np.float32)
    col = (t % 64).astype(np.float32)
    inv = (10000.0 ** (-np.arange(0, 16, 2, dtype=np.float32) / np.float32(16))).astype(np.float32)
    ang = np.concatenate([row[:, None] * inv, col[:, None] * inv], -1)
    ang = np.concatenate([ang, ang], -1).astype(np.float32)
    return np.cos(ang).astype(np.float32), np.sin(ang).astype(np.float32)


def prep_core(inp, b, hh):
    f32 = np.float32
    m = {}
    x = inp["x"][b]
    ctx = inp["ctx"][b]
    if hh == 1:
        x = x[::-1]
        ctx = ctx[::-1]
    m["xf"] = np.ascontiguousarray(x, f32)
    m["ctxf"] = np.ascontiguousarray(ctx, f32)
    cT = np.stack([inp["c"][b].reshape(8, 128).T, inp["c_ctx"].reshape(8, 128).T], -1)
    m["cT"] = np.ascontiguousarray(cT, f32)
    m["w_ada"] = inp["w_ada"][0]
    ba = inp["b_ada"][0]
    m["b_ada_2rows"] = np.ascontiguousarray(np.stack([ba, ba], 0), f32)
    m["w_in"] = inp["w_in"][0]
    m["qg_cols"] = np.ascontiguousarray(inp["q_norm_g"][0].reshape(2, 128).T, f32)
    m["kvg_col"] = np.ascontiguousarray(inp["kv_norm_g"][0].reshape(1, 128).T, f32)
    m["w_uq"] = inp["w_uq"][0]
    m["w_ukv"] = inp["w_ukv"][0]
    mt, j, i = np.meshgrid(np.arange(2), np.arange(16), np.arange(128), indexing="ij")
    tau = (16 * (128 * mt + i) + j).reshape(-1)
    tok = tau if hh == 0 else (NTOK - 1 - tau)
    cos, sin = _rope_tables(tok)
    rope = np.zeros((128, 2, NKEY), f32)
    rope[64:96, 0, :NTOK] = cos.T
    rope[64:96, 1, :NTOK] = sin.T
    rope[64:96, 0, NTOK:] = 1.0
    m["rope"] = rope
    dirs = [0, 1] if hh == 0 else [1, 0]

    def lay(a):
        a = a[dirs]
        sh = a.shape[3:]
        a = a.reshape(2, 16, 2, 64, *sh)
        a = np.moveaxis(a, (2, 3), (0, 1))
        return a.reshape(128, 32, *sh)

    m["ssm_a"] = np.ascontiguousarray(np.stack([lay(inp["ssm_a_re"][0]), lay(inp["ssm_a_im"][0])], 1), f32)
    ldt = np.broadcast_to(inp["ssm_log_dt"][0][:, :, None], (2, 32, 64))
    m["ssm_ldt"] = np.ascontiguousarray(lay(ldt), f32)
    m["ssm_b"] = np.ascontiguousarray(np.stack([lay(inp["ssm_b_re"][0]), lay(inp["ssm_b_im"][0])], 1), f32)
    cre = np.swapaxes(inp["ssm_c_re"][0], 2, 3)
    cim = np.swapaxes(inp["ssm_c_im"][0], 2, 3)
    m["ssm_c"] = np.ascontiguousarray(np.stack([lay(cre), lay(cim)], 1), f32)
    dd = inp["ssm_d"][0].reshape(32, 16)
    m["ssm_dcols"] = np.ascontiguousarray(np.broadcast_to(dd.T[None], (8, 16, 32)).reshape(128, 32), f32)
    jl = np.arange(128) // 16
    m0 = (jl[None, :] >= jl[:, None]).astype(f32)
    m1 = (jl[:, None] >= jl[None, :]).astype(f32)
    m["masks"] = np.ascontiguousarray(np.stack([m0, m1], 1), f32)
    m["ident"] = np.eye(128, dtype=f32)
    m["w_glu"] = inp["w_glu"][0]
    m["b_glu_row"] = np.ascontiguousarray(np.broadcast_to(inp["b_glu"][0][None], (128, 512)), f32)
    gn = np.concatenate([inp["gn_attn_g"][0], inp["gn_ssm_g"][0]])
    m["gn_cols"] = np.ascontiguousarray(gn.reshape(8, 128).T, f32)
    m["w_o"] = inp["w_o"][0]
    ln = np.stack([inp["ln1_g"][0], inp["ln1_b"][0], inp["ln2_g"][0], inp["ln2_b"][0]], 0)
    m["ln_rows"] = np.ascontiguousarray(np.broadcast_to(ln[None], (128, 4, D)), f32)
    m["w_r"] = np.ascontiguousarray(np.concatenate([inp["w_router_group"][0], inp["w_router_expert"][0]], 1), f32)
    br_ = np.concatenate([inp["b_router_group"][0], inp["b_router_expert"][0]])
    m["b_r_row"] = np.ascontiguousarray(np.broadcast_to(br_[None], (128, 36)), f32)
    m["w_exp_gate"] = inp["w_exp_gate"][0]
    m["w_exp_up"] = inp["w_exp_up"][0]
    m["w_exp_down"] = inp["w_exp_down"][0]
    sel = np.zeros((32, NE, 128), f32)
    sel[np.arange(32), np.arange(32), :] = 1.0
    m["sel"] = sel.reshape(32, NE * 128)
    return m


def run(inputs, stop_after=None, dbg_names=(), cores=8):
    inp = {k: np.asarray(v) for k, v in inputs.items()}
    nc = build(stop_after=stop_after, dbg_names=dbg_names)
    in_maps = [prep_core(inp, c // 2, c % 2) for c in range(cores)]
    res = run_bass_kernel_spmd(nc, in_maps, core_ids=list(range(cores)))
    return res.results


def kernel(**inputs):
    res = run(inputs)
    out = np.zeros((4, NTOK, D), np.float32)
    for c in range(8):
        b, hh = c // 2, c % 2
        o = res[c]["out"]
        o = o.reshape(16, 128, D).transpose(1, 0, 2).reshape(NOWN, D)
        if hh == 0:
            out[b, :NOWN] = o
        else:
            out[b, NOWN:] = o[::-1]
    return out
```

```python
import math
import os
from contextlib import ExitStack
import numpy as np
import concourse.bass as bass
import concourse.mybir as mybir
from concourse.ap import AP
from concourse.bass_utils import run_bass_kernel_spmd

F32 = mybir.dt.float32
BF16 = mybir.dt.bfloat16
AF = mybir.ActivationFunctionType
ALU = mybir.AluOpType
AX = mybir.AxisListType

D = 1024
NTOK = 4096
NOWN = 2048
NCTX = 256
NKEY = NTOK + NCTX
NH = 8
SCALE = 96 ** -0.5
ALPHA = 2.0 ** 0.25
EPS = 1e-6
NS = 134
WINDOW = False
NE = 32


class Buf:
    __slots__ = ("name", "w", "r", "excl")

    def __init__(self, name, excl=False):
        self.name = name
        self.w = None
        self.r = []
        self.excl = excl


def bufs(name, n):
    return [Buf("%s%d" % (name, i)) for i in range(n)]


class Q:
    def __init__(self, fw, name, eng, same=False):
        self.name = name
        self.eng = eng
        self.sem = fw.es.enter_context(fw.nc.semaphore("s_" + name))
        self.cnt = 0
        self.seen = {}
        self.same = same
        self.window = False


class DmaTok:
    def __init__(self, fw, name):
        self.sem = fw.es.enter_context(fw.nc.semaphore("d_" + str(name)))
        self.cnt = 0
        self.name = name
        self.same = True


class FW:
    def __init__(self, nc, es):
        self.nc = nc
        self.es = es
        self.pe = Q(self, "pe", nc.tensor)
        self.dve = Q(self, "dve", nc.vector, same=True)
        self.act = Q(self, "act", nc.scalar, same=True)
        self.dve.window = WINDOW
        self.pool = Q(self, "pool", nc.gpsimd, same=True)
        self.sp = Q(self, "sp", nc.sync)
        self.dmasems = {}

    def _waits(self, q, reads, writes, sreads=()):
        need = {}

        def add(dep, force=False):
            if dep is None:
                return
            dq, n = dep
            if dq is q and not force:
                if not q.same:
                    return
                if q.window and n < q.cnt:
                    return
            if need.get(dq, 0) < n:
                need[dq] = n

        for b in sreads:
            add(b.w, True)
        for b in reads:
            add(b.w)
            if b.excl:
                for d in b.r:
                    add(d)
        for b in writes:
            add(b.w)
            for d in b.r:
                add(d)
        for dq, n in need.items():
            if q.seen.get(id(dq), 0) >= n:
                continue
            q.eng.wait_ge(dq.sem, n)
            q.seen[id(dq)] = n

    def op(self, q, fn, reads=(), writes=(), sreads=()):
        self._waits(q, reads, writes, sreads)
        reads = list(reads) + list(sreads)
        ins = fn(q.eng)
        ins.then_inc(q.sem, 1)
        q.cnt += 1
        tok = (q, q.cnt)
        for b in reads:
            b.r.append(tok)
        for b in writes:
            b.w = tok
            b.r = []
        return ins

    def dma(self, q, out, in_, reads=(), writes=(), sem=None, **kw):
        self._waits(q, reads, writes)
        ds = self.dmasems.get(sem)
        if ds is None:
            ds = DmaTok(self, sem)
            self.dmasems[sem] = ds
        if out.dtype != in_.dtype and in_.ap[-1][1] > 2048:
            kw.setdefault("max_dma_last_dim", 4096)
        ins = q.eng.dma_start(out=out, in_=in_, **kw)
        ds.cnt += 16
        ins.then_inc(ds.sem, 16)
        tok = (ds, ds.cnt)
        for b in reads:
            b.r.append(tok)
        for b in writes:
            b.w = tok
            b.r = []
        return tok

    def barrier(self):
        qs = [self.pe, self.dve, self.act, self.pool, self.sp]
        for q in qs:
            for dq in qs + list(self.dmasems.values()):
                if dq is q or dq.cnt == 0:
                    continue
                if q.seen.get(id(dq), 0) >= dq.cnt:
                    continue
                q.eng.wait_ge(dq.sem, dq.cnt)
                q.seen[id(dq)] = dq.cnt

    def group_done(self, sem, blist):
        ds = self.dmasems[sem]
        for b in blist:
            if b.w is not None and b.w[0] is ds:
                b.w = (ds, ds.cnt)


def sbap(t, off, dims):
    full = t[:] if not isinstance(t, AP) else t
    return AP(full.tensor, full.offset + off, [list(full.ap[0])] + [list(d) for d in dims])


def part_ap(t, p0, npart, off, dims):
    full = t[:]
    pstep = full.ap[0][0]
    sub = t[p0:p0 + npart]
    return AP(sub.tensor, sub.offset + off, [[pstep, npart]] + [list(d) for d in dims])


def build(stop_after=None, dbg_names=()):
    nc = bass.Bass("TRN2", target_bir_lowering=False)
    es = ExitStack()
    fw = FW(nc, es)
    pe, dve, act, pool, sp = fw.pe, fw.dve, fw.act, fw.pool, fw.sp
    dbg = {}

    def din(name, shape, dt=F32):
        return nc.dram_tensor(name, list(shape), dt, kind="ExternalInput").ap()

    xf = din("xf", [NTOK, D])
    ctxf = din("ctxf", [NCTX, D])
    cT_d = din("cT", [128, 8, 2])
    w_ada_d = din("w_ada", [D, 6 * D])
    b_ada_2rows_d = din("b_ada_2rows", [2, 6 * D])
    w_in_d = din("w_in", [D, 928])
    qg_d = din("qg_cols", [128, 2])
    kvg_d = din("kvg_col", [128, 1])
    w_uq_d = din("w_uq", [256, 768])
    w_ukv_d = din("w_ukv", [128, 1024])
    rope_d = din("rope", [128, 2, NKEY])
    ssm_a_d = din("ssm_a", [128, 2, 32])
    ssm_ldt_d = din("ssm_ldt", [128, 32])
    ssm_b_d = din("ssm_b", [128, 2, 32, 16])
    ssm_c_d = din("ssm_c", [128, 2, 32, 16])
    ssm_dcols_d = din("ssm_dcols", [128, 32])
    masks_d = din("masks", [128, 2, 128])
    ident_d = din("ident", [128, 128])
    w_glu_d = din("w_glu", [512, 512])
    b_glu_d = din("b_glu_row", [128, 512])
    gn_d = din("gn_cols", [128, 8])
    w_o_d = din("w_o", [D, D])
    ln_d = din("ln_rows", [128, 4, D])
    w_r_d = din("w_r", [D, 36])
    b_r_d = din("b_r_row", [128, 36])
    wg_d = din("w_exp_gate", [NE, D, 256])
    wu_d = din("w_exp_up", [NE, D, 256])
    wd_d = din("w_exp_down", [NE, 256, D])
    sel_d = din("sel", [32, NE * 128])
    out_d = nc.dram_tensor("out", [NOWN, D], F32, kind="ExternalOutput").ap()
    ug_s = nc.dram_tensor("ug_s", [128, 32 * 2 * 272], BF16, kind="Internal").ap()
    attn_s = nc.dram_tensor("attn_s", [128, 16 * 512], F32, kind="Internal").ap()
    x1_s = nc.dram_tensor("x1_s", [128, 16 * D], F32, kind="Internal").ap()
    g_s = nc.dram_tensor("g_s", [128, 16 * 512], F32, kind="Internal").ap()
    dbg_out = {}
    for ent in dbg_names:
        nm, shp = ent[0], ent[1]
        dbg_out[nm] = nc.dram_tensor("dbg_" + nm, list(shp), BF16 if (len(ent) > 2 and ent[2] == "bf16") else F32, kind="ExternalOutput").ap()

    def sb(name, shape, dt=F32, stack=None):
        return (stack or es).enter_context(nc.sbuf_tensor("sb_" + name, list(shape), dt))

    PS = [es.enter_context(nc.psum_tensor("ps%d" % i, [128, 512], F32)) for i in range(8)]
    PSB = [Buf("ps%d" % i, excl=True) for i in range(8)]
    ps_rr = [0]

    def ps_next(k=None):
        i = ps_rr[0] % 8
        ps_rr[0] += 1
        return PS[i], PSB[i]

    ident = sb("ident", [128, 128]); B_ident = Buf("ident")
    ones_bf = sb("ones_bf", [128, 128], BF16); B_ones = Buf("ones")
    modc = sb("modc", [128, 96]); B_modc = Buf("modc")
    grow = sb("grow", [128, 4, D]); B_grow = Buf("grow")
    fw.dma(sp, ident[:], ident_d[:, :], writes=[B_ident], sem="c0")
    fw.op(dve, lambda e: e.memset(ones_bf[:], 1.0), writes=[B_ones])

    def dump(name, ap, reads):
        if name in dbg_out:
            fw.dma(sp, dbg_out[name], ap, reads=reads, writes=[], sem="dbg")

    def finish():
        for ds in fw.dmasems.values():
            sp.eng.wait_ge(ds.sem, ds.cnt)
        es.close()
        return nc

    with ExitStack() as ph:
        cT = sb("cT", [128, 8, 2], stack=ph); B_cT = Buf("cT")
        sil2 = sb("sil2", [128, 8, 2], stack=ph); B_sil = Buf("sil2")
        b2 = sb("b2rows", [2, 6 * D], stack=ph); B_b2 = Buf("b2")
        modrow = sb("modrow", [2, 6 * D], stack=ph); B_mr = Buf("modrow")
        ones_f = sb("ones_f", [1, 128], stack=ph); B_onesf = Buf("ones_f")
        wa = [sb("wa%d" % i, [128, 8, D], stack=ph) for i in range(2)]
        B_wa = bufs("wa", 2)
        fw.dma(sp, cT[:], cT_d[:, :, :], writes=[B_cT], sem="c1")
        fw.dma(sp, b2[:], b_ada_2rows_d[:, :], writes=[B_b2], sem="c2")
        fw.op(dve, lambda e: e.memset(ones_f[:], 1.0), writes=[B_onesf])
        fw.op(act, lambda e: e.activation(out=sil2[:], in_=cT[:], func=AF.Silu), reads=[B_cT], writes=[B_sil])
        w_ada_v = w_ada_d.rearrange("(k p) n -> p k n", p=128)
        for v in range(6):
            slot = v % 2
            fw.dma(sp if v % 2 == 0 else act, wa[slot][:], w_ada_v[:, :, v * D:(v + 1) * D], writes=[B_wa[slot]], sem="wa%d" % slot)
            for half in range(2):
                pr, Bpr = PS[half], PSB[half]
                for k in range(8):
                    fw.op(pe, lambda e: e.matmul(pr[0:2, :], lhsT=sil2[:, k, :], rhs=wa[slot][:, k, half * 512:(half + 1) * 512],
                                                 start=(k == 0), stop=(k == 7)), reads=[B_wa[slot], B_sil], writes=[Bpr])
                c0 = v * D + half * 512
                fw.op(dve, lambda e: e.tensor_tensor(out=modrow[0:2, c0:c0 + 512], in0=pr[0:2, :], in1=b2[0:2, c0:c0 + 512], op=ALU.add),
                      reads=[Bpr, B_b2], writes=[B_mr])
        for v in (1, 4):
            fw.op(dve, lambda e: e.tensor_scalar_add(out=modrow[0:2, v * D:(v + 1) * D], in0=modrow[0:2, v * D:(v + 1) * D], scalar1=1.0),
                  reads=[B_mr], writes=[B_mr])
        psc, Bpsc = PS[2], PSB[2]
        for vc in range(48):
            fw.op(pe, lambda e: e.matmul(psc[:, vc * 2:vc * 2 + 2], lhsT=modrow[0:2, vc * 128:(vc + 1) * 128], rhs=ident[0:2, 0:2],
                                         start=True, stop=True, is_transpose=True), reads=[B_mr, B_ident], writes=[Bpsc])
        fw.op(dve, lambda e: e.tensor_copy(out=modc[:], in_=psc[:, 0:96]), reads=[Bpsc], writes=[B_modc])
        for ri, v in enumerate((2, 3, 4, 5)):
            for half in range(2):
                pr, Bpr = PS[3 + (ri * 2 + half) % 2], PSB[3 + (ri * 2 + half) % 2]
                c0 = v * D + half * 512
                fw.op(pe, lambda e: e.matmul(pr[:, :], lhsT=ones_f[0:1, :], rhs=modrow[0:1, c0:c0 + 512], start=True, stop=True),
                      reads=[B_onesf, B_mr], writes=[Bpr])
                fw.op(act, lambda e: e.activation(out=grow[:, ri, half * 512:(half + 1) * 512], in_=pr[:, :], func=AF.Copy),
                      reads=[Bpr], writes=[B_grow])
    fw.barrier()
    dump("modc", modc[:], [B_modc])
    dump("grow", grow[:, 0:2].rearrange("p a b -> p (a b)"), [B_grow])
    if stop_after == "adaln":
        return finish()

    def mcol(v, ch, who):
        i = (v * 8 + ch) * 2 + who
        return modc[:, i:i + 1]

    def A(q, out, in_, func, reads, writes, **kw):
        return fw.op(q, lambda e: e.activation(out=out, in_=in_, func=func, **kw), reads=reads, writes=writes)

    def TT(q, out, in0, in1, op, reads, writes):
        return fw.op(q, lambda e: e.tensor_tensor(out=out, in0=in0, in1=in1, op=op), reads=reads, writes=writes)

    def TS(q, out, in0, s1, s2, op0, op1, reads, writes, sreads=()):
        return fw.op(q, lambda e: e.tensor_scalar(out=out, in0=in0, scalar1=s1, scalar2=s2, op0=op0, op1=op1), reads=reads, writes=writes, sreads=sreads)

    def STT(out, in0, scalar, in1, op0, op1, reads, writes):
        return fw.op(dve, lambda e: e.scalar_tensor_tensor(out=out, in0=in0, scalar=scalar, in1=in1, op0=op0, op1=op1), reads=reads, writes=writes)

    def CP(q, out, in_, reads, writes):
        return fw.op(q, lambda e: e.tensor_copy(out=out, in_=in_), reads=reads, writes=writes)

    def MM(out, lhsT, rhs, start, stop, reads, writes):
        return fw.op(pe, lambda e: e.matmul(out, lhsT=lhsT, rhs=rhs, start=start, stop=stop), reads=reads, writes=writes)

    def TR(out, in_, reads, writes, n=128):
        return fw.op(pe, lambda e: e.matmul(out, lhsT=in_, rhs=ident[0:n, 0:n], start=True, stop=True, is_transpose=True),
                     reads=list(reads) + [B_ident], writes=writes)

    def rstd_from_ss(out, ss_ps, B_ss, inv_n, tmp, B_tmp, B_out):
        A(act, tmp, ss_ps, AF.Ln, [B_ss, B_eps], [B_tmp], scale=inv_n, bias=epsc[:, 0:1])
        A(act, out, tmp, AF.Exp, [B_tmp], [B_out], scale=-0.5)

    epsc = sb("epsc", [128, 1]); B_eps = Buf("eps")
    fw.op(dve, lambda e: e.memset(epsc[:], EPS), writes=[B_eps])

    att = ExitStack()
    cqT = sb("cqT", [128, 2, NOWN], BF16, stack=att); B_cqT = Buf("cqT")
    rstdq = sb("rstdq", [128, NOWN], stack=att); B_rstdq = Buf("rstdq")
    ckvnT = sb("ckvnT", [128, NKEY], BF16, stack=att); B_ckvnT = Buf("ckvnT")
    KRT = sb("KRT", [128, NKEY], BF16, stack=att); B_KRT = Buf("KRT")

    with ExitStack() as ph:
        w_in = sb("w_in", [128, 8, 928], BF16, stack=ph); B_win = Buf("w_in")
        w_krot = sb("w_krot", [128, 8, 32], BF16, stack=ph); B_wkrot = Buf("w_krot")
        fw.dma(pool, w_in[:], w_in_d.rearrange("(k p) n -> p k n", p=128), writes=[B_win], sem="w_in")
        CP(dve, w_krot[:, :, 16:32], w_in[:, :, 384:400], [B_win], [B_wkrot])
        TS(dve, w_krot[:, :, 0:16], w_in[:, :, 400:416], -1.0, None, ALU.mult, ALU.bypass, [B_win], [B_wkrot])
        xin = [sb("xin%d" % i, [128, D], stack=ph) for i in range(4)]; B_xin = bufs("xin", 4)
        xmT = [sb("xmT%d" % i, [128, 8, 512], BF16, stack=ph) for i in range(2)]; B_xmT = bufs("xmT", 2)
        ropeg = [sb("ropeg%d" % i, [128, 2, 512], stack=ph) for i in range(2)]; B_ropeg = bufs("ropeg", 2)
        u_tm = sb("u_tm", [128, 32, 128], stack=ph); B_utm = Buf("u_tm")
        Ug = sb("Ug", [128, 32, 2, 272], BF16, stack=ph); B_Ug = Buf("Ug")
        sqb = sb("sqb", [128, 512], BF16, stack=ph); B_sqb = Buf("sqb")
        sqq = sb("sqq", [128, 2, 512], BF16, stack=ph); B_sqq = Buf("sqq")
        rawkv = sb("rawkv", [128, 512], stack=ph); B_rawkv = Buf("rawkv")
        lnt = sb("lnt", [128, 512], stack=ph); B_lnt = Buf("lnt")
        rstdkv = sb("rstdkv", [128, 512], stack=ph); B_rstdkv = Buf("rstdkv")
        rt1 = sb("rt1", [128, 512], stack=ph); B_rt1 = Buf("rt1")
        rt2 = sb("rt2", [128, 512], stack=ph); B_rt2 = Buf("rt2")
        xf_v = xf.rearrange("(m j) d -> j m d", j=16)
        ctx_v = ctxf.rearrange("(m j) d -> j m d", j=16)
        gcount = [0]

        def in_group(kind, mt, jg):
            gs = gcount[0] % 2
            gcount[0] += 1
            lat = kind == "lat"
            ncols = 512 if lat else 256
            col0 = (mt * 16 + jg * 4) * 128 if lat else NTOK
            own = lat and mt == 0
            np_ = 128 if lat else 16
            nt = 4 if lat else 16
            tw = 128 if lat else 16
            if lat:
                for t in range(4):
                    j = jg * 4 + t
                    fw.dma(sp, xin[t][:], xf_v[j, mt * 128:(mt + 1) * 128, :], writes=[B_xin[t]], sem="xin%d" % t)
            fw.dma(sp, ropeg[gs][64:96, :, 0:ncols], rope_d[64:96, :, col0:col0 + ncols], writes=[B_ropeg[gs]], sem="ropeg%d" % gs)
            who = 0 if lat else 1
            if lat:
                for ch in range(8):
                    ps, Bp = ps_next()
                    for t in range(4):
                        TR(ps[:, t * 128:(t + 1) * 128], xin[t][:, ch * 128:(ch + 1) * 128], [B_xin[t]], [Bp])
                    A(act, xmT[gs][:, ch, 0:ncols], ps[:, 0:ncols], AF.Identity, [Bp, B_modc], [B_xmT[gs]],
                      scale=mcol(1, ch, who), bias=mcol(0, ch, who))
            else:
                banks = [ps_next() for _ in range(8)]
                for t in range(16):
                    fw.dma(sp, xin[t % 4][0:16, :], ctx_v[t, 0:16, :], writes=[B_xin[t % 4]], sem="xin%d" % (t % 4))
                    for ch in range(8):
                        ps, Bp = banks[ch]
                        TR(ps[:, t * 16:(t + 1) * 16], xin[t % 4][0:16, ch * 128:(ch + 1) * 128], [B_xin[t % 4]], [Bp], n=16)
                for ch in range(8):
                    ps, Bp = banks[ch]
                    A(act, xmT[gs][:, ch, 0:ncols], ps[:, 0:ncols], AF.Identity, [Bp, B_modc], [B_xmT[gs]],
                      scale=mcol(1, ch, who), bias=mcol(0, ch, who))
            X = xmT[gs]; BX = B_xmT[gs]
            kstep = 9
            yield
            ps, Bp = ps_next()
            for ch in range(8):
                MM(ps[:, 0:ncols], w_in[:, ch, 256:384], X[:, ch, 0:ncols], ch == 0, ch == 7, [B_win, BX], [Bp])
            A(act, sqb[:, 0:ncols], ps[:, 0:ncols], AF.Square, [Bp], [B_sqb])
            A(act, rawkv[:, 0:ncols], ps[:, 0:ncols], AF.Copy, [Bp], [B_rawkv])
            ps2, Bp2 = ps_next()
            MM(ps2[:, 0:ncols], ones_bf[:, :], sqb[:, 0:ncols], True, True, [B_ones, B_sqb], [Bp2])
            rstd_from_ss(rstdkv[:, 0:ncols], ps2[:, 0:ncols], Bp2, 1.0 / 128, lnt[:, 0:ncols], B_lnt, B_rstdkv)
            TT(dve, ckvnT[:, col0:col0 + ncols], rawkv[:, 0:ncols], rstdkv[:, 0:ncols], ALU.mult, [B_rawkv, B_rstdkv], [B_ckvnT])
            psa, Bpa = ps_next()
            psb, Bpb = ps_next()
            for ch in range(8):
                MM(psa[64:96, 0:ncols], w_in[:, ch, 384:416], X[:, ch, 0:ncols], ch == 0, ch == 7, [B_win, BX], [Bpa])
            for ch in range(8):
                MM(psb[64:96, 0:ncols], w_krot[:, ch, :], X[:, ch, 0:ncols], ch == 0, ch == 7, [B_wkrot, BX], [Bpb])
            TT(dve, rt1[64:96, 0:ncols], psa[64:96, 0:ncols], ropeg[gs][64:96, 0, 0:ncols], ALU.mult, [Bpa, B_ropeg[gs]], [B_rt1])
            TT(dve, rt2[64:96, 0:ncols], psb[64:96, 0:ncols], ropeg[gs][64:96, 1, 0:ncols], ALU.mult, [Bpb, B_ropeg[gs]], [B_rt2])
            TT(dve, KRT[64:96, col0:col0 + ncols], rt1[64:96, 0:ncols], rt2[64:96, 0:ncols], ALU.add, [B_rt1, B_rt2], [B_KRT])
            if own:
                pss = []
                for fc in range(2):
                    ps, Bp = ps_next()
                    for ch in range(8):
                        MM(ps[:, :], w_in[:, ch, fc * 128:(fc + 1) * 128], X[:, ch, :], ch == 0, ch == 7, [B_win, BX], [Bp])
                    A(act, cqT[:, fc, col0:col0 + 512], ps[:, :], AF.Copy, [Bp], [B_cqT])
                    pss.append((ps, Bp))
                ksub = int(os.environ.get("K_SUB", "9"))
                if ksub >= 1:
                    ps2, Bp2 = ps_next()
                    for fc in range(2):
                        ps, Bp = pss[fc]
                        A(act, sqq[:, fc, :], ps[:, :], AF.Square, [Bp], [B_sqq])
                    for fc in range(2):
                        MM(ps2[:, :], ones_bf[:, :], sqq[:, fc, :], fc == 0, fc == 1, [B_ones, B_sqq], [Bp2])
                if ksub >= 2:
                    rstd_from_ss(rstdq[:, col0:col0 + 512], ps2[:, :], Bp2, 1.0 / 256, lnt[:, :], B_lnt, B_rstdq)
            for t in range(nt):
                ps, Bp = ps_next()
                for ch in range(8):
                    MM(ps[0:np_, :], X[:, ch, t * tw:(t + 1) * tw], w_in[:, ch, 416:928], ch == 0, ch == 7, [B_win, BX], [Bp])
                if lat:
                    jl = (jg * 4 + t) % 8
                    uo = sbap(u_tm, jl * 16, [[128, 32], [1, 16]])
                    ui = sbap(ps, 0, [[16, 32], [1, 16]])
                    if t % 2 == 0:
                        CP(dve, uo, ui, [Bp], [B_utm])
                    else:
                        A(act, uo, ui, AF.Copy, [Bp], [B_utm])
                else:
                    A(act, part_ap(u_tm, 0, 16, (t % 8) * 16, [[128, 32], [1, 16]]), part_ap(ps, 0, 16, 0, [[16, 32], [1, 16]]),
                      AF.Copy, [Bp], [B_utm])
                    if t % 8 == 7:
                        ug_transposes(u_tm, B_utm, 16, t // 8, 256, 0)

        def ug_transposes(src, Bsrc, np_, jt, ucol0, jbase):
            for g4 in range(8):
                ps, Bp = ps_next()
                for gg in range(4):
                    g = g4 * 4 + gg
                    inap = part_ap(src, 0, np_, g * 128, [[1, 128]])
                    TR(ps[:, gg * 128:gg * 128 + np_], inap, [Bsrc], [Bp], n=np_)
                outap = sbap(Ug, (g4 * 4) * 2 * 272 + jt * 272 + ucol0, [[2 * 272, 4], [1, np_]])
                inp_ = sbap(ps, 0, [[128, 4], [1, np_]])
                if g4 % 2 == 0:
                    CP(dve, outap, inp_, [Bp], [B_Ug])
                else:
                    A(act, outap, inp_, AF.Copy, [Bp], [B_Ug])

        glist = [("lat", mt, jg) for mt in range(2) for jg in range(4)] + [("ctx", 0, 0)]
        gens = [in_group(*g_) for g_ in glist]
        next(gens[0])
        for gi_, g_ in enumerate(glist):
            if gi_ + 1 < len(glist):
                next(gens[gi_ + 1])
            for _ in gens[gi_]:
                pass
            if g_[0] == "lat" and g_[2] % 2 == 1:
                ug_transposes(u_tm, B_utm, 128, g_[2] // 2, g_[1] * 128, 0)
        dump("ckvnT", ckvnT[:], [B_ckvnT])
        dump("KRT", KRT[64:96, :], [B_KRT])
        dump("cqT", cqT[:].rearrange("p a b -> p (a b)"), [B_cqT])
        dump("rstdq", rstdq[:], [B_rstdq])
        dump("Ug", Ug[:].rearrange("p a b c -> p (a b c)"), [B_Ug])
        fw.dma(sp, ug_s[:, :], Ug[:].rearrange("p a b c -> p (a b c)"), reads=[B_Ug], writes=[], sem="ugs")
    fw.barrier()
    if stop_after == "inproj":
        return finish()

    attn_tm = sb("attn_tm", [128, 16, 512], stack=att); B_attn = Buf("attn_tm")
    B_attn_s = Buf("attn_s")
    with ExitStack() as ph:
        wq_f = sb("wq_f", [128, 2, 768], stack=ph); B_wqf = Buf("wq_f")
        wkv_f = sb("wkv_f", [128, 1024], stack=ph); B_wkvf = Buf("wkv_f")
        qg = sb("qg", [128, 2], stack=ph); kvg = sb("kvg", [128, 1], stack=ph); B_g = Buf("qkvg")
        w_uq_s = sb("w_uq_s", [128, 2, 768], BF16, stack=ph); B_wuq = Buf("w_uq_s")
        w_uq_rot = sb("w_uq_rot", [128, 2, 8, 32], BF16, stack=ph); B_wuqr = Buf("w_uq_rot")
        w_k = sb("w_k", [128, 1024], BF16, stack=ph); B_wk = Buf("w_k")
        w_v = sb("w_v", [128, 512], BF16, stack=ph); B_wv = Buf("w_v")
        V_all = sb("V_all", [128, 34, 8, 65], BF16, stack=ph); B_V = Buf("V_all")
        KT = [sb("KT%d" % i, [128, NKEY], BF16, stack=ph) for i in range(2)]; B_KT = bufs("KT", 2)
        QT = [sb("QT%d" % i, [128, NOWN], BF16, stack=ph) for i in range(2)]; B_QT = bufs("QT", 2)
        ropeq = sb("ropeq", [128, 2, NOWN], stack=ph); B_ropeq = Buf("ropeq")
        PT = [sb("PT%d" % i, [128, 512], BF16, stack=ph) for i in range(4)]; B_PT = bufs("PT", 4)
        ot = [sb("ot%d" % i, [128, 512], stack=ph) for i in range(2)]; B_ot = bufs("ot", 2)
        qt1 = sb("qt1", [128, 512], stack=ph); B_qt1 = Buf("qt1")
        qt2 = sb("qt2", [128, 512], stack=ph); B_qt2 = Buf("qt2")
        rd = sb("rd", [128, 4], stack=ph); B_rd = Buf("rd")
        fw.dma(sp, wq_f[:], w_uq_d.rearrange("(c p) n -> p c n", p=128), writes=[B_wqf], sem="a0")
        fw.dma(sp, wkv_f[:], w_ukv_d[:, :], writes=[B_wkvf], sem="a1")
        fw.dma(sp, qg[:], qg_d[:, :], writes=[B_g], sem="a2")
        fw.dma(sp, kvg[:], kvg_d[:, :], writes=[B_g], sem="a3")
        fw.dma(sp, ropeq[64:96, :, :], rope_d[64:96, :, 0:NOWN], writes=[B_ropeq], sem="a4")
        for c in range(2):
            TS(dve, w_uq_s[:, c, :], wq_f[:, c, :], qg[:, c:c + 1], None, ALU.mult, ALU.bypass, [B_wqf, B_g], [B_wuq])
            TS(dve, w_uq_rot[:, c, :, 0:16], sbap(w_uq_s, c * 768 + 80, [[96, 8], [1, 16]]), -1.0, None, ALU.mult, ALU.bypass, [B_wuq], [B_wuqr])
            CP(dve, w_uq_rot[:, c, :, 16:32], sbap(w_uq_s, c * 768 + 64, [[96, 8], [1, 16]]), [B_wuq], [B_wuqr])
        TS(dve, w_k[:, :], wkv_f[:, :], kvg[:, 0:1], None, ALU.mult, ALU.bypass, [B_wkvf, B_g], [B_wk])
        CP(dve, w_v[:].rearrange("p (h d) -> p h d", h=8), sbap(w_k, 64, [[128, 8], [1, 64]]), [B_wk], [B_wv])
        fw.op(dve, lambda e: e.memset(V_all[:, :, :, 64:65], 1.0), writes=[B_V])
        for i in range(2):
            fw.op(dve, lambda e: e.memset(KT[i][96:128, :], 0.0), writes=[B_KT[i]])
            fw.op(dve, lambda e: e.memset(QT[i][96:128, :], 0.0), writes=[B_QT[i]])
        for kt in range(34):
            ps, Bp = PS[6 + kt % 2], PSB[6 + kt % 2]
            MM(ps[:, :], ckvnT[:, kt * 128:(kt + 1) * 128], w_v[:, :], True, True, [B_ckvnT, B_wv], [Bp])
            vo = V_all[:, kt, :, 0:64]
            vi = ps[:, :].rearrange("p (h d) -> p h d", h=8)
            if kt % 2 == 0:
                CP(dve, vo, vi, [Bp], [B_V])
            else:
                A(act, vo, vi, AF.Copy, [Bp], [B_V])

        def gen(h):
            s = h % 2
            for ct in range(9):
                n = 512 if ct < 8 else 256
                c0 = ct * 512
                ps, Bp = PS[6], PSB[6]
                MM(ps[0:64, 0:n], w_k[:, h * 128:h * 128 + 64], ckvnT[:, c0:c0 + n], True, True, [B_wk, B_ckvnT], [Bp])
                CP(dve, KT[s][0:64, c0:c0 + n], ps[0:64, 0:n], [Bp], [B_KT[s]])
                yield
            CP(pool, KT[s][64:96, :], KRT[64:96, :], [B_KRT], [B_KT[s]])
            for ct in range(4):
                c0 = ct * 512
                psq, Bq = PS[6], PSB[6]
                psr, Br = PS[7], PSB[7]
                for c in range(2):
                    MM(psq[0:96, :], w_uq_s[:, c, h * 96:(h + 1) * 96], cqT[:, c, c0:c0 + 512], c == 0, c == 1, [B_wuq, B_cqT], [Bq])
                for c in range(2):
                    MM(psr[64:96, :], w_uq_rot[:, c, h, :], cqT[:, c, c0:c0 + 512], c == 0, c == 1, [B_wuqr, B_cqT], [Br])
                TT(dve, QT[s][0:64, c0:c0 + 512], psq[0:64, :], rstdq[0:64, c0:c0 + 512], ALU.mult, [Bq, B_rstdq], [B_QT[s]])
                TT(dve, qt1[64:96, :], psq[64:96, :], ropeq[64:96, 0, c0:c0 + 512], ALU.mult, [Bq, B_ropeq], [B_qt1])
                TT(dve, qt2[64:96, :], psr[64:96, :], ropeq[64:96, 1, c0:c0 + 512], ALU.mult, [Br, B_ropeq], [B_qt2])
                TT(dve, qt1[64:96, :], qt1[64:96, :], qt2[64:96, :], ALU.add, [B_qt1, B_qt2], [B_qt1])
                TT(dve, QT[s][64:96, c0:c0 + 512], qt1[64:96, :], rstdq[64:96, c0:c0 + 512], ALU.mult, [B_qt1, B_rstdq], [B_QT[s]])
                yield

        scount = [0]
        ocount = [0]

        pend = [None]

        def epilogue(h, qt, ob, osl):
            CP(dve, ot[osl][0:65, :], PS[ob][0:65, :], [PSB[ob]], [B_ot[osl]])
            pso, Bo = PS[7], PSB[7]
            for t in range(4):
                TR(pso[:, t * 128:t * 128 + 65], ot[osl][0:65, t * 128:(t + 1) * 128], [B_ot[osl]], [Bo], n=65)
            fw.op(dve, lambda e: e.reciprocal(out=rd[:, :], in_=sbap(pso, 64, [[128, 4]])), reads=[Bo], writes=[B_rd])
            for t in range(4):
                TS(dve, attn_tm[:, qt * 4 + t, h * 64:(h + 1) * 64], pso[:, t * 128:t * 128 + 64], rd[:, t:t + 1], None, ALU.mult, ALU.bypass,
                   [Bo], [B_attn], sreads=[B_rd])

        def attend(h, nxt):
            s = h % 2
            itc = [0]
            for qt in range(4):
                ob = 4 + ocount[0] % 2
                osl = ocount[0] % 2
                ocount[0] += 1
                base = scount[0]
                scount[0] += 34

                def S(kt):
                    bi = (base + kt) % 4
                    MM(PS[bi][:, :], KT[s][:, kt * 128:(kt + 1) * 128], QT[s][:, qt * 512:(qt + 1) * 512], True, True,
                       [B_KT[s], B_QT[s]], [PSB[bi]])
                S(0)
                S(1)
                if pend[0] is not None:
                    epilogue(*pend[0])
                    pend[0] = None
                for kt in range(34):
                    if kt + 2 < 34:
                        S(kt + 2)
                    bi = (base + kt) % 4
                    A(act, PT[bi][:, :], PS[bi][:, :], AF.Exp, [PSB[bi]], [B_PT[bi]], scale=SCALE)
                    MM(PS[ob][0:65, :], V_all[:, kt, h, 0:65], PT[bi][:, :], kt == 0, kt == 33, [B_V, B_PT[bi]], [PSB[ob]])
                    itc[0] += 1
                    if nxt is not None and itc[0] % 9 == 0:
                        next(nxt, None)
                pend[0] = (h, qt, ob, osl)

        nheads = int(os.environ.get("K_HEADS", "8"))
        for _ in gen(0):
            pass
        for h in range(nheads):
            nxt = gen(h + 1) if h + 1 < nheads else None
            attend(h, nxt)
            if nxt is not None:
                for _ in nxt:
                    pass
        epilogue(*pend[0])
        dump("attn", attn_tm[:].rearrange("p a b -> p (a b)"), [B_attn])
        fw.dma(sp, attn_s[:, :], attn_tm[:].rearrange("p a b -> p (a b)"), reads=[B_attn], writes=[B_attn_s], sem="attns")
        dump("KT0", KT[0][0:96, :], [B_KT[0]])
        dump("QT0", QT[0][0:96, :], [B_QT[0]])
        dump("V0", V_all[:].rearrange("p a b c -> p (a b c)"), [B_V])
    fw.barrier()
    att.close()
    if stop_after == "attn":
        return finish()

    PI = math.pi
    B_gs = Buf("g_s")
    with ExitStack() as S:
        sa = sb("ssm_a", [128, 2, 32], stack=S); sl = sb("ssm_l", [128, 32], stack=S)
        sbb = sb("ssm_b", [128, 2, 32, 16], stack=S); scc = sb("ssm_c", [128, 2, 32, 16], stack=S)
        dcols = sb("dcols", [128, 32], stack=S); masks = sb("masks4", [128, 4, 128], stack=S)
        B_par = Buf("ssm_par")
        fw.dma(sp, sa[:], ssm_a_d[:, :, :], writes=[B_par], sem="s0")
        fw.dma(sp, sl[:], ssm_ldt_d[:, :], writes=[B_par], sem="s1")
        fw.dma(sp, sbb[:], ssm_b_d[:, :, :, :], writes=[B_par], sem="s2")
        fw.dma(sp, scc[:], ssm_c_d[:, :, :, :], writes=[B_par], sem="s3")
        fw.dma(sp, dcols[:], ssm_dcols_d[:, :], writes=[B_par], sem="s4")
        fw.dma(sp, masks[:, 0:2, :], masks_d[:, :, :], writes=[B_par], sem="s5")
        fw.dma(sp, masks[:, 2:4, :], masks_d[:, :, :], writes=[B_par], sem="s6")
        w_glu = sb("w_glu", [128, 4, 512], BF16, stack=S); B_wglu = Buf("w_glu")
        fw.dma(pool, w_glu[:], w_glu_d.rearrange("(k p) n -> p k n", p=128), writes=[B_wglu], sem="s7")
        bglu = sb("bglu", [128, 512], stack=S); B_bglu = Buf("bglu")
        fw.dma(sp, bglu[:], b_glu_d[:, :], writes=[B_bglu], sem="s8")
        sm = sb("ssm_sm", [128, 32, 32], stack=S); B_sm = Buf("ssm_sm")
        _smi = [0]

        def smt():
            i = _smi[0]; _smi[0] += 1
            return sm[:, i, :]
        Bbar = sb("Bbar", [128, 2, 32, 16], stack=S)
        PWr = sb("PWr", [128, 17, 32], stack=S); PWi = sb("PWi", [128, 17, 32], stack=S)
        IPr = sb("IPr", [128, 17, 32], stack=S); IPi = sb("IPi", [128, 17, 32], stack=S)
        AA = sb("AA", [128, 2, 2, 16], stack=S); AB = sb("AB", [128, 2, 2, 16], stack=S)
        BS = [B_par, B_sm]

        def tt(out, a, b, op):
            TT(dve, out, a, b, op, BS, [B_sm])

        a_re, a_im = sa[:, 0, :], sa[:, 1, :]
        dt_ = smt(); lre = smt(); ang = smt(); mag = smt()
        A(act, dt_, sl[:, :], AF.Exp, [B_par], [B_sm])
        tt(lre, a_re, dt_, ALU.mult)
        tt(ang, a_im, dt_, ALU.mult)
        A(act, mag, lre, AF.Exp, [B_sm], [B_sm])

        def sin_of(src_ang, shift):
            a2 = smt(); k = smt(); r = smt(); o = smt()
            TS(dve, a2, src_ang, shift, None, ALU.add, ALU.bypass, BS, [B_sm])
            TS(dve, k, a2, PI, None, ALU.is_ge, ALU.bypass, BS, [B_sm])
            for i in range(2, 9):
                STT(k, a2, (2 * i - 1) * PI, k, ALU.is_ge, ALU.add, BS, [B_sm])
            STT(r, k, -2.0 * PI, a2, ALU.mult, ALU.add, BS, [B_sm])
            A(act, o, r, AF.Sin, [B_sm], [B_sm])
            return o
        sn = sin_of(ang, 0.0)
        cs = sin_of(ang, PI / 2)
        abre = smt(); abim = smt()
        tt(abre, mag, cs, ALU.mult)
        tt(abim, mag, sn, ALU.mult)
        t1 = smt(); t2 = smt(); den = smt(); rden = smt(); nre = smt(); cre = smt(); cim = smt()
        tt(t1, a_re, a_re, ALU.mult); tt(t2, a_im, a_im, ALU.mult); tt(den, t1, t2, ALU.add)
        fw.op(dve, lambda e: e.reciprocal(out=rden, in_=den), reads=BS, writes=[B_sm])
        TS(dve, nre, abre, -1.0, None, ALU.add, ALU.bypass, BS, [B_sm])
        tt(t1, nre, a_re, ALU.mult); tt(t2, abim, a_im, ALU.mult); tt(t1, t1, t2, ALU.add); tt(cre, t1, rden, ALU.mult)
        tt(t1, abim, a_re, ALU.mult); tt(t2, nre, a_im, ALU.mult); tt(t1, t1, t2, ALU.subtract); tt(cim, t1, rden, ALU.mult)
        tb = sb("tb", [128, 32, 16], stack=S)

        def bc16(v):
            return AP(v.tensor, v.offset, [list(v.ap[0]), [1, 32], [0, 16]])
        tt(Bbar[:, 0], sbb[:, 0], bc16(cre), ALU.mult); tt(tb[:], sbb[:, 1], bc16(cim), ALU.mult); tt(Bbar[:, 0], Bbar[:, 0], tb[:], ALU.subtract)
        tt(Bbar[:, 1], sbb[:, 1], bc16(cre), ALU.mult); tt(tb[:], sbb[:, 0], bc16(cim), ALU.mult); tt(Bbar[:, 1], Bbar[:, 1], tb[:], ALU.add)
        m2 = smt(); rm2 = smt(); iabre = smt(); iabim = smt()
        tt(t1, abre, abre, ALU.mult); tt(t2, abim, abim, ALU.mult); tt(m2, t1, t2, ALU.add)
        fw.op(dve, lambda e: e.reciprocal(out=rm2, in_=m2), reads=BS, writes=[B_sm])
        tt(iabre, abre, rm2, ALU.mult)
        STT(iabim, abim, -1.0, rm2, ALU.mult, ALU.mult, BS, [B_sm])
        for (Pr, Pi, br, bi) in ((PWr, PWi, abre, abim), (IPr, IPi, iabre, iabim)):
            fw.op(dve, lambda e: e.memset(Pr[:, 0, :], 1.0), writes=[B_sm])
            fw.op(dve, lambda e: e.memset(Pi[:, 0, :], 0.0), writes=[B_sm])
            CP(dve, Pr[:, 1, :], br, BS, [B_sm])
            CP(dve, Pi[:, 1, :], bi, BS, [B_sm])
            for k in range(2, 17):
                tt(t1, Pr[:, k - 1, :], br, ALU.mult); tt(t2, Pi[:, k - 1, :], bi, ALU.mult); tt(Pr[:, k, :], t1, t2, ALU.subtract)
                tt(t1, Pr[:, k - 1, :], bi, ALU.mult); tt(t2, Pi[:, k - 1, :], br, ALU.mult); tt(Pi[:, k, :], t1, t2, ALU.add)
        for d in range(2):
            for c in range(2):
                CP(dve, AA[:, d, c, :], PWr[:, 16, d * 16:(d + 1) * 16], BS, [B_sm])
            TS(dve, AB[:, d, 0, :], PWi[:, 16, d * 16:(d + 1) * 16], -1.0, None, ALU.mult, ALU.bypass, BS, [B_sm])
            CP(dve, AB[:, d, 1, :], PWi[:, 16, d * 16:(d + 1) * 16], BS, [B_sm])
        dump("sm", sm[:].rearrange("p a b -> p (a b)"), BS)
        dump("abar", sbap(PWr, 32, [[1, 32]]), BS)
        dump("abari", sbap(PWi, 32, [[1, 32]]), BS)
        dump("Bbar", Bbar[:].rearrange("p a b c -> p (a b c)"), BS)

        ctmp = [sb("ctmp%d" % i, [128, 4, 256], stack=S) for i in range(2)]; B_ct = Buf("ctmp")

        def cplx_build(q, out_t, P_r, P_i, k0, kstep, Vt, d, neg_im):
            for half in range(4):
                p0 = half * 4

                def Pv(Pt):
                    return sbap(Pt, k0 * 32 + d * 16 + p0, [[1, 4], [kstep * 32, 16], [0, 16]])

                def Vv(c):
                    return sbap(Vt, c * 512 + (d * 16 + p0) * 16, [[16, 4], [0, 16], [1, 16]])

                def Ov(c):
                    return sbap(out_t, p0 * 512 + c * 256, [[512, 4], [16, 16], [1, 16]])
                ta = ctmp[0][:].rearrange("p a (k h) -> p a k h", h=16)
                tb_ = ctmp[1][:].rearrange("p a (k h) -> p a k h", h=16)
                R = BS + [B_ct]
                TT(q, ta, Pv(P_r), Vv(0), ALU.mult, R, [B_ct])
                TT(q, tb_, Pv(P_i), Vv(1), ALU.mult, R, [B_ct])
                TT(q, Ov(0), ta, tb_, ALU.subtract, R, [B_sm])
                TT(q, ta, Pv(P_r), Vv(1), ALU.mult, R, [B_ct])
                TT(q, tb_, Pv(P_i), Vv(0), ALU.mult, R, [B_ct])
                if neg_im:
                    TT(q, ta, ta, tb_, ALU.add, R, [B_ct])
                    TS(q, Ov(1), ta, -1.0, None, ALU.mult, ALU.bypass, R, [B_sm])
                else:
                    TT(q, Ov(1), ta, tb_, ALU.add, R, [B_sm])

        Sbf = sb("Sbf", [128, 2, 2, 16, 128], BF16, stack=S); B_Sbf = Buf("Sbf")
        with ExitStack() as S2:
            Ug = sb("Ug2", [128, 32, 2, 272], BF16, stack=S2); B_Ug2 = Buf("Ug2")
            fw.dma(sp, Ug[:].rearrange("p a b c -> p (a b c)"), ug_s[:, :], writes=[B_Ug2], sem="s9")
            L = sb("L", [128, 2, 2, 16, 272], BF16, stack=S2); B_L = Buf("L")
            with ExitStack() as S2a:
                Wsrc = sb("Wsrc", [128, 16, 2, 256], stack=S2a)
                Wb = sb("Wb", [128, 16, 4, 128], BF16, stack=S2a); B_Wb = Buf("Wb")
                for d in range(2):
                    if d == 0:
                        cplx_build(dve, Wsrc, PWr, PWi, 15, -1, Bbar, 0, False)
                    else:
                        cplx_build(dve, Wsrc, PWr, PWi, 0, 1, Bbar, 1, False)
                    for pt in range(16):
                        ps, Bp = ps_next()
                        for blk in range(4):
                            TR(ps[:, blk * 128:(blk + 1) * 128], sbap(Wsrc, pt * 512 + blk * 128, [[1, 128]]), BS, [Bp])
                        if pt % 2 == 0:
                            A(act, Wb[:, pt, :, :], ps[:, :].rearrange("p (a b) -> p a b", a=4), AF.Copy, [Bp], [B_Wb])
                        else:
                            CP(dve, Wb[:, pt, :, :], ps[:, :].rearrange("p (a b) -> p a b", a=4), [Bp], [B_Wb])
                    for pt in range(16):
                        for c in range(2):
                            ps, Bp = ps_next()
                            if d == 1:
                                ranges = [(0, 272, 0)]
                                ncol = 272
                            else:
                                ranges = [(256, 16, 0), (0, 128, 16)]
                                ncol = 144
                            for (u0, n, o0) in ranges:
                                for gl in range(2):
                                    for jt in range(2):
                                        MM(ps[gl * 64:(gl + 1) * 64, o0:o0 + n], Wb[:, pt, c * 2 + jt, gl * 64:(gl + 1) * 64],
                                           Ug[:, 2 * pt + gl, jt, u0:u0 + n], jt == 0, jt == 1, [B_Wb, B_Ug2], [Bp])
                            lo = L[:, d, c, pt, 272 - ncol:272]
                            if c == 0:
                                A(act, lo, ps[:, 0:ncol], AF.Copy, [Bp], [B_L])
                            else:
                                CP(dve, lo, ps[:, 0:ncol], [Bp], [B_L])
                    if d == 0:
                        dump("Wb0", Wb[:].rearrange("p a b c -> p (a b c)"), [B_Wb])
            dump("L", L[:].rearrange("p a b c e -> p (a b c e)"), [B_L])
            fw.barrier()
            Sh = sb("Sh", [128, 2, 2, 16, NS], stack=S2); B_Sh = Buf("Sh")
            fw.op(pool, lambda e: e.memset(Sh[:], 0.0), writes=[B_Sh])
            tm1 = sb("tm1", [128, 2, 2, 16], stack=S2); tm2 = sb("tm2", [128, 2, 2, 16], stack=S2); B_tm = Buf("tm")

            def f0(st):
                return st - 140 if st >= 142 else st % 2

            def f1(st):
                return 273 - st if st >= 142 else 132 + st % 2
            B_tmh = bufs("tmh", 2); B_Shh = bufs("Shh", 2)
            for b_ in B_Shh:
                b_.w = B_Sh.w
            RSh = [[B_Shh[h_], B_L, B_sm, B_tmh[h_]] for h_ in range(2)]
            for s in range(272):
                both = s >= 128
                ops = []
                NH_ = 2 if WINDOW else 1
                PW_ = 16 // NH_
                for ph_ in range(NH_):
                    po = ph_ * PW_
                    if both:
                        def hv(t_, n_, c0, c1, rev=False):
                            dstep = 32 * n_ + (c1 - c0)
                            if rev:
                                return sbap(t_, c0 + 16 * n_ + po * n_, [[dstep, 2], [-16 * n_, 2], [n_, PW_]])
                            return sbap(t_, c0 + po * n_, [[dstep, 2], [16 * n_, 2], [n_, PW_]])
                        sr = hv(Sh, NS, f0(s), f1(s)); srev = hv(Sh, NS, f0(s), f1(s), True)
                        sw = hv(Sh, NS, f0(s + 1), f1(s + 1))
                        lv = hv(L, 272, s, 271 - s)
                        aa, ab = AA[:, :, :, po:po + PW_], AB[:, :, :, po:po + PW_]
                        x1, x2 = tm1[:, :, :, po:po + PW_], tm2[:, :, :, po:po + PW_]
                    else:
                        def hv(t_, n_, c1, rev=False):
                            if rev:
                                return sbap(t_, 32 * n_ + c1 + 16 * n_ + po * n_, [[-16 * n_, 2], [n_, PW_]])
                            return sbap(t_, 32 * n_ + c1 + po * n_, [[16 * n_, 2], [n_, PW_]])
                        sr = hv(Sh, NS, f1(s)); srev = hv(Sh, NS, f1(s), True); sw = hv(Sh, NS, f1(s + 1))
                        lv = hv(L, 272, 271 - s)
                        aa, ab = AA[:, 1, :, po:po + PW_], AB[:, 1, :, po:po + PW_]
                        x1, x2 = tm1[:, 1, :, po:po + PW_], tm2[:, 1, :, po:po + PW_]
                    ops.append((sr, srev, sw, lv, aa, ab, x1, x2))
                for h_, (sr, srev, sw, lv, aa, ab, x1, x2) in enumerate(ops):
                    TT(dve, x1, sr, aa, ALU.mult, RSh[h_], [B_tmh[h_]])
                for h_, (sr, srev, sw, lv, aa, ab, x1, x2) in enumerate(ops):
                    TT(dve, x2, srev, ab, ALU.mult, RSh[h_], [B_tmh[h_]])
                for h_, (sr, srev, sw, lv, aa, ab, x1, x2) in enumerate(ops):
                    TT(dve, x1, x1, lv, ALU.add, RSh[h_], [B_tmh[h_]])
                for h_, (sr, srev, sw, lv, aa, ab, x1, x2) in enumerate(ops):
                    TT(dve, sw, x1, x2, ALU.add, RSh[h_], [B_Shh[h_]])
            CP(dve, Sbf[:, 0].rearrange("p a b c -> p (a b) c"), sbap(Sh, 4, [[NS, 32], [1, 128]]), B_Shh, [B_Sbf])
            CP(dve, Sbf[:, 1].rearrange("p a b c -> p (a b) c"), sbap(Sh, 32 * NS + 2, [[NS, 32], [1, 128]]), B_Shh, [B_Sbf])
            dump("Sbf", Sbf[:].rearrange("p a b c e -> p (a b c e)"), [B_Sbf])
        fw.barrier()
        if stop_after == "scan":
            S.close()
            return finish()
        with ExitStack() as S3:
            Kmat = sb("Kmat", [128, 32, 4, 128], BF16, stack=S3); B_Km = Buf("Kmat")
            Ca = sb("Ca", [128, 2, 16, 2, 256], BF16, stack=S3); B_Ca = Buf("Ca")
            Ugo = sb("Ugo", [128, 64, 128], BF16, stack=S3); B_Ugo = Buf("Ugo")
            fw.dma(sp, Ugo[:], ug_s.rearrange("p (a c) -> p a c", c=272)[:, :, 0:128], writes=[B_Ugo], sem="s10")
            S3a = ExitStack()
            Xt = sb("Xt", [128, 16, 2, 256], BF16, stack=S3a); Zt = sb("Zt", [128, 16, 2, 256], BF16, stack=S3a)
            tmpK = sb("tmpK", [128, 4, 128], stack=S3a); tmpS = sb("tmpS", [128, 2, 128], stack=S3a); B_tk = Buf("tmpK")
            cplx_build(dve, Ca[:, 0], PWr, PWi, 1, 1, scc, 0, True)
            cplx_build(dve, Ca[:, 1], PWr, PWi, 16, -1, scc, 1, True)
            KS = BS + [B_tk]
            for d in range(2):
                if d == 0:
                    cplx_build(dve, Xt, IPr, IPi, 0, 1, Bbar, 0, False)
                    cplx_build(dve, Zt, PWr, PWi, 0, 1, scc, 0, True)
                else:
                    cplx_build(dve, Xt, PWr, PWi, 0, 1, Bbar, 1, False)
                    cplx_build(dve, Zt, IPr, IPi, 0, 1, scc, 1, True)
                for g in range(32):
                    pt, gl = g // 2, g % 2
                    r0, r1 = gl * 64, gl * 64 + 64
                    psA, BpA = ps_next()

                    def kblock(ps, col, jt, tt_):
                        for c in range(2):
                            MM(ps[:, col * 128:(col + 1) * 128], Xt[r0:r1, pt, c, jt * 128:(jt + 1) * 128],
                               Zt[r0:r1, pt, c, tt_ * 128:(tt_ + 1) * 128], c == 0, c == 1, BS, [BpA])
                    kblock(psA, 0, 0, 0)
                    kblock(psA, 1, 1, 1)
                    if d == 0:
                        kblock(psA, 2, 0, 1)
                    else:
                        kblock(psA, 2, 1, 0)
                    mk = sbap(masks, d * 128, [[0, 2], [1, 128]])
                    kd = sbap(Kmat, g * 512, [[384, 2], [1, 128]])
                    ko_ = Kmat[:, g, 1 if d == 0 else 2, :]
                    if d == 0:
                        TT(dve, tmpS[:], psA[:, 0:256].rearrange("p (a b) -> p a b", a=2), mk, ALU.mult, [BpA, B_par], [B_tk])
                        STT(kd, sbap(ident, 0, [[0, 2], [1, 128]]), dcols[:, g:g + 1], tmpS[:], ALU.mult, ALU.add, [B_ident, B_par, B_tk], [B_Km])
                    else:
                        TT(dve, tmpS[:], psA[:, 0:256].rearrange("p (a b) -> p a b", a=2), mk, ALU.mult, [BpA, B_par], [B_tk])
                        TT(dve, kd, kd, tmpS[:], ALU.add, [B_tk, B_Km], [B_Km])
                    A(act, ko_, psA[:, 256:384], AF.Copy, [BpA], [B_Km])
            dump("Kmat", Kmat[:].rearrange("p a b c -> p (a b c)"), [B_Km])
            dump("Ca", Ca[:].rearrange("p a b c e -> p (a b c e)"), BS)
            fw.barrier()
            S3a.close()
            g_tm = sb("g_tm", [128, 16, 512], stack=S3); B_gtm = Buf("g_tm")
            glsb = [sb("glsb%d" % i, [128, 512], stack=S3) for i in range(2)]; B_gl = bufs("glsb", 2)
            for gp in range(16):
                psY, BpY = ps_next()
                for gi in range(2):
                    g = gp * 2 + gi
                    pt, gl = g // 2, g % 2
                    r0, r1 = gl * 64, gl * 64 + 64
                    for tt_ in range(2):
                        yo = psY[:, (gi * 2 + tt_) * 128:(gi * 2 + tt_ + 1) * 128]
                        first = True
                        for jt in range(2):
                            MM(yo, Kmat[:, g, jt * 2 + tt_, :], Ugo[:, g * 2 + jt, :], first, False, [B_Km, B_Ugo], [BpY])
                            first = False
                        for d in range(2):
                            for c in range(2):
                                MM(yo, Ca[r0:r1, d, pt, c, tt_ * 128:(tt_ + 1) * 128], Sbf[r0:r1, d, c, pt, :], False, (d == 1 and c == 1),
                                   BS + [B_Sbf], [BpY])
                gs_ = gp % 2
                A(act, glsb[gs_][:, :], psY[:, :], AF.Gelu, [BpY], [B_gl[gs_]])
                psT, BpT = ps_next()
                for blk in range(4):
                    TR(psT[:, blk * 128:(blk + 1) * 128], glsb[gs_][:, blk * 128:(blk + 1) * 128], [B_gl[gs_]], [BpT])
                for gi in range(2):
                    g = gp * 2 + gi
                    oo = sbap(g_tm, 16 * g, [[512, 16], [1, 16]])
                    ii = sbap(psT, gi * 256, [[16, 16], [1, 16]])
                    CP(dve, oo, ii, [BpT], [B_gtm])
            dump("gtm", g_tm[:].rearrange("p a b -> p (a b)"), [B_gtm])
            gT = [sb("gT%d" % i, [128, 4, 128], BF16, stack=S3) for i in range(2)]; B_gT = bufs("gT", 2)
            zt = sb("zt", [128, 512], stack=S3); B_zt = Buf("zt")
            for t in range(16):
                gs_ = t % 2
                ps, Bp = ps_next()
                for fc in range(4):
                    TR(ps[:, fc * 128:(fc + 1) * 128], g_tm[:, t, fc * 128:(fc + 1) * 128], [B_gtm], [Bp])
                A(act, gT[gs_][:], ps[:, :].rearrange("p (a b) -> p a b", a=4), AF.Copy, [Bp], [B_gT[gs_]])
                ps2, Bp2 = ps_next()
                for fc in range(4):
                    MM(ps2[:, :], gT[gs_][:, fc, :], w_glu[:, fc, :], fc == 0, fc == 3, [B_gT[gs_], B_wglu], [Bp2])
                TT(dve, zt[:], ps2[:, :], bglu[:], ALU.add, [Bp2, B_bglu], [B_zt])
                A(act, zt[:], zt[:], AF.Sigmoid, [B_zt], [B_zt])
                TT(dve, g_tm[:, t, :], g_tm[:, t, :], zt[:], ALU.mult, [B_gtm, B_zt], [B_gtm])
            dump("ssm", g_tm[:].rearrange("p a b -> p (a b)"), [B_gtm])
            fw.dma(sp, g_s[:, :], g_tm[:].rearrange("p a b -> p (a b)"), reads=[B_gtm], writes=[B_gs], sem="gs")
    fw.barrier()
    if stop_after == "ssm":
        return finish()

    hT = sb("hT", [128, 8, NOWN], BF16); B_hT = Buf("hT")
    combT = sb("combT", [32, NOWN], BF16); B_combT = Buf("combT")
    lnbox = [None, None]
    B_x1s = Buf("x1_s")

    def layer_norm(pre, B_pre, gi, out, B_out, st, mv, sc_, B_st):
        for hf in range(2):
            fw.op(dve, lambda e: e.bn_stats(out=st[:, hf * 6:(hf + 1) * 6], in_=pre[:, hf * 512:(hf + 1) * 512]), reads=[B_pre], writes=[B_st])
        fw.op(dve, lambda e: e.bn_aggr(out=mv[:, 0:2], in_=st[:, 0:12]), reads=[B_st], writes=[B_st])
        A(act, sc_[:, 0:1], mv[:, 1:2], AF.Ln, [B_st, B_eps], [B_st], bias=epsc[:, 0:1])
        A(act, sc_[:, 1:2], sc_[:, 0:1], AF.Exp, [B_st], [B_st], scale=-0.5)
        TS(dve, out, pre, mv[:, 0:1], sc_[:, 1:2], ALU.subtract, ALU.mult, [B_pre], [B_out], sreads=[B_st])
        lnrows, B_ln = lnbox
        TT(dve, out, out, lnrows[:, 0, :], ALU.mult, [B_out, B_ln], [B_out])
        TT(dve, out, out, lnrows[:, 1, :], ALU.add, [B_out, B_ln], [B_out])

    n_exp = int(os.environ.get("K_NEXP", str(NE)))
    NSLOT = 3
    Wg = [sb("Wg%d" % i, [128, 8, 256], BF16, stack=es) for i in range(NSLOT)]
    Wu = [sb("Wu%d" % i, [128, 8, 256], BF16, stack=es) for i in range(NSLOT)]
    Wd = [sb("Wd%d" % i, [128, 2, D], BF16, stack=es) for i in range(NSLOT)]
    B_W = bufs("Wexp", NSLOT)
    stg = [sb("stg%d" % i, [128, 2048], stack=es) for i in range(2)]; B_stg = bufs("stg", 2)
    stc = [0]

    def load_expert(e):
        sl_ = e % NSLOT
        for (src_, dst, kk) in ((wg_d[e], Wg[sl_], 8), (wu_d[e], Wu[sl_], 8), (wd_d[e], Wd[sl_], 2)):
            k_ = stc[0] % 2
            stc[0] += 1
            fw.dma(sp, stg[k_][:].rearrange("p (k n) -> p k n", k=kk), src_.rearrange("(k p) n -> p k n", p=128),
                   writes=[B_stg[k_]], sem="stg%d" % k_)
            CP(pool, dst[:].rearrange("p k n -> p (k n)"), stg[k_][:], [B_stg[k_]], [B_W[sl_]])


    with ExitStack() as M:
        ln1 = sb("ln1rows", [128, 2, D], stack=M); lnbox[0] = ln1; lnbox[1] = Buf("ln1")
        fw.dma(sp, ln1[:], ln_d[:, 0:2, :], writes=[lnbox[1]], sem="m0")
        w_o_s = sb("w_o_s", [128, 8, D], BF16, stack=M); B_wo = Buf("w_o_s")
        wof = [sb("wof%d" % i, [128, D], stack=M) for i in range(2)]; B_wof = bufs("wof", 2)
        gn = sb("gn", [128, 8], stack=M); B_gn = Buf("gn")
        w_r = sb("w_r", [128, 8, 36], stack=M); b_r = sb("b_r", [128, 36], stack=M); B_wr = Buf("w_r")
        fw.dma(sp, gn[:], gn_d[:, :], writes=[B_gn], sem="m1")
        fw.dma(sp, w_r[:], w_r_d.rearrange("(k p) n -> p k n", p=128), writes=[B_wr], sem="m2")
        fw.dma(sp, b_r[:], b_r_d[:, :], writes=[B_wr], sem="m3")
        for fc in range(8):
            fw.dma(sp, wof[fc % 2][:], w_o_d[fc * 128:(fc + 1) * 128, :], writes=[B_wof[fc % 2]], sem="wof%d" % (fc % 2))
            TS(dve, w_o_s[:, fc, :], wof[fc % 2][:], gn[:, fc:fc + 1], None, ALU.mult, ALU.bypass, [B_wof[fc % 2], B_gn], [B_wo])
        load_expert(0)
        cat = [sb("cat%d" % i, [128, D], stack=M) for i in range(2)]; B_cat = bufs("cat", 2)
        xo = [sb("xo%d" % i, [128, D], stack=M) for i in range(2)]; B_xo = bufs("xo", 2)
        catT = [sb("catT%d" % i, [128, 8, 128], BF16, stack=M) for i in range(2)]; B_catT = bufs("catT", 2)
        pre = [sb("pre%d" % i, [128, D], stack=M) for i in range(2)]; B_pre = bufs("pre", 2)
        hTf = [sb("hTf%d" % i, [128, 8, 128], stack=M) for i in range(2)]; B_hTf = bufs("hTf", 2)
        htm = [sb("htm%d" % i, [128, D], stack=M) for i in range(2)]; B_htm = bufs("htm", 2)
        junk = sb("junk", [128, 512], stack=M); B_junk = Buf("junk")
        st = sb("st", [128, 12], stack=M); mv = sb("mv", [128, 2], stack=M); sc_ = sb("sc_", [128, 8], stack=M); B_st = Buf("st")
        rt = sb("rt", [128, 160], stack=M); B_rt = Buf("rt")
        scA = sb("scA", [128, 8], stack=M); B_stA = Buf("stA")
        comb = sb("comb", [128, 32], stack=M); B_comb = Buf("comb")
        mixps = [[], []]

        def stageA(t):
            s = t % 2
            fw.dma(sp, cat[s][:, 0:512], attn_s[:, t * 512:(t + 1) * 512], reads=[B_attn_s], writes=[B_cat[s]], sem="cat%d" % s)
            fw.dma(sp, cat[s][:, 512:1024], g_s[:, t * 512:(t + 1) * 512], reads=[B_gs], writes=[B_cat[s]], sem="cat%d" % s)
            fw.group_done("cat%d" % s, [B_cat[s]])
            fw.dma(sp, xo[s][:], xf_v[t, 0:128, :], writes=[B_xo[s]], sem="xo%d" % s)
            for hf in range(2):
                A(act, junk[:], cat[s][:, hf * 512:(hf + 1) * 512], AF.Square, [B_cat[s]], [B_junk, B_stA], accum_out=scA[:, 2 + hf:3 + hf])
                A(act, scA[:, 4 + hf:5 + hf], scA[:, 2 + hf:3 + hf], AF.Ln, [B_stA, B_eps], [B_stA], scale=1.0 / 512, bias=epsc[:, 0:1])
                A(act, scA[:, 6 + hf:7 + hf], scA[:, 4 + hf:5 + hf], AF.Exp, [B_stA], [B_stA], scale=-0.5)
                A(act, cat[s][:, hf * 512:(hf + 1) * 512], cat[s][:, hf * 512:(hf + 1) * 512], AF.Copy, [B_cat[s], B_stA], [B_cat[s]],
                  scale=scA[:, 6 + hf:7 + hf])
            for hf in range(2):
                ps, Bp = ps_next()
                for q4 in range(4):
                    fc = hf * 4 + q4
                    TR(ps[:, q4 * 128:(q4 + 1) * 128], cat[s][:, fc * 128:(fc + 1) * 128], [B_cat[s]], [Bp])
                A(act, catT[s][:, hf * 4:(hf + 1) * 4, :], ps[:, :].rearrange("p (a b) -> p a b", a=4), AF.Copy, [Bp], [B_catT[s]])
            for hf in range(2):
                ps, Bp = ps_next()
                for fc in range(8):
                    MM(ps[:, :], catT[s][:, fc, :], w_o_s[:, fc, hf * 512:(hf + 1) * 512], fc == 0, fc == 7, [B_catT[s], B_wo], [Bp])
                mixps[t % 2].append((ps, Bp))

        def stageA_pre(t):
            s = t % 2
            for hf in range(2):
                ps, Bp = mixps[t % 2][hf]
                TT(dve, pre[s][:, hf * 512:(hf + 1) * 512], ps[:, :], grow[:, 0, hf * 512:(hf + 1) * 512], ALU.mult, [Bp, B_grow], [B_pre[s]])
            mixps[t % 2].clear()
            STT(pre[s][:], xo[s][:], ALPHA, pre[s][:], ALU.mult, ALU.add, [B_xo[s], B_pre[s]], [B_pre[s]])

        def stageB(t):
            s = t % 2
            layer_norm(pre[s][:], B_pre[s], 0, pre[s][:], B_pre[s], st, mv, sc_, B_st)
            fw.dma(sp, x1_s[:, t * D:(t + 1) * D], pre[s][:], reads=[B_pre[s]], writes=[B_x1s], sem="x1s")
            TT(dve, htm[s][:], pre[s][:], grow[:, 2, :], ALU.mult, [B_pre[s], B_grow], [B_htm[s]])
            TT(dve, htm[s][:], htm[s][:], grow[:, 1, :], ALU.add, [B_htm[s], B_grow], [B_htm[s]])
            for hf in range(2):
                ps, Bp = ps_next()
                for q4 in range(4):
                    fc = hf * 4 + q4
                    TR(ps[:, q4 * 128:(q4 + 1) * 128], htm[s][:, fc * 128:(fc + 1) * 128], [B_htm[s]], [Bp])
                A(act, hT[:, hf * 4:(hf + 1) * 4, t * 128:(t + 1) * 128], ps[:, :].rearrange("p (a b) -> p a b", a=4), AF.Copy, [Bp], [B_hT])
                A(act, hTf[s][:, hf * 4:(hf + 1) * 4, :], ps[:, :].rearrange("p (a b) -> p a b", a=4), AF.Copy, [Bp], [B_hTf[s]])

        rps = [None]

        def stageC_mm(t):
            s = t % 2
            ps, Bp = ps_next()
            for fc in range(8):
                MM(ps[:, 0:36], hTf[s][:, fc, :], w_r[:, fc, :], fc == 0, fc == 7, [B_hTf[s], B_wr], [Bp])
            rps[0] = (ps, Bp)

        def stageC(t):
            s = t % 2
            ps, Bp = rps[0]
            lg = rt[:, 0:36]
            gsel = rt[:, 36:40]; gmax = rt[:, 40:41]; ngmax = rt[:, 41:42]; gsum = rt[:, 42:43]; g_w = rt[:, 43:44]
            esel = rt[:, 44:52]; m1 = rt[:, 52:53]; mask1 = rt[:, 56:64]; esel2 = rt[:, 64:72]; m2 = rt[:, 53:54]; mask2 = rt[:, 72:80]
            nm1 = rt[:, 54:55]; e2 = rt[:, 55:56]; den_ = rt[:, 80:81]; w1 = rt[:, 81:82]; w2 = rt[:, 82:83]; cw8 = rt[:, 88:96]; gex = rt[:, 96:100]
            RR = [B_rt]
            TT(dve, lg, ps[:, 0:36], b_r[:, :], ALU.add, [Bp, B_wr], RR)
            fw.op(dve, lambda e: e.reduce_max(out=gmax, in_=rt[:, 0:4], axis=AX.X), reads=RR, writes=RR)
            TS(dve, gsel, rt[:, 0:4], gmax, None, ALU.is_ge, ALU.bypass, RR, RR, sreads=RR)
            TS(dve, ngmax, gmax, -1.0, None, ALU.mult, ALU.bypass, RR, RR)
            A(act, gex, rt[:, 0:4], AF.Exp, RR, RR, bias=ngmax, accum_out=gsum)
            fw.op(dve, lambda e: e.reciprocal(out=g_w, in_=gsum), reads=RR, writes=RR)
            TS(dve, esel, rt[:, 4:12], gsel[:, 0:1], None, ALU.mult, ALU.bypass, RR, RR, sreads=RR)
            for g in range(1, 4):
                fw.op(dve, lambda e: e.scalar_tensor_tensor(out=esel, in0=rt[:, 4 + 8 * g:12 + 8 * g], scalar=gsel[:, g:g + 1], in1=esel,
                                                            op0=ALU.mult, op1=ALU.add), reads=RR, writes=RR, sreads=RR)
            fw.op(dve, lambda e: e.reduce_max(out=m1, in_=esel, axis=AX.X), reads=RR, writes=RR)
            TS(dve, mask1, esel, m1, None, ALU.is_ge, ALU.bypass, RR, RR, sreads=RR)
            STT(esel2, mask1, -1e30, esel, ALU.mult, ALU.add, RR, RR)
            fw.op(dve, lambda e: e.reduce_max(out=m2, in_=esel2, axis=AX.X), reads=RR, writes=RR)
            TS(dve, mask2, esel2, m2, None, ALU.is_ge, ALU.bypass, RR, RR, sreads=RR)
            TS(dve, nm1, m1, -1.0, None, ALU.mult, ALU.bypass, RR, RR)
            A(act, e2, m2, AF.Exp, RR, RR, bias=nm1)
            TS(dve, den_, e2, 1.0, None, ALU.add, ALU.bypass, RR, RR)
            fw.op(dve, lambda e: e.reciprocal(out=w1, in_=den_), reads=RR, writes=RR)
            TT(dve, w2, e2, w1, ALU.mult, RR, RR)
            TT(dve, w1, w1, g_w, ALU.mult, RR, RR)
            TT(dve, w2, w2, g_w, ALU.mult, RR, RR)
            TS(dve, cw8, mask1, w1, None, ALU.mult, ALU.bypass, RR, RR, sreads=RR)
            fw.op(dve, lambda e: e.scalar_tensor_tensor(out=cw8, in0=mask2, scalar=w2, in1=cw8, op0=ALU.mult, op1=ALU.add),
                  reads=RR, writes=RR, sreads=RR)
            for g in range(4):
                TS(dve, comb[:, g * 8:(g + 1) * 8], cw8, gsel[:, g:g + 1], None, ALU.mult, ALU.bypass, RR, [B_comb], sreads=RR)

        def stageC_tail(t):
            s = t % 2
            ps, Bp = ps_next()
            TR(ps[0:32, 0:128], comb[:, :], [B_comb], [Bp])
            CP(dve, combT[:, t * 128:(t + 1) * 128], ps[0:32, 0:128], [Bp], [B_combT])
            if t == 0:
                dump("x1_0", pre[s][:], [B_pre[s]])
                dump("comb0", comb[:], [B_comb])

        stageA(0)
        stageA_pre(0)
        for t in range(17):
            if t >= 1:
                stageC_mm(t - 1)
            if t + 1 < 16:
                stageA(t + 1)
            if t < 16:
                stageB(t)
            if t >= 1:
                stageC(t - 1)
            if t + 1 < 16:
                stageA_pre(t + 1)
            if t >= 1:
                stageC_tail(t - 1)
    fw.barrier()
    if stop_after == "merge":
        return finish()

    with ExitStack() as E:
        ln2 = sb("ln2rows", [128, 2, D], stack=E); lnbox[0] = ln2; lnbox[1] = Buf("ln2")
        fw.dma(sp, ln2[:], ln_d[:, 2:4, :], writes=[lnbox[1]], sem="e9")
        acc = sb("acc", [128, 16, D], stack=E); B_acc = bufs("acc", 16)
        sel = sb("sel", [32, NE * 128], BF16, stack=E); B_sel = Buf("sel")
        fw.dma(pool, sel[:], sel_d[:, :], writes=[B_sel], sem="e0")
        sg = [sb("sg%d" % i, [128, 512], stack=E) for i in range(2)]; B_sg = bufs("sg", 2)
        tu = [sb("tu%d" % i, [128, 512], stack=E) for i in range(2)]; B_tu = bufs("tu", 2)
        gT2 = [sb("gT2_%d" % i, [128, 2, 256], BF16, stack=E) for i in range(2)]; B_gT2 = bufs("gT2", 2)
        n_exp = int(os.environ.get("K_NEXP", str(NE)))

        for e in range(1, min(NSLOT, n_exp)):
            load_expert(e)
        items = [(e, tg) for e in range(n_exp) for tg in range(8)]
        dcnt = [0]

        csb = [sb("csb%d" % i, [128, 256], BF16, stack=E) for i in range(2)]; B_csb = bufs("csb", 2)

        def AUC(i):
            e, tg = items[i]
            sl_ = e % NSLOT
            k2 = i % 2
            pa, Bpa = PS[k2 * 2], PSB[k2 * 2]
            pu, Bpu = PS[k2 * 2 + 1], PSB[k2 * 2 + 1]
            pc_, Bpc = PS[4], PSB[4]
            MM(pc_[:, 0:256], sel[0:32, e * 128:(e + 1) * 128], combT[0:32, tg * 256:(tg + 1) * 256], True, True, [B_sel, B_combT], [Bpc])
            for fc in range(2):
                for ch in range(8):
                    MM(pa[:, fc * 256:(fc + 1) * 256], Wg[sl_][:, ch, fc * 128:(fc + 1) * 128], hT[:, ch, tg * 256:(tg + 1) * 256],
                       ch == 0, ch == 7, [B_W[sl_], B_hT], [Bpa])
            for fc in range(2):
                for ch in range(8):
                    MM(pu[:, fc * 256:(fc + 1) * 256], Wu[sl_][:, ch, fc * 128:(fc + 1) * 128], hT[:, ch, tg * 256:(tg + 1) * 256],
                       ch == 0, ch == 7, [B_W[sl_], B_hT], [Bpu])

        def REST(i):
            e, tg = items[i]
            sl_ = e % NSLOT
            k2 = i % 2
            pa, Bpa = PS[k2 * 2], PSB[k2 * 2]
            pu, Bpu = PS[k2 * 2 + 1], PSB[k2 * 2 + 1]
            A(act, sg[k2][:, :], pa[:, :], AF.Silu, [Bpa], [B_sg[k2]])
            if i + 1 < len(items):
                A(act, csb[1 - k2][:, :], PS[4][:, 0:256], AF.Copy, [PSB[4]], [B_csb[1 - k2]])
            TT(dve, tu[k2][:, :], pu[:, :], sg[k2][:, :], ALU.mult, [Bpu, B_sg[k2]], [B_tu[k2]])
            TT(dve, gT2[k2][:], tu[k2][:, :].rearrange("p (a b) -> p a b", a=2), sbap(csb[k2], 0, [[0, 2], [1, 256]]), ALU.mult,
               [B_tu[k2], B_csb[k2]], [B_gT2[k2]])
            for tt_ in range(2):
                tile_ = tg * 2 + tt_
                for hf in range(2):
                    po, Bpo = PS[5 + dcnt[0] % 3], PSB[5 + dcnt[0] % 3]
                    dcnt[0] += 1
                    for fc in range(2):
                        MM(po[:, :], gT2[k2][:, fc, tt_ * 128:(tt_ + 1) * 128], Wd[sl_][:, fc, hf * 512:(hf + 1) * 512],
                           fc == 0, fc == 1, [B_gT2[k2], B_W[sl_]], [Bpo])
                    ao = acc[:, tile_, hf * 512:(hf + 1) * 512]
                    if e == 0:
                        CP(dve, ao, po[:, :], [Bpo], [B_acc[tile_]])
                    else:
                        TT(dve, ao, po[:, :], ao, ALU.add, [Bpo, B_acc[tile_]], [B_acc[tile_]])
            if tg == 7 and e + NSLOT < n_exp:
                load_expert(e + NSLOT)

        x1t = [stg[i][:, 0:D] for i in range(2)]; B_x1t = B_stg
        st2 = sb("st2", [128, 12], stack=E); mv2 = sb("mv2", [128, 2], stack=E); sc2 = sb("sc2", [128, 8], stack=E); B_st2 = Buf("st2")
        B_out = Buf("out")
        st2s = [sb("st2_%d" % i, [128, 12], stack=E) for i in range(2)]; mv2s = [sb("mv2_%d" % i, [128, 2], stack=E) for i in range(2)]
        sc2s = [sb("sc2_%d" % i, [128, 8], stack=E) for i in range(2)]; B_st2s = bufs("st2s", 2)

        def F1(t):
            s = t % 2
            fw.dma(sp, x1t[s], x1_s[:, t * D:(t + 1) * D], reads=[B_x1s], writes=[B_x1t[s]], sem="stg%d" % s)
            TT(dve, acc[:, t, :], acc[:, t, :], grow[:, 3, :], ALU.mult, [B_acc[t], B_grow], [B_acc[t]])
            STT(acc[:, t, :], x1t[s], ALPHA, acc[:, t, :], ALU.mult, ALU.add, [B_x1t[s], B_acc[t]], [B_acc[t]])
            pre_ = acc[:, t, :]
            for hf in range(2):
                fw.op(dve, lambda e: e.bn_stats(out=st2s[s][:, hf * 6:(hf + 1) * 6], in_=pre_[:, hf * 512:(hf + 1) * 512]), reads=[B_acc[t]], writes=[B_st2s[s]])
            fw.op(dve, lambda e: e.bn_aggr(out=mv2s[s][:, 0:2], in_=st2s[s][:, 0:12]), reads=[B_st2s[s]], writes=[B_st2s[s]])
            A(act, sc2s[s][:, 0:1], mv2s[s][:, 1:2], AF.Ln, [B_st2s[s], B_eps], [B_st2s[s]], bias=epsc[:, 0:1])
            A(act, sc2s[s][:, 1:2], sc2s[s][:, 0:1], AF.Exp, [B_st2s[s]], [B_st2s[s]], scale=-0.5)

        def F2(t):
            s = t % 2
            o_ = acc[:, t, :]
            lnrows, B_ln = lnbox
            TS(dve, o_, o_, mv2s[s][:, 0:1], sc2s[s][:, 1:2], ALU.subtract, ALU.mult, [B_acc[t]], [B_acc[t]], sreads=[B_st2s[s]])
            TT(dve, o_, o_, lnrows[:, 0, :], ALU.mult, [B_acc[t], B_ln], [B_acc[t]])
            TT(dve, o_, o_, lnrows[:, 1, :], ALU.add, [B_acc[t], B_ln], [B_acc[t]])
            fw.dma(sp, out_d[t * 128:(t + 1) * 128, :], o_, reads=[B_acc[t]], writes=[B_out], sem="out")
        AUC(0)
        A(act, csb[0][:, :], PS[4][:, 0:256], AF.Copy, [PSB[4]], [B_csb[0]])
        for i in range(len(items)):
            if i + 1 < len(items):
                AUC(i + 1)
            REST(i)
            if items[i][0] == n_exp - 1:
                tg_ = items[i][1]
                F1(2 * tg_); F1(2 * tg_ + 1); F2(2 * tg_); F2(2 * tg_ + 1)

    return finish()


def _rope_tables(frame_tok):
    t = np.asarray(frame_tok)
    row = (t // 64).astype(np.float32)
    col = (t % 64).astype(np.float32)
    inv = (10000.0 ** (-np.arange(0, 16, 2, dtype=np.float32) / np.float32(16))).astype(np.float32)
    ang = np.concatenate([row[:, None] * inv, col[:, None] * inv], -1)
    ang = np.concatenate([ang, ang], -1).astype(np.float32)
    return np.cos(ang).astype(np.float32), np.sin(ang).astype(np.float32)


def prep_core(inp, b, hh):
    f32 = np.float32
    m = {}
    x = inp["x"][b]
    ctx = inp["ctx"][b]
    if hh == 1:
        x = x[::-1]
        ctx = ctx[::-1]
    m["xf"] = np.ascontiguousarray(x, f32)
    m["ctxf"] = np.ascontiguousarray(ctx, f32)
    cT = np.stack([inp["c"][b].reshape(8, 128).T, inp["c_ctx"].reshape(8, 128).T], -1)
    m["cT"] = np.ascontiguousarray(cT, f32)
    m["w_ada"] = inp["w_ada"][0]
    ba = inp["b_ada"][0]
    m["b_ada_2rows"] = np.ascontiguousarray(np.stack([ba, ba], 0), f32)
    m["w_in"] = inp["w_in"][0]
    m["qg_cols"] = np.ascontiguousarray(inp["q_norm_g"][0].reshape(2, 128).T, f32)
    m["kvg_col"] = np.ascontiguousarray(inp["kv_norm_g"][0].reshape(1, 128).T, f32)
    m["w_uq"] = inp["w_uq"][0]
    m["w_ukv"] = inp["w_ukv"][0]
    mt, j, i = np.meshgrid(np.arange(2), np.arange(16), np.arange(128), indexing="ij")
    tau = (16 * (128 * mt + i) + j).reshape(-1)
    tok = tau if hh == 0 else (NTOK - 1 - tau)
    cos, sin = _rope_tables(tok)
    rope = np.zeros((128, 2, NKEY), f32)
    rope[64:96, 0, :NTOK] = cos.T
    rope[64:96, 1, :NTOK] = sin.T
    rope[64:96, 0, NTOK:] = 1.0
    m["rope"] = rope
    dirs = [0, 1] if hh == 0 else [1, 0]

    def lay(a):
        a = a[dirs]
        sh = a.shape[3:]
        a = a.reshape(2, 16, 2, 64, *sh)
        a = np.moveaxis(a, (2, 3), (0, 1))
        return a.reshape(128, 32, *sh)

    m["ssm_a"] = np.ascontiguousarray(np.stack([lay(inp["ssm_a_re"][0]), lay(inp["ssm_a_im"][0])], 1), f32)
    ldt = np.broadcast_to(inp["ssm_log_dt"][0][:, :, None], (2, 32, 64))
    m["ssm_ldt"] = np.ascontiguousarray(lay(ldt), f32)
    m["ssm_b"] = np.ascontiguousarray(np.stack([lay(inp["ssm_b_re"][0]), lay(inp["ssm_b_im"][0])], 1), f32)
    cre = np.swapaxes(inp["ssm_c_re"][0], 2, 3)
    cim = np.swapaxes(inp["ssm_c_im"][0], 2, 3)
    m["ssm_c"] = np.ascontiguousarray(np.stack([lay(cre), lay(cim)], 1), f32)
    dd = inp["ssm_d"][0].reshape(32, 16)
    m["ssm_dcols"] = np.ascontiguousarray(np.broadcast_to(dd.T[None], (8, 16, 32)).reshape(128, 32), f32)
    jl = np.arange(128) // 16
    m0 = (jl[None, :] >= jl[:, None]).astype(f32)
    m1 = (jl[:, None] >= jl[None, :]).astype(f32)
    m["masks"] = np.ascontiguousarray(np.stack([m0, m1], 1), f32)
    m["ident"] = np.eye(128, dtype=f32)
    m["w_glu"] = inp["w_glu"][0]
    m["b_glu_row"] = np.ascontiguousarray(np.broadcast_to(inp["b_glu"][0][None], (128, 512)), f32)
    gn = np.concatenate([inp["gn_attn_g"][0], inp["gn_ssm_g"][0]])
    m["gn_cols"] = np.ascontiguousarray(gn.reshape(8, 128).T, f32)
    m["w_o"] = inp["w_o"][0]
    ln = np.stack([inp["ln1_g"][0], inp["ln1_b"][0], inp["ln2_g"][0], inp["ln2_b"][0]], 0)
    m["ln_rows"] = np.ascontiguousarray(np.broadcast_to(ln[None], (128, 4, D)), f32)
    m["w_r"] = np.ascontiguousarray(np.concatenate([inp["w_router_group"][0], inp["w_router_expert"][0]], 1), f32)
    br_ = np.concatenate([inp["b_router_group"][0], inp["b_router_expert"][0]])
    m["b_r_row"] = np.ascontiguousarray(np.broadcast_to(br_[None], (128, 36)), f32)
    m["w_exp_gate"] = inp["w_exp_gate"][0]
    m["w_exp_up"] = inp["w_exp_up"][0]
    m["w_exp_down"] = inp["w_exp_down"][0]
    sel = np.zeros((32, NE, 128), f32)
    sel[np.arange(32), np.arange(32), :] = 1.0
    m["sel"] = sel.reshape(32, NE * 128)
    return m


def run(inputs, stop_after=None, dbg_names=(), cores=8):
    inp = {k: np.asarray(v) for k, v in inputs.items()}
    nc = build(stop_after=stop_after, dbg_names=dbg_names)
    in_maps = [prep_core(inp, c // 2, c % 2) for c in range(cores)]
    res = run_bass_kernel_spmd(nc, in_maps, core_ids=list(range(cores)))
    return res.results


def kernel(**inputs):
    res = run(inputs)
    out = np.zeros((4, NTOK, D), np.float32)
    for c in range(8):
        b, hh = c // 2, c % 2
        o = res[c]["out"]
        o = o.reshape(16, 128, D).transpose(1, 0, 2).reshape(NOWN, D)
        if hh == 0:
            out[b, :NOWN] = o
        else:
            out[b, NOWN:] = o[::-1]
    return out
```

```python
import math
import os
from contextlib import ExitStack
import numpy as np
import concourse.bass as bass
import concourse.mybir as mybir
from concourse.ap import AP
from concourse.bass_utils import run_bass_kernel_spmd

F32 = mybir.dt.float32
BF16 = mybir.dt.bfloat16
AF = mybir.ActivationFunctionType
ALU = mybir.AluOpType
AX = mybir.AxisListType

D = 1024
NTOK = 4096
NOWN = 2048
NCTX = 256
NKEY = NTOK + NCTX
NH = 8
SCALE = 96 ** -0.5
ALPHA = 2.0 ** 0.25
EPS = 1e-6
NS = 134
WINDOW = True
NE = 32


class Buf:
    __slots__ = ("name", "w", "r", "excl")

    def __init__(self, name, excl=False):
        self.name = name
        self.w = None
        self.r = []
        self.excl = excl


def bufs(name, n):
    return [Buf("%s%d" % (name, i)) for i in range(n)]


class Q:
    def __init__(self, fw, name, eng, same=False):
        self.name = name
        self.eng = eng
        self.sem = fw.es.enter_context(fw.nc.semaphore("s_" + name))
        self.cnt = 0
        self.seen = {}
        self.same = same
        self.window = False


class DmaTok:
    def __init__(self, fw, name):
        self.sem = fw.es.enter_context(fw.nc.semaphore("d_" + str(name)))
        self.cnt = 0
        self.name = name
        self.same = True


class FW:
    def __init__(self, nc, es):
        self.nc = nc
        self.es = es
        self.pe = Q(self, "pe", nc.tensor)
        self.dve = Q(self, "dve", nc.vector, same=True)
        self.act = Q(self, "act", nc.scalar, same=True)
        self.dve.window = WINDOW
        self.pool = Q(self, "pool", nc.gpsimd, same=True)
        self.sp = Q(self, "sp", nc.sync)
        self.dmasems = {}

    def _waits(self, q, reads, writes, sreads=()):
        need = {}

        def add(dep, force=False):
            if dep is None:
                return
            dq, n = dep
            if dq is q and not force:
                if not q.same:
                    return
                if q.window and n < q.cnt:
                    return
            if need.get(dq, 0) < n:
                need[dq] = n

        for b in sreads:
            add(b.w, True)
        for b in reads:
            add(b.w)
            if b.excl:
                for d in b.r:
                    add(d)
        for b in writes:
            add(b.w)
            for d in b.r:
                add(d)
        for dq, n in need.items():
            if q.seen.get(id(dq), 0) >= n:
                continue
            q.eng.wait_ge(dq.sem, n)
            q.seen[id(dq)] = n

    def op(self, q, fn, reads=(), writes=(), sreads=()):
        self._waits(q, reads, writes, sreads)
        reads = list(reads) + list(sreads)
        ins = fn(q.eng)
        ins.then_inc(q.sem, 1)
        q.cnt += 1
        tok = (q, q.cnt)
        for b in reads:
            b.r.append(tok)
        for b in writes:
            b.w = tok
            b.r = []
        return ins

    def dma(self, q, out, in_, reads=(), writes=(), sem=None, **kw):
        self._waits(q, reads, writes)
        ds = self.dmasems.get(sem)
        if ds is None:
            ds = DmaTok(self, sem)
            self.dmasems[sem] = ds
        if out.dtype != in_.dtype and in_.ap[-1][1] > 2048:
            kw.setdefault("max_dma_last_dim", 4096)
        ins = q.eng.dma_start(out=out, in_=in_, **kw)
        ds.cnt += 16
        ins.then_inc(ds.sem, 16)
        tok = (ds, ds.cnt)
        for b in reads:
            b.r.append(tok)
        for b in writes:
            b.w = tok
            b.r = []
        return tok

    def barrier(self):
        qs = [self.pe, self.dve, self.act, self.pool, self.sp]
        for q in qs:
            for dq in qs + list(self.dmasems.values()):
                if dq is q or dq.cnt == 0:
                    continue
                if q.seen.get(id(dq), 0) >= dq.cnt:
                    continue
                q.eng.wait_ge(dq.sem, dq.cnt)
                q.seen[id(dq)] = dq.cnt

    def group_done(self, sem, blist):
        ds = self.dmasems[sem]
        for b in blist:
            if b.w is not None and b.w[0] is ds:
                b.w = (ds, ds.cnt)


def sbap(t, off, dims):
    full = t[:] if not isinstance(t, AP) else t
    return AP(full.tensor, full.offset + off, [list(full.ap[0])] + [list(d) for d in dims])


def part_ap(t, p0, npart, off, dims):
    full = t[:]
    pstep = full.ap[0][0]
    sub = t[p0:p0 + npart]
    return AP(sub.tensor, sub.offset + off, [[pstep, npart]] + [list(d) for d in dims])


def build(stop_after=None, dbg_names=()):
    nc = bass.Bass("TRN2", target_bir_lowering=False)
    es = ExitStack()
    fw = FW(nc, es)
    pe, dve, act, pool, sp = fw.pe, fw.dve, fw.act, fw.pool, fw.sp
    dbg = {}

    def din(name, shape, dt=F32):
        return nc.dram_tensor(name, list(shape), dt, kind="ExternalInput").ap()

    xf = din("xf", [NTOK, D])
    ctxf = din("ctxf", [NCTX, D])
    cT_d = din("cT", [128, 8, 2])
    w_ada_d = din("w_ada", [D, 6 * D])
    b_ada_2rows_d = din("b_ada_2rows", [2, 6 * D])
    w_in_d = din("w_in", [D, 928])
    qg_d = din("qg_cols", [128, 2])
    kvg_d = din("kvg_col", [128, 1])
    w_uq_d = din("w_uq", [256, 768])
    w_ukv_d = din("w_ukv", [128, 1024])
    rope_d = din("rope", [128, 2, NKEY])
    ssm_a_d = din("ssm_a", [128, 2, 32])
    ssm_ldt_d = din("ssm_ldt", [128, 32])
    ssm_b_d = din("ssm_b", [128, 2, 32, 16])
    ssm_c_d = din("ssm_c", [128, 2, 32, 16])
    ssm_dcols_d = din("ssm_dcols", [128, 32])
    masks_d = din("masks", [128, 2, 128])
    ident_d = din("ident", [128, 128])
    w_glu_d = din("w_glu", [512, 512])
    b_glu_d = din("b_glu_row", [128, 512])
    gn_d = din("gn_cols", [128, 8])
    w_o_d = din("w_o", [D, D])
    ln_d = din("ln_rows", [128, 4, D])
    w_r_d = din("w_r", [D, 36])
    b_r_d = din("b_r_row", [128, 36])
    wg_d = din("w_exp_gate", [NE, D, 256])
    wu_d = din("w_exp_up", [NE, D, 256])
    wd_d = din("w_exp_down", [NE, 256, D])
    sel_d = din("sel", [32, NE * 128])
    out_d = nc.dram_tensor("out", [NOWN, D], F32, kind="ExternalOutput").ap()
    ug_s = nc.dram_tensor("ug_s", [128, 32 * 2 * 272], BF16, kind="Internal").ap()
    attn_s = nc.dram_tensor("attn_s", [128, 16 * 512], F32, kind="Internal").ap()
    x1_s = nc.dram_tensor("x1_s", [128, 16 * D], F32, kind="Internal").ap()
    g_s = nc.dram_tensor("g_s", [128, 16 * 512], F32, kind="Internal").ap()
    dbg_out = {}
    for ent in dbg_names:
        nm, shp = ent[0], ent[1]
        dbg_out[nm] = nc.dram_tensor("dbg_" + nm, list(shp), BF16 if (len(ent) > 2 and ent[2] == "bf16") else F32, kind="ExternalOutput").ap()

    def sb(name, shape, dt=F32, stack=None):
        return (stack or es).enter_context(nc.sbuf_tensor("sb_" + name, list(shape), dt))

    PS = [es.enter_context(nc.psum_tensor("ps%d" % i, [128, 512], F32)) for i in range(8)]
    PSB = [Buf("ps%d" % i, excl=True) for i in range(8)]
    ps_rr = [0]

    def ps_next(k=None):
        i = ps_rr[0] % 8
        ps_rr[0] += 1
        return PS[i], PSB[i]

    ident = sb("ident", [128, 128]); B_ident = Buf("ident")
    ones_bf = sb("ones_bf", [128, 128], BF16); B_ones = Buf("ones")
    modc = sb("modc", [128, 96]); B_modc = Buf("modc")
    grow = sb("grow", [128, 4, D]); B_grow = Buf("grow")
    fw.dma(sp, ident[:], ident_d[:, :], writes=[B_ident], sem="c0")
    fw.op(dve, lambda e: e.memset(ones_bf[:], 1.0), writes=[B_ones])

    def dump(name, ap, reads):
        if name in dbg_out:
            fw.dma(sp, dbg_out[name], ap, reads=reads, writes=[], sem="dbg")

    def finish():
        for ds in fw.dmasems.values():
            sp.eng.wait_ge(ds.sem, ds.cnt)
        es.close()
        return nc

    with ExitStack() as ph:
        cT = sb("cT", [128, 8, 2], stack=ph); B_cT = Buf("cT")
        sil2 = sb("sil2", [128, 8, 2], stack=ph); B_sil = Buf("sil2")
        b2 = sb("b2rows", [2, 6 * D], stack=ph); B_b2 = Buf("b2")
        modrow = sb("modrow", [2, 6 * D], stack=ph); B_mr = Buf("modrow")
        ones_f = sb("ones_f", [1, 128], stack=ph); B_onesf = Buf("ones_f")
        wa = [sb("wa%d" % i, [128, 8, D], stack=ph) for i in range(2)]
        B_wa = bufs("wa", 2)
        fw.dma(sp, cT[:], cT_d[:, :, :], writes=[B_cT], sem="c1")
        fw.dma(sp, b2[:], b_ada_2rows_d[:, :], writes=[B_b2], sem="c2")
        fw.op(dve, lambda e: e.memset(ones_f[:], 1.0), writes=[B_onesf])
        fw.op(act, lambda e: e.activation(out=sil2[:], in_=cT[:], func=AF.Silu), reads=[B_cT], writes=[B_sil])
        w_ada_v = w_ada_d.rearrange("(k p) n -> p k n", p=128)
        for v in range(6):
            slot = v % 2
            fw.dma(sp if v % 2 == 0 else act, wa[slot][:], w_ada_v[:, :, v * D:(v + 1) * D], writes=[B_wa[slot]], sem="wa%d" % slot)
            for half in range(2):
                pr, Bpr = PS[half], PSB[half]
                for k in range(8):
                    fw.op(pe, lambda e: e.matmul(pr[0:2, :], lhsT=sil2[:, k, :], rhs=wa[slot][:, k, half * 512:(half + 1) * 512],
                                                 start=(k == 0), stop=(k == 7)), reads=[B_wa[slot], B_sil], writes=[Bpr])
                c0 = v * D + half * 512
                fw.op(dve, lambda e: e.tensor_tensor(out=modrow[0:2, c0:c0 + 512], in0=pr[0:2, :], in1=b2[0:2, c0:c0 + 512], op=ALU.add),
                      reads=[Bpr, B_b2], writes=[B_mr])
        for v in (1, 4):
            fw.op(dve, lambda e: e.tensor_scalar_add(out=modrow[0:2, v * D:(v + 1) * D], in0=modrow[0:2, v * D:(v + 1) * D], scalar1=1.0),
                  reads=[B_mr], writes=[B_mr])
        psc, Bpsc = PS[2], PSB[2]
        for vc in range(48):
            fw.op(pe, lambda e: e.matmul(psc[:, vc * 2:vc * 2 + 2], lhsT=modrow[0:2, vc * 128:(vc + 1) * 128], rhs=ident[0:2, 0:2],
                                         start=True, stop=True, is_transpose=True), reads=[B_mr, B_ident], writes=[Bpsc])
        fw.op(dve, lambda e: e.tensor_copy(out=modc[:], in_=psc[:, 0:96]), reads=[Bpsc], writes=[B_modc])
        for ri, v in enumerate((2, 3, 4, 5)):
            for half in range(2):
                pr, Bpr = PS[3 + (ri * 2 + half) % 2], PSB[3 + (ri * 2 + half) % 2]
                c0 = v * D + half * 512
                fw.op(pe, lambda e: e.matmul(pr[:, :], lhsT=ones_f[0:1, :], rhs=modrow[0:1, c0:c0 + 512], start=True, stop=True),
                      reads=[B_onesf, B_mr], writes=[Bpr])
                fw.op(act, lambda e: e.activation(out=grow[:, ri, half * 512:(half + 1) * 512], in_=pr[:, :], func=AF.Copy),
                      reads=[Bpr], writes=[B_grow])
    fw.barrier()
    dump("modc", modc[:], [B_modc])
    dump("grow", grow[:, 0:2].rearrange("p a b -> p (a b)"), [B_grow])
    if stop_after == "adaln":
        return finish()

    def mcol(v, ch, who):
        i = (v * 8 + ch) * 2 + who
        return modc[:, i:i + 1]

    def A(q, out, in_, func, reads, writes, **kw):
        return fw.op(q, lambda e: e.activation(out=out, in_=in_, func=func, **kw), reads=reads, writes=writes)

    def TT(q, out, in0, in1, op, reads, writes):
        return fw.op(q, lambda e: e.tensor_tensor(out=out, in0=in0, in1=in1, op=op), reads=reads, writes=writes)

    def TS(q, out, in0, s1, s2, op0, op1, reads, writes, sreads=()):
        return fw.op(q, lambda e: e.tensor_scalar(out=out, in0=in0, scalar1=s1, scalar2=s2, op0=op0, op1=op1), reads=reads, writes=writes, sreads=sreads)

    def STT(out, in0, scalar, in1, op0, op1, reads, writes):
        return fw.op(dve, lambda e: e.scalar_tensor_tensor(out=out, in0=in0, scalar=scalar, in1=in1, op0=op0, op1=op1), reads=reads, writes=writes)

    def CP(q, out, in_, reads, writes):
        return fw.op(q, lambda e: e.tensor_copy(out=out, in_=in_), reads=reads, writes=writes)

    def MM(out, lhsT, rhs, start, stop, reads, writes):
        return fw.op(pe, lambda e: e.matmul(out, lhsT=lhsT, rhs=rhs, start=start, stop=stop), reads=reads, writes=writes)

    def TR(out, in_, reads, writes, n=128):
        return fw.op(pe, lambda e: e.matmul(out, lhsT=in_, rhs=ident[0:n, 0:n], start=True, stop=True, is_transpose=True),
                     reads=list(reads) + [B_ident], writes=writes)

    def rstd_from_ss(out, ss_ps, B_ss, inv_n, tmp, B_tmp, B_out):
        A(act, tmp, ss_ps, AF.Ln, [B_ss, B_eps], [B_tmp], scale=inv_n, bias=epsc[:, 0:1])
        A(act, out, tmp, AF.Exp, [B_tmp], [B_out], scale=-0.5)

    epsc = sb("epsc", [128, 1]); B_eps = Buf("eps")
    fw.op(dve, lambda e: e.memset(epsc[:], EPS), writes=[B_eps])

    att = ExitStack()
    cqT = sb("cqT", [128, 2, NOWN], BF16, stack=att); B_cqT = Buf("cqT")
    rstdq = sb("rstdq", [128, NOWN], stack=att); B_rstdq = Buf("rstdq")
    ckvnT = sb("ckvnT", [128, NKEY], BF16, stack=att); B_ckvnT = Buf("ckvnT")
    KRT = sb("KRT", [128, NKEY], BF16, stack=att); B_KRT = Buf("KRT")

    with ExitStack() as ph:
        w_in = sb("w_in", [128, 8, 928], BF16, stack=ph); B_win = Buf("w_in")
        w_krot = sb("w_krot", [128, 8, 32], BF16, stack=ph); B_wkrot = Buf("w_krot")
        fw.dma(pool, w_in[:], w_in_d.rearrange("(k p) n -> p k n", p=128), writes=[B_win], sem="w_in")
        CP(dve, w_krot[:, :, 16:32], w_in[:, :, 384:400], [B_win], [B_wkrot])
        TS(dve, w_krot[:, :, 0:16], w_in[:, :, 400:416], -1.0, None, ALU.mult, ALU.bypass, [B_win], [B_wkrot])
        xin = [sb("xin%d" % i, [128, D], stack=ph) for i in range(4)]; B_xin = bufs("xin", 4)
        xmT = [sb("xmT%d" % i, [128, 8, 512], BF16, stack=ph) for i in range(2)]; B_xmT = bufs("xmT", 2)
        ropeg = [sb("ropeg%d" % i, [128, 2, 512], stack=ph) for i in range(2)]; B_ropeg = bufs("ropeg", 2)
        u_tm = sb("u_tm", [128, 32, 128], stack=ph); B_utm = Buf("u_tm")
        Ug = sb("Ug", [128, 32, 2, 272], BF16, stack=ph); B_Ug = Buf("Ug")
        sqb = sb("sqb", [128, 512], BF16, stack=ph); B_sqb = Buf("sqb")
        sqq = sb("sqq", [128, 2, 512], BF16, stack=ph); B_sqq = Buf("sqq")
        rawkv = sb("rawkv", [128, 512], stack=ph); B_rawkv = Buf("rawkv")
        lnt = sb("lnt", [128, 512], stack=ph); B_lnt = Buf("lnt")
        rstdkv = sb("rstdkv", [128, 512], stack=ph); B_rstdkv = Buf("rstdkv")
        rt1 = sb("rt1", [128, 512], stack=ph); B_rt1 = Buf("rt1")
        rt2 = sb("rt2", [128, 512], stack=ph); B_rt2 = Buf("rt2")
        xf_v = xf.rearrange("(m j) d -> j m d", j=16)
        ctx_v = ctxf.rearrange("(m j) d -> j m d", j=16)
        gcount = [0]

        def in_group(kind, mt, jg):
            gs = gcount[0] % 2
            gcount[0] += 1
            lat = kind == "lat"
            ncols = 512 if lat else 256
            col0 = (mt * 16 + jg * 4) * 128 if lat else NTOK
            own = lat and mt == 0
            np_ = 128 if lat else 16
            nt = 4 if lat else 16
            tw = 128 if lat else 16
            if lat:
                for t in range(4):
                    j = jg * 4 + t
                    fw.dma(sp, xin[t][:], xf_v[j, mt * 128:(mt + 1) * 128, :], writes=[B_xin[t]], sem="xin%d" % t)
            fw.dma(sp, ropeg[gs][64:96, :, 0:ncols], rope_d[64:96, :, col0:col0 + ncols], writes=[B_ropeg[gs]], sem="ropeg%d" % gs)
            who = 0 if lat else 1
            if lat:
                for ch in range(8):
                    ps, Bp = ps_next()
                    for t in range(4):
                        TR(ps[:, t * 128:(t + 1) * 128], xin[t][:, ch * 128:(ch + 1) * 128], [B_xin[t]], [Bp])
                    A(act, xmT[gs][:, ch, 0:ncols], ps[:, 0:ncols], AF.Identity, [Bp, B_modc], [B_xmT[gs]],
                      scale=mcol(1, ch, who), bias=mcol(0, ch, who))
            else:
                banks = [ps_next() for _ in range(8)]
                for t in range(16):
                    fw.dma(sp, xin[t % 4][0:16, :], ctx_v[t, 0:16, :], writes=[B_xin[t % 4]], sem="xin%d" % (t % 4))
                    for ch in range(8):
                        ps, Bp = banks[ch]
                        TR(ps[:, t * 16:(t + 1) * 16], xin[t % 4][0:16, ch * 128:(ch + 1) * 128], [B_xin[t % 4]], [Bp], n=16)
                for ch in range(8):
                    ps, Bp = banks[ch]
                    A(act, xmT[gs][:, ch, 0:ncols], ps[:, 0:ncols], AF.Identity, [Bp, B_modc], [B_xmT[gs]],
                      scale=mcol(1, ch, who), bias=mcol(0, ch, who))
            X = xmT[gs]; BX = B_xmT[gs]
            kstep = 9
            yield
            ps, Bp = ps_next()
            for ch in range(8):
                MM(ps[:, 0:ncols], w_in[:, ch, 256:384], X[:, ch, 0:ncols], ch == 0, ch == 7, [B_win, BX], [Bp])
            A(act, sqb[:, 0:ncols], ps[:, 0:ncols], AF.Square, [Bp], [B_sqb])
            A(act, rawkv[:, 0:ncols], ps[:, 0:ncols], AF.Copy, [Bp], [B_rawkv])
            ps2, Bp2 = ps_next()
            MM(ps2[:, 0:ncols], ones_bf[:, :], sqb[:, 0:ncols], True, True, [B_ones, B_sqb], [Bp2])
            rstd_from_ss(rstdkv[:, 0:ncols], ps2[:, 0:ncols], Bp2, 1.0 / 128, lnt[:, 0:ncols], B_lnt, B_rstdkv)
            TT(dve, ckvnT[:, col0:col0 + ncols], rawkv[:, 0:ncols], rstdkv[:, 0:ncols], ALU.mult, [B_rawkv, B_rstdkv], [B_ckvnT])
            psa, Bpa = ps_next()
            psb, Bpb = ps_next()
            for ch in range(8):
                MM(psa[64:96, 0:ncols], w_in[:, ch, 384:416], X[:, ch, 0:ncols], ch == 0, ch == 7, [B_win, BX], [Bpa])
            for ch in range(8):
                MM(psb[64:96, 0:ncols], w_krot[:, ch, :], X[:, ch, 0:ncols], ch == 0, ch == 7, [B_wkrot, BX], [Bpb])
            TT(dve, rt1[64:96, 0:ncols], psa[64:96, 0:ncols], ropeg[gs][64:96, 0, 0:ncols], ALU.mult, [Bpa, B_ropeg[gs]], [B_rt1])
            TT(dve, rt2[64:96, 0:ncols], psb[64:96, 0:ncols], ropeg[gs][64:96, 1, 0:ncols], ALU.mult, [Bpb, B_ropeg[gs]], [B_rt2])
            TT(dve, KRT[64:96, col0:col0 + ncols], rt1[64:96, 0:ncols], rt2[64:96, 0:ncols], ALU.add, [B_rt1, B_rt2], [B_KRT])
            if own:
                pss = []
                for fc in range(2):
                    ps, Bp = ps_next()
                    for ch in range(8):
                        MM(ps[:, :], w_in[:, ch, fc * 128:(fc + 1) * 128], X[:, ch, :], ch == 0, ch == 7, [B_win, BX], [Bp])
                    A(act, cqT[:, fc, col0:col0 + 512], ps[:, :], AF.Copy, [Bp], [B_cqT])
                    pss.append((ps, Bp))
                ksub = int(os.environ.get("K_SUB", "9"))
                if ksub >= 1:
                    ps2, Bp2 = ps_next()
                    for fc in range(2):
                        ps, Bp = pss[fc]
                        A(act, sqq[:, fc, :], ps[:, :], AF.Square, [Bp], [B_sqq])
                    for fc in range(2):
                        MM(ps2[:, :], ones_bf[:, :], sqq[:, fc, :], fc == 0, fc == 1, [B_ones, B_sqq], [Bp2])
                if ksub >= 2:
                    rstd_from_ss(rstdq[:, col0:col0 + 512], ps2[:, :], Bp2, 1.0 / 256, lnt[:, :], B_lnt, B_rstdq)
            for t in range(nt):
                ps, Bp = ps_next()
                for ch in range(8):
                    MM(ps[0:np_, :], X[:, ch, t * tw:(t + 1) * tw], w_in[:, ch, 416:928], ch == 0, ch == 7, [B_win, BX], [Bp])
                if lat:
                    jl = (jg * 4 + t) % 8
                    uo = sbap(u_tm, jl * 16, [[128, 32], [1, 16]])
                    ui = sbap(ps, 0, [[16, 32], [1, 16]])
                    if t % 2 == 0:
                        CP(dve, uo, ui, [Bp], [B_utm])
                    else:
                        A(act, uo, ui, AF.Copy, [Bp], [B_utm])
                else:
                    A(act, part_ap(u_tm, 0, 16, (t % 8) * 16, [[128, 32], [1, 16]]), part_ap(ps, 0, 16, 0, [[16, 32], [1, 16]]),
                      AF.Copy, [Bp], [B_utm])
                    if t % 8 == 7:
                        ug_transposes(u_tm, B_utm, 16, t // 8, 256, 0)

        def ug_transposes(src, Bsrc, np_, jt, ucol0, jbase):
            for g4 in range(8):
                ps, Bp = ps_next()
                for gg in range(4):
                    g = g4 * 4 + gg
                    inap = part_ap(src, 0, np_, g * 128, [[1, 128]])
                    TR(ps[:, gg * 128:gg * 128 + np_], inap, [Bsrc], [Bp], n=np_)
                outap = sbap(Ug, (g4 * 4) * 2 * 272 + jt * 272 + ucol0, [[2 * 272, 4], [1, np_]])
                inp_ = sbap(ps, 0, [[128, 4], [1, np_]])
                if g4 % 2 == 0:
                    CP(dve, outap, inp_, [Bp], [B_Ug])
                else:
                    A(act, outap, inp_, AF.Copy, [Bp], [B_Ug])

        glist = [("lat", mt, jg) for mt in range(2) for jg in range(4)] + [("ctx", 0, 0)]
        gens = [in_group(*g_) for g_ in glist]
        next(gens[0])
        for gi_, g_ in enumerate(glist):
            if gi_ + 1 < len(glist):
                next(gens[gi_ + 1])
            for _ in gens[gi_]:
                pass
            if g_[0] == "lat" and g_[2] % 2 == 1:
                ug_transposes(u_tm, B_utm, 128, g_[2] // 2, g_[1] * 128, 0)
        dump("ckvnT", ckvnT[:], [B_ckvnT])
        dump("KRT", KRT[64:96, :], [B_KRT])
        dump("cqT", cqT[:].rearrange("p a b -> p (a b)"), [B_cqT])
        dump("rstdq", rstdq[:], [B_rstdq])
        dump("Ug", Ug[:].rearrange("p a b c -> p (a b c)"), [B_Ug])
        fw.dma(sp, ug_s[:, :], Ug[:].rearrange("p a b c -> p (a b c)"), reads=[B_Ug], writes=[], sem="ugs")
    fw.barrier()
    if stop_after == "inproj":
        return finish()

    attn_tm = sb("attn_tm", [128, 16, 512], stack=att); B_attn = Buf("attn_tm")
    B_attn_s = Buf("attn_s")
    with ExitStack() as ph:
        wq_f = sb("wq_f", [128, 2, 768], stack=ph); B_wqf = Buf("wq_f")
        wkv_f = sb("wkv_f", [128, 1024], stack=ph); B_wkvf = Buf("wkv_f")
        qg = sb("qg", [128, 2], stack=ph); kvg = sb("kvg", [128, 1], stack=ph); B_g = Buf("qkvg")
        w_uq_s = sb("w_uq_s", [128, 2, 768], BF16, stack=ph); B_wuq = Buf("w_uq_s")
        w_uq_rot = sb("w_uq_rot", [128, 2, 8, 32], BF16, stack=ph); B_wuqr = Buf("w_uq_rot")
        w_k = sb("w_k", [128, 1024], BF16, stack=ph); B_wk = Buf("w_k")
        w_v = sb("w_v", [128, 512], BF16, stack=ph); B_wv = Buf("w_v")
        V_all = sb("V_all", [128, 34, 8, 65], BF16, stack=ph); B_V = Buf("V_all")
        KT = [sb("KT%d" % i, [128, NKEY], BF16, stack=ph) for i in range(2)]; B_KT = bufs("KT", 2)
        QT = [sb("QT%d" % i, [128, NOWN], BF16, stack=ph) for i in range(2)]; B_QT = bufs("QT", 2)
        ropeq = sb("ropeq", [128, 2, NOWN], stack=ph); B_ropeq = Buf("ropeq")
        PT = [sb("PT%d" % i, [128, 512], BF16, stack=ph) for i in range(4)]; B_PT = bufs("PT", 4)
        ot = [sb("ot%d" % i, [128, 512], stack=ph) for i in range(2)]; B_ot = bufs("ot", 2)
        qt1 = sb("qt1", [128, 512], stack=ph); B_qt1 = Buf("qt1")
        qt2 = sb("qt2", [128, 512], stack=ph); B_qt2 = Buf("qt2")
        rd = sb("rd", [128, 4], stack=ph); B_rd = Buf("rd")
        fw.dma(sp, wq_f[:], w_uq_d.rearrange("(c p) n -> p c n", p=128), writes=[B_wqf], sem="a0")
        fw.dma(sp, wkv_f[:], w_ukv_d[:, :], writes=[B_wkvf], sem="a1")
        fw.dma(sp, qg[:], qg_d[:, :], writes=[B_g], sem="a2")
        fw.dma(sp, kvg[:], kvg_d[:, :], writes=[B_g], sem="a3")
        fw.dma(sp, ropeq[64:96, :, :], rope_d[64:96, :, 0:NOWN], writes=[B_ropeq], sem="a4")
        for c in range(2):
            TS(dve, w_uq_s[:, c, :], wq_f[:, c, :], qg[:, c:c + 1], None, ALU.mult, ALU.bypass, [B_wqf, B_g], [B_wuq])
            TS(dve, w_uq_rot[:, c, :, 0:16], sbap(w_uq_s, c * 768 + 80, [[96, 8], [1, 16]]), -1.0, None, ALU.mult, ALU.bypass, [B_wuq], [B_wuqr])
            CP(dve, w_uq_rot[:, c, :, 16:32], sbap(w_uq_s, c * 768 + 64, [[96, 8], [1, 16]]), [B_wuq], [B_wuqr])
        TS(dve, w_k[:, :], wkv_f[:, :], kvg[:, 0:1], None, ALU.mult, ALU.bypass, [B_wkvf, B_g], [B_wk])
        CP(dve, w_v[:].rearrange("p (h d) -> p h d", h=8), sbap(w_k, 64, [[128, 8], [1, 64]]), [B_wk], [B_wv])
        fw.op(dve, lambda e: e.memset(V_all[:, :, :, 64:65], 1.0), writes=[B_V])
        for i in range(2):
            fw.op(dve, lambda e: e.memset(KT[i][96:128, :], 0.0), writes=[B_KT[i]])
            fw.op(dve, lambda e: e.memset(QT[i][96:128, :], 0.0), writes=[B_QT[i]])
        for kt in range(34):
            ps, Bp = PS[6 + kt % 2], PSB[6 + kt % 2]
            MM(ps[:, :], ckvnT[:, kt * 128:(kt + 1) * 128], w_v[:, :], True, True, [B_ckvnT, B_wv], [Bp])
            vo = V_all[:, kt, :, 0:64]
            vi = ps[:, :].rearrange("p (h d) -> p h d", h=8)
            if kt % 2 == 0:
                CP(dve, vo, vi, [Bp], [B_V])
            else:
                A(act, vo, vi, AF.Copy, [Bp], [B_V])

        def gen(h):
            s = h % 2
            for ct in range(9):
                n = 512 if ct < 8 else 256
                c0 = ct * 512
                ps, Bp = PS[6], PSB[6]
                MM(ps[0:64, 0:n], w_k[:, h * 128:h * 128 + 64], ckvnT[:, c0:c0 + n], True, True, [B_wk, B_ckvnT], [Bp])
                CP(dve, KT[s][0:64, c0:c0 + n], ps[0:64, 0:n], [Bp], [B_KT[s]])
                yield
            CP(pool, KT[s][64:96, :], KRT[64:96, :], [B_KRT], [B_KT[s]])
            for ct in range(4):
                c0 = ct * 512
                psq, Bq = PS[6], PSB[6]
                psr, Br = PS[7], PSB[7]
                for c in range(2):
                    MM(psq[0:96, :], w_uq_s[:, c, h * 96:(h + 1) * 96], cqT[:, c, c0:c0 + 512], c == 0, c == 1, [B_wuq, B_cqT], [Bq])
                for c in range(2):
                    MM(psr[64:96, :], w_uq_rot[:, c, h, :], cqT[:, c, c0:c0 + 512], c == 0, c == 1, [B_wuqr, B_cqT], [Br])
                TT(dve, QT[s][0:64, c0:c0 + 512], psq[0:64, :], rstdq[0:64, c0:c0 + 512], ALU.mult, [Bq, B_rstdq], [B_QT[s]])
                TT(dve, qt1[64:96, :], psq[64:96, :], ropeq[64:96, 0, c0:c0 + 512], ALU.mult, [Bq, B_ropeq], [B_qt1])
                TT(dve, qt2[64:96, :], psr[64:96, :], ropeq[64:96, 1, c0:c0 + 512], ALU.mult, [Br, B_ropeq], [B_qt2])
                TT(dve, qt1[64:96, :], qt1[64:96, :], qt2[64:96, :], ALU.add, [B_qt1, B_qt2], [B_qt1])
                TT(dve, QT[s][64:96, c0:c0 + 512], qt1[64:96, :], rstdq[64:96, c0:c0 + 512], ALU.mult, [B_qt1, B_rstdq], [B_QT[s]])
                yield

        scount = [0]
        ocount = [0]

        pend = [None]

        def epilogue(h, qt, ob, osl):
            CP(dve, ot[osl][0:65, :], PS[ob][0:65, :], [PSB[ob]], [B_ot[osl]])
            pso, Bo = PS[7], PSB[7]
            for t in range(4):
                TR(pso[:, t * 128:t * 128 + 65], ot[osl][0:65, t * 128:(t + 1) * 128], [B_ot[osl]], [Bo], n=65)
            fw.op(dve, lambda e: e.reciprocal(out=rd[:, :], in_=sbap(pso, 64, [[128, 4]])), reads=[Bo], writes=[B_rd])
            for t in range(4):
                TS(dve, attn_tm[:, qt * 4 + t, h * 64:(h + 1) * 64], pso[:, t * 128:t * 128 + 64], rd[:, t:t + 1], None, ALU.mult, ALU.bypass,
                   [Bo], [B_attn], sreads=[B_rd])

        def attend(h, nxt):
            s = h % 2
            itc = [0]
            for qt in range(4):
                ob = 4 + ocount[0] % 2
                osl = ocount[0] % 2
                ocount[0] += 1
                base = scount[0]
                scount[0] += 34

                def S(kt):
                    bi = (base + kt) % 4
                    MM(PS[bi][:, :], KT[s][:, kt * 128:(kt + 1) * 128], QT[s][:, qt * 512:(qt + 1) * 512], True, True,
                       [B_KT[s], B_QT[s]], [PSB[bi]])
                S(0)
                S(1)
                if pend[0] is not None:
                    epilogue(*pend[0])
                    pend[0] = None
                for kt in range(34):
                    if kt + 2 < 34:
                        S(kt + 2)
                    bi = (base + kt) % 4
                    A(act, PT[bi][:, :], PS[bi][:, :], AF.Exp, [PSB[bi]], [B_PT[bi]], scale=SCALE)
                    MM(PS[ob][0:65, :], V_all[:, kt, h, 0:65], PT[bi][:, :], kt == 0, kt == 33, [B_V, B_PT[bi]], [PSB[ob]])
                    itc[0] += 1
                    if nxt is not None and itc[0] % 9 == 0:
                        next(nxt, None)
                pend[0] = (h, qt, ob, osl)

        nheads = int(os.environ.get("K_HEADS", "8"))
        for _ in gen(0):
            pass
        for h in range(nheads):
            nxt = gen(h + 1) if h + 1 < nheads else None
            attend(h, nxt)
            if nxt is not None:
                for _ in nxt:
                    pass
        epilogue(*pend[0])
        dump("attn", attn_tm[:].rearrange("p a b -> p (a b)"), [B_attn])
        fw.dma(sp, attn_s[:, :], attn_tm[:].rearrange("p a b -> p (a b)"), reads=[B_attn], writes=[B_attn_s], sem="attns")
        dump("KT0", KT[0][0:96, :], [B_KT[0]])
        dump("QT0", QT[0][0:96, :], [B_QT[0]])
        dump("V0", V_all[:].rearrange("p a b c -> p (a b c)"), [B_V])
    fw.barrier()
    att.close()
    if stop_after == "attn":
        return finish()

    PI = math.pi
    B_gs = Buf("g_s")
    with ExitStack() as S:
        sa = sb("ssm_a", [128, 2, 32], stack=S); sl = sb("ssm_l", [128, 32], stack=S)
        sbb = sb("ssm_b", [128, 2, 32, 16], stack=S); scc = sb("ssm_c", [128, 2, 32, 16], stack=S)
        dcols = sb("dcols", [128, 32], stack=S); masks = sb("masks4", [128, 4, 128], stack=S)
        B_par = Buf("ssm_par")
        fw.dma(sp, sa[:], ssm_a_d[:, :, :], writes=[B_par], sem="s0")
        fw.dma(sp, sl[:], ssm_ldt_d[:, :], writes=[B_par], sem="s1")
        fw.dma(sp, sbb[:], ssm_b_d[:, :, :, :], writes=[B_par], sem="s2")
        fw.dma(sp, scc[:], ssm_c_d[:, :, :, :], writes=[B_par], sem="s3")
        fw.dma(sp, dcols[:], ssm_dcols_d[:, :], writes=[B_par], sem="s4")
        fw.dma(sp, masks[:, 0:2, :], masks_d[:, :, :], writes=[B_par], sem="s5")
        fw.dma(sp, masks[:, 2:4, :], masks_d[:, :, :], writes=[B_par], sem="s6")
        w_glu = sb("w_glu", [128, 4, 512], BF16, stack=S); B_wglu = Buf("w_glu")
        fw.dma(pool, w_glu[:], w_glu_d.rearrange("(k p) n -> p k n", p=128), writes=[B_wglu], sem="s7")
        bglu = sb("bglu", [128, 512], stack=S); B_bglu = Buf("bglu")
        fw.dma(sp, bglu[:], b_glu_d[:, :], writes=[B_bglu], sem="s8")
        sm = sb("ssm_sm", [128, 32, 32], stack=S); B_sm = Buf("ssm_sm")
        _smi = [0]

        def smt():
            i = _smi[0]; _smi[0] += 1
            return sm[:, i, :]
        Bbar = sb("Bbar", [128, 2, 32, 16], stack=S)
        PWr = sb("PWr", [128, 17, 32], stack=S); PWi = sb("PWi", [128, 17, 32], stack=S)
        IPr = sb("IPr", [128, 17, 32], stack=S); IPi = sb("IPi", [128, 17, 32], stack=S)
        AA = sb("AA", [128, 2, 2, 16], stack=S); AB = sb("AB", [128, 2, 2, 16], stack=S)
        BS = [B_par, B_sm]

        def tt(out, a, b, op):
            TT(dve, out, a, b, op, BS, [B_sm])

        a_re, a_im = sa[:, 0, :], sa[:, 1, :]
        dt_ = smt(); lre = smt(); ang = smt(); mag = smt()
        A(act, dt_, sl[:, :], AF.Exp, [B_par], [B_sm])
        tt(lre, a_re, dt_, ALU.mult)
        tt(ang, a_im, dt_, ALU.mult)
        A(act, mag, lre, AF.Exp, [B_sm], [B_sm])

        def sin_of(src_ang, shift):
            a2 = smt(); k = smt(); r = smt(); o = smt()
            TS(dve, a2, src_ang, shift, None, ALU.add, ALU.bypass, BS, [B_sm])
            TS(dve, k, a2, PI, None, ALU.is_ge, ALU.bypass, BS, [B_sm])
            for i in range(2, 9):
                STT(k, a2, (2 * i - 1) * PI, k, ALU.is_ge, ALU.add, BS, [B_sm])
            STT(r, k, -2.0 * PI, a2, ALU.mult, ALU.add, BS, [B_sm])
            A(act, o, r, AF.Sin, [B_sm], [B_sm])
            return o
        sn = sin_of(ang, 0.0)
        cs = sin_of(ang, PI / 2)
        abre = smt(); abim = smt()
        tt(abre, mag, cs, ALU.mult)
        tt(abim, mag, sn, ALU.mult)
        t1 = smt(); t2 = smt(); den = smt(); rden = smt(); nre = smt(); cre = smt(); cim = smt()
        tt(t1, a_re, a_re, ALU.mult); tt(t2, a_im, a_im, ALU.mult); tt(den, t1, t2, ALU.add)
        fw.op(dve, lambda e: e.reciprocal(out=rden, in_=den), reads=BS, writes=[B_sm])
        TS(dve, nre, abre, -1.0, None, ALU.add, ALU.bypass, BS, [B_sm])
        tt(t1, nre, a_re, ALU.mult); tt(t2, abim, a_im, ALU.mult); tt(t1, t1, t2, ALU.add); tt(cre, t1, rden, ALU.mult)
        tt(t1, abim, a_re, ALU.mult); tt(t2, nre, a_im, ALU.mult); tt(t1, t1, t2, ALU.subtract); tt(cim, t1, rden, ALU.mult)
        tb = sb("tb", [128, 32, 16], stack=S)

        def bc16(v):
            return AP(v.tensor, v.offset, [list(v.ap[0]), [1, 32], [0, 16]])
        tt(Bbar[:, 0], sbb[:, 0], bc16(cre), ALU.mult); tt(tb[:], sbb[:, 1], bc16(cim), ALU.mult); tt(Bbar[:, 0], Bbar[:, 0], tb[:], ALU.subtract)
        tt(Bbar[:, 1], sbb[:, 1], bc16(cre), ALU.mult); tt(tb[:], sbb[:, 0], bc16(cim), ALU.mult); tt(Bbar[:, 1], Bbar[:, 1], tb[:], ALU.add)
        m2 = smt(); rm2 = smt(); iabre = smt(); iabim = smt()
        tt(t1, abre, abre, ALU.mult); tt(t2, abim, abim, ALU.mult); tt(m2, t1, t2, ALU.add)
        fw.op(dve, lambda e: e.reciprocal(out=rm2, in_=m2), reads=BS, writes=[B_sm])
        tt(iabre, abre, rm2, ALU.mult)
        STT(iabim, abim, -1.0, rm2, ALU.mult, ALU.mult, BS, [B_sm])
        for (Pr, Pi, br, bi) in ((PWr, PWi, abre, abim), (IPr, IPi, iabre, iabim)):
            fw.op(dve, lambda e: e.memset(Pr[:, 0, :], 1.0), writes=[B_sm])
            fw.op(dve, lambda e: e.memset(Pi[:, 0, :], 0.0), writes=[B_sm])
            CP(dve, Pr[:, 1, :], br, BS, [B_sm])
            CP(dve, Pi[:, 1, :], bi, BS, [B_sm])
            for k in range(2, 17):
                tt(t1, Pr[:, k - 1, :], br, ALU.mult); tt(t2, Pi[:, k - 1, :], bi, ALU.mult); tt(Pr[:, k, :], t1, t2, ALU.subtract)
                tt(t1, Pr[:, k - 1, :], bi, ALU.mult); tt(t2, Pi[:, k - 1, :], br, ALU.mult); tt(Pi[:, k, :], t1, t2, ALU.add)
        for d in range(2):
            for c in range(2):
                CP(dve, AA[:, d, c, :], PWr[:, 16, d * 16:(d + 1) * 16], BS, [B_sm])
            TS(dve, AB[:, d, 0, :], PWi[:, 16, d * 16:(d + 1) * 16], -1.0, None, ALU.mult, ALU.bypass, BS, [B_sm])
            CP(dve, AB[:, d, 1, :], PWi[:, 16, d * 16:(d + 1) * 16], BS, [B_sm])
        dump("sm", sm[:].rearrange("p a b -> p (a b)"), BS)
        dump("abar", sbap(PWr, 32, [[1, 32]]), BS)
        dump("abari", sbap(PWi, 32, [[1, 32]]), BS)
        dump("Bbar", Bbar[:].rearrange("p a b c -> p (a b c)"), BS)

        ctmp = [sb("ctmp%d" % i, [128, 4, 256], stack=S) for i in range(2)]; B_ct = Buf("ctmp")

        def cplx_build(q, out_t, P_r, P_i, k0, kstep, Vt, d, neg_im):
            for half in range(4):
                p0 = half * 4

                def Pv(Pt):
                    return sbap(Pt, k0 * 32 + d * 16 + p0, [[1, 4], [kstep * 32, 16], [0, 16]])

                def Vv(c):
                    return sbap(Vt, c * 512 + (d * 16 + p0) * 16, [[16, 4], [0, 16], [1, 16]])

                def Ov(c):
                    return sbap(out_t, p0 * 512 + c * 256, [[512, 4], [16, 16], [1, 16]])
                ta = ctmp[0][:].rearrange("p a (k h) -> p a k h", h=16)
                tb_ = ctmp[1][:].rearrange("p a (k h) -> p a k h", h=16)
                R = BS + [B_ct]
                TT(q, ta, Pv(P_r), Vv(0), ALU.mult, R, [B_ct])
                TT(q, tb_, Pv(P_i), Vv(1), ALU.mult, R, [B_ct])
                TT(q, Ov(0), ta, tb_, ALU.subtract, R, [B_sm])
                TT(q, ta, Pv(P_r), Vv(1), ALU.mult, R, [B_ct])
                TT(q, tb_, Pv(P_i), Vv(0), ALU.mult, R, [B_ct])
                if neg_im:
                    TT(q, ta, ta, tb_, ALU.add, R, [B_ct])
                    TS(q, Ov(1), ta, -1.0, None, ALU.mult, ALU.bypass, R, [B_sm])
                else:
                    TT(q, Ov(1), ta, tb_, ALU.add, R, [B_sm])

        Sbf = sb("Sbf", [128, 2, 2, 16, 128], BF16, stack=S); B_Sbf = Buf("Sbf")
        with ExitStack() as S2:
            Ug = sb("Ug2", [128, 32, 2, 272], BF16, stack=S2); B_Ug2 = Buf("Ug2")
            fw.dma(sp, Ug[:].rearrange("p a b c -> p (a b c)"), ug_s[:, :], writes=[B_Ug2], sem="s9")
            L = sb("L", [128, 2, 2, 16, 272], BF16, stack=S2); B_L = Buf("L")
            with ExitStack() as S2a:
                Wsrc = sb("Wsrc", [128, 16, 2, 256], stack=S2a)
                Wb = sb("Wb", [128, 16, 4, 128], BF16, stack=S2a); B_Wb = Buf("Wb")
                for d in range(2):
                    if d == 0:
                        cplx_build(dve, Wsrc, PWr, PWi, 15, -1, Bbar, 0, False)
                    else:
                        cplx_build(dve, Wsrc, PWr, PWi, 0, 1, Bbar, 1, False)
                    for pt in range(16):
                        ps, Bp = ps_next()
                        for blk in range(4):
                            TR(ps[:, blk * 128:(blk + 1) * 128], sbap(Wsrc, pt * 512 + blk * 128, [[1, 128]]), BS, [Bp])
                        if pt % 2 == 0:
                            A(act, Wb[:, pt, :, :], ps[:, :].rearrange("p (a b) -> p a b", a=4), AF.Copy, [Bp], [B_Wb])
                        else:
                            CP(dve, Wb[:, pt, :, :], ps[:, :].rearrange("p (a b) -> p a b", a=4), [Bp], [B_Wb])
                    for pt in range(16):
                        for c in range(2):
                            ps, Bp = ps_next()
                            if d == 1:
                                ranges = [(0, 272, 0)]
                                ncol = 272
                            else:
                                ranges = [(256, 16, 0), (0, 128, 16)]
                                ncol = 144
                            for (u0, n, o0) in ranges:
                                for gl in range(2):
                                    for jt in range(2):
                                        MM(ps[gl * 64:(gl + 1) * 64, o0:o0 + n], Wb[:, pt, c * 2 + jt, gl * 64:(gl + 1) * 64],
                                           Ug[:, 2 * pt + gl, jt, u0:u0 + n], jt == 0, jt == 1, [B_Wb, B_Ug2], [Bp])
                            lo = L[:, d, c, pt, 272 - ncol:272]
                            if c == 0:
                                A(act, lo, ps[:, 0:ncol], AF.Copy, [Bp], [B_L])
                            else:
                                CP(dve, lo, ps[:, 0:ncol], [Bp], [B_L])
                    if d == 0:
                        dump("Wb0", Wb[:].rearrange("p a b c -> p (a b c)"), [B_Wb])
            dump("L", L[:].rearrange("p a b c e -> p (a b c e)"), [B_L])
            fw.barrier()
            Sh = sb("Sh", [128, 2, 2, 16, NS], stack=S2); B_Sh = Buf("Sh")
            fw.op(pool, lambda e: e.memset(Sh[:], 0.0), writes=[B_Sh])
            tm1 = sb("tm1", [128, 2, 2, 16], stack=S2); tm2 = sb("tm2", [128, 2, 2, 16], stack=S2); B_tm = Buf("tm")

            def f0(st):
                return st - 140 if st >= 142 else st % 2

            def f1(st):
                return 273 - st if st >= 142 else 132 + st % 2
            B_tmh = bufs("tmh", 2); B_Shh = bufs("Shh", 2)
            for b_ in B_Shh:
                b_.w = B_Sh.w
            RSh = [[B_Shh[h_], B_L, B_sm, B_tmh[h_]] for h_ in range(2)]
            for s in range(272):
                both = s >= 128
                ops = []
                NH_ = 2 if WINDOW else 1
                PW_ = 16 // NH_
                for ph_ in range(NH_):
                    po = ph_ * PW_
                    if both:
                        def hv(t_, n_, c0, c1, rev=False):
                            dstep = 32 * n_ + (c1 - c0)
                            if rev:
                                return sbap(t_, c0 + 16 * n_ + po * n_, [[dstep, 2], [-16 * n_, 2], [n_, PW_]])
                            return sbap(t_, c0 + po * n_, [[dstep, 2], [16 * n_, 2], [n_, PW_]])
                        sr = hv(Sh, NS, f0(s), f1(s)); srev = hv(Sh, NS, f0(s), f1(s), True)
                        sw = hv(Sh, NS, f0(s + 1), f1(s + 1))
                        lv = hv(L, 272, s, 271 - s)
                        aa, ab = AA[:, :, :, po:po + PW_], AB[:, :, :, po:po + PW_]
                        x1, x2 = tm1[:, :, :, po:po + PW_], tm2[:, :, :, po:po + PW_]
                    else:
                        def hv(t_, n_, c1, rev=False):
                            if rev:
                                return sbap(t_, 32 * n_ + c1 + 16 * n_ + po * n_, [[-16 * n_, 2], [n_, PW_]])
                            return sbap(t_, 32 * n_ + c1 + po * n_, [[16 * n_, 2], [n_, PW_]])
                        sr = hv(Sh, NS, f1(s)); srev = hv(Sh, NS, f1(s), True); sw = hv(Sh, NS, f1(s + 1))
                        lv = hv(L, 272, 271 - s)
                        aa, ab = AA[:, 1, :, po:po + PW_], AB[:, 1, :, po:po + PW_]
                        x1, x2 = tm1[:, 1, :, po:po + PW_], tm2[:, 1, :, po:po + PW_]
                    ops.append((sr, srev, sw, lv, aa, ab, x1, x2))
                for h_, (sr, srev, sw, lv, aa, ab, x1, x2) in enumerate(ops):
                    TT(dve, x1, sr, aa, ALU.mult, RSh[h_], [B_tmh[h_]])
                for h_, (sr, srev, sw, lv, aa, ab, x1, x2) in enumerate(ops):
                    TT(dve, x2, srev, ab, ALU.mult, RSh[h_], [B_tmh[h_]])
                for h_, (sr, srev, sw, lv, aa, ab, x1, x2) in enumerate(ops):
                    TT(dve, x1, x1, lv, ALU.add, RSh[h_], [B_tmh[h_]])
                for h_, (sr, srev, sw, lv, aa, ab, x1, x2) in enumerate(ops):
                    TT(dve, sw, x1, x2, ALU.add, RSh[h_], [B_Shh[h_]])
            CP(dve, Sbf[:, 0].rearrange("p a b c -> p (a b) c"), sbap(Sh, 4, [[NS, 32], [1, 128]]), B_Shh, [B_Sbf])
            CP(dve, Sbf[:, 1].rearrange("p a b c -> p (a b) c"), sbap(Sh, 32 * NS + 2, [[NS, 32], [1, 128]]), B_Shh, [B_Sbf])
            dump("Sbf", Sbf[:].rearrange("p a b c e -> p (a b c e)"), [B_Sbf])
        fw.barrier()
        if stop_after == "scan":
            S.close()
            return finish()
        with ExitStack() as S3:
            Kmat = sb("Kmat", [128, 32, 4, 128], BF16, stack=S3); B_Km = Buf("Kmat")
            Ca = sb("Ca", [128, 2, 16, 2, 256], BF16, stack=S3); B_Ca = Buf("Ca")
            Ugo = sb("Ugo", [128, 64, 128], BF16, stack=S3); B_Ugo = Buf("Ugo")
            fw.dma(sp, Ugo[:], ug_s.rearrange("p (a c) -> p a c", c=272)[:, :, 0:128], writes=[B_Ugo], sem="s10")
            S3a = ExitStack()
            Xt = sb("Xt", [128, 16, 2, 256], BF16, stack=S3a); Zt = sb("Zt", [128, 16, 2, 256], BF16, stack=S3a)
            tmpK = sb("tmpK", [128, 4, 128], stack=S3a); tmpS = sb("tmpS", [128, 2, 128], stack=S3a); B_tk = Buf("tmpK")
            cplx_build(dve, Ca[:, 0], PWr, PWi, 1, 1, scc, 0, True)
            cplx_build(dve, Ca[:, 1], PWr, PWi, 16, -1, scc, 1, True)
            KS = BS + [B_tk]
            for d in range(2):
                if d == 0:
                    cplx_build(dve, Xt, IPr, IPi, 0, 1, Bbar, 0, False)
                    cplx_build(dve, Zt, PWr, PWi, 0, 1, scc, 0, True)
                else:
                    cplx_build(dve, Xt, PWr, PWi, 0, 1, Bbar, 1, False)
                    cplx_build(dve, Zt, IPr, IPi, 0, 1, scc, 1, True)
                for g in range(32):
                    pt, gl = g // 2, g % 2
                    r0, r1 = gl * 64, gl * 64 + 64
                    psA, BpA = ps_next()

                    def kblock(ps, col, jt, tt_):
                        for c in range(2):
                            MM(ps[:, col * 128:(col + 1) * 128], Xt[r0:r1, pt, c, jt * 128:(jt + 1) * 128],
                               Zt[r0:r1, pt, c, tt_ * 128:(tt_ + 1) * 128], c == 0, c == 1, BS, [BpA])
                    kblock(psA, 0, 0, 0)
                    kblock(psA, 1, 1, 1)
                    if d == 0:
                        kblock(psA, 2, 0, 1)
                    else:
                        kblock(psA, 2, 1, 0)
                    mk = sbap(masks, d * 128, [[0, 2], [1, 128]])
                    kd = sbap(Kmat, g * 512, [[384, 2], [1, 128]])
                    ko_ = Kmat[:, g, 1 if d == 0 else 2, :]
                    if d == 0:
                        TT(dve, tmpS[:], psA[:, 0:256].rearrange("p (a b) -> p a b", a=2), mk, ALU.mult, [BpA, B_par], [B_tk])
                        STT(kd, sbap(ident, 0, [[0, 2], [1, 128]]), dcols[:, g:g + 1], tmpS[:], ALU.mult, ALU.add, [B_ident, B_par, B_tk], [B_Km])
                    else:
                        TT(dve, tmpS[:], psA[:, 0:256].rearrange("p (a b) -> p a b", a=2), mk, ALU.mult, [BpA, B_par], [B_tk])
                        TT(dve, kd, kd, tmpS[:], ALU.add, [B_tk, B_Km], [B_Km])
                    A(act, ko_, psA[:, 256:384], AF.Copy, [BpA], [B_Km])
            dump("Kmat", Kmat[:].rearrange("p a b c -> p (a b c)"), [B_Km])
            dump("Ca", Ca[:].rearrange("p a b c e -> p (a b c e)"), BS)
            fw.barrier()
            S3a.close()
            g_tm = sb("g_tm", [128, 16, 512], stack=S3); B_gtm = Buf("g_tm")
            glsb = [sb("glsb%d" % i, [128, 512], stack=S3) for i in range(2)]; B_gl = bufs("glsb", 2)
            for gp in range(16):
                psY, BpY = ps_next()
                for gi in range(2):
                    g = gp * 2 + gi
                    pt, gl = g // 2, g % 2
                    r0, r1 = gl * 64, gl * 64 + 64
                    for tt_ in range(2):
                        yo = psY[:, (gi * 2 + tt_) * 128:(gi * 2 + tt_ + 1) * 128]
                        first = True
                        for jt in range(2):
                            MM(yo, Kmat[:, g, jt * 2 + tt_, :], Ugo[:, g * 2 + jt, :], first, False, [B_Km, B_Ugo], [BpY])
                            first = False
                        for d in range(2):
                            for c in range(2):
                                MM(yo, Ca[r0:r1, d, pt, c, tt_ * 128:(tt_ + 1) * 128], Sbf[r0:r1, d, c, pt, :], False, (d == 1 and c == 1),
                                   BS + [B_Sbf], [BpY])
                gs_ = gp % 2
                A(act, glsb[gs_][:, :], psY[:, :], AF.Gelu, [BpY], [B_gl[gs_]])
                psT, BpT = ps_next()
                for blk in range(4):
                    TR(psT[:, blk * 128:(blk + 1) * 128], glsb[gs_][:, blk * 128:(blk + 1) * 128], [B_gl[gs_]], [BpT])
                for gi in range(2):
                    g = gp * 2 + gi
                    oo = sbap(g_tm, 16 * g, [[512, 16], [1, 16]])
                    ii = sbap(psT, gi * 256, [[16, 16], [1, 16]])
                    CP(dve, oo, ii, [BpT], [B_gtm])
            dump("gtm", g_tm[:].rearrange("p a b -> p (a b)"), [B_gtm])
            gT = [sb("gT%d" % i, [128, 4, 128], BF16, stack=S3) for i in range(2)]; B_gT = bufs("gT", 2)
            zt = sb("zt", [128, 512], stack=S3); B_zt = Buf("zt")
            for t in range(16):
                gs_ = t % 2
                ps, Bp = ps_next()
                for fc in range(4):
                    TR(ps[:, fc * 128:(fc + 1) * 128], g_tm[:, t, fc * 128:(fc + 1) * 128], [B_gtm], [Bp])
                A(act, gT[gs_][:], ps[:, :].rearrange("p (a b) -> p a b", a=4), AF.Copy, [Bp], [B_gT[gs_]])
                ps2, Bp2 = ps_next()
                for fc in range(4):
                    MM(ps2[:, :], gT[gs_][:, fc, :], w_glu[:, fc, :], fc == 0, fc == 3, [B_gT[gs_], B_wglu], [Bp2])
                TT(dve, zt[:], ps2[:, :], bglu[:], ALU.add, [Bp2, B_bglu], [B_zt])
                A(act, zt[:], zt[:], AF.Sigmoid, [B_zt], [B_zt])
                TT(dve, g_tm[:, t, :], g_tm[:, t, :], zt[:], ALU.mult, [B_gtm, B_zt], [B_gtm])
            dump("ssm", g_tm[:].rearrange("p a b -> p (a b)"), [B_gtm])
            fw.dma(sp, g_s[:, :], g_tm[:].rearrange("p a b -> p (a b)"), reads=[B_gtm], writes=[B_gs], sem="gs")
    fw.barrier()
    if stop_after == "ssm":
        return finish()

    hT = sb("hT", [128, 8, NOWN], BF16); B_hT = Buf("hT")
    combT = sb("combT", [32, NOWN], BF16); B_combT = Buf("combT")
    lnbox = [None, None]
    B_x1s = Buf("x1_s")

    def layer_norm(pre, B_pre, gi, out, B_out, st, mv, sc_, B_st):
        for hf in range(2):
            fw.op(dve, lambda e: e.bn_stats(out=st[:, hf * 6:(hf + 1) * 6], in_=pre[:, hf * 512:(hf + 1) * 512]), reads=[B_pre], writes=[B_st])
        fw.op(dve, lambda e: e.bn_aggr(out=mv[:, 0:2], in_=st[:, 0:12]), reads=[B_st], writes=[B_st])
        A(act, sc_[:, 0:1], mv[:, 1:2], AF.Ln, [B_st, B_eps], [B_st], bias=epsc[:, 0:1])
        A(act, sc_[:, 1:2], sc_[:, 0:1], AF.Exp, [B_st], [B_st], scale=-0.5)
        TS(dve, out, pre, mv[:, 0:1], sc_[:, 1:2], ALU.subtract, ALU.mult, [B_pre], [B_out], sreads=[B_st])
        lnrows, B_ln = lnbox
        TT(dve, out, out, lnrows[:, 0, :], ALU.mult, [B_out, B_ln], [B_out])
        TT(dve, out, out, lnrows[:, 1, :], ALU.add, [B_out, B_ln], [B_out])

    n_exp = int(os.environ.get("K_NEXP", str(NE)))
    NSLOT = 3
    Wg = [sb("Wg%d" % i, [128, 8, 256], BF16, stack=es) for i in range(NSLOT)]
    Wu = [sb("Wu%d" % i, [128, 8, 256], BF16, stack=es) for i in range(NSLOT)]
    Wd = [sb("Wd%d" % i, [128, 2, D], BF16, stack=es) for i in range(NSLOT)]
    B_W = bufs("Wexp", NSLOT)
    stg = [sb("stg%d" % i, [128, 2048], stack=es) for i in range(2)]; B_stg = bufs("stg", 2)
    stc = [0]

    def load_expert(e):
        sl_ = e % NSLOT
        for (src_, dst, kk) in ((wg_d[e], Wg[sl_], 8), (wu_d[e], Wu[sl_], 8), (wd_d[e], Wd[sl_], 2)):
            k_ = stc[0] % 2
            stc[0] += 1
            fw.dma(sp, stg[k_][:].rearrange("p (k n) -> p k n", k=kk), src_.rearrange("(k p) n -> p k n", p=128),
                   writes=[B_stg[k_]], sem="stg%d" % k_)
            CP(pool, dst[:].rearrange("p k n -> p (k n)"), stg[k_][:], [B_stg[k_]], [B_W[sl_]])


    with ExitStack() as M:
        ln1 = sb("ln1rows", [128, 2, D], stack=M); lnbox[0] = ln1; lnbox[1] = Buf("ln1")
        fw.dma(sp, ln1[:], ln_d[:, 0:2, :], writes=[lnbox[1]], sem="m0")
        w_o_s = sb("w_o_s", [128, 8, D], BF16, stack=M); B_wo = Buf("w_o_s")
        wof = [sb("wof%d" % i, [128, D], stack=M) for i in range(2)]; B_wof = bufs("wof", 2)
        gn = sb("gn", [128, 8], stack=M); B_gn = Buf("gn")
        w_r = sb("w_r", [128, 8, 36], stack=M); b_r = sb("b_r", [128, 36], stack=M); B_wr = Buf("w_r")
        fw.dma(sp, gn[:], gn_d[:, :], writes=[B_gn], sem="m1")
        fw.dma(sp, w_r[:], w_r_d.rearrange("(k p) n -> p k n", p=128), writes=[B_wr], sem="m2")
        fw.dma(sp, b_r[:], b_r_d[:, :], writes=[B_wr], sem="m3")
        for fc in range(8):
            fw.dma(sp, wof[fc % 2][:], w_o_d[fc * 128:(fc + 1) * 128, :], writes=[B_wof[fc % 2]], sem="wof%d" % (fc % 2))
            TS(dve, w_o_s[:, fc, :], wof[fc % 2][:], gn[:, fc:fc + 1], None, ALU.mult, ALU.bypass, [B_wof[fc % 2], B_gn], [B_wo])
        load_expert(0)
        cat = [sb("cat%d" % i, [128, D], stack=M) for i in range(2)]; B_cat = bufs("cat", 2)
        xo = [sb("xo%d" % i, [128, D], stack=M) for i in range(2)]; B_xo = bufs("xo", 2)
        catT = [sb("catT%d" % i, [128, 8, 128], BF16, stack=M) for i in range(2)]; B_catT = bufs("catT", 2)
        pre = [sb("pre%d" % i, [128, D], stack=M) for i in range(2)]; B_pre = bufs("pre", 2)
        hTf = [sb("hTf%d" % i, [128, 8, 128], stack=M) for i in range(2)]; B_hTf = bufs("hTf", 2)
        htm = [sb("htm%d" % i, [128, D], stack=M) for i in range(2)]; B_htm = bufs("htm", 2)
        junk = sb("junk", [128, 512], stack=M); B_junk = Buf("junk")
        st = sb("st", [128, 12], stack=M); mv = sb("mv", [128, 2], stack=M); sc_ = sb("sc_", [128, 8], stack=M); B_st = Buf("st")
        rt = sb("rt", [128, 160], stack=M); B_rt = Buf("rt")
        scA = sb("scA", [128, 8], stack=M); B_stA = Buf("stA")
        comb = sb("comb", [128, 32], stack=M); B_comb = Buf("comb")
        mixps = [[], []]

        def stageA(t):
            s = t % 2
            fw.dma(sp, cat[s][:, 0:512], attn_s[:, t * 512:(t + 1) * 512], reads=[B_attn_s], writes=[B_cat[s]], sem="cat%d" % s)
            fw.dma(sp, cat[s][:, 512:1024], g_s[:, t * 512:(t + 1) * 512], reads=[B_gs], writes=[B_cat[s]], sem="cat%d" % s)
            fw.group_done("cat%d" % s, [B_cat[s]])
            fw.dma(sp, xo[s][:], xf_v[t, 0:128, :], writes=[B_xo[s]], sem="xo%d" % s)
            for hf in range(2):
                A(act, junk[:], cat[s][:, hf * 512:(hf + 1) * 512], AF.Square, [B_cat[s]], [B_junk, B_stA], accum_out=scA[:, 2 + hf:3 + hf])
                A(act, scA[:, 4 + hf:5 + hf], scA[:, 2 + hf:3 + hf], AF.Ln, [B_stA, B_eps], [B_stA], scale=1.0 / 512, bias=epsc[:, 0:1])
                A(act, scA[:, 6 + hf:7 + hf], scA[:, 4 + hf:5 + hf], AF.Exp, [B_stA], [B_stA], scale=-0.5)
                A(act, cat[s][:, hf * 512:(hf + 1) * 512], cat[s][:, hf * 512:(hf + 1) * 512], AF.Copy, [B_cat[s], B_stA], [B_cat[s]],
                  scale=scA[:, 6 + hf:7 + hf])
            for hf in range(2):
                ps, Bp = ps_next()
                for q4 in range(4):
                    fc = hf * 4 + q4
                    TR(ps[:, q4 * 128:(q4 + 1) * 128], cat[s][:, fc * 128:(fc + 1) * 128], [B_cat[s]], [Bp])
                A(act, catT[s][:, hf * 4:(hf + 1) * 4, :], ps[:, :].rearrange("p (a b) -> p a b", a=4), AF.Copy, [Bp], [B_catT[s]])
            for hf in range(2):
                ps, Bp = ps_next()
                for fc in range(8):
                    MM(ps[:, :], catT[s][:, fc, :], w_o_s[:, fc, hf * 512:(hf + 1) * 512], fc == 0, fc == 7, [B_catT[s], B_wo], [Bp])
                mixps[t % 2].append((ps, Bp))

        def stageA_pre(t):
            s = t % 2
            for hf in range(2):
                ps, Bp = mixps[t % 2][hf]
                TT(dve, pre[s][:, hf * 512:(hf + 1) * 512], ps[:, :], grow[:, 0, hf * 512:(hf + 1) * 512], ALU.mult, [Bp, B_grow], [B_pre[s]])
            mixps[t % 2].clear()
            STT(pre[s][:], xo[s][:], ALPHA, pre[s][:], ALU.mult, ALU.add, [B_xo[s], B_pre[s]], [B_pre[s]])

        def stageB(t):
            s = t % 2
            layer_norm(pre[s][:], B_pre[s], 0, pre[s][:], B_pre[s], st, mv, sc_, B_st)
            fw.dma(sp, x1_s[:, t * D:(t + 1) * D], pre[s][:], reads=[B_pre[s]], writes=[B_x1s], sem="x1s")
            TT(dve, htm[s][:], pre[s][:], grow[:, 2, :], ALU.mult, [B_pre[s], B_grow], [B_htm[s]])
            TT(dve, htm[s][:], htm[s][:], grow[:, 1, :], ALU.add, [B_htm[s], B_grow], [B_htm[s]])
            for hf in range(2):
                ps, Bp = ps_next()
                for q4 in range(4):
                    fc = hf * 4 + q4
                    TR(ps[:, q4 * 128:(q4 + 1) * 128], htm[s][:, fc * 128:(fc + 1) * 128], [B_htm[s]], [Bp])
                A(act, hT[:, hf * 4:(hf + 1) * 4, t * 128:(t + 1) * 128], ps[:, :].rearrange("p (a b) -> p a b", a=4), AF.Copy, [Bp], [B_hT])
                A(act, hTf[s][:, hf * 4:(hf + 1) * 4, :], ps[:, :].rearrange("p (a b) -> p a b", a=4), AF.Copy, [Bp], [B_hTf[s]])

        rps = [None]

        def stageC_mm(t):
            s = t % 2
            ps, Bp = ps_next()
            for fc in range(8):
                MM(ps[:, 0:36], hTf[s][:, fc, :], w_r[:, fc, :], fc == 0, fc == 7, [B_hTf[s], B_wr], [Bp])
            rps[0] = (ps, Bp)

        def stageC(t):
            s = t % 2
            ps, Bp = rps[0]
            lg = rt[:, 0:36]
            gsel = rt[:, 36:40]; gmax = rt[:, 40:41]; ngmax = rt[:, 41:42]; gsum = rt[:, 42:43]; g_w = rt[:, 43:44]
            esel = rt[:, 44:52]; m1 = rt[:, 52:53]; mask1 = rt[:, 56:64]; esel2 = rt[:, 64:72]; m2 = rt[:, 53:54]; mask2 = rt[:, 72:80]
            nm1 = rt[:, 54:55]; e2 = rt[:, 55:56]; den_ = rt[:, 80:81]; w1 = rt[:, 81:82]; w2 = rt[:, 82:83]; cw8 = rt[:, 88:96]; gex = rt[:, 96:100]
            RR = [B_rt]
            TT(dve, lg, ps[:, 0:36], b_r[:, :], ALU.add, [Bp, B_wr], RR)
            fw.op(dve, lambda e: e.reduce_max(out=gmax, in_=rt[:, 0:4], axis=AX.X), reads=RR, writes=RR)
            TS(dve, gsel, rt[:, 0:4], gmax, None, ALU.is_ge, ALU.bypass, RR, RR, sreads=RR)
            TS(dve, ngmax, gmax, -1.0, None, ALU.mult, ALU.bypass, RR, RR)
            A(act, gex, rt[:, 0:4], AF.Exp, RR, RR, bias=ngmax, accum_out=gsum)
            fw.op(dve, lambda e: e.reciprocal(out=g_w, in_=gsum), reads=RR, writes=RR)
            TS(dve, esel, rt[:, 4:12], gsel[:, 0:1], None, ALU.mult, ALU.bypass, RR, RR, sreads=RR)
            for g in range(1, 4):
                fw.op(dve, lambda e: e.scalar_tensor_tensor(out=esel, in0=rt[:, 4 + 8 * g:12 + 8 * g], scalar=gsel[:, g:g + 1], in1=esel,
                                                            op0=ALU.mult, op1=ALU.add), reads=RR, writes=RR, sreads=RR)
            fw.op(dve, lambda e: e.reduce_max(out=m1, in_=esel, axis=AX.X), reads=RR, writes=RR)
            TS(dve, mask1, esel, m1, None, ALU.is_ge, ALU.bypass, RR, RR, sreads=RR)
            STT(esel2, mask1, -1e30, esel, ALU.mult, ALU.add, RR, RR)
            fw.op(dve, lambda e: e.reduce_max(out=m2, in_=esel2, axis=AX.X), reads=RR, writes=RR)
            TS(dve, mask2, esel2, m2, None, ALU.is_ge, ALU.bypass, RR, RR, sreads=RR)
            TS(dve, nm1, m1, -1.0, None, ALU.mult, ALU.bypass, RR, RR)
            A(act, e2, m2, AF.Exp, RR, RR, bias=nm1)
            TS(dve, den_, e2, 1.0, None, ALU.add, ALU.bypass, RR, RR)
            fw.op(dve, lambda e: e.reciprocal(out=w1, in_=den_), reads=RR, writes=RR)
            TT(dve, w2, e2, w1, ALU.mult, RR, RR)
            TT(dve, w1, w1, g_w, ALU.mult, RR, RR)
            TT(dve, w2, w2, g_w, ALU.mult, RR, RR)
            TS(dve, cw8, mask1, w1, None, ALU.mult, ALU.bypass, RR, RR, sreads=RR)
            fw.op(dve, lambda e: e.scalar_tensor_tensor(out=cw8, in0=mask2, scalar=w2, in1=cw8, op0=ALU.mult, op1=ALU.add),
                  reads=RR, writes=RR, sreads=RR)
            for g in range(4):
                TS(dve, comb[:, g * 8:(g + 1) * 8], cw8, gsel[:, g:g + 1], None, ALU.mult, ALU.bypass, RR, [B_comb], sreads=RR)

        def stageC_tail(t):
            s = t % 2
            ps, Bp = ps_next()
            TR(ps[0:32, 0:128], comb[:, :], [B_comb], [Bp])
            CP(dve, combT[:, t * 128:(t + 1) * 128], ps[0:32, 0:128], [Bp], [B_combT])
            if t == 0:
                dump("x1_0", pre[s][:], [B_pre[s]])
                dump("comb0", comb[:], [B_comb])

        stageA(0)
        stageA_pre(0)
        for t in range(17):
            if t >= 1:
                stageC_mm(t - 1)
            if t + 1 < 16:
                stageA(t + 1)
            if t < 16:
                stageB(t)
            if t >= 1:
                stageC(t - 1)
            if t + 1 < 16:
                stageA_pre(t + 1)
            if t >= 1:
                stageC_tail(t - 1)
    fw.barrier()
    if stop_after == "merge":
        return finish()

    with ExitStack() as E:
        ln2 = sb("ln2rows", [128, 2, D], stack=E); lnbox[0] = ln2; lnbox[1] = Buf("ln2")
        fw.dma(sp, ln2[:], ln_d[:, 2:4, :], writes=[lnbox[1]], sem="e9")
        acc = sb("acc", [128, 16, D], stack=E); B_acc = bufs("acc", 16)
        sel = sb("sel", [32, NE * 128], BF16, stack=E); B_sel = Buf("sel")
        fw.dma(pool, sel[:], sel_d[:, :], writes=[B_sel], sem="e0")
        sg = [sb("sg%d" % i, [128, 512], stack=E) for i in range(2)]; B_sg = bufs("sg", 2)
        tu = [sb("tu%d" % i, [128, 512], stack=E) for i in range(2)]; B_tu = bufs("tu", 2)
        gT2 = [sb("gT2_%d" % i, [128, 2, 256], BF16, stack=E) for i in range(2)]; B_gT2 = bufs("gT2", 2)
        n_exp = int(os.environ.get("K_NEXP", str(NE)))

        for e in range(1, min(NSLOT, n_exp)):
            load_expert(e)
        items = [(e, tg) for e in range(n_exp) for tg in range(8)]
        dcnt = [0]

        csb = [sb("csb%d" % i, [128, 256], BF16, stack=E) for i in range(2)]; B_csb = bufs("csb", 2)

        def AUC(i):
            e, tg = items[i]
            sl_ = e % NSLOT
            k2 = i % 2
            pa, Bpa = PS[k2 * 2], PSB[k2 * 2]
            pu, Bpu = PS[k2 * 2 + 1], PSB[k2 * 2 + 1]
            pc_, Bpc = PS[4], PSB[4]
            MM(pc_[:, 0:256], sel[0:32, e * 128:(e + 1) * 128], combT[0:32, tg * 256:(tg + 1) * 256], True, True, [B_sel, B_combT], [Bpc])
            for fc in range(2):
                for ch in range(8):
                    MM(pa[:, fc * 256:(fc + 1) * 256], Wg[sl_][:, ch, fc * 128:(fc + 1) * 128], hT[:, ch, tg * 256:(tg + 1) * 256],
                       ch == 0, ch == 7, [B_W[sl_], B_hT], [Bpa])
            for fc in range(2):
                for ch in range(8):
                    MM(pu[:, fc * 256:(fc + 1) * 256], Wu[sl_][:, ch, fc * 128:(fc + 1) * 128], hT[:, ch, tg * 256:(tg + 1) * 256],
                       ch == 0, ch == 7, [B_W[sl_], B_hT], [Bpu])

        def REST(i):
            e, tg = items[i]
            sl_ = e % NSLOT
            k2 = i % 2
            pa, Bpa = PS[k2 * 2], PSB[k2 * 2]
            pu, Bpu = PS[k2 * 2 + 1], PSB[k2 * 2 + 1]
            A(act, sg[k2][:, :], pa[:, :], AF.Silu, [Bpa], [B_sg[k2]])
            if i + 1 < len(items):
                A(act, csb[1 - k2][:, :], PS[4][:, 0:256], AF.Copy, [PSB[4]], [B_csb[1 - k2]])
            TT(dve, tu[k2][:, :], pu[:, :], sg[k2][:, :], ALU.mult, [Bpu, B_sg[k2]], [B_tu[k2]])
            TT(dve, gT2[k2][:], tu[k2][:, :].rearrange("p (a b) -> p a b", a=2), sbap(csb[k2], 0, [[0, 2], [1, 256]]), ALU.mult,
               [B_tu[k2], B_csb[k2]], [B_gT2[k2]])
            for tt_ in range(2):
                tile_ = tg * 2 + tt_
                for hf in range(2):
                    po, Bpo = PS[5 + dcnt[0] % 3], PSB[5 + dcnt[0] % 3]
                    dcnt[0] += 1
                    for fc in range(2):
                        MM(po[:, :], gT2[k2][:, fc, tt_ * 128:(tt_ + 1) * 128], Wd[sl_][:, fc, hf * 512:(hf + 1) * 512],
                           fc == 0, fc == 1, [B_gT2[k2], B_W[sl_]], [Bpo])
                    ao = acc[:, tile_, hf * 512:(hf + 1) * 512]
                    if e == 0:
                        CP(dve, ao, po[:, :], [Bpo], [B_acc[tile_]])
                    else:
                        TT(dve, ao, po[:, :], ao, ALU.add, [Bpo, B_acc[tile_]], [B_acc[tile_]])
            if tg == 7 and e + NSLOT < n_exp:
                load_expert(e + NSLOT)

        x1t = [stg[i][:, 0:D] for i in range(2)]; B_x1t = B_stg
        st2 = sb("st2", [128, 12], stack=E); mv2 = sb("mv2", [128, 2], stack=E); sc2 = sb("sc2", [128, 8], stack=E); B_st2 = Buf("st2")
        B_out = Buf("out")
        st2s = [sb("st2_%d" % i, [128, 12], stack=E) for i in range(2)]; mv2s = [sb("mv2_%d" % i, [128, 2], stack=E) for i in range(2)]
        sc2s = [sb("sc2_%d" % i, [128, 8], stack=E) for i in range(2)]; B_st2s = bufs("st2s", 2)

        def F1(t):
            s = t % 2
            fw.dma(sp, x1t[s], x1_s[:, t * D:(t + 1) * D], reads=[B_x1s], writes=[B_x1t[s]], sem="stg%d" % s)
            TT(dve, acc[:, t, :], acc[:, t, :], grow[:, 3, :], ALU.mult, [B_acc[t], B_grow], [B_acc[t]])
            STT(acc[:, t, :], x1t[s], ALPHA, acc[:, t, :], ALU.mult, ALU.add, [B_x1t[s], B_acc[t]], [B_acc[t]])
            pre_ = acc[:, t, :]
            for hf in range(2):
                fw.op(dve, lambda e: e.bn_stats(out=st2s[s][:, hf * 6:(hf + 1) * 6], in_=pre_[:, hf * 512:(hf + 1) * 512]), reads=[B_acc[t]], writes=[B_st2s[s]])
            fw.op(dve, lambda e: e.bn_aggr(out=mv2s[s][:, 0:2], in_=st2s[s][:, 0:12]), reads=[B_st2s[s]], writes=[B_st2s[s]])
            A(act, sc2s[s][:, 0:1], mv2s[s][:, 1:2], AF.Ln, [B_st2s[s], B_eps], [B_st2s[s]], bias=epsc[:, 0:1])
            A(act, sc2s[s][:, 1:2], sc2s[s][:, 0:1], AF.Exp, [B_st2s[s]], [B_st2s[s]], scale=-0.5)

        def F2(t):
            s = t % 2
            o_ = acc[:, t, :]
            lnrows, B_ln = lnbox
            TS(dve, o_, o_, mv2s[s][:, 0:1], sc2s[s][:, 1:2], ALU.subtract, ALU.mult, [B_acc[t]], [B_acc[t]], sreads=[B_st2s[s]])
            TT(dve, o_, o_, lnrows[:, 0, :], ALU.mult, [B_acc[t], B_ln], [B_acc[t]])
            TT(dve, o_, o_, lnrows[:, 1, :], ALU.add, [B_acc[t], B_ln], [B_acc[t]])
            fw.dma(sp, out_d[t * 128:(t + 1) * 128, :], o_, reads=[B_acc[t]], writes=[B_out], sem="out")
        AUC(0)
        A(act, csb[0][:, :], PS[4][:, 0:256], AF.Copy, [PSB[4]], [B_csb[0]])
        for i in range(len(items)):
            if i + 1 < len(items):
                AUC(i + 1)
            REST(i)
            if items[i][0] == n_exp - 1:
                tg_ = items[i][1]
                F1(2 * tg_); F1(2 * tg_ + 1); F2(2 * tg_); F2(2 * tg_ + 1)

    return finish()


def _rope_tables(frame_tok):
    t = np.asarray(frame_tok)
    row = (t // 64).astype(np.float32)
    col = (t % 64).astype(np.float32)
    inv = (10000.0 ** (-np.arange(0, 16, 2, dtype=np.float32) / np.float32(16))).astype(np.float32)
    ang = np.concatenate([row[:, None] * inv, col[:, None] * inv], -1)
    ang = np.concatenate([ang, ang], -1).astype(np.float32)
    return np.cos(ang).astype(np.float32), np.sin(ang).astype(np.float32)


def prep_core(inp, b, hh):
    f32 = np.float32
    m = {}
    x = inp["x"][b]
    ctx = inp["ctx"][b]
    if hh == 1:
        x = x[::-1]
        ctx = ctx[::-1]
    m["xf"] = np.ascontiguousarray(x, f32)
    m["ctxf"] = np.ascontiguousarray(ctx, f32)
    cT = np.stack([inp["c"][b].reshape(8, 128).T, inp["c_ctx"].reshape(8, 128).T], -1)
    m["cT"] = np.ascontiguousarray(cT, f32)
    m["w_ada"] = inp["w_ada"][0]
    ba = inp["b_ada"][0]
    m["b_ada_2rows"] = np.ascontiguousarray(np.stack([ba, ba], 0), f32)
    m["w_in"] = inp["w_in"][0]
    m["qg_cols"] = np.ascontiguousarray(inp["q_norm_g"][0].reshape(2, 128).T, f32)
    m["kvg_col"] = np.ascontiguousarray(inp["kv_norm_g"][0].reshape(1, 128).T, f32)
    m["w_uq"] = inp["w_uq"][0]
    m["w_ukv"] = inp["w_ukv"][0]
    mt, j, i = np.meshgrid(np.arange(2), np.arange(16), np.arange(128), indexing="ij")
    tau = (16 * (128 * mt + i) + j).reshape(-1)
    tok = tau if hh == 0 else (NTOK - 1 - tau)
    cos, sin = _rope_tables(tok)
    rope = np.zeros((128, 2, NKEY), f32)
    rope[64:96, 0, :NTOK] = cos.T
    rope[64:96, 1, :NTOK] = sin.T
    rope[64:96, 0, NTOK:] = 1.0
    m["rope"] = rope
    dirs = [0, 1] if hh == 0 else [1, 0]

    def lay(a):
        a = a[dirs]
        sh = a.shape[3:]
        a = a.reshape(2, 16, 2, 64, *sh)
        a = np.moveaxis(a, (2, 3), (0, 1))
        return a.reshape(128, 32, *sh)

    m["ssm_a"] = np.ascontiguousarray(np.stack([lay(inp["ssm_a_re"][0]), lay(inp["ssm_a_im"][0])], 1), f32)
    ldt = np.broadcast_to(inp["ssm_log_dt"][0][:, :, None], (2, 32, 64))
    m["ssm_ldt"] = np.ascontiguousarray(lay(ldt), f32)
    m["ssm_b"] = np.ascontiguousarray(np.stack([lay(inp["ssm_b_re"][0]), lay(inp["ssm_b_im"][0])], 1), f32)
    cre = np.swapaxes(inp["ssm_c_re"][0], 2, 3)
    cim = np.swapaxes(inp["ssm_c_im"][0], 2, 3)
    m["ssm_c"] = np.ascontiguousarray(np.stack([lay(cre), lay(cim)], 1), f32)
    dd = inp["ssm_d"][0].reshape(32, 16)
    m["ssm_dcols"] = np.ascontiguousarray(np.broadcast_to(dd.T[None], (8, 16, 32)).reshape(128, 32), f32)
    jl = np.arange(128) // 16
    m0 = (jl[None, :] >= jl[:, None]).astype(f32)
    m1 = (jl[:, None] >= jl[None, :]).astype(f32)
    m["masks"] = np.ascontiguousarray(np.stack([m0, m1], 1), f32)
    m["ident"] = np.eye(128, dtype=f32)
    m["w_glu"] = inp["w_glu"][0]
    m["b_glu_row"] = np.ascontiguousarray(np.broadcast_to(inp["b_glu"][0][None], (128, 512)), f32)
    gn = np.concatenate([inp["gn_attn_g"][0], inp["gn_ssm_g"][0]])
    m["gn_cols"] = np.ascontiguousarray(gn.reshape(8, 128).T, f32)
    m["w_o"] = inp["w_o"][0]
    ln = np.stack([inp["ln1_g"][0], inp["ln1_b"][0], inp["ln2_g"][0], inp["ln2_b"][0]], 0)
    m["ln_rows"] = np.ascontiguousarray(np.broadcast_to(ln[None], (128, 4, D)), f32)
    m["w_r"] = np.ascontiguousarray(np.concatenate([inp["w_router_group"][0], inp["w_router_expert"][0]], 1), f32)
    br_ = np.concatenate([inp["b_router_group"][0], inp["b_router_expert"][0]])
    m["b_r_row"] = np.ascontiguousarray(np.broadcast_to(br_[None], (128, 36)), f32)
    m["w_exp_gate"] = inp["w_exp_gate"][0]
    m["w_exp_up"] = inp["w_exp_up"][0]
    m["w_exp_down"] = inp["w_exp_down"][0]
    sel = np.zeros((32, NE, 128), f32)
    sel[np.arange(32), np.arange(32), :] = 1.0
    m["sel"] = sel.reshape(32, NE * 128)
    return m


def run(inputs, stop_after=None, dbg_names=(), cores=8):
    inp = {k: np.asarray(v) for k, v in inputs.items()}
    nc = build(stop_after=stop_after, dbg_names=dbg_names)
    in_maps = [prep_core(inp, c // 2, c % 2) for c in range(cores)]
    res = run_bass_kernel_spmd(nc, in_maps, core_ids=list(range(cores)))
    return res.results


def kernel(**inputs):
    res = run(inputs)
    out = np.zeros((4, NTOK, D), np.float32)
    for c in range(8):
        b, hh = c // 2, c % 2
        o = res[c]["out"]
        o = o.reshape(16, 128, D).transpose(1, 0, 2).reshape(NOWN, D)
        if hh == 0:
            out[b, :NOWN] = o
        else:
            out[b, NOWN:] = o[::-1]
    return out
```
